# Optimizing a Trainium2 kernel written in Bass

```python
import math
import jax, jax.numpy as jnp
from jax import lax
import numpy as np


D_MODEL = 1024
BATCH = 32
SEQ = 2048
DEPTH = 2

CHUNK = 64
QBLOCK = 128
N_HEADS = 8
D_QLAT = 256
D_LAT = 128
D_VHEAD = 64
IDX_HEADS = 8
IDX_DIM = 32
DSA_TOPK_MAX = 256
SSM_GROUPS = 16
SSM_GROUP_DIM = 16
SSM_STATE = 64
W_SSM = SSM_GROUPS * SSM_GROUP_DIM
POOL_WINDOWS = (2, 4, 8, 16)
POOL_GROUPS = 4
POOL_GROUP_DIM = 64
W_POOL = POOL_GROUPS * POOL_GROUP_DIM
W_ATTN = N_HEADS * D_VHEAD
N_BRANCH = 3
D_FF = 2048
CONV_WIDTH = 3
RMS_EPS = 1e-6
NEG_INF = -1e30
ATTN_SCALE = D_LAT ** -0.5
IDX_SCALE = IDX_DIM ** -0.5
IDX_HEAD_SCALE = IDX_HEADS ** -0.5
IN_SPLITS = (D_QLAT, D_LAT, IDX_DIM, IDX_HEADS, W_SSM, W_POOL, N_BRANCH * D_MODEL)
N_IN = 4008

kernel_name = 'chunk_causal_hybrid_dsa_s5_pool_block'


def _rmsnorm(x, g):
    xf = x.astype(jnp.float32)
    y = xf * lax.rsqrt(jnp.mean(xf * xf, axis=-1, keepdims=True) + RMS_EPS)
    return (y * g.astype(jnp.float32)).astype(x.dtype)


def _alibi_slopes(n):
    return jnp.exp2(-8.0 * jnp.arange(1, n + 1, dtype=jnp.float32) / n)


def _dsa_branch(cq, ckv, kidx, widx, g_cq, w_uq, w_qi, g_ckv, w_uv):
    Bsz, S, _ = cq.shape
    cq = _rmsnorm(cq, g_cq)
    ckv = _rmsnorm(ckv, g_ckv)
    q = jnp.einsum('bsr,rhd->bshd', cq, w_uq)
    qi = jnp.einsum('bsr,rhd->bshd', cq, w_qi)
    top_k = min(DSA_TOPK_MAX, S // 4)
    slopes = _alibi_slopes(N_HEADS)
    nb = S // QBLOCK
    key_pos = jnp.arange(S, dtype=jnp.int32)

    def to_blocks(a):
        return jnp.moveaxis(a.reshape((Bsz, nb, QBLOCK) + a.shape[2:]), 1, 0)

    def attend(blk):
        q_b, qi_b, w_b, t_b = blk
        limit = (t_b // CHUNK + 1) * CHUNK
        admissible = key_pos[None, :] < limit[:, None]
        idx_logits = jnp.einsum('bqhd,bsd->bqhs', qi_b, kidx).astype(jnp.float32) * IDX_SCALE
        score = jnp.einsum('bqhs,bqh->bqs', jax.nn.relu(idx_logits), w_b.astype(jnp.float32)) * IDX_HEAD_SCALE
        score = jnp.where(admissible[None], score, NEG_INF)
        _, sel = lax.top_k(score, top_k)
        kv_sel = jax.vmap(lambda kv, i: kv[i])(ckv, sel)
        logits = jnp.einsum('bqhd,bqkd->bqhk', q_b, kv_sel).astype(jnp.float32) * ATTN_SCALE
        dist = jnp.abs(t_b[None, :, None] - sel).astype(jnp.float32)
        logits = logits - slopes[None, None, :, None] * dist[:, :, None, :]
        valid = sel < limit[None, :, None]
        logits = jnp.where(valid[:, :, None, :], logits, NEG_INF)
        probs = jax.nn.softmax(logits, axis=-1).astype(kv_sel.dtype)
        return jnp.einsum('bqhk,bqkd->bqhd', probs, kv_sel)

    pos = jnp.arange(S, dtype=jnp.int32).reshape(nb, QBLOCK)
    o = lax.map(attend, (to_blocks(q), to_blocks(qi), to_blocks(widx), pos))
    o = jnp.moveaxis(o, 0, 1).reshape(Bsz, S, N_HEADS, D_LAT)
    return jnp.einsum('bshd,hde->bshe', o, w_uv).reshape(Bsz, S, W_ATTN)


def _ssm_combine(left, right):
    a_l, b_l = left
    a_r, b_r = right
    return (a_r * a_l, a_r * b_l + b_r)


def _s5_branch(u, a_re, a_im, b_re, b_im, c_re, c_im, d_skip, log_step, w_glu, b_glu):
    Bsz, S, _ = u.shape
    f32 = jnp.float32
    uf = u.astype(f32).reshape(Bsz, S, SSM_GROUPS, SSM_GROUP_DIM)
    lam = lax.complex(a_re.astype(f32), a_im.astype(f32))
    step = jnp.exp(log_step.astype(f32))[:, None]
    lam_bar = jnp.exp(lam * step)
    b_mat = lax.complex(b_re.astype(f32), b_im.astype(f32))
    b_bar = ((lam_bar - 1.0) / lam)[:, :, None] * b_mat
    bu = jnp.einsum('bsgp,gnp->bsgn', uf, b_bar)
    a_seq = jnp.broadcast_to(lam_bar, (1, S, SSM_GROUPS, SSM_STATE))
    _, states = lax.associative_scan(_ssm_combine, (a_seq, bu), axis=1)
    c_mat = lax.complex(c_re.astype(f32), c_im.astype(f32))
    y = jnp.real(jnp.einsum('bsgn,gpn->bsgp', states, c_mat))
    y = y + d_skip.astype(f32).reshape(SSM_GROUPS, SSM_GROUP_DIM) * uf
    z = jax.nn.gelu(y.reshape(Bsz, S, W_SSM))
    out = z * jax.nn.sigmoid(z @ w_glu.astype(f32) + b_glu.astype(f32))
    return out.astype(u.dtype)


def _pool_branch(u, w_pool, pool_scale):
    Bsz, S, _ = u.shape
    f32 = jnp.float32
    uf = u.astype(f32).reshape(Bsz, S, POOL_GROUPS, POOL_GROUP_DIM)
    csum = jnp.pad(jnp.cumsum(uf, axis=1), ((0, 0), (1, 0), (0, 0), (0, 0)))
    t = jnp.arange(S)
    means = []
    for g, win in enumerate(POOL_WINDOWS):
        cs_g = csum[:, :, g]
        lo = jnp.maximum(t + 1 - win, 0)
        count = jnp.minimum(t + 1, win).astype(f32)[None, :, None]
        means.append((cs_g[:, 1:] - cs_g[:, lo]) / count)
    pooled = jnp.stack(means, axis=2) - uf
    y = jnp.einsum('bsgc,gcd->bsgd', pooled, w_pool.astype(f32))
    y = y * pool_scale.astype(f32).reshape(POOL_GROUPS, POOL_GROUP_DIM)
    return y.reshape(Bsz, S, W_POOL).astype(u.dtype)


def _token_mixer(h, w_in, g_cq, w_uq, w_qi, g_ckv, w_uv, a_re, a_im, b_re, b_im, c_re, c_im,
                 d_skip, log_step, w_glu, b_glu, w_pool, pool_scale, p_a, p_b, p_c, w_out):
    Bsz, S, _ = h.shape
    z = h @ w_in
    splits = [int(v) for v in np.cumsum(IN_SPLITS)[:-1]]
    cq, ckv, kidx, widx, u_ssm, u_pool, gate_logits = jnp.split(z, splits, axis=-1)
    y_attn = _dsa_branch(cq, ckv, kidx, widx, g_cq, w_uq, w_qi, g_ckv, w_uv)
    y_ssm = _s5_branch(u_ssm, a_re, a_im, b_re, b_im, c_re, c_im, d_skip, log_step, w_glu, b_glu)
    y_pool = _pool_branch(u_pool, w_pool, pool_scale)
    gates = jax.nn.sigmoid(gate_logits).reshape(Bsz, S, N_BRANCH, D_MODEL)
    merged = (gates[:, :, 0] * (y_attn @ p_a)
              + gates[:, :, 1] * (y_ssm @ p_b)
              + gates[:, :, 2] * (y_pool @ p_c))
    return merged @ w_out


def _conv_gated_ffn(h, w_up, conv_w, conv_b, w_down):
    S = h.shape[1]
    u = h @ w_up
    up = jnp.pad(u, ((0, 0), (CONV_WIDTH - 1, 0), (0, 0)))
    acc = conv_b + conv_w[0] * up[:, 0:S]
    for k in range(1, CONV_WIDTH):
        acc = acc + conv_w[k] * up[:, k:k + S]
    g, v = jnp.split(acc, 2, axis=-1)
    return (jax.nn.gelu(g) * v) @ w_down


def setup_inputs(seed: int = 0) -> dict:
    key = jax.random.key(seed)
    ks = iter(jax.random.split(key, 40))
    f32 = jnp.float32
    L = DEPTH

    def nrm(shape, scale):
        return jax.random.normal(next(ks), shape, f32) * scale

    def gain(shape):
        return 1.0 + nrm(shape, 0.02)

    n_idx = jnp.arange(SSM_STATE, dtype=f32)
    return {
        'x': nrm((BATCH, SEQ, D_MODEL), 1.0),
        'c': nrm((BATCH, D_MODEL), 1.0),
        'mod_w': nrm((L, D_MODEL, 6 * D_MODEL), 0.5 * D_MODEL ** -0.5),
        'mod_b': nrm((L, 6 * D_MODEL), 0.02),
        'mix_pre_g': gain((L, D_MODEL)),
        'mix_post_g': gain((L, D_MODEL)),
        'ffn_pre_g': gain((L, D_MODEL)),
        'ffn_post_g': gain((L, D_MODEL)),
        'w_in': nrm((L, D_MODEL, N_IN), D_MODEL ** -0.5),
        'g_cq': gain((L, D_QLAT)),
        'w_uq': nrm((L, D_QLAT, N_HEADS, D_LAT), D_QLAT ** -0.5),
        'w_qi': nrm((L, D_QLAT, IDX_HEADS, IDX_DIM), D_QLAT ** -0.5),
        'g_ckv': gain((L, D_LAT)),
        'w_uv': nrm((L, N_HEADS, D_LAT, D_VHEAD), D_LAT ** -0.5),
        'a_re': -0.5 + nrm((L, SSM_GROUPS, SSM_STATE), 0.01),
        'a_im': math.pi * n_idx + nrm((L, SSM_GROUPS, SSM_STATE), 0.01),
        'b_re': nrm((L, SSM_GROUPS, SSM_STATE, SSM_GROUP_DIM), (2 * SSM_GROUP_DIM) ** -0.5),
        'b_im': nrm((L, SSM_GROUPS, SSM_STATE, SSM_GROUP_DIM), (2 * SSM_GROUP_DIM) ** -0.5),
        'c_re': nrm((L, SSM_GROUPS, SSM_GROUP_DIM, SSM_STATE), (2 * SSM_STATE) ** -0.5 * 4.0),
        'c_im': nrm((L, SSM_GROUPS, SSM_GROUP_DIM, SSM_STATE), (2 * SSM_STATE) ** -0.5 * 4.0),
        'd_skip': nrm((L, W_SSM), 1.0),
        'log_step': jax.random.uniform(next(ks), (L, SSM_GROUPS), f32, math.log(1e-3), math.log(1e-1)),
        'w_glu': nrm((L, W_SSM, W_SSM), W_SSM ** -0.5),
        'b_glu': nrm((L, W_SSM), 0.01),
        'w_pool': nrm((L, POOL_GROUPS, POOL_GROUP_DIM, POOL_GROUP_DIM), POOL_GROUP_DIM ** -0.5),
        'pool_scale': 1.0 + nrm((L, W_POOL), 0.1),
        'p_a': nrm((L, W_ATTN, D_MODEL), W_ATTN ** -0.5),
        'p_b': nrm((L, W_SSM, D_MODEL), W_SSM ** -0.5),
        'p_c': nrm((L, W_POOL, D_MODEL), W_POOL ** -0.5),
        'w_out': nrm((L, D_MODEL, D_MODEL), D_MODEL ** -0.5),
        'w_up': nrm((L, D_MODEL, 2 * D_FF), D_MODEL ** -0.5),
        'conv_w': nrm((L, CONV_WIDTH, 2 * D_FF), CONV_WIDTH ** -0.5),
        'conv_b': nrm((L, 2 * D_FF), 0.01),
        'w_down': nrm((L, D_FF, D_MODEL), D_FF ** -0.5),
    }


def reference(x, c, mod_w, mod_b, mix_pre_g, mix_post_g, ffn_pre_g, ffn_post_g, w_in,
              g_cq, w_uq, w_qi, g_ckv, w_uv, a_re, a_im, b_re, b_im, c_re, c_im, d_skip,
              log_step, w_glu, b_glu, w_pool, pool_scale, p_a, p_b, p_c, w_out,
              w_up, conv_w, conv_b, w_down):
    cond = jax.nn.silu(c)
    for l in range(DEPTH):
        mod = (cond @ mod_w[l] + mod_b[l])[:, None, :]
        sh_m, sc_m, gt_m, sh_f, sc_f, gt_f = jnp.split(mod, 6, axis=-1)
        h = _rmsnorm(x, mix_pre_g[l]) * (1.0 + sc_m) + sh_m
        y = _token_mixer(h, w_in[l], g_cq[l], w_uq[l], w_qi[l], g_ckv[l], w_uv[l],
                         a_re[l], a_im[l], b_re[l], b_im[l], c_re[l], c_im[l], d_skip[l],
                         log_step[l], w_glu[l], b_glu[l], w_pool[l], pool_scale[l],
                         p_a[l], p_b[l], p_c[l], w_out[l])
        x = x + gt_m * _rmsnorm(y, mix_post_g[l])
        h = _rmsnorm(x, ffn_pre_g[l]) * (1.0 + sc_f) + sh_f
        y = _conv_gated_ffn(h, w_up[l], conv_w[l], conv_b[l], w_down[l])
        x = x + gt_f * _rmsnorm(y, ffn_post_g[l])
    return x
```

```python
import types
from functools import partial
import numpy as np
import ml_dtypes
from contextlib import ExitStack
import concourse.bass as bass
import concourse.mybir as mybir
from concourse.bass_utils import run_bass_kernel_spmd

F32 = mybir.dt.float32
BF16 = mybir.dt.bfloat16
AF = mybir.ActivationFunctionType
ALU = mybir.AluOpType
AX = mybir.AxisListType

D = 1024
SEQ = 2048
NT = 16
NL = 2
NB = 4
EPS = 1e-6
ATT_SC = 128.0 ** -0.5
IDXW = (32.0 ** -0.5) * (8.0 ** -0.5)
NIT = 18
NEG = -1.0e30
POOL_WINS = (2, 4, 8, 16)


_BASE = {}
_UNIQ = [0]


def _nm(t):
    return _BASE[t.name]


def _freeze(fn):
    if fn.__closure__ is None:
        return fn
    cells = []
    for c in fn.__closure__:
        try:
            cells.append(types.CellType(c.cell_contents))
        except ValueError:
            cells.append(c)
    return types.FunctionType(fn.__code__, fn.__globals__, fn.__name__, fn.__defaults__, tuple(cells))


class Sched:
    ENG = ("pe", "act", "dve", "pool", "sp")

    def __init__(self, nc, es, n_dma_sems=12):
        self.nc = nc
        self.sems = {e: es.enter_context(nc.semaphore("s_" + e)) for e in self.ENG}
        self.cnt = {e: 0 for e in self.ENG}
        self.dma_sems = [es.enter_context(nc.semaphore("d%d" % i)) for i in range(n_dma_sems)]
        self.dma_cnt = [0] * n_dma_sems
        self.dma_rr = 0
        self.prog = {e: [] for e in self.ENG}
        self.seen = {e: {} for e in self.ENG}
        self.lastw = {}
        self.readers = {}
        self.ninst = 0
        self.excl = set()

    def _sem_of(self, key):
        if isinstance(key, tuple):
            return self.dma_sems[key[1]], 16
        return self.sems[key], 1

    def _needed(self, eng, deps):
        best = {}
        for (key, val) in deps:
            if key == eng and eng in ("pe", "sp"):
                continue
            if self.seen[eng].get(key, 0) >= val:
                continue
            if best.get(key, 0) < val:
                best[key] = val
        waits = []
        for key, val in best.items():
            self.seen[eng][key] = val
            sem, step = self._sem_of(key)
            waits.append((sem, val * step))
        return waits

    def _deps(self, reads, writes):
        deps = []
        for r in reads:
            t = self.lastw.get(r)
            if t is not None:
                deps.append(t)
        for w in writes:
            t = self.lastw.get(w)
            if t is not None:
                deps.append(t)
            rd = self.readers.get(w)
            if rd:
                deps.extend(rd.items())
        return deps

    def _commit(self, tok, reads, writes):
        for w in writes:
            self.lastw[w] = tok
            self.readers[w] = {}
        for r in reads:
            d = self.readers.setdefault(r, {})
            if d.get(tok[0], 0) < tok[1]:
                d[tok[0]] = tok[1]

    def op(self, eng, fn, reads=(), writes=()):
        fn = _freeze(fn)
        if self.excl:
            ex = [r for r in reads if r in self.excl]
            if ex:
                writes = list(writes) + [r for r in ex if r not in writes]
        waits = self._needed(eng, self._deps(reads, writes))
        self.cnt[eng] += 1
        tok = (eng, self.cnt[eng])
        sem = self.sems[eng]

        def emit(e, waits=waits, fn=fn, sem=sem):
            for (s, v) in waits:
                e.wait_ge(s, v)
            fn(e).then_inc(sem, 1)

        self.prog[eng].append(emit)
        self._commit(tok, reads, writes)
        self.ninst += 1
        return tok

    def dma(self, out, in_, reads=(), writes=(), q="sp", **kw):
        k = self.dma_rr
        self.dma_rr = (k + 1) % len(self.dma_sems)
        deps = self._deps(reads, writes)
        if self.dma_cnt[k] > 0:
            deps.append((("dma", k), self.dma_cnt[k]))
        waits = self._needed(q, deps)
        self.dma_cnt[k] += 1
        tok = (("dma", k), self.dma_cnt[k])
        sem = self.dma_sems[k]

        def emit(e, waits=waits, sem=sem, out=out, in_=in_, kw=kw):
            for (s, v) in waits:
                e.wait_ge(s, v)
            e.dma_start(out=out, in_=in_, **kw).then_inc(sem, 16)

        self.prog[q].append(emit)
        self._commit(tok, reads, writes)
        self.ninst += 1
        return tok

    def end_phase(self, q="sp"):
        waits = [(self.dma_sems[k], 16 * c) for k, c in enumerate(self.dma_cnt) if c > 0]
        waits += [(self.sems[e], self.cnt[e]) for e in self.ENG if self.cnt[e] > 0 and e != q]

        def emit(e, waits=waits):
            for (s, v) in waits:
                e.wait_ge(s, v)

        self.prog[q].append(emit)
        nc = self.nc
        prog = self.prog
        with nc.Block() as block:
            if prog["pe"]:
                @block.tensor
                def _(e):
                    for f in prog["pe"]:
                        f(e)
            if prog["act"]:
                @block.scalar
                def _(e):
                    for f in prog["act"]:
                        f(e)
            if prog["dve"]:
                @block.vector
                def _(e):
                    for f in prog["dve"]:
                        f(e)
            if prog["pool"]:
                @block.gpsimd
                def _(e):
                    for f in prog["pool"]:
                        f(e)
            if prog["sp"]:
                @block.sync
                def _(e):
                    for f in prog["sp"]:
                        f(e)
        self.prog = {e: [] for e in self.ENG}


def _consts():
    bf = ml_dtypes.bfloat16
    c = {}
    c["identf"] = np.eye(128, dtype=np.float32)
    c["identb"] = np.eye(128, dtype=np.float32).astype(bf)
    c["i4b"] = np.tile(np.eye(128, dtype=np.float32), (1, 4)).astype(bf)
    c["onesb"] = np.ones((128, 128), np.float32).astype(bf)
    c["onesf"] = np.ones((128, 128), np.float32)
    s = np.arange(128)
    c["triub"] = (s[:, None] <= s[None, :]).astype(np.float32).astype(bf)
    m0 = np.zeros((128, 128), np.float32)
    m0[:64, 64:] = NEG
    c["m0"] = m0
    slopes = 2.0 ** (-8.0 * np.arange(1, 9) / 8.0)
    altd = np.zeros((128, 8, 128), np.float32)
    for h in range(8):
        altd[:, h, :] = -slopes[h] * np.abs(s[None, :] - s[:, None])
    c["altd"] = altd.reshape(128, 1024).astype(bf)
    at3 = np.zeros((3, 16, 128), np.float32)
    at3[0] = 1.0
    at3[1] = (128.0 * np.arange(16))[:, None]
    at3[2] = s[None, :].astype(np.float32)
    c["at3"] = at3.reshape(3, 2048).astype(bf)
    bt3 = np.zeros((3, 8, 128), np.float32)
    for h in range(8):
        bt3[0, h] = -slopes[h] * s
        bt3[1, h] = -slopes[h]
        bt3[2, h] = slopes[h]
    c["bt3"] = bt3.reshape(3, 1024).astype(bf)
    sg = np.ones((128, 2), np.float32)
    sg[:64, 0] = -1.0
    sg[64:, 1] = -1.0
    c["sgn"] = sg
    bm = np.zeros((128, 8), np.float32)
    for p in range(128):
        bm[p, p // 16] = 1.0
    c["bmask"] = bm
    m2 = np.zeros((128, 16, 8), np.float32)
    for g in range(16):
        m2[:, g, g % 8] = 1.0
    c["mask2"] = m2
    c["pow2"] = np.tile((2.0 ** -(np.arange(NIT + 1) + 1.0))[None, :], (128, 1)).astype(np.float32)
    c["invt"] = np.tile((1.0 / (np.arange(16) + 1.0))[None, :], (128, 1)).astype(np.float32)
    return c


def _layout_weights(inp):
    f = lambda a: np.ascontiguousarray(a, dtype=np.float32)
    w = {}
    w["modw"] = f(inp["mod_w"].reshape(NL, 8, 128, 12, 512).transpose(0, 3, 2, 1, 4))
    w["modb"] = f(inp["mod_b"].reshape(NL, 48, 128).transpose(0, 2, 1))
    gv = np.stack([inp["mix_pre_g"], inp["mix_post_g"], inp["ffn_pre_g"], inp["ffn_post_g"]], axis=1)
    w["gvec"] = f(gv.reshape(NL, 4, 8, 128).transpose(0, 1, 3, 2))
    win = inp["w_in"].reshape(NL, 8, 128, 4008)
    w["w1"] = f(win[:, :, :, :936].transpose(0, 2, 1, 3))
    w["wg"] = f(win[:, :, :, 936:].reshape(NL, 8, 128, 24, 128).transpose(0, 3, 2, 1, 4))
    w["gcq"] = f(inp["g_cq"].reshape(NL, 2, 128).transpose(0, 2, 1))
    w["gckv"] = f(inp["g_ckv"])
    w["wuq"] = f(inp["w_uq"].reshape(NL, 2, 128, 1024).transpose(0, 2, 1, 3))
    w["wqi"] = f(inp["w_qi"].reshape(NL, 2, 128, 256).transpose(0, 2, 1, 3))
    w["wuv"] = f(inp["w_uv"].transpose(0, 2, 1, 3))
    dup = lambda a: np.concatenate([a, a], axis=1)
    w["are"] = f(dup(inp["a_re"].transpose(0, 2, 1)))
    w["aim"] = f(dup(inp["a_im"].transpose(0, 2, 1)))
    w["lstep"] = f(np.broadcast_to(inp["log_step"][:, None, :], (NL, 128, 16)))
    w["bre"] = f(inp["b_re"].transpose(0, 2, 1, 3))
    w["bim"] = f(inp["b_im"].transpose(0, 2, 1, 3))
    w["cre"] = f(inp["c_re"].transpose(0, 3, 1, 2))
    w["cim"] = f(inp["c_im"].transpose(0, 3, 1, 2))
    w["dskip"] = f(inp["d_skip"].reshape(NL, 2, 128).transpose(0, 2, 1))
    w["wglu"] = f(inp["w_glu"].reshape(NL, 2, 128, 256).transpose(0, 2, 1, 3))
    w["bglu"] = f(inp["b_glu"].reshape(NL, 2, 128).transpose(0, 2, 1))
    w["wpool"] = f(inp["w_pool"])
    w["pscale"] = f(inp["pool_scale"].reshape(NL, 2, 128).transpose(0, 2, 1))
    w["pa"] = f(inp["p_a"].reshape(NL, 4, 128, 1024).transpose(0, 2, 1, 3))
    w["pb"] = f(inp["p_b"].reshape(NL, 2, 128, 1024).transpose(0, 2, 1, 3))
    w["pc"] = f(inp["p_c"].reshape(NL, 2, 128, 1024).transpose(0, 2, 1, 3))
    w["wout"] = f(inp["w_out"].reshape(NL, 8, 128, 1024).transpose(0, 2, 1, 3))
    w["wup"] = f(inp["w_up"].reshape(NL, 8, 128, 32, 128).transpose(0, 3, 2, 1, 4))
    w["convw"] = f(inp["conv_w"].reshape(NL, 3, 32, 128).transpose(0, 3, 1, 2))
    w["convb"] = f(inp["conv_b"].reshape(NL, 32, 128).transpose(0, 2, 1))
    w["wdown"] = f(inp["w_down"].reshape(NL, 16, 128, 1024).transpose(0, 2, 1, 3))
    return w


def build_nc(shapes, nb=NB, nl=NL, do_mix=True, do_ffn=True, att=True, ssm=True, pool=True):
    nc = bass.Bass("TRN2", target_bir_lowering=False)
    DBG["nc"] = nc
    DBG["outs"] = {}
    dr = {}
    for name, (shape, dt) in shapes.items():
        dr[name] = nc.dram_tensor("d_" + name, list(shape), dt, kind="ExternalInput").ap()
    out = nc.dram_tensor("out", [nb, SEQ, D], F32, kind="ExternalOutput").ap()
    TABW = 2 * 1024 + 2 * 2048 + 64
    tab = nc.dram_tensor("tabscratch", [NL, 128, TABW], F32, kind="Internal").ap()
    tabb = nc.dram_tensor("tabscratchb", [NL, 128, 2048 + 2048], BF16, kind="Internal").ap()

    with ExitStack() as es:
        S = Sched(nc, es)

        def sb(st, name, shape, dt):
            _UNIQ[0] += 1
            actual = "%s__%d" % (name, _UNIQ[0])
            _BASE[actual] = name
            return st.enter_context(nc.sbuf_tensor(actual, shape, dt))

        def ps(st, name, shape, dt):
            S.excl.add(name)
            _UNIQ[0] += 1
            actual = "%s__%d" % (name, _UNIQ[0])
            _BASE[actual] = name
            return st.enter_context(nc.psum_tensor(actual, shape, dt))

        X = sb(es, "X", [128, NT, D], F32)
        hT = sb(es, "hT", [128, 8, SEQ], BF16)
        identf = sb(es, "identf", [128, 128], F32)
        identb = sb(es, "identb", [128, 128], BF16)
        onesb = sb(es, "onesb", [128, 128], BF16)
        onesf = sb(es, "onesf", [128, 128], F32)
        cols = sb(es, "cols", [128, NL, 6, 8, NB], F32)
        GG = sb(es, "GG", [128, D], F32)
        for nm, t in (("identf", identf), ("identb", identb), ("onesb", onesb), ("onesf", onesf)):
            S.dma(t[:], dr[nm], writes=[nm])

        with ExitStack() as ph:
            condT = sb(ph, "condT", [128, 8, NB], F32)
            modT = sb(ph, "modT", [128, 48, NB], F32)
            modb = sb(ph, "modb", [128, 48], F32)
            gvec = sb(ph, "gvec", [128, 4, 8], F32)
            mw = [sb(ph, "mw%d" % i, [128, 8, 512], F32) for i in range(2)]
            pm = ps(ph, "pm", [128, 512], F32)
            S.dma(condT[:], dr["cT"], writes=["condT"])
            S.op("act", lambda e: e.activation(out=condT[:], in_=condT[:], func=AF.Silu),
                 reads=["condT"], writes=["condT"])
            for l in range(nl):
                S.dma(modb[:], dr["modb"][l], writes=["modb"])
                S.dma(gvec[:], dr["gvec"][l].rearrange("w p c -> p w c"), writes=["gvec"])
                for j in range(12):
                    buf = mw[j % 2]
                    S.dma(buf[:], dr["modw"][l, j], writes=["mw%d" % (j % 2)])
                    for f4 in range(4):
                        fc = j * 4 + f4
                        for k in range(8):
                            S.op("pe", lambda e, buf=buf, f4=f4, k=k, fc=fc: e.matmul(
                                pm[:, fc * NB:(fc + 1) * NB], lhsT=buf[:, k, f4 * 128:(f4 + 1) * 128],
                                rhs=condT[:, k, :], start=(k == 0), stop=(k == 7)),
                                reads=["mw%d" % (j % 2), "condT"], writes=["pm"])
                S.op("dve", lambda e: e.tensor_tensor(
                    out=modT[:], in0=pm[:, 0:48 * NB].rearrange("p (f b) -> p f b", b=NB),
                    in1=modb[:].unsqueeze(2).to_broadcast([128, 48, NB]), op=ALU.add),
                    reads=["pm", "modb"], writes=["modT"])
                def gb(w):
                    return gvec[:, w, :].unsqueeze(2).to_broadcast([128, 8, NB])
                for (dst, sc0, gw) in ((0, 8, 0), (3, 32, 2)):
                    S.op("dve", lambda e, dst=dst, sc0=sc0, gw=gw: e.scalar_tensor_tensor(
                        out=cols[:, l, dst], in0=modT[:, sc0:sc0 + 8, :], scalar=1.0, in1=gb(gw),
                        op0=ALU.add, op1=ALU.mult), reads=["modT", "gvec"], writes=["cols"])
                for (dst, sh0) in ((1, 0), (4, 24)):
                    S.op("dve", lambda e, dst=dst, sh0=sh0: e.tensor_copy(
                        out=cols[:, l, dst], in_=modT[:, sh0:sh0 + 8, :]), reads=["modT"], writes=["cols"])
                for (dst, g0, gw) in ((2, 16, 1), (5, 40, 3)):
                    S.op("dve", lambda e, dst=dst, g0=g0, gw=gw: e.tensor_tensor(
                        out=cols[:, l, dst], in0=modT[:, g0:g0 + 8, :], in1=gb(gw), op=ALU.mult),
                        reads=["modT", "gvec"], writes=["cols"])
            dbg(S, "cols", cols[:].rearrange("p l s c b -> p (l s c b)"), [128, NL * 6 * 8 * NB], F32, ["cols"])
            S.end_phase()

        def rstd_from_ss(ss, n, width):
            S.op("dve", lambda e: e.tensor_scalar(out=ss[:, 0:width], in0=ss[:, 0:width], scalar1=1.0 / n,
                                                  scalar2=EPS, op0=ALU.mult, op1=ALU.add),
                 reads=[_nm(ss)], writes=[_nm(ss)])
            S.op("act", lambda e: e.activation(out=ss[:, 0:width], in_=ss[:, 0:width], func=AF.Sqrt),
                 reads=[_nm(ss)], writes=[_nm(ss)])
            S.op("dve", lambda e: e.reciprocal(out=ss[:, 0:width], in_=ss[:, 0:width]),
                 reads=[_nm(ss)], writes=[_nm(ss)])

        def norm_to_hT(ph, l, b, ca, cb):
            ss = sb(ph, "n_ss", [128, NT], F32)
            junk = sb(ph, "n_junk", [128, D], BF16)
            xn = [sb(ph, "n_xn%d" % i, [128, D], BF16) for i in range(2)]
            pt = [ps(ph, "n_pt%d" % i, [128, 1024], BF16) for i in range(2)]
            for t in range(NT):
                S.op("act", lambda e, t=t: e.activation(out=junk[:], in_=X[:, t, :], func=AF.Square,
                                                        accum_out=ss[:, t:t + 1]),
                     reads=[("X", t)], writes=["n_junk", "n_ss"])
            rstd_from_ss(ss, float(D), NT)
            for t in range(NT):
                xb = xn[t % 2]
                pp = pt[t % 2]
                S.op("dve", lambda e, t=t, xb=xb: e.tensor_scalar(out=xb[:], in0=X[:, t, :],
                                                               scalar1=ss[:, t:t + 1], scalar2=None, op0=ALU.mult),
                     reads=[("X", t), "n_ss"], writes=[_nm(xb)])
                for c in range(8):
                    S.op("pe", lambda e, c=c, xb=xb, pp=pp: e.transpose(
                        out=pp[:, c * 128:(c + 1) * 128], in_=xb[:, c * 128:(c + 1) * 128], identity=identb[:]),
                        reads=[_nm(xb), "identb"], writes=[_nm(pp)])
                for c in range(8):
                    eng = "act" if c % 2 else "dve"
                    if eng == "dve":
                        S.op("dve", lambda e, c=c, t=t, pp=pp: e.tensor_scalar(
                            out=hT[:, c, t * 128:(t + 1) * 128], in0=pp[:, c * 128:(c + 1) * 128],
                            scalar1=cols[:, l, ca, c, b:b + 1], scalar2=cols[:, l, cb, c, b:b + 1],
                            op0=ALU.mult, op1=ALU.add), reads=[_nm(pp), "cols"], writes=[("hT", t)])
                    else:
                        S.op("act", lambda e, c=c, t=t, pp=pp: e.activation(
                            out=hT[:, c, t * 128:(t + 1) * 128], in_=pp[:, c * 128:(c + 1) * 128],
                            func=AF.Identity, scale=cols[:, l, ca, c, b:b + 1], bias=cols[:, l, cb, c, b:b + 1]),
                            reads=[_nm(pp), "cols"], writes=[("hT", t)])

        def make_GG(ph, l, b, cg):
            dg = sb(ph, "gg_diag", [128, 128], F32)
            pg = [ps(ph, "gg_ps%d" % i, [128, 512], F32) for i in range(2)]
            for c in range(8):
                S.op("dve", lambda e, c=c: e.tensor_scalar(out=dg[:], in0=identf[:], scalar1=cols[:, l, cg, c, b:b + 1],
                                                        scalar2=None, op0=ALU.mult),
                     reads=["identf", "cols"], writes=["gg_diag"])
                S.op("pe", lambda e, c=c: e.matmul(pg[c // 4][:, (c % 4) * 128:(c % 4 + 1) * 128], lhsT=onesf[:],
                                                   rhs=dg[:], start=True, stop=True),
                     reads=["gg_diag", "onesf"], writes=[_nm(pg[c // 4])])
            for i in range(2):
                S.op("act", lambda e, i=i: e.copy(out=GG[:, i * 512:(i + 1) * 512], in_=pg[i][:]),
                     reads=[_nm(pg[i])], writes=["GG"])

        def resid_update(ph_tiles, t, py):
            ssy, junk, tmp = ph_tiles
            for i in range(2):
                S.op("act", lambda e, i=i: e.activation(out=junk[:, 0:512], in_=py[i][:], func=AF.Square,
                                                        accum_out=ssy[:, i:i + 1]),
                     reads=[_nm(py[i])], writes=[_nm(junk), _nm(ssy)])
            S.op("dve", lambda e: e.tensor_tensor(out=ssy[:, 2:3], in0=ssy[:, 0:1], in1=ssy[:, 1:2], op=ALU.add),
                 reads=[_nm(ssy)], writes=[_nm(ssy)])
            S.op("dve", lambda e: e.tensor_scalar(out=ssy[:, 2:3], in0=ssy[:, 2:3], scalar1=1.0 / D, scalar2=EPS,
                                                  op0=ALU.mult, op1=ALU.add), reads=[_nm(ssy)], writes=[_nm(ssy)])
            S.op("act", lambda e: e.activation(out=ssy[:, 2:3], in_=ssy[:, 2:3], func=AF.Sqrt),
                 reads=[_nm(ssy)], writes=[_nm(ssy)])
            S.op("dve", lambda e: e.reciprocal(out=ssy[:, 2:3], in_=ssy[:, 2:3]), reads=[_nm(ssy)], writes=[_nm(ssy)])
            for i in range(2):
                S.op("dve", lambda e, i=i: e.scalar_tensor_tensor(
                    out=tmp[:, i * 512:(i + 1) * 512], in0=py[i][:], scalar=ssy[:, 2:3],
                    in1=GG[:, i * 512:(i + 1) * 512], op0=ALU.mult, op1=ALU.mult),
                    reads=[_nm(py[i]), _nm(ssy), "GG"], writes=[_nm(tmp)])
            if t == 8:
                dbg(S, "r_ssy", ssy[:], [128, 4], F32, [_nm(ssy)])
                dbg(S, "r_tmp", tmp[:], [128, D], F32, [_nm(tmp)])
            S.op("dve", lambda e: e.tensor_tensor(out=X[:, t, :], in0=X[:, t, :], in1=tmp[:], op=ALU.add),
                 reads=[_nm(tmp), ("X", t)], writes=[("X", t)])

        if do_mix and ssm:
            for l in range(nl):
                ssm_tables(nc, S, dr, l, tab, tabb, sb, ps, identf)

        for b in range(nb):
            with ExitStack() as ph:
                for t in range(NT):
                    S.dma(X[:, t, :], dr["x"][b, t * 128:(t + 1) * 128, :], writes=[("X", t)])
                S.end_phase()
            for l in range(nl):
                if do_mix:
                    mixer(nc, S, dr, l, b, X, hT, GG, cols, tab, tabb, sb, ps, norm_to_hT, make_GG, resid_update,
                          rstd_from_ss, identb, identf, onesb, att, ssm, pool)
                if do_ffn:
                    ffn(nc, S, dr, l, b, X, hT, GG, sb, ps, norm_to_hT, make_GG, resid_update)
            with ExitStack() as ph:
                for t in range(NT):
                    S.dma(out[b, t * 128:(t + 1) * 128, :], X[:, t, :], reads=[("X", t)])
                S.end_phase()
        print("instructions:", S.ninst)
    return nc


DBG = {"on": False, "nc": None, "outs": {}}


def dbg(S, name, ap, shape, dt, reads, st=None):
    if not DBG["on"] or name in DBG["outs"]:
        return
    nc = DBG["nc"]
    d = nc.dram_tensor("dbg_" + name, list(shape), F32, kind="ExternalOutput").ap()
    DBG["outs"][name] = d
    if dt != F32:
        tmpf = st.enter_context(nc.sbuf_tensor("dbgt_" + name, list(shape), F32))
        S.op("dve", lambda e: e.tensor_copy(out=tmpf[:], in_=ap), reads=reads, writes=["dbgt_" + name])
        S.dma(d, tmpf[:], reads=["dbgt_" + name])
    else:
        S.dma(d, ap, reads=reads)


def cast_load(S, dst, src, wname):
    S.dma(dst, src, writes=[wname], q="pool")


def ffn(nc, S, dr, l, b, X, hT, GG, sb, ps, norm_to_hT, make_GG, resid_update):
    with ExitStack() as ph:
        norm_to_hT(ph, l, b, 3, 4)
        make_GG(ph, l, b, 5)
        dbg(S, "f_hT", hT[:].rearrange("p c t -> p (c t)"), [128, 8 * SEQ], BF16, [("hT", t) for t in range(NT)], ph)
        dbg(S, "f_GG", GG[:], [128, D], F32, ["GG"])
        S.end_phase()
    with ExitStack() as ph:
        wd = sb(ph, "f_wd", [128, 16, D], BF16)
        m = sb(ph, "f_m", [128, 16, 1024], BF16)
        cw = sb(ph, "f_cw", [128, 3, 32], F32)
        cbt = sb(ph, "f_cb", [128, 32], F32)
        halo = sb(ph, "f_halo", [128, 32, 2], F32)
        wu = [sb(ph, "f_wu%d" % i, [128, 8, 128], BF16) for i in range(3)]
        ub = [sb(ph, "f_ub%d" % i, [128, 1026], F32) for i in range(2)]
        acc = [sb(ph, "f_acc%d" % i, [128, 1024], F32) for i in range(2)]
        gl = sb(ph, "f_gl", [128, 1024], F32)
        ssy = sb(ph, "f_ssy", [128, 4], F32)
        junk = sb(ph, "f_junk", [128, 512], BF16)
        tmp = sb(ph, "f_tmp", [128, D], F32)
        pu = [ps(ph, "f_pu%d" % i, [128, 512], F32) for i in range(4)]
        py = [ps(ph, "f_py%d" % i, [128, 512], F32) for i in range(4)]
        S.dma(cw[:], dr["convw"][l], writes=["f_cw"])
        S.dma(cbt[:], dr["convb"][l], writes=["f_cb"])
        for k in range(16):
            cast_load(S, wd[:, k, :], dr["wdown"][l][:, k, :], ("f_wd", k))
        nload = 0
        for half in range(2):
            t0 = half * 1024
            for cc in range(16):
                for gv in range(2):
                    j = gv * 16 + cc
                    wb = wu[nload % 3]
                    nload += 1
                    cast_load(S, wb[:], dr["wup"][l, j], _nm(wb))
                    pp = pu[(gv * 2):(gv * 2 + 2)]
                    for s2 in range(2):
                        for k in range(8):
                            S.op("pe", lambda e, wb=wb, k=k, s2=s2, pp=pp: e.matmul(
                                pp[s2][:], lhsT=wb[:, k, :], rhs=hT[:, k, t0 + s2 * 512:t0 + (s2 + 1) * 512],
                                start=(k == 0), stop=(k == 7)),
                                reads=[_nm(wb)] + [("hT", (t0 + s2 * 512) // 128 + q) for q in range(4)],
                                writes=[_nm(pp[s2])])
                    u = ub[gv]
                    a = acc[gv]
                    dbg(S, "f_wb", wb[:].rearrange("p k n -> p (k n)"), [128, 1024], BF16, [_nm(wb)], ph)
                    if half == 0:
                        S.op("dve", lambda e, u=u: e.memset(u[:, 0:2], 0.0), writes=[_nm(u)])
                    else:
                        S.op("dve", lambda e, u=u, j=j: e.tensor_copy(out=u[:, 0:2], in_=halo[:, j, :]),
                             reads=["f_halo"], writes=[_nm(u)])
                    for s2 in range(2):
                        S.op("act", lambda e, u=u, s2=s2, pp=pp: e.copy(out=u[:, 2 + s2 * 512:2 + (s2 + 1) * 512],
                                                                        in_=pp[s2][:]),
                             reads=[_nm(pp[s2])], writes=[_nm(u)])
                    if half == 0:
                        S.op("dve", lambda e, u=u, j=j: e.tensor_copy(out=halo[:, j, :], in_=u[:, 1024:1026]),
                             reads=[_nm(u)], writes=["f_halo"])
                    dbg(S, "f_u", u[:], [128, 1026], F32, [_nm(u)], ph)
                    for s2 in range(2):
                        S.op("act", lambda e, a=a, s2=s2, pp=pp, j=j: e.activation(
                            out=a[:, s2 * 512:(s2 + 1) * 512], in_=pp[s2][:], func=AF.Identity,
                            scale=cw[:, 2, j:j + 1], bias=cbt[:, j:j + 1]),
                            reads=[_nm(pp[s2]), "f_cw", "f_cb"], writes=[_nm(a)])
                    S.op("dve", lambda e, u=u, a=a, j=j: e.scalar_tensor_tensor(
                        out=a[:], in0=u[:, 1:1025], scalar=cw[:, 1, j:j + 1], in1=a[:], op0=ALU.mult, op1=ALU.add),
                        reads=[_nm(u), "f_cw"], writes=[_nm(a)])
                    S.op("dve", lambda e, u=u, a=a, j=j: e.scalar_tensor_tensor(
                        out=a[:], in0=u[:, 0:1024], scalar=cw[:, 0, j:j + 1], in1=a[:], op0=ALU.mult, op1=ALU.add),
                        reads=[_nm(u), "f_cw"], writes=[_nm(a)])
                dbg(S, "f_acc0", acc[0][:], [128, 1024], F32, [_nm(acc[0])], ph)
                dbg(S, "f_acc1", acc[1][:], [128, 1024], F32, [_nm(acc[1])], ph)
                S.op("act", lambda e: e.activation(out=gl[:], in_=acc[0][:], func=AF.Gelu_apprx_tanh),
                     reads=[_nm(acc[0])], writes=["f_gl"])
                S.op("dve", lambda e, cc=cc: e.tensor_tensor(out=m[:, cc, :], in0=gl[:], in1=acc[1][:], op=ALU.mult),
                     reads=["f_gl", _nm(acc[1])], writes=[("f_m", cc)])
            for tt in range(8):
                t = half * 8 + tt
                pyy = py[(tt % 2) * 2:(tt % 2) * 2 + 2]
                for i in range(2):
                    for k in range(16):
                        S.op("pe", lambda e, i=i, k=k, tt=tt, pyy=pyy: e.matmul(
                            pyy[i][:], lhsT=m[:, k, tt * 128:(tt + 1) * 128], rhs=wd[:, k, i * 512:(i + 1) * 512],
                            start=(k == 0), stop=(k == 15)),
                            reads=[("f_m", k), ("f_wd", k)], writes=[_nm(pyy[i])])
                resid_update((ssy, junk, tmp), t, pyy)
        S.end_phase()


def ssm_tables(nc, S, dr, l, tab, tabb, sb, ps, identf):
    with ExitStack() as ph:
        are = sb(ph, "t_are", [128, 16], F32)
        aim = sb(ph, "t_aim", [128, 16], F32)
        stp = sb(ph, "t_stp", [128, 16], F32)
        th = sb(ph, "t_th", [128, 16], F32)
        r = sb(ph, "t_r", [128, 16], F32)
        sn = sb(ph, "t_sn", [128, 16], F32)
        cs = sb(ph, "t_cs", [128, 16], F32)
        t1 = sb(ph, "t_t1", [128, 16], F32)
        t2 = sb(ph, "t_t2", [128, 16], F32)
        lre = sb(ph, "t_lre", [128, 16], F32)
        lim = sb(ph, "t_lim", [128, 16], F32)
        ire = sb(ph, "t_ire", [128, 16], F32)
        iim = sb(ph, "t_iim", [128, 16], F32)
        pr = sb(ph, "t_pr", [128, 16], F32)
        pi = sb(ph, "t_pi", [128, 16], F32)
        cfr = sb(ph, "t_cfr", [128, 16], F32)
        cfi = sb(ph, "t_cfi", [128, 16], F32)
        Ere = sb(ph, "t_Ere", [128, 16, 256], F32)
        Eim = sb(ph, "t_Eim", [128, 16, 256], F32)
        ta = sb(ph, "t_ta", [128, 16, 128], F32)
        tb = sb(ph, "t_tb", [128, 16, 128], F32)
        sgn = sb(ph, "t_sgn", [128, 2], F32)
        outc = sb(ph, "t_outc", [128, 64], F32)
        halfpi = sb(ph, "t_hpi", [128, 1], F32)
        R = ["t"]

        def dv(fn, w=("t",)):
            S.op("dve", fn, reads=R, writes=list(w))

        S.dma(are[:], dr["are"][l], writes=R)
        S.dma(aim[:], dr["aim"][l], writes=R)
        S.dma(stp[:], dr["lstep"][l], writes=R)
        S.dma(sgn[:], dr["sgn"], writes=R)
        S.op("act", lambda e: e.activation(out=stp[:], in_=stp[:], func=AF.Exp), reads=R, writes=R)
        dv(lambda e: e.tensor_tensor(out=th[:], in0=aim[:], in1=stp[:], op=ALU.mult))
        dv(lambda e: e.tensor_tensor(out=t1[:], in0=are[:], in1=stp[:], op=ALU.mult))
        S.op("act", lambda e: e.activation(out=r[:], in_=t1[:], func=AF.Exp), reads=R, writes=R)
        dv(lambda e: e.memset(halfpi[:], float(np.pi / 2)))
        S.op("act", lambda e: e.activation(out=sn[:], in_=th[:], func=AF.Sin, scale=1.0 / 16), reads=R, writes=R)
        S.op("act", lambda e: e.activation(out=cs[:], in_=th[:], func=AF.Sin, scale=1.0 / 16, bias=halfpi[:]),
             reads=R, writes=R)
        for _ in range(4):
            dv(lambda e: e.tensor_tensor(out=t1[:], in0=sn[:], in1=cs[:], op=ALU.mult))
            dv(lambda e: e.tensor_tensor(out=t2[:], in0=sn[:], in1=sn[:], op=ALU.mult))
            dv(lambda e: e.tensor_scalar(out=sn[:], in0=t1[:], scalar1=2.0, scalar2=None, op0=ALU.mult))
            dv(lambda e: e.tensor_scalar(out=cs[:], in0=t2[:], scalar1=-2.0, scalar2=1.0, op0=ALU.mult, op1=ALU.add))
        dv(lambda e: e.tensor_tensor(out=lre[:], in0=r[:], in1=cs[:], op=ALU.mult))
        dv(lambda e: e.tensor_tensor(out=lim[:], in0=r[:], in1=sn[:], op=ALU.mult))
        dv(lambda e: e.reciprocal(out=t1[:], in_=r[:]))
        dv(lambda e: e.tensor_tensor(out=ire[:], in0=t1[:], in1=cs[:], op=ALU.mult))
        dv(lambda e: e.tensor_tensor(out=iim[:], in0=t1[:], in1=sn[:], op=ALU.mult))
        dv(lambda e: e.tensor_scalar(out=iim[:], in0=iim[:], scalar1=-1.0, scalar2=None, op0=ALU.mult))
        dv(lambda e: e.tensor_tensor(out=t1[:], in0=are[:], in1=are[:], op=ALU.mult))
        dv(lambda e: e.tensor_tensor(out=t2[:], in0=aim[:], in1=aim[:], op=ALU.mult))
        dv(lambda e: e.tensor_tensor(out=t1[:], in0=t1[:], in1=t2[:], op=ALU.add))
        dv(lambda e: e.reciprocal(out=t1[:], in_=t1[:]))
        dv(lambda e: e.tensor_scalar(out=t2[:], in0=lre[:], scalar1=-1.0, scalar2=None, op0=ALU.add))
        dv(lambda e: e.tensor_tensor(out=cfr[:], in0=t2[:], in1=are[:], op=ALU.mult))
        dv(lambda e: e.tensor_tensor(out=pr[:], in0=lim[:], in1=aim[:], op=ALU.mult))
        dv(lambda e: e.tensor_tensor(out=cfr[:], in0=cfr[:], in1=pr[:], op=ALU.add))
        dv(lambda e: e.tensor_tensor(out=cfr[:], in0=cfr[:], in1=t1[:], op=ALU.mult))
        dv(lambda e: e.tensor_tensor(out=cfi[:], in0=lim[:], in1=are[:], op=ALU.mult))
        dv(lambda e: e.tensor_tensor(out=pr[:], in0=t2[:], in1=aim[:], op=ALU.mult))
        dv(lambda e: e.tensor_tensor(out=cfi[:], in0=cfi[:], in1=pr[:], op=ALU.subtract))
        dv(lambda e: e.tensor_tensor(out=cfi[:], in0=cfi[:], in1=t1[:], op=ALU.mult))

        def power_table(bre_, bim_, nlev):
            dv(lambda e: e.memset(Ere[:, :, 0:1], 1.0))
            dv(lambda e: e.memset(Eim[:, :, 0:1], 0.0))
            dv(lambda e: e.tensor_copy(out=pr[:], in_=bre_[:]))
            dv(lambda e: e.tensor_copy(out=pi[:], in_=bim_[:]))
            for k in range(nlev):
                n = 1 << k
                prb = pr[:].unsqueeze(2).to_broadcast([128, 16, n])
                pib = pi[:].unsqueeze(2).to_broadcast([128, 16, n])
                dv(lambda e, n=n, prb=prb: e.tensor_tensor(out=ta[:, :, 0:n], in0=Ere[:, :, 0:n], in1=prb, op=ALU.mult))
                dv(lambda e, n=n, pib=pib: e.tensor_tensor(out=tb[:, :, 0:n], in0=Eim[:, :, 0:n], in1=pib, op=ALU.mult))
                dv(lambda e, n=n: e.tensor_tensor(out=Ere[:, :, n:2 * n], in0=ta[:, :, 0:n], in1=tb[:, :, 0:n],
                                                  op=ALU.subtract))
                dv(lambda e, n=n, pib=pib: e.tensor_tensor(out=ta[:, :, 0:n], in0=Ere[:, :, 0:n], in1=pib, op=ALU.mult))
                dv(lambda e, n=n, prb=prb: e.tensor_tensor(out=tb[:, :, 0:n], in0=Eim[:, :, 0:n], in1=prb, op=ALU.mult))
                dv(lambda e, n=n: e.tensor_tensor(out=Eim[:, :, n:2 * n], in0=ta[:, :, 0:n], in1=tb[:, :, 0:n],
                                                  op=ALU.add))
                if k < nlev - 1:
                    dv(lambda e: e.tensor_tensor(out=t1[:], in0=pr[:], in1=pr[:], op=ALU.mult))
                    dv(lambda e: e.tensor_tensor(out=t2[:], in0=pi[:], in1=pi[:], op=ALU.mult))
                    dv(lambda e: e.tensor_tensor(out=pi[:], in0=pr[:], in1=pi[:], op=ALU.mult))
                    dv(lambda e: e.tensor_scalar(out=pi[:], in0=pi[:], scalar1=2.0, scalar2=None, op0=ALU.mult))
                    dv(lambda e: e.tensor_tensor(out=pr[:], in0=t1[:], in1=t2[:], op=ALU.subtract))

        power_table(lre, lim, 8)
        o_t1 = 2048
        o_t2 = 2048 + 2048
        o_c = 2048 + 4096
        S.dma(tab[l][:, o_t1:o_t1 + 2048].rearrange("p (g t) -> p g t", t=128), Ere[:, :, 0:128], reads=R)
        dv(lambda e: e.tensor_scalar(out=ta[:], in0=Eim[:, :, 0:128], scalar1=sgn[:, 0:1], scalar2=None, op0=ALU.mult))
        S.dma(tab[l][:, o_t2:o_t2 + 2048].rearrange("p (g t) -> p g t", t=128), ta[:], reads=R)
        dv(lambda e: e.tensor_copy(out=outc[:, 0:16], in_=Ere[:, :, 128]))
        dv(lambda e: e.tensor_scalar(out=outc[:, 16:32], in0=Eim[:, :, 128], scalar1=sgn[:, 0:1], scalar2=None,
                                     op0=ALU.mult))
        dv(lambda e: e.tensor_scalar(out=outc[:, 32:48], in0=Eim[:, :, 128], scalar1=sgn[:, 1:2], scalar2=None,
                                     op0=ALU.mult))
        dv(lambda e: e.memset(outc[:, 48:64], 0.0))
        S.dma(tab[l][:, o_c:o_c + 64], outc[:], reads=R)
        S.end_phase()
        power_table(ire, iim, 7)
        ptr = [ps(ph, "t_ptr%d" % i, [128, 512], F32) for i in range(4)]
        arai = sb(ph, "t_arai", [128, 2, 16, 64], F32)
        for ri, E in ((0, Ere), (1, Eim)):
            for g in range(16):
                S.op("pe", lambda e, g=g, E=E, ri=ri: e.transpose(
                    out=ptr[ri * 2 + g // 8][:, (g % 8) * 64:(g % 8 + 1) * 64], in_=E[0:64, g, 0:128],
                    identity=identf[0:64, 0:64]), reads=R + ["identf"], writes=["t_ptr"])
            for hh in range(2):
                S.op("act", lambda e, hh=hh, ri=ri: e.copy(
                    out=arai[:, ri, hh * 8:(hh + 1) * 8, :].rearrange("p g n -> p (g n)"), in_=ptr[ri * 2 + hh][:]),
                    reads=["t_ptr"], writes=["t_arai"])
        S.dma(tab[l][:, 0:2048], arai[:].rearrange("p r g n -> p (r g n)"), reads=["t_arai"])
        bre = sb(ph, "t_bre", [64, 16, 16], F32)
        bim = sb(ph, "t_bim", [64, 16, 16], F32)
        Bre = sb(ph, "t_Bre", [64, 16, 16], F32)
        Bim = sb(ph, "t_Bim", [64, 16, 16], F32)
        tq = sb(ph, "t_tq", [64, 16, 16], F32)
        bmask = sb(ph, "t_bmask", [128, 8], F32)
        BT = sb(ph, "t_BT", [128, 2, 128], F32)
        Bblk = sb(ph, "t_Bblk", [128, 2, 8, 128], BF16)
        S.dma(bre[:], dr["bre"][l], writes=R)
        S.dma(bim[:], dr["bim"][l], writes=R)
        S.dma(bmask[:], dr["bmask"], writes=R)
        cfrb = cfr[0:64, :].unsqueeze(2).to_broadcast([64, 16, 16])
        cfib = cfi[0:64, :].unsqueeze(2).to_broadcast([64, 16, 16])
        dv(lambda e: e.tensor_tensor(out=Bre[:], in0=bre[:], in1=cfrb, op=ALU.mult))
        dv(lambda e: e.tensor_tensor(out=tq[:], in0=bim[:], in1=cfib, op=ALU.mult))
        dv(lambda e: e.tensor_tensor(out=Bre[:], in0=Bre[:], in1=tq[:], op=ALU.subtract))
        dv(lambda e: e.tensor_tensor(out=Bim[:], in0=bim[:], in1=cfrb, op=ALU.mult))
        dv(lambda e: e.tensor_tensor(out=tq[:], in0=bre[:], in1=cfib, op=ALU.mult))
        dv(lambda e: e.tensor_tensor(out=Bim[:], in0=Bim[:], in1=tq[:], op=ALU.add))
        for half in range(2):
            for ri, Bsrc in ((0, Bre), (1, Bim)):
                S.op("pe", lambda e, half=half, ri=ri, Bsrc=Bsrc: e.transpose(
                    out=ptr[0][:, (half * 2 + ri) * 64:(half * 2 + ri + 1) * 64],
                    in_=Bsrc[:, half * 8:(half + 1) * 8, :].rearrange("n g p -> n (g p)"),
                    identity=identf[0:64, 0:64]), reads=R + ["identf", "t_arai"], writes=["t_ptr0b"])
        S.op("act", lambda e: e.copy(out=BT[:].rearrange("p h c -> p (h c)"), in_=ptr[0][:, 0:256]),
             reads=["t_ptr0b"], writes=R)
        for half in range(2):
            S.op("dve", lambda e, half=half: e.tensor_tensor(
                out=Bblk[:, half], in0=BT[:, half, :].unsqueeze(1).to_broadcast([128, 8, 128]),
                in1=bmask[:].unsqueeze(2).to_broadcast([128, 8, 128]), op=ALU.mult), reads=R, writes=R)
        S.dma(tabb[l][:, 0:2048], Bblk[:].rearrange("p h g c -> p (h g c)"), reads=R)
        Cm = sb(ph, "t_Cm", [128, 16, 16], F32)
        mask2 = sb(ph, "t_mask2", [128, 16, 8], F32)
        Cpad = sb(ph, "t_Cpad", [128, 16, 8, 16], BF16)
        S.dma(Cm[0:64], dr["cre"][l], writes=R)
        S.dma(Cm[64:128], dr["cim"][l], writes=R)
        S.dma(mask2[:], dr["mask2"], writes=R)
        dv(lambda e: e.tensor_scalar(out=Cm[64:128], in0=Cm[64:128], scalar1=-1.0, scalar2=None, op0=ALU.mult))
        dv(lambda e: e.tensor_tensor(out=Cpad[:], in0=Cm[:].unsqueeze(2).to_broadcast([128, 16, 8, 16]),
                                     in1=mask2[:].unsqueeze(3).to_broadcast([128, 16, 8, 16]), op=ALU.mult))
        S.dma(tabb[l][:, 2048:4096], Cpad[:].rearrange("p g a b -> p (g a b)"), reads=R)
        S.end_phase()


def mixer(nc, S, dr, l, b, X, hT, GG, cols, tab, tabb, sb, ps, norm_to_hT, make_GG, resid_update, rstd_from_ss,
          identb, identf, onesb, att, ssm, pool):
    with ExitStack() as ph:
        norm_to_hT(ph, l, b, 0, 1)
        make_GG(ph, l, b, 2)
        S.end_phase()
    with ExitStack() as mx:
        yaT = sb(mx, "yaT", [128, 4, SEQ], BF16)
        ysT = sb(mx, "ysT", [128, 2, SEQ], BF16)
        ypT = sb(mx, "ypT", [128, 2, SEQ], BF16)
        if att:
            attention(nc, S, dr, l, b, hT, yaT, sb, ps, rstd_from_ss, identb, onesb)
        else:
            S.op("dve", lambda e: e.memset(yaT[:], 0.0), writes=["yaT"])
            S.end_phase()
        if ssm:
            ssm_branch(nc, S, dr, l, b, hT, ysT, tab, tabb, sb, ps)
        else:
            S.op("dve", lambda e: e.memset(ysT[:], 0.0), writes=["ysT"])
            S.end_phase()
        if pool:
            pool_branch(nc, S, dr, l, b, hT, ypT, sb, ps)
        else:
            S.op("dve", lambda e: e.memset(ypT[:], 0.0), writes=["ypT"])
            S.end_phase()
        merge(nc, S, dr, l, b, X, hT, GG, yaT, ysT, ypT, sb, ps, resid_update)


def merge(nc, S, dr, l, b, X, hT, GG, yaT, ysT, ypT, sb, ps, resid_update):
    with ExitStack() as ph:
        wg = [sb(ph, "m_wg%d" % i, [128, 8, 128], BF16) for i in range(4)]
        pa = sb(ph, "m_pa", [128, 4, D], BF16)
        pb = sb(ph, "m_pb", [128, 2, D], BF16)
        pc = sb(ph, "m_pc", [128, 2, D], BF16)
        wo = sb(ph, "m_wo", [128, 8, D], BF16)
        mT = sb(ph, "m_mT", [128, 8, 1024], BF16)
        sg = [sb(ph, "m_sg%d" % i, [128, 512], F32) for i in range(2)]
        tm = sb(ph, "m_tm", [128, 512], F32)
        ac = sb(ph, "m_ac", [128, 512], F32)
        ssy = sb(ph, "m_ssy", [128, 4], F32)
        junk = sb(ph, "m_junk", [128, 512], BF16)
        tmp = sb(ph, "m_tmp", [128, D], F32)
        pg = [ps(ph, "m_pg%d" % i, [128, 512], F32) for i in range(2)]
        pp = [ps(ph, "m_pp%d" % i, [128, 512], F32) for i in range(2)]
        py = [ps(ph, "m_py%d" % i, [128, 512], F32) for i in range(4)]
        for k in range(4):
            cast_load(S, pa[:, k, :], dr["pa"][l][:, k, :], "m_pa")
        cast_load(S, pb[:], dr["pb"][l], "m_pb")
        cast_load(S, pc[:], dr["pc"][l], "m_pc")
        for k in range(8):
            cast_load(S, wo[:, k, :], dr["wout"][l][:, k, :], "m_wo")
        brs = ((yaT, pa, 4, "yaT", "m_pa"), (ysT, pb, 2, "ysT", "m_pb"), (ypT, pc, 2, "ypT", "m_pc"))
        nl_ = 0
        ng = 0
        for half in range(2):
            for dc in range(8):
                wgs = []
                for br in range(3):
                    wb = wg[nl_ % 4]
                    nl_ += 1
                    cast_load(S, wb[:], dr["wg"][l, br * 8 + dc], _nm(wb))
                    wgs.append(wb)
                for s2 in range(2):
                    t0 = half * 1024 + s2 * 512
                    hreads = [("hT", t0 // 128 + q) for q in range(4)]
                    for br in range(3):
                        yT, pw, nk, yn, pn = brs[br]
                        g_ = pg[ng % 2]
                        p_ = pp[ng % 2]
                        s_ = sg[ng % 2]
                        ng += 1
                        for k in range(8):
                            S.op("pe", lambda e, k=k, g_=g_, wb=wgs[br], t0=t0: e.matmul(
                                g_[:], lhsT=wb[:, k, :], rhs=hT[:, k, t0:t0 + 512], start=(k == 0), stop=(k == 7)),
                                reads=[_nm(wgs[br])] + hreads, writes=[_nm(g_)])
                        for k in range(nk):
                            S.op("pe", lambda e, k=k, p_=p_, pw=pw, yT=yT, t0=t0, nk=nk, dc=dc: e.matmul(
                                p_[:], lhsT=pw[:, k, dc * 128:(dc + 1) * 128], rhs=yT[:, k, t0:t0 + 512],
                                start=(k == 0), stop=(k == nk - 1)), reads=[pn, yn], writes=[_nm(p_)])
                        S.op("act", lambda e, g_=g_, s_=s_: e.activation(out=s_[:], in_=g_[:], func=AF.Sigmoid),
                             reads=[_nm(g_)], writes=[_nm(s_)])
                        if br == 0:
                            S.op("dve", lambda e, p_=p_, s_=s_: e.tensor_tensor(out=ac[:], in0=p_[:], in1=s_[:], op=ALU.mult),
                                 reads=[_nm(p_), _nm(s_)], writes=["m_ac"])
                        elif br == 1:
                            S.op("dve", lambda e, p_=p_, s_=s_: e.tensor_tensor(out=tm[:], in0=p_[:], in1=s_[:], op=ALU.mult),
                                 reads=[_nm(p_), _nm(s_)], writes=["m_tm"])
                            S.op("dve", lambda e: e.tensor_tensor(out=ac[:], in0=ac[:], in1=tm[:], op=ALU.add),
                                 reads=["m_tm"], writes=["m_ac"])
                        else:
                            S.op("dve", lambda e, p_=p_, s_=s_: e.tensor_tensor(out=tm[:], in0=p_[:], in1=s_[:], op=ALU.mult),
                                 reads=[_nm(p_), _nm(s_)], writes=["m_tm"])
                            S.op("dve", lambda e, dc=dc, s2=s2: e.tensor_tensor(
                                out=mT[:, dc, s2 * 512:(s2 + 1) * 512], in0=ac[:], in1=tm[:], op=ALU.add),
                                reads=["m_tm", "m_ac"], writes=[("m_mT", dc)])
            for tt in range(8):
                t = half * 8 + tt
                pyy = py[(tt % 2) * 2:(tt % 2) * 2 + 2]
                for i in range(2):
                    for k in range(8):
                        S.op("pe", lambda e, i=i, k=k, tt=tt, pyy=pyy: e.matmul(
                            pyy[i][:], lhsT=mT[:, k, tt * 128:(tt + 1) * 128], rhs=wo[:, k, i * 512:(i + 1) * 512],
                            start=(k == 0), stop=(k == 7)), reads=[("m_mT", k), "m_wo"], writes=[_nm(pyy[i])])
                resid_update((ssy, junk, tmp), t, pyy)
        S.end_phase()


def pool_branch(nc, S, dr, l, b, hT, ypT, sb, ps):
    with ExitStack() as ph:
        w1 = sb(ph, "p_w1", [128, 8, 256], BF16)
        wp = sb(ph, "p_wp", [128, 2, 128], F32)
        wpb = sb(ph, "p_wpb", [128, 2, 128], BF16)
        psc = sb(ph, "p_psc", [128, 2], F32)
        invt = sb(ph, "p_invt", [128, 16], F32)
        U = sb(ph, "p_U", [128, 16 + SEQ], F32)
        s_a = sb(ph, "p_sa", [128, 16 + SEQ], F32)
        s_b = sb(ph, "p_sb", [128, 16 + SEQ], F32)
        pl = sb(ph, "p_pl", [128, SEQ], BF16)
        fx = sb(ph, "p_fx", [128, 16], F32)
        pu = [ps(ph, "p_pu%d" % i, [128, 512], F32) for i in range(4)]
        cast_load(S, w1[:], dr["w1"][l][:, :, 680:936], "p_w1")
        S.dma(psc[:], dr["pscale"][l], writes=["p_psc"])
        S.dma(invt[:], dr["invt"], writes=["p_invt"])
        S.op("dve", lambda e: e.memset(wp[:], 0.0), writes=["p_wp"])
        for g in range(4):
            S.dma(wp[(g % 2) * 64:(g % 2 + 1) * 64, g // 2, (g % 2) * 64:(g % 2 + 1) * 64], dr["wpool"][l, g],
                  reads=[], writes=["p_wp"])
        S.op("dve", lambda e: e.tensor_copy(out=wpb[:], in_=wp[:]), reads=["p_wp"], writes=["p_wpb"])
        for buf in (U, s_a, s_b):
            S.op("dve", lambda e, buf=buf: e.memset(buf[:, 0:16], 0.0), writes=[_nm(buf)])
        for ct in range(2):
            for tc in range(4):
                for k in range(8):
                    S.op("pe", lambda e, k=k, tc=tc, ct=ct: e.matmul(
                        pu[tc][:], lhsT=w1[:, k, ct * 128:(ct + 1) * 128], rhs=hT[:, k, tc * 512:(tc + 1) * 512],
                        start=(k == 0), stop=(k == 7)), reads=["p_w1"] + [("hT", tc * 4 + q) for q in range(4)],
                        writes=[_nm(pu[tc])])
                S.op("act", lambda e, tc=tc: e.copy(out=U[:, 16 + tc * 512:16 + (tc + 1) * 512], in_=pu[tc][:]),
                     reads=[_nm(pu[tc])], writes=["p_U"])
            def shadd(dst, src, sh):
                S.op("dve", lambda e: e.tensor_tensor(out=dst[:, 16:16 + SEQ], in0=src[:, 16:16 + SEQ],
                                                      in1=src[:, 16 - sh:16 - sh + SEQ], op=ALU.add),
                     reads=[_nm(src)], writes=[_nm(dst)])
            shadd(s_a, U, 1)
            shadd(s_b, s_a, 2)
            if ct == 0:
                halves = ((0, s_a, 2), (64, s_b, 4))
            else:
                shadd(s_a, s_b, 4)
                shadd(s_b, s_a, 8)
                halves = ((0, s_a, 8), (64, s_b, 16))
            for (p0, sbuf, win) in halves:
                S.op("dve", lambda e, p0=p0, sbuf=sbuf, win=win: e.scalar_tensor_tensor(
                    out=pl[p0:p0 + 64, :], in0=sbuf[p0:p0 + 64, 16:16 + SEQ], scalar=1.0 / win,
                    in1=U[p0:p0 + 64, 16:16 + SEQ], op0=ALU.mult, op1=ALU.subtract),
                    reads=[_nm(sbuf), "p_U"], writes=["p_pl"])
                S.op("dve", lambda e, p0=p0, sbuf=sbuf, win=win: e.tensor_tensor(
                    out=fx[p0:p0 + 64, 0:win - 1], in0=sbuf[p0:p0 + 64, 16:16 + win - 1],
                    in1=invt[p0:p0 + 64, 0:win - 1], op=ALU.mult), reads=[_nm(sbuf), "p_invt"], writes=["p_fx"])
                S.op("dve", lambda e, p0=p0, win=win: e.tensor_tensor(
                    out=pl[p0:p0 + 64, 0:win - 1], in0=fx[p0:p0 + 64, 0:win - 1], in1=U[p0:p0 + 64, 16:16 + win - 1],
                    op=ALU.subtract), reads=["p_fx", "p_U"], writes=["p_pl"])
            for tc in range(4):
                S.op("pe", lambda e, tc=tc, ct=ct: e.matmul(pu[tc][:], lhsT=wpb[:, ct, :],
                                                         rhs=pl[:, tc * 512:(tc + 1) * 512], start=True, stop=True),
                     reads=["p_wpb", "p_pl"], writes=[_nm(pu[tc])])
                S.op("act", lambda e, tc=tc, ct=ct: e.activation(
                    out=ypT[:, ct, tc * 512:(tc + 1) * 512], in_=pu[tc][:], func=AF.Copy, scale=psc[:, ct:ct + 1]),
                    reads=[_nm(pu[tc]), "p_psc"], writes=["ypT"])
        S.end_phase()


def ssm_branch(nc, S, dr, l, b, hT, ysT, tab, tabb, sb, ps):
    with ExitStack() as ph:
        w1 = sb(ph, "s_w1", [128, 8, 256], BF16)
        uT = sb(ph, "s_uT", [128, 2, SEQ], BF16)
        zT = sb(ph, "s_zT", [128, 2, SEQ], BF16)
        arai = sb(ph, "s_arai", [128, 2, 16, 64], F32)
        T1 = sb(ph, "s_T1", [128, 16, 128], F32)
        T2 = sb(ph, "s_T2", [128, 16, 128], F32)
        pcs = sb(ph, "s_pcs", [128, 64], F32)
        Bblk = sb(ph, "s_Bblk", [128, 2, 1024], BF16)
        Cpad = sb(ph, "s_Cpad", [128, 16, 128], BF16)
        triub = sb(ph, "s_triub", [128, 128], BF16)
        dsk = sb(ph, "s_dsk", [128, 2], F32)
        bgl = sb(ph, "s_bgl", [128, 2], F32)
        wgl = sb(ph, "s_wgl", [128, 2, 256], BF16)
        t1s = [sb(ph, "s_t1%d" % i, [128, 4, 2, 64], BF16) for i in range(2)]
        t2s = [sb(ph, "s_t2%d" % i, [128, 4, 2, 64], BF16) for i in range(2)]
        v = sb(ph, "s_v", [128, 16, 2, 64], BF16)
        xs = sb(ph, "s_xs", [128, 16, 128], BF16)
        xas = [sb(ph, "s_xa%d" % i, [128, 128], F32) for i in range(2)]
        xbs = [sb(ph, "s_xb%d" % i, [128, 128], F32) for i in range(2)]
        cA = [sb(ph, "s_cA%d" % i, [128, 16], F32) for i in range(2)]
        cB = [sb(ph, "s_cB%d" % i, [128, 16], F32) for i in range(2)]
        sA = sb(ph, "s_sA", [128, 16], F32)
        sB = sb(ph, "s_sB", [128, 16], F32)
        q1 = sb(ph, "s_q1", [128, 16], F32)
        yv = sb(ph, "s_yv", [128, 256], F32)
        sg = sb(ph, "s_sg", [128, 512], BF16)
        pbu = [ps(ph, "s_pbu%d" % i, [128, 512], F32) for i in range(2)]
        pA = [ps(ph, "s_pA%d" % i, [128, 512], F32) for i in range(2)]
        pB = [ps(ph, "s_pB%d" % i, [128, 512], F32) for i in range(2)]
        pyy = ps(ph, "s_py", [128, 512], F32)
        pg = ps(ph, "s_pg", [128, 512], F32)
        cast_load(S, w1[:], dr["w1"][l][:, :, 424:680], "s_w1")
        S.dma(arai[:].rearrange("p r g n -> p (r g n)"), tab[l][:, 0:2048], writes=["s_arai"])
        S.dma(T1[:].rearrange("p g t -> p (g t)"), tab[l][:, 2048:4096], writes=["s_T1"])
        S.dma(T2[:].rearrange("p g t -> p (g t)"), tab[l][:, 4096:6144], writes=["s_T2"])
        S.dma(pcs[:], tab[l][:, 6144:6208], writes=["s_pcs"])
        S.dma(Bblk[:].rearrange("p h c -> p (h c)"), tabb[l][:, 0:2048], writes=["s_Bblk"])
        S.dma(Cpad[:].rearrange("p g c -> p (g c)"), tabb[l][:, 2048:4096], writes=["s_Cpad"])
        S.dma(triub[:], dr["triub"], writes=["s_triub"])
        S.dma(dsk[:], dr["dskip"][l], writes=["s_dsk"])
        S.dma(bgl[:], dr["bglu"][l], writes=["s_bgl"])
        cast_load(S, wgl[:], dr["wglu"][l], "s_wgl")
        for ct in range(2):
            for tc in range(4):
                pp = pbu[tc % 2]
                for k in range(8):
                    S.op("pe", lambda e, k=k, tc=tc, ct=ct, pp=pp: e.matmul(
                        pp[:], lhsT=w1[:, k, ct * 128:(ct + 1) * 128], rhs=hT[:, k, tc * 512:(tc + 1) * 512],
                        start=(k == 0), stop=(k == 7)), reads=["s_w1"] + [("hT", tc * 4 + q) for q in range(4)],
                        writes=[_nm(pp)])
                S.op("act", lambda e, tc=tc, ct=ct, pp=pp: e.copy(out=uT[:, ct, tc * 512:(tc + 1) * 512], in_=pp[:]),
                     reads=[_nm(pp)], writes=["s_uT"])
        for i in range(2):
            S.op("dve", lambda e, i=i: e.memset(cA[i][:], 0.0), writes=[_nm(cA[i])])
            S.op("dve", lambda e, i=i: e.memset(cB[i][:], 0.0), writes=[_nm(cB[i])])
        Pr = pcs[:, 0:16]
        PiA = pcs[:, 16:32]
        PiB = pcs[:, 32:48]
        for t in range(NT):
            cAo, cBo = cA[t % 2], cB[t % 2]
            cAn, cBn = cA[(t + 1) % 2], cB[(t + 1) % 2]
            for half in range(2):
                for q2 in range(2):
                    S.op("pe", lambda e, half=half, q2=q2, t=t: e.matmul(
                        pbu[q2][:], lhsT=uT[:, half, t * 128:(t + 1) * 128], rhs=Bblk[:, half, q2 * 512:(q2 + 1) * 512],
                        start=True, stop=True), reads=["s_uT", "s_Bblk"], writes=[_nm(pbu[q2])])
                for q2 in range(2):
                    g0 = half * 8 + q2 * 4
                    t1 = t1s[q2]
                    t2 = t2s[q2]
                    bu = pbu[q2][:].rearrange("p (g r n) -> p g r n", g=4, r=2)
                    Ar = arai[:, 0, g0:g0 + 4, :]
                    Ai = arai[:, 1, g0:g0 + 4, :]
                    S.op("dve", lambda e, bu=bu, Ar=Ar: e.tensor_tensor(
                        out=t1[:], in0=bu, in1=Ar.unsqueeze(2).to_broadcast([128, 4, 2, 64]), op=ALU.mult),
                        reads=[_nm(pbu[q2]), "s_arai"], writes=[_nm(t1)])
                    S.op("dve", lambda e, bu=bu, Ai=Ai: e.tensor_tensor(
                        out=t2[:, :, 0, :], in0=bu[:, :, 1, :], in1=Ai, op=ALU.mult),
                        reads=[_nm(pbu[q2]), "s_arai"], writes=[_nm(t2)])
                    S.op("dve", lambda e, bu=bu, Ai=Ai: e.tensor_tensor(
                        out=t2[:, :, 1, :], in0=bu[:, :, 0, :], in1=Ai, op=ALU.mult),
                        reads=[_nm(pbu[q2]), "s_arai"], writes=[_nm(t2)])
                    S.op("pool", lambda e, g0=g0: e.tensor_tensor(
                        out=v[:, g0:g0 + 4, 0, :], in0=t1[:, :, 0, :], in1=t2[:, :, 0, :], op=ALU.subtract),
                        reads=[_nm(t1), _nm(t2)], writes=[("s_v", g0)])
                    S.op("pool", lambda e, g0=g0: e.tensor_tensor(
                        out=v[:, g0:g0 + 4, 1, :], in0=t1[:, :, 1, :], in1=t2[:, :, 1, :], op=ALU.add),
                        reads=[_nm(t1), _nm(t2)], writes=[("s_v", g0)])
                    pa_, pb_ = pA[q2], pB[q2]
                    for gg in range(4):
                        g = g0 + gg
                        S.op("pe", lambda e, g=g, gg=gg, pa_=pa_: e.matmul(
                            pa_[:, gg * 128:(gg + 1) * 128], lhsT=v[:, g].rearrange("p r n -> p (r n)"), rhs=triub[:],
                            start=True, stop=True), reads=[("s_v", g0), "s_triub"], writes=[_nm(pa_)])
                        S.op("pe", lambda e, g=g, gg=gg, pb_=pb_: e.matmul(
                            pb_[0:64, gg * 128:(gg + 1) * 128], lhsT=v[:, g, 1, :], rhs=triub[:],
                            start=True, stop=True), reads=[("s_v", g0), "s_triub"], writes=[_nm(pb_)])
                        S.op("pe", lambda e, g=g, gg=gg, pb_=pb_: e.matmul(
                            pb_[64:128, gg * 128:(gg + 1) * 128], lhsT=v[:, g, 0, :], rhs=triub[:],
                            start=True, stop=True), reads=[("s_v", g0), "s_triub"], writes=[_nm(pb_)])
                    A127 = pa_[:].rearrange("p (g t) -> p g t", t=128)[:, :, 127]
                    B127 = pb_[:].rearrange("p (g t) -> p g t", t=128)[:, :, 127]
                    gs = slice(g0, g0 + 4)
                    S.op("dve", lambda e, A127=A127, gs=gs: e.tensor_tensor(out=sA[:, gs], in0=A127, in1=cAo[:, gs], op=ALU.add),
                         reads=[_nm(pa_), _nm(cAo)], writes=["s_sA"])
                    S.op("dve", lambda e, B127=B127, gs=gs: e.tensor_tensor(out=sB[:, gs], in0=B127, in1=cBo[:, gs], op=ALU.add),
                         reads=[_nm(pb_), _nm(cBo)], writes=["s_sB"])
                    for gg in range(4):
                        g = g0 + gg
                        xa = xas[gg % 2]
                        xb = xbs[gg % 2]
                        S.op("dve", lambda e, g=g, gg=gg, pa_=pa_, xa=xa: e.scalar_tensor_tensor(
                            out=xa[:], in0=pa_[:, gg * 128:(gg + 1) * 128], scalar=cAo[:, g:g + 1], in1=T1[:, g, :],
                            op0=ALU.add, op1=ALU.mult), reads=[_nm(pa_), _nm(cAo), "s_T1"], writes=[_nm(xa)])
                        S.op("dve", lambda e, g=g, gg=gg, pb_=pb_, xb=xb: e.scalar_tensor_tensor(
                            out=xb[:], in0=pb_[:, gg * 128:(gg + 1) * 128], scalar=cBo[:, g:g + 1], in1=T2[:, g, :],
                            op0=ALU.add, op1=ALU.mult), reads=[_nm(pb_), _nm(cBo), "s_T2"], writes=[_nm(xb)])
                        S.op("pool", lambda e, g=g, xa=xa, xb=xb: e.tensor_tensor(out=xs[:, g, :], in0=xa[:], in1=xb[:], op=ALU.add),
                             reads=[_nm(xa), _nm(xb)], writes=[("s_xs", g)])
                    S.op("dve", lambda e, gs=gs: e.tensor_tensor(out=cAn[:, gs], in0=sA[:, gs], in1=Pr[:, gs], op=ALU.mult),
                         reads=["s_sA", "s_pcs"], writes=[_nm(cAn)])
                    S.op("dve", lambda e, gs=gs: e.tensor_tensor(out=q1[:, gs], in0=sB[:, gs], in1=PiA[:, gs], op=ALU.mult),
                         reads=["s_sB", "s_pcs"], writes=["s_q1"])
                    S.op("dve", lambda e, gs=gs: e.tensor_tensor(out=cAn[:, gs], in0=cAn[:, gs], in1=q1[:, gs], op=ALU.add),
                         reads=["s_q1"], writes=[_nm(cAn)])
                    S.op("dve", lambda e, gs=gs: e.tensor_tensor(out=cBn[:, gs], in0=sB[:, gs], in1=Pr[:, gs], op=ALU.mult),
                         reads=["s_sB", "s_pcs"], writes=[_nm(cBn)])
                    S.op("dve", lambda e, gs=gs: e.tensor_tensor(out=q1[:, gs], in0=sA[:, gs], in1=PiB[:, gs], op=ALU.mult),
                         reads=["s_sA", "s_pcs"], writes=["s_q1"])
                    S.op("dve", lambda e, gs=gs: e.tensor_tensor(out=cBn[:, gs], in0=cBn[:, gs], in1=q1[:, gs], op=ALU.add),
                         reads=["s_q1"], writes=[_nm(cBn)])
                for g8 in range(8):
                    g = half * 8 + g8
                    S.op("pe", lambda e, g=g, g8=g8, half=half: e.matmul(
                        pyy[:, half * 128:(half + 1) * 128], lhsT=Cpad[:, g, :], rhs=xs[:, g, :],
                        start=(g8 == 0), stop=(g8 == 7)), reads=[("s_xs", g), "s_Cpad"], writes=["s_py"])
                S.op("dve", lambda e, half=half, t=t: e.scalar_tensor_tensor(
                    out=yv[:, half * 128:(half + 1) * 128], in0=uT[:, half, t * 128:(t + 1) * 128],
                    scalar=dsk[:, half:half + 1], in1=pyy[:, half * 128:(half + 1) * 128], op0=ALU.mult, op1=ALU.add),
                    reads=["s_py", "s_uT", "s_dsk"], writes=["s_yv"])
                S.op("act", lambda e, half=half, t=t: e.activation(
                    out=zT[:, half, t * 128:(t + 1) * 128], in_=yv[:, half * 128:(half + 1) * 128],
                    func=AF.Gelu_apprx_tanh), reads=["s_yv"], writes=[("s_zT", t // 4)])
            if t % 4 == 3:
                tc = t // 4
                for ct in range(2):
                    for k in range(2):
                        S.op("pe", lambda e, k=k, ct=ct, tc=tc: e.matmul(
                            pg[:], lhsT=wgl[:, k, ct * 128:(ct + 1) * 128], rhs=zT[:, k, tc * 512:(tc + 1) * 512],
                            start=(k == 0), stop=(k == 1)), reads=["s_wgl", ("s_zT", tc)], writes=["s_pg"])
                    S.op("act", lambda e, ct=ct: e.activation(out=sg[:], in_=pg[:], func=AF.Sigmoid,
                                                              bias=bgl[:, ct:ct + 1]),
                         reads=["s_pg", "s_bgl"], writes=["s_sg"])
                    S.op("dve", lambda e, ct=ct, tc=tc: e.tensor_tensor(
                        out=ysT[:, ct, tc * 512:(tc + 1) * 512], in0=zT[:, ct, tc * 512:(tc + 1) * 512], in1=sg[:],
                        op=ALU.mult), reads=["s_sg", ("s_zT", tc)], writes=["ysT"])
        S.end_phase()


def attention(nc, S, dr, l, b, hT, yaT, sb, ps, rstd_from_ss, identb, onesb):
    with ExitStack() as at:
        cqnT = sb(at, "a_cqnT", [128, 2, SEQ], BF16)
        ckvT = sb(at, "a_ckvT", [128, SEQ], BF16)
        Vg = sb(at, "a_V", [128, NT, 128], BF16)
        kidT = sb(at, "a_kidT", [128, SEQ], BF16)
        Wsc = sb(at, "a_Wsc", [128, NT, 8], F32)
        with ExitStack() as ph:
            w1 = sb(ph, "a_w1", [128, 8, 424], BF16)
            gcq = sb(ph, "a_gcq", [128, 2], F32)
            gkv = sb(ph, "a_gkv", [128, 128], F32)
            ss = sb(ph, "a_ss", [128, 2], F32)
            junk = sb(ph, "a_junk", [128, 256], BF16)
            cqn = sb(ph, "a_cqn", [128, 256], BF16)
            kid4 = sb(ph, "a_kid4", [128, 4, 32], BF16)
            pz = [ps(ph, "a_pz%d" % i, [128, 512], F32) for i in range(2)]
            pt = [ps(ph, "a_pt%d" % i, [128, 1024], BF16) for i in range(2)]
            cast_load(S, w1[:], dr["w1"][l][:, :, 0:424], "a_w1")
            S.dma(gcq[:], dr["gcq"][l], writes=["a_gcq"])
            S.dma(gkv[:], dr["gckv"][l:l + 1, :].to_broadcast([128, 128]), writes=["a_gkv"])
            for t in range(NT):
                p = pz[t % 2]
                q = pt[t % 2]
                for k in range(8):
                    S.op("pe", lambda e, k=k, t=t, p=p: e.matmul(p[:, 0:424], lhsT=hT[:, k, t * 128:(t + 1) * 128],
                                                                 rhs=w1[:, k, :], start=(k == 0), stop=(k == 7)),
                         reads=["a_w1", ("hT", t)], writes=[_nm(p)])
                S.op("act", lambda e, p=p: e.activation(out=junk[:], in_=p[:, 0:256], func=AF.Square,
                                                        accum_out=ss[:, 0:1]), reads=[_nm(p)], writes=["a_junk", "a_ss"])
                S.op("act", lambda e, p=p: e.activation(out=junk[:, 0:128], in_=p[:, 256:384], func=AF.Square,
                                                        accum_out=ss[:, 1:2]), reads=[_nm(p)], writes=["a_junk", "a_ss"])
                S.op("dve", lambda e: e.tensor_scalar(out=ss[:, 0:1], in0=ss[:, 0:1], scalar1=1.0 / 256, scalar2=EPS,
                                                      op0=ALU.mult, op1=ALU.add), reads=["a_ss"], writes=["a_ss"])
                S.op("dve", lambda e: e.tensor_scalar(out=ss[:, 1:2], in0=ss[:, 1:2], scalar1=1.0 / 128, scalar2=EPS,
                                                      op0=ALU.mult, op1=ALU.add), reads=["a_ss"], writes=["a_ss"])
                S.op("act", lambda e: e.activation(out=ss[:], in_=ss[:], func=AF.Sqrt), reads=["a_ss"], writes=["a_ss"])
                S.op("dve", lambda e: e.reciprocal(out=ss[:], in_=ss[:]), reads=["a_ss"], writes=["a_ss"])
                S.op("dve", lambda e, p=p: e.tensor_scalar(out=cqn[:], in0=p[:, 0:256], scalar1=ss[:, 0:1], scalar2=None,
                                                           op0=ALU.mult), reads=[_nm(p), "a_ss"], writes=["a_cqn"])
                S.op("dve", lambda e, p=p, t=t: e.scalar_tensor_tensor(
                    out=Vg[:, t, :], in0=p[:, 256:384], scalar=ss[:, 1:2], in1=gkv[:], op0=ALU.mult, op1=ALU.mult),
                    reads=[_nm(p), "a_ss", "a_gkv"], writes=[("a_V", t)])
                S.op("dve", lambda e, p=p: e.tensor_copy(
                    out=kid4[:], in_=p[:, 384:416].unsqueeze(1).to_broadcast([128, 4, 32])),
                    reads=[_nm(p)], writes=["a_kid4"])
                S.op("dve", lambda e, p=p, t=t: e.tensor_scalar(out=Wsc[:, t, :], in0=p[:, 416:424], scalar1=IDXW,
                                                                scalar2=None, op0=ALU.mult),
                     reads=[_nm(p)], writes=["a_Wsc"])
                for c in range(2):
                    S.op("pe", lambda e, c=c, q=q: e.transpose(out=q[:, c * 128:(c + 1) * 128],
                                                              in_=cqn[:, c * 128:(c + 1) * 128], identity=identb[:]),
                         reads=["a_cqn", "identb"], writes=[_nm(q)])
                S.op("pe", lambda e, q=q, t=t: e.transpose(out=q[:, 256:384], in_=Vg[:, t, :], identity=identb[:]),
                     reads=[("a_V", t), "identb"], writes=[_nm(q)])
                S.op("pe", lambda e, q=q: e.transpose(out=q[:, 384:512], in_=kid4[:].rearrange("p a b -> p (a b)"),
                                                      identity=identb[:]), reads=["a_kid4", "identb"], writes=[_nm(q)])
                for c in range(2):
                    S.op("act", lambda e, c=c, q=q, t=t: e.activation(
                        out=cqnT[:, c, t * 128:(t + 1) * 128], in_=q[:, c * 128:(c + 1) * 128], func=AF.Copy,
                        scale=gcq[:, c:c + 1]), reads=[_nm(q), "a_gcq"], writes=[("a_cqnT", t)])
                S.op("dve", lambda e, q=q, t=t: e.tensor_copy(out=ckvT[:, t * 128:(t + 1) * 128], in_=q[:, 256:384]),
                     reads=[_nm(q)], writes=[("a_ckvT", t)])
                S.op("dve", lambda e, q=q, t=t: e.tensor_copy(out=kidT[:, t * 128:(t + 1) * 128], in_=q[:, 384:512]),
                     reads=[_nm(q)], writes=[("a_kidT", t)])
            S.end_phase()
        with ExitStack() as ph:
            wuq = sb(ph, "a_wuq", [128, 2, 1024], BF16)
            wqi = sb(ph, "a_wqi", [128, 2, 256], BF16)
            wuv = sb(ph, "a_wuv", [128, 8, 64], BF16)
            i4b = sb(ph, "a_i4b", [128, 512], BF16)
            altd = sb(ph, "a_altd", [128, 1024], BF16)
            at3 = sb(ph, "a_at3", [3, 2048], BF16)
            bt3 = sb(ph, "a_bt3", [3, 1024], BF16)
            m0 = sb(ph, "a_m0", [128, 128], F32)
            pow2 = sb(ph, "a_pow2", [128, NIT + 1], F32)
            qT = sb(ph, "a_qT", [128, 8, 128], BF16)
            qiT = sb(ph, "a_qiT", [128, 3, 128], BF16)
            acc = sb(ph, "a_acc", [128, SEQ], F32)
            jk = sb(ph, "a_jk", [128, SEQ], BF16)
            MBs = [sb(ph, "a_MB%d" % i, [128, SEQ], BF16) for i in range(2)]
            Rb = [sb(ph, "a_R%d" % i, [128, 512], F32) for i in range(2)]
            PT = [sb(ph, "a_PT%d" % i, [128, 512], BF16) for i in range(2)]
            rec = sb(ph, "a_rec", [128, 512], F32)
            oT = sb(ph, "a_oT", [128, 1024], BF16)
            sm = sb(ph, "a_sm", [128, 8], F32)
            Wk = sb(ph, "a_Wk", [128, NIT + 1], F32)
            pS = [ps(ph, "a_pS%d" % i, [128, 512], F32) for i in range(2)]
            pL = [ps(ph, "a_pL%d" % i, [128, 512], F32) for i in range(2)]
            pQ = ps(ph, "a_pQ", [128, 512], F32)
            pO = ps(ph, "a_pO", [128, 512], F32)
            pD = ps(ph, "a_pD", [128, 512], F32)
            pY = ps(ph, "a_pY", [128, 512], F32)
            cast_load(S, wuq[:], dr["wuq"][l], "a_wuq")
            cast_load(S, wqi[:], dr["wqi"][l], "a_wqi")
            cast_load(S, wuv[:], dr["wuv"][l], "a_wuv")
            for nm, tl in (("i4b", i4b), ("altd", altd), ("at3", at3), ("bt3", bt3), ("m0", m0), ("pow2", pow2)):
                S.dma(tl[:], dr[nm], writes=["a_" + nm])
            hi, lo, w0, mid, cnt, tt, thr = (sm[:, i:i + 1] for i in range(7))
            ACCR = [("a_acc", c_) for c_ in range(4)]

            def stageA(i):
                steps = []
                N = 128 * (i + 1)
                qs = slice(i * 128, (i + 1) * 128)
                MB = MBs[i % 2]
                mbn = "a_MB%d" % (i % 2)

                def qi_proj(tl):
                    ncol = 96 if tl < 2 else 64
                    for k in range(2):
                        S.op("pe", lambda e: e.matmul(
                            pQ[0:ncol, tl * 128:(tl + 1) * 128], lhsT=wqi[:, k, tl * 96:tl * 96 + ncol],
                            rhs=cqnT[:, k, qs], start=(k == 0), stop=(k == 1)),
                            reads=["a_wqi", ("a_cqnT", i)], writes=["a_pQ"])
                    S.op("act", lambda e: e.copy(out=qiT[0:ncol, tl, :], in_=pQ[0:ncol, tl * 128:(tl + 1) * 128]),
                         reads=["a_pQ"], writes=["a_qiT"])
                for tl in range(3):
                    steps.append(partial(qi_proj, tl))

                nch = (N + 511) // 512

                def idx_step(h, cc, nr):
                    p0 = (h % 3) * 32
                    n = min(512, N - cc * 512)
                    pl_ = pL[nr % 2]
                    rb = Rb[nr % 2]
                    S.op("pe", lambda e: e.matmul(
                        pl_[:, 0:n], lhsT=qiT[p0:p0 + 32, h // 3, :], rhs=kidT[p0:p0 + 32, cc * 512:cc * 512 + n],
                        start=True, stop=True), reads=["a_qiT"] + [("a_kidT", cc * 4 + q) for q in range((n + 127) // 128)],
                        writes=[_nm(pl_)])
                    S.op("act", lambda e: e.activation(out=rb[:, 0:n], in_=pl_[:, 0:n], func=AF.Relu),
                         reads=[_nm(pl_)], writes=[_nm(rb)])
                    if h == 0:
                        S.op("pool", lambda e: e.tensor_scalar(
                            out=acc[:, cc * 512:cc * 512 + n], in0=rb[:, 0:n], scalar1=Wsc[:, i, 0:1], scalar2=None,
                            op0=ALU.mult), reads=[_nm(rb), "a_Wsc"], writes=[("a_acc", cc)])
                    else:
                        S.op("pool", lambda e: e.tensor_scalar(
                            out=rb[:, 0:n], in0=rb[:, 0:n], scalar1=Wsc[:, i, h:h + 1], scalar2=None,
                            op0=ALU.mult), reads=["a_Wsc"], writes=[_nm(rb)])
                        S.op("pool", lambda e: e.tensor_tensor(
                            out=acc[:, cc * 512:cc * 512 + n], in0=acc[:, cc * 512:cc * 512 + n], in1=rb[:, 0:n],
                            op=ALU.add), reads=[_nm(rb)], writes=[("a_acc", cc)])
                nr = 0
                for h in range(8):
                    for cc in range(nch):
                        steps.append(partial(idx_step, h, cc, nr))
                        nr += 1

                def bounds():
                    if i >= 2:
                        S.op("dve", lambda e: e.tensor_reduce(out=hi, in_=acc[:, 0:N], axis=AX.X, op=ALU.max),
                             reads=ACCR, writes=["a_sm"])
                        S.op("dve", lambda e: e.tensor_reduce(out=lo, in_=acc[:, 0:N], axis=AX.X, op=ALU.min),
                             reads=ACCR, writes=["a_sm"])
                    S.op("dve", lambda e: e.tensor_tensor(out=acc[:, N - 128:N], in0=acc[:, N - 128:N], in1=m0[:],
                                                          op=ALU.add), reads=["a_m0"] + ACCR, writes=ACCR)
                    if i >= 2:
                        S.op("dve", lambda e: e.tensor_tensor(out=w0, in0=hi, in1=lo, op=ALU.subtract),
                             reads=["a_sm"], writes=["a_sm"])
                        S.op("dve", lambda e: e.tensor_scalar(out=Wk[:], in0=pow2[:], scalar1=w0, scalar2=None,
                                                              op0=ALU.mult), reads=["a_sm", "a_pow2"], writes=["a_Wk"])
                        S.op("dve", lambda e: e.tensor_tensor(out=mid, in0=lo, in1=Wk[:, 0:1], op=ALU.add),
                             reads=["a_sm", "a_Wk"], writes=["a_sm"])
                    else:
                        S.op("dve", lambda e: e.memset(thr, -1.0e29), writes=["a_sm"])
                steps.append(bounds)

                def bis(k):
                    S.op("dve", lambda e: e.tensor_scalar(
                        out=jk[:, 0:N], in0=acc[:, 0:N], scalar1=mid, scalar2=0.0, op0=ALU.is_ge, op1=ALU.add,
                        accum_out=cnt), reads=ACCR + ["a_sm"], writes=["a_jk", "a_sm"])
                    S.op("dve", lambda e: e.tensor_scalar(out=tt, in0=cnt, scalar1=255.5, scalar2=0.5, op0=ALU.is_ge,
                                                          op1=ALU.subtract), reads=["a_sm"], writes=["a_sm"])
                    S.op("dve", lambda e: e.scalar_tensor_tensor(out=mid, in0=tt, scalar=Wk[:, k:k + 1], in1=mid,
                                                                 op0=ALU.mult, op1=ALU.add),
                         reads=["a_sm", "a_Wk"], writes=["a_sm"])
                if i >= 2:
                    for k in range(NIT):
                        steps.append(partial(bis, k))

                def fin():
                    if i >= 2:
                        S.op("dve", lambda e: e.tensor_tensor(out=thr, in0=mid, in1=Wk[:, NIT:NIT + 1], op=ALU.subtract),
                             reads=["a_sm", "a_Wk"], writes=["a_sm"])
                    S.op("dve", lambda e: e.tensor_scalar(out=MB[:, 0:N], in0=acc[:, 0:N], scalar1=thr, scalar2=-30000.0,
                                                          op0=ALU.is_lt, op1=ALU.mult),
                         reads=ACCR + ["a_sm"], writes=[mbn])
                steps.append(fin)
                return steps

            def stageB(i):
                steps = []
                qs = slice(i * 128, (i + 1) * 128)
                MB = MBs[i % 2]
                mbn = "a_MB%d" % (i % 2)

                def q_proj(g2):
                    for h in range(g2 * 4, g2 * 4 + 4):
                        for k in range(2):
                            S.op("pe", lambda e: e.matmul(
                                pS[g2][:, (h % 4) * 128:(h % 4 + 1) * 128], lhsT=wuq[:, k, h * 128:(h + 1) * 128],
                                rhs=cqnT[:, k, qs], start=(k == 0), stop=(k == 1)),
                                reads=["a_wuq", ("a_cqnT", i)], writes=[_nm(pS[g2])])
                    S.op("act", lambda e: e.activation(
                        out=qT[:, g2 * 4:(g2 + 1) * 4, :].rearrange("p h q -> p (h q)"), in_=pS[g2][:], func=AF.Copy,
                        scale=ATT_SC), reads=[_nm(pS[g2])], writes=["a_qT"])
                steps.append(partial(q_proj, 0))
                steps.append(partial(q_proj, 1))
                its = [(g, j) for g in range(2) for j in range(i + 1)]

                def emitS(n):
                    grp, j = its[n]
                    s_ = pS[n % 2]
                    S.op("pe", lambda e: e.matmul(
                        s_[:], lhsT=ckvT[:, j * 128:(j + 1) * 128],
                        rhs=qT[:, grp * 4:(grp + 1) * 4, :].rearrange("p h q -> p (h q)"), start=True, stop=False),
                        reads=[("a_ckvT", j), "a_qT"], writes=[_nm(s_)])
                    S.op("pe", lambda e: e.matmul(
                        s_[:], lhsT=MB[:, j * 128:(j + 1) * 128], rhs=i4b[:], start=False, stop=False),
                        reads=[mbn, "a_i4b"], writes=[_nm(s_)])
                    if j < i:
                        dd = i - j
                        S.op("pe", lambda e: e.matmul(
                            s_[:], lhsT=at3[0:3, dd * 128:(dd + 1) * 128], rhs=bt3[0:3, grp * 512:(grp + 1) * 512],
                            start=False, stop=True), reads=["a_at3", "a_bt3"], writes=[_nm(s_)])
                    else:
                        S.op("pe", lambda e: e.matmul(
                            s_[:], lhsT=identb[:], rhs=altd[:, grp * 512:(grp + 1) * 512], start=False, stop=True),
                            reads=["identb", "a_altd"], writes=[_nm(s_)])

                def it_step(n):
                    grp, j = its[n]
                    if n == 0:
                        emitS(0)
                    if n + 1 < len(its):
                        emitS(n + 1)
                    s_ = pS[n % 2]
                    ptb = PT[n % 2]
                    S.op("act", lambda e: e.activation(out=ptb[:], in_=s_[:], func=AF.Exp),
                         reads=[_nm(s_)], writes=[_nm(ptb)])
                    S.op("pe", lambda e: e.matmul(pO[:], lhsT=Vg[:, j, :], rhs=ptb[:], start=(j == 0), stop=(j == i)),
                         reads=[("a_V", j), _nm(ptb)], writes=["a_pO"])
                    S.op("pe", lambda e: e.matmul(pD[:], lhsT=onesb[:], rhs=ptb[:], start=(j == 0), stop=(j == i)),
                         reads=["onesb", _nm(ptb)], writes=["a_pD"])
                    if j == i:
                        S.op("dve", lambda e: e.reciprocal(out=rec[:], in_=pD[:]), reads=["a_pD"], writes=["a_rec"])
                        S.op("dve", lambda e: e.tensor_tensor(out=oT[:, grp * 512:(grp + 1) * 512], in0=pO[:],
                                                              in1=rec[:], op=ALU.mult),
                             reads=["a_pO", "a_rec"], writes=["a_oT"])
                for n in range(len(its)):
                    steps.append(partial(it_step, n))

                def y_proj():
                    for h in range(8):
                        S.op("pe", lambda e: e.matmul(
                            pY[(h % 2) * 64:(h % 2 + 1) * 64, (h // 2) * 128:(h // 2 + 1) * 128], lhsT=wuv[:, h, :],
                            rhs=oT[:, h * 128:(h + 1) * 128], start=True, stop=True),
                            reads=["a_wuv", "a_oT"], writes=["a_pY"])
                    S.op("act", lambda e: e.copy(out=yaT[:, :, qs], in_=pY[:].rearrange("p (c q) -> p c q", q=128)),
                         reads=["a_pY"], writes=["yaT"])
                steps.append(y_proj)
                return steps

            def run_merged(la, lb):
                na, nb_ = len(la), len(lb)
                ia = ib = 0
                while ia < na or ib < nb_:
                    if ib >= nb_ or (ia < na and ia * nb_ <= ib * na):
                        la[ia]()
                        ia += 1
                    else:
                        lb[ib]()
                        ib += 1

            run_merged(stageA(0), [])
            for i in range(NT):
                run_merged(stageA(i + 1) if i + 1 < NT else [], stageB(i))
            S.end_phase()


_BF = {"identb", "i4b", "onesb", "triub", "altd", "at3", "bt3"}


def kernel(**inputs):
    inp = {k: np.asarray(v) for k, v in inputs.items()}
    n_cores = 8
    shared = _layout_weights(inp)
    shared.update(_consts())
    x = np.ascontiguousarray(inp["x"], dtype=np.float32)
    c = np.asarray(inp["c"], dtype=np.float32)
    in_maps = []
    for ci in range(n_cores):
        m = dict(shared)
        m["x"] = x[ci * NB:(ci + 1) * NB]
        cc = c[ci * NB:(ci + 1) * NB]
        m["cT"] = np.ascontiguousarray(cc.reshape(NB, 8, 128).transpose(2, 1, 0))
        in_maps.append(m)
    shapes = {k: (v.shape, BF16 if k in _BF else F32) for k, v in in_maps[0].items()}
    nc = build_nc(shapes)
    in_maps = [{"d_" + k: v for k, v in m.items()} for m in in_maps]
    res = run_bass_kernel_spmd(nc, in_maps, core_ids=list(range(n_cores)))
    return np.concatenate([np.asarray(r["out"], dtype=np.float32) for r in res.results], axis=0)
```

```python
import types
from functools import partial
import numpy as np
import ml_dtypes
from contextlib import ExitStack
import concourse.bass as bass
import concourse.mybir as mybir
from concourse.bass_utils import run_bass_kernel_spmd

F32 = mybir.dt.float32
BF16 = mybir.dt.bfloat16
AF = mybir.ActivationFunctionType
ALU = mybir.AluOpType
AX = mybir.AxisListType

D = 1024
SEQ = 2048
NT = 16
NL = 2
NB = 4
EPS = 1e-6
ATT_SC = 128.0 ** -0.5
IDXW = (32.0 ** -0.5) * (8.0 ** -0.5)
NIT = 18
NEG = -1.0e30
POOL_WINS = (2, 4, 8, 16)


_BASE = {}
_UNIQ = [0]


def _nm(t):
    return _BASE[t.name]


def _freeze(fn):
    if fn.__closure__ is None:
        return fn
    cells = []
    for c in fn.__closure__:
        try:
            cells.append(types.CellType(c.cell_contents))
        except ValueError:
            cells.append(c)
    return types.FunctionType(fn.__code__, fn.__globals__, fn.__name__, fn.__defaults__, tuple(cells))


class Sched:
    ENG = ("pe", "act", "dve", "pool", "sp")

    def __init__(self, nc, es, n_dma_sems=12):
        self.nc = nc
        self.sems = {e: es.enter_context(nc.semaphore("s_" + e)) for e in self.ENG}
        self.cnt = {e: 0 for e in self.ENG}
        self.dma_sems = [es.enter_context(nc.semaphore("d%d" % i)) for i in range(n_dma_sems)]
        self.dma_cnt = [0] * n_dma_sems
        self.dma_rr = 0
        self.prog = {e: [] for e in self.ENG}
        self.seen = {e: {} for e in self.ENG}
        self.lastw = {}
        self.readers = {}
        self.ninst = 0
        self.excl = set()

    def _sem_of(self, key):
        if isinstance(key, tuple):
            return self.dma_sems[key[1]], 16
        return self.sems[key], 1

    def _needed(self, eng, deps):
        best = {}
        for (key, val) in deps:
            if key == eng and eng in ("pe", "sp"):
                continue
            if self.seen[eng].get(key, 0) >= val:
                continue
            if best.get(key, 0) < val:
                best[key] = val
        waits = []
        for key, val in best.items():
            self.seen[eng][key] = val
            sem, step = self._sem_of(key)
            waits.append((sem, val * step))
        return waits

    def _deps(self, reads, writes):
        deps = []
        for r in reads:
            t = self.lastw.get(r)
            if t is not None:
                deps.append(t)
        for w in writes:
            t = self.lastw.get(w)
            if t is not None:
                deps.append(t)
            rd = self.readers.get(w)
            if rd:
                deps.extend(rd.items())
        return deps

    def _commit(self, tok, reads, writes):
        for w in writes:
            self.lastw[w] = tok
            self.readers[w] = {}
        for r in reads:
            d = self.readers.setdefault(r, {})
            if d.get(tok[0], 0) < tok[1]:
                d[tok[0]] = tok[1]

    def op(self, eng, fn, reads=(), writes=()):
        fn = _freeze(fn)
        if self.excl:
            ex = [r for r in reads if r in self.excl]
            if ex:
                writes = list(writes) + [r for r in ex if r not in writes]
        waits = self._needed(eng, self._deps(reads, writes))
        self.cnt[eng] += 1
        tok = (eng, self.cnt[eng])
        sem = self.sems[eng]

        def emit(e, waits=waits, fn=fn, sem=sem):
            for (s, v) in waits:
                e.wait_ge(s, v)
            fn(e).then_inc(sem, 1)

        self.prog[eng].append(emit)
        self._commit(tok, reads, writes)
        self.ninst += 1
        return tok

    def dma(self, out, in_, reads=(), writes=(), q="sp", **kw):
        k = self.dma_rr
        self.dma_rr = (k + 1) % len(self.dma_sems)
        deps = self._deps(reads, writes)
        if self.dma_cnt[k] > 0:
            deps.append((("dma", k), self.dma_cnt[k]))
        waits = self._needed(q, deps)
        self.dma_cnt[k] += 1
        tok = (("dma", k), self.dma_cnt[k])
        sem = self.dma_sems[k]

        def emit(e, waits=waits, sem=sem, out=out, in_=in_, kw=kw):
            for (s, v) in waits:
                e.wait_ge(s, v)
            e.dma_start(out=out, in_=in_, **kw).then_inc(sem, 16)

        self.prog[q].append(emit)
        self._commit(tok, reads, writes)
        self.ninst += 1
        return tok

    def end_phase(self, q="sp"):
        waits = [(self.dma_sems[k], 16 * c) for k, c in enumerate(self.dma_cnt) if c > 0]
        waits += [(self.sems[e], self.cnt[e]) for e in self.ENG if self.cnt[e] > 0 and e != q]

        def emit(e, waits=waits):
            for (s, v) in waits:
                e.wait_ge(s, v)

        self.prog[q].append(emit)
        nc = self.nc
        prog = self.prog
        with nc.Block() as block:
            if prog["pe"]:
                @block.tensor
                def _(e):
                    for f in prog["pe"]:
                        f(e)
            if prog["act"]:
                @block.scalar
                def _(e):
                    for f in prog["act"]:
                        f(e)
            if prog["dve"]:
                @block.vector
                def _(e):
                    for f in prog["dve"]:
                        f(e)
            if prog["pool"]:
                @block.gpsimd
                def _(e):
                    for f in prog["pool"]:
                        f(e)
            if prog["sp"]:
                @block.sync
                def _(e):
                    for f in prog["sp"]:
                        f(e)
        self.prog = {e: [] for e in self.ENG}


def _consts():
    bf = ml_dtypes.bfloat16
    c = {}
    c["identf"] = np.eye(128, dtype=np.float32)
    c["identb"] = np.eye(128, dtype=np.float32).astype(bf)
    c["i4b"] = np.tile(np.eye(128, dtype=np.float32), (1, 4)).astype(bf)
    c["onesb"] = np.ones((128, 128), np.float32).astype(bf)
    c["onesf"] = np.ones((128, 128), np.float32)
    s = np.arange(128)
    c["triub"] = (s[:, None] <= s[None, :]).astype(np.float32).astype(bf)
    m0 = np.zeros((128, 128), np.float32)
    m0[:64, 64:] = NEG
    c["m0"] = m0
    slopes = 2.0 ** (-8.0 * np.arange(1, 9) / 8.0)
    altd = np.zeros((128, 8, 128), np.float32)
    for h in range(8):
        altd[:, h, :] = -slopes[h] * np.abs(s[None, :] - s[:, None])
    c["altd"] = altd.reshape(128, 1024).astype(bf)
    at3 = np.zeros((3, 16, 128), np.float32)
    at3[0] = 1.0
    at3[1] = (128.0 * np.arange(16))[:, None]
    at3[2] = s[None, :].astype(np.float32)
    c["at3"] = at3.reshape(3, 2048).astype(bf)
    bt3 = np.zeros((3, 8, 128), np.float32)
    for h in range(8):
        bt3[0, h] = -slopes[h] * s
        bt3[1, h] = -slopes[h]
        bt3[2, h] = slopes[h]
    c["bt3"] = bt3.reshape(3, 1024).astype(bf)
    sg = np.ones((128, 2), np.float32)
    sg[:64, 0] = -1.0
    sg[64:, 1] = -1.0
    c["sgn"] = sg
    bm = np.zeros((128, 8), np.float32)
    for p in range(128):
        bm[p, p // 16] = 1.0
    c["bmask"] = bm
    m2 = np.zeros((128, 16, 8), np.float32)
    for g in range(16):
        m2[:, g, g % 8] = 1.0
    c["mask2"] = m2
    c["pow2"] = np.tile((2.0 ** -(np.arange(NIT + 1) + 1.0))[None, :], (128, 1)).astype(np.float32)
    c["invt"] = np.tile((1.0 / (np.arange(16) + 1.0))[None, :], (128, 1)).astype(np.float32)
    return c


def _layout_weights(inp):
    f = lambda a: np.ascontiguousarray(a, dtype=np.float32)
    w = {}
    w["modw"] = f(inp["mod_w"].reshape(NL, 8, 128, 12, 512).transpose(0, 3, 2, 1, 4))
    w["modb"] = f(inp["mod_b"].reshape(NL, 48, 128).transpose(0, 2, 1))
    gv = np.stack([inp["mix_pre_g"], inp["mix_post_g"], inp["ffn_pre_g"], inp["ffn_post_g"]], axis=1)
    w["gvec"] = f(gv.reshape(NL, 4, 8, 128).transpose(0, 1, 3, 2))
    win = inp["w_in"].reshape(NL, 8, 128, 4008)
    w["w1"] = f(win[:, :, :, :936].transpose(0, 2, 1, 3))
    w["wg"] = f(win[:, :, :, 936:].reshape(NL, 8, 128, 24, 128).transpose(0, 3, 2, 1, 4))
    w["gcq"] = f(inp["g_cq"].reshape(NL, 2, 128).transpose(0, 2, 1))
    w["gckv"] = f(inp["g_ckv"])
    w["wuq"] = f(inp["w_uq"].reshape(NL, 2, 128, 1024).transpose(0, 2, 1, 3))
    w["wqi"] = f(inp["w_qi"].reshape(NL, 2, 128, 256).transpose(0, 2, 1, 3))
    w["wuv"] = f(inp["w_uv"].transpose(0, 2, 1, 3))
    dup = lambda a: np.concatenate([a, a], axis=1)
    w["are"] = f(dup(inp["a_re"].transpose(0, 2, 1)))
    w["aim"] = f(dup(inp["a_im"].transpose(0, 2, 1)))
    w["lstep"] = f(np.broadcast_to(inp["log_step"][:, None, :], (NL, 128, 16)))
    w["bre"] = f(inp["b_re"].transpose(0, 2, 1, 3))
    w["bim"] = f(inp["b_im"].transpose(0, 2, 1, 3))
    w["cre"] = f(inp["c_re"].transpose(0, 3, 1, 2))
    w["cim"] = f(inp["c_im"].transpose(0, 3, 1, 2))
    w["dskip"] = f(inp["d_skip"].reshape(NL, 2, 128).transpose(0, 2, 1))
    w["wglu"] = f(inp["w_glu"].reshape(NL, 2, 128, 256).transpose(0, 2, 1, 3))
    w["bglu"] = f(inp["b_glu"].reshape(NL, 2, 128).transpose(0, 2, 1))
    w["wpool"] = f(inp["w_pool"])
    w["pscale"] = f(inp["pool_scale"].reshape(NL, 2, 128).transpose(0, 2, 1))
    w["pa"] = f(inp["p_a"].reshape(NL, 4, 128, 1024).transpose(0, 2, 1, 3))
    w["pb"] = f(inp["p_b"].reshape(NL, 2, 128, 1024).transpose(0, 2, 1, 3))
    w["pc"] = f(inp["p_c"].reshape(NL, 2, 128, 1024).transpose(0, 2, 1, 3))
    w["wout"] = f(inp["w_out"].reshape(NL, 8, 128, 1024).transpose(0, 2, 1, 3))
    w["wup"] = f(inp["w_up"].reshape(NL, 8, 128, 32, 128).transpose(0, 3, 2, 1, 4))
    w["convw"] = f(inp["conv_w"].reshape(NL, 3, 32, 128).transpose(0, 3, 1, 2))
    w["convb"] = f(inp["conv_b"].reshape(NL, 32, 128).transpose(0, 2, 1))
    w["wdown"] = f(inp["w_down"].reshape(NL, 16, 128, 1024).transpose(0, 2, 1, 3))
    return w


def build_nc(shapes, nb=NB, nl=NL, do_mix=True, do_ffn=True, att=True, ssm=True, pool=True):
    nc = bass.Bass("TRN2", target_bir_lowering=False)
    DBG["nc"] = nc
    DBG["outs"] = {}
    dr = {}
    for name, (shape, dt) in shapes.items():
        dr[name] = nc.dram_tensor("d_" + name, list(shape), dt, kind="ExternalInput").ap()
    out = nc.dram_tensor("out", [nb, SEQ, D], F32, kind="ExternalOutput").ap()
    TABW = 2 * 1024 + 2 * 2048 + 64
    tab = nc.dram_tensor("tabscratch", [NL, 128, TABW], F32, kind="Internal").ap()
    tabb = nc.dram_tensor("tabscratchb", [NL, 128, 2048 + 2048], BF16, kind="Internal").ap()

    with ExitStack() as es:
        S = Sched(nc, es)

        def sb(st, name, shape, dt):
            _UNIQ[0] += 1
            actual = "%s__%d" % (name, _UNIQ[0])
            _BASE[actual] = name
            return st.enter_context(nc.sbuf_tensor(actual, shape, dt))

        def ps(st, name, shape, dt):
            S.excl.add(name)
            _UNIQ[0] += 1
            actual = "%s__%d" % (name, _UNIQ[0])
            _BASE[actual] = name
            return st.enter_context(nc.psum_tensor(actual, shape, dt))

        X = sb(es, "X", [128, NT, D], F32)
        hT = sb(es, "hT", [128, 8, SEQ], BF16)
        identf = sb(es, "identf", [128, 128], F32)
        identb = sb(es, "identb", [128, 128], BF16)
        onesb = sb(es, "onesb", [128, 128], BF16)
        onesf = sb(es, "onesf", [128, 128], F32)
        cols = sb(es, "cols", [128, NL, 6, 8, NB], F32)
        GG = sb(es, "GG", [128, D], F32)
        for nm, t in (("identf", identf), ("identb", identb), ("onesb", onesb), ("onesf", onesf)):
            S.dma(t[:], dr[nm], writes=[nm])

        with ExitStack() as ph:
            condT = sb(ph, "condT", [128, 8, NB], F32)
            modT = sb(ph, "modT", [128, 48, NB], F32)
            modb = sb(ph, "modb", [128, 48], F32)
            gvec = sb(ph, "gvec", [128, 4, 8], F32)
            mw = [sb(ph, "mw%d" % i, [128, 8, 512], F32) for i in range(2)]
            pm = ps(ph, "pm", [128, 512], F32)
            S.dma(condT[:], dr["cT"], writes=["condT"])
            S.op("act", lambda e: e.activation(out=condT[:], in_=condT[:], func=AF.Silu),
                 reads=["condT"], writes=["condT"])
            for l in range(nl):
                S.dma(modb[:], dr["modb"][l], writes=["modb"])
                S.dma(gvec[:], dr["gvec"][l].rearrange("w p c -> p w c"), writes=["gvec"])
                for j in range(12):
                    buf = mw[j % 2]
                    S.dma(buf[:], dr["modw"][l, j], writes=["mw%d" % (j % 2)])
                    for f4 in range(4):
                        fc = j * 4 + f4
                        for k in range(8):
                            S.op("pe", lambda e, buf=buf, f4=f4, k=k, fc=fc: e.matmul(
                                pm[:, fc * NB:(fc + 1) * NB], lhsT=buf[:, k, f4 * 128:(f4 + 1) * 128],
                                rhs=condT[:, k, :], start=(k == 0), stop=(k == 7)),
                                reads=["mw%d" % (j % 2), "condT"], writes=["pm"])
                S.op("dve", lambda e: e.tensor_tensor(
                    out=modT[:], in0=pm[:, 0:48 * NB].rearrange("p (f b) -> p f b", b=NB),
                    in1=modb[:].unsqueeze(2).to_broadcast([128, 48, NB]), op=ALU.add),
                    reads=["pm", "modb"], writes=["modT"])
                def gb(w):
                    return gvec[:, w, :].unsqueeze(2).to_broadcast([128, 8, NB])
                for (dst, sc0, gw) in ((0, 8, 0), (3, 32, 2)):
                    S.op("dve", lambda e, dst=dst, sc0=sc0, gw=gw: e.scalar_tensor_tensor(
                        out=cols[:, l, dst], in0=modT[:, sc0:sc0 + 8, :], scalar=1.0, in1=gb(gw),
                        op0=ALU.add, op1=ALU.mult), reads=["modT", "gvec"], writes=["cols"])
                for (dst, sh0) in ((1, 0), (4, 24)):
                    S.op("dve", lambda e, dst=dst, sh0=sh0: e.tensor_copy(
                        out=cols[:, l, dst], in_=modT[:, sh0:sh0 + 8, :]), reads=["modT"], writes=["cols"])
                for (dst, g0, gw) in ((2, 16, 1), (5, 40, 3)):
                    S.op("dve", lambda e, dst=dst, g0=g0, gw=gw: e.tensor_tensor(
                        out=cols[:, l, dst], in0=modT[:, g0:g0 + 8, :], in1=gb(gw), op=ALU.mult),
                        reads=["modT", "gvec"], writes=["cols"])
            dbg(S, "cols", cols[:].rearrange("p l s c b -> p (l s c b)"), [128, NL * 6 * 8 * NB], F32, ["cols"])
            S.end_phase()

        def rstd_from_ss(ss, n, width):
            S.op("dve", lambda e: e.tensor_scalar(out=ss[:, 0:width], in0=ss[:, 0:width], scalar1=1.0 / n,
                                                  scalar2=EPS, op0=ALU.mult, op1=ALU.add),
                 reads=[_nm(ss)], writes=[_nm(ss)])
            S.op("act", lambda e: e.activation(out=ss[:, 0:width], in_=ss[:, 0:width], func=AF.Sqrt),
                 reads=[_nm(ss)], writes=[_nm(ss)])
            S.op("dve", lambda e: e.reciprocal(out=ss[:, 0:width], in_=ss[:, 0:width]),
                 reads=[_nm(ss)], writes=[_nm(ss)])

        def norm_to_hT(ph, l, b, ca, cb):
            ss = sb(ph, "n_ss", [128, NT], F32)
            junk = sb(ph, "n_junk", [128, D], BF16)
            xn = [sb(ph, "n_xn%d" % i, [128, D], BF16) for i in range(2)]
            pt = [ps(ph, "n_pt%d" % i, [128, 1024], BF16) for i in range(2)]
            for t in range(NT):
                S.op("act", lambda e, t=t: e.activation(out=junk[:], in_=X[:, t, :], func=AF.Square,
                                                        accum_out=ss[:, t:t + 1]),
                     reads=[("X", t)], writes=["n_junk", "n_ss"])
            rstd_from_ss(ss, float(D), NT)
            for t in range(NT):
                xb = xn[t % 2]
                pp = pt[t % 2]
                S.op("dve", lambda e, t=t, xb=xb: e.tensor_scalar(out=xb[:], in0=X[:, t, :],
                                                               scalar1=ss[:, t:t + 1], scalar2=None, op0=ALU.mult),
                     reads=[("X", t), "n_ss"], writes=[_nm(xb)])
                for c in range(8):
                    S.op("pe", lambda e, c=c, xb=xb, pp=pp: e.transpose(
                        out=pp[:, c * 128:(c + 1) * 128], in_=xb[:, c * 128:(c + 1) * 128], identity=identb[:]),
                        reads=[_nm(xb), "identb"], writes=[_nm(pp)])
                for c in range(8):
                    eng = "act" if c % 2 else "dve"
                    if eng == "dve":
                        S.op("dve", lambda e, c=c, t=t, pp=pp: e.tensor_scalar(
                            out=hT[:, c, t * 128:(t + 1) * 128], in0=pp[:, c * 128:(c + 1) * 128],
                            scalar1=cols[:, l, ca, c, b:b + 1], scalar2=cols[:, l, cb, c, b:b + 1],
                            op0=ALU.mult, op1=ALU.add), reads=[_nm(pp), "cols"], writes=[("hT", t)])
                    else:
                        S.op("act", lambda e, c=c, t=t, pp=pp: e.activation(
                            out=hT[:, c, t * 128:(t + 1) * 128], in_=pp[:, c * 128:(c + 1) * 128],
                            func=AF.Identity, scale=cols[:, l, ca, c, b:b + 1], bias=cols[:, l, cb, c, b:b + 1]),
                            reads=[_nm(pp), "cols"], writes=[("hT", t)])

        def make_GG(ph, l, b, cg):
            dg = sb(ph, "gg_diag", [128, 128], F32)
            pg = [ps(ph, "gg_ps%d" % i, [128, 512], F32) for i in range(2)]
            for c in range(8):
                S.op("dve", lambda e, c=c: e.tensor_scalar(out=dg[:], in0=identf[:], scalar1=cols[:, l, cg, c, b:b + 1],
                                                        scalar2=None, op0=ALU.mult),
                     reads=["identf", "cols"], writes=["gg_diag"])
                S.op("pe", lambda e, c=c: e.matmul(pg[c // 4][:, (c % 4) * 128:(c % 4 + 1) * 128], lhsT=onesf[:],
                                                   rhs=dg[:], start=True, stop=True),
                     reads=["gg_diag", "onesf"], writes=[_nm(pg[c // 4])])
            for i in range(2):
                S.op("act", lambda e, i=i: e.copy(out=GG[:, i * 512:(i + 1) * 512], in_=pg[i][:]),
                     reads=[_nm(pg[i])], writes=["GG"])

        def resid_update(ph_tiles, t, py):
            ssy, junk, tmp = ph_tiles
            for i in range(2):
                S.op("act", lambda e, i=i: e.activation(out=junk[:, 0:512], in_=py[i][:], func=AF.Square,
                                                        accum_out=ssy[:, i:i + 1]),
                     reads=[_nm(py[i])], writes=[_nm(junk), _nm(ssy)])
            S.op("dve", lambda e: e.tensor_tensor(out=ssy[:, 2:3], in0=ssy[:, 0:1], in1=ssy[:, 1:2], op=ALU.add),
                 reads=[_nm(ssy)], writes=[_nm(ssy)])
            S.op("dve", lambda e: e.tensor_scalar(out=ssy[:, 2:3], in0=ssy[:, 2:3], scalar1=1.0 / D, scalar2=EPS,
                                                  op0=ALU.mult, op1=ALU.add), reads=[_nm(ssy)], writes=[_nm(ssy)])
            S.op("act", lambda e: e.activation(out=ssy[:, 2:3], in_=ssy[:, 2:3], func=AF.Sqrt),
                 reads=[_nm(ssy)], writes=[_nm(ssy)])
            S.op("dve", lambda e: e.reciprocal(out=ssy[:, 2:3], in_=ssy[:, 2:3]), reads=[_nm(ssy)], writes=[_nm(ssy)])
            for i in range(2):
                S.op("dve", lambda e, i=i: e.scalar_tensor_tensor(
                    out=tmp[:, i * 512:(i + 1) * 512], in0=py[i][:], scalar=ssy[:, 2:3],
                    in1=GG[:, i * 512:(i + 1) * 512], op0=ALU.mult, op1=ALU.mult),
                    reads=[_nm(py[i]), _nm(ssy), "GG"], writes=[_nm(tmp)])
            if t == 8:
                dbg(S, "r_ssy", ssy[:], [128, 4], F32, [_nm(ssy)])
                dbg(S, "r_tmp", tmp[:], [128, D], F32, [_nm(tmp)])
            S.op("dve", lambda e: e.tensor_tensor(out=X[:, t, :], in0=X[:, t, :], in1=tmp[:], op=ALU.add),
                 reads=[_nm(tmp), ("X", t)], writes=[("X", t)])

        if do_mix and ssm:
            for l in range(nl):
                ssm_tables(nc, S, dr, l, tab, tabb, sb, ps, identf)

        for b in range(nb):
            with ExitStack() as ph:
                for t in range(NT):
                    S.dma(X[:, t, :], dr["x"][b, t * 128:(t + 1) * 128, :], writes=[("X", t)])
                S.end_phase()
            for l in range(nl):
                if do_mix:
                    mixer(nc, S, dr, l, b, X, hT, GG, cols, tab, tabb, sb, ps, norm_to_hT, make_GG, resid_update,
                          rstd_from_ss, identb, identf, onesb, att, ssm, pool)
                if do_ffn:
                    ffn(nc, S, dr, l, b, X, hT, GG, sb, ps, norm_to_hT, make_GG, resid_update)
            with ExitStack() as ph:
                for t in range(NT):
                    S.dma(out[b, t * 128:(t + 1) * 128, :], X[:, t, :], reads=[("X", t)])
                S.end_phase()
        print("instructions:", S.ninst)
    return nc


DBG = {"on": False, "nc": None, "outs": {}}


def dbg(S, name, ap, shape, dt, reads, st=None):
    if not DBG["on"] or name in DBG["outs"]:
        return
    nc = DBG["nc"]
    d = nc.dram_tensor("dbg_" + name, list(shape), F32, kind="ExternalOutput").ap()
    DBG["outs"][name] = d
    if dt != F32:
        tmpf = st.enter_context(nc.sbuf_tensor("dbgt_" + name, list(shape), F32))
        S.op("dve", lambda e: e.tensor_copy(out=tmpf[:], in_=ap), reads=reads, writes=["dbgt_" + name])
        S.dma(d, tmpf[:], reads=["dbgt_" + name])
    else:
        S.dma(d, ap, reads=reads)


def cast_load(S, dst, src, wname):
    S.dma(dst, src, writes=[wname], q="pool")


def ffn(nc, S, dr, l, b, X, hT, GG, sb, ps, norm_to_hT, make_GG, resid_update):
    with ExitStack() as ph:
        norm_to_hT(ph, l, b, 3, 4)
        make_GG(ph, l, b, 5)
        dbg(S, "f_hT", hT[:].rearrange("p c t -> p (c t)"), [128, 8 * SEQ], BF16, [("hT", t) for t in range(NT)], ph)
        dbg(S, "f_GG", GG[:], [128, D], F32, ["GG"])
        S.end_phase()
    with ExitStack() as ph:
        wd = sb(ph, "f_wd", [128, 16, D], BF16)
        m = sb(ph, "f_m", [128, 16, 1024], BF16)
        cw = sb(ph, "f_cw", [128, 3, 32], F32)
        cbt = sb(ph, "f_cb", [128, 32], F32)
        halo = sb(ph, "f_halo", [128, 32, 2], F32)
        wu = [sb(ph, "f_wu%d" % i, [128, 8, 128], BF16) for i in range(3)]
        ub = [sb(ph, "f_ub%d" % i, [128, 1026], F32) for i in range(2)]
        acc = [sb(ph, "f_acc%d" % i, [128, 1024], F32) for i in range(2)]
        gl = sb(ph, "f_gl", [128, 1024], F32)
        ssy = sb(ph, "f_ssy", [128, 4], F32)
        junk = sb(ph, "f_junk", [128, 512], BF16)
        tmp = sb(ph, "f_tmp", [128, D], F32)
        pu = [ps(ph, "f_pu%d" % i, [128, 512], F32) for i in range(4)]
        py = [ps(ph, "f_py%d" % i, [128, 512], F32) for i in range(4)]
        S.dma(cw[:], dr["convw"][l], writes=["f_cw"])
        S.dma(cbt[:], dr["convb"][l], writes=["f_cb"])
        for k in range(16):
            cast_load(S, wd[:, k, :], dr["wdown"][l][:, k, :], ("f_wd", k))
        nload = 0
        for half in range(2):
            t0 = half * 1024
            for cc in range(16):
                for gv in range(2):
                    j = gv * 16 + cc
                    wb = wu[nload % 3]
                    nload += 1
                    cast_load(S, wb[:], dr["wup"][l, j], _nm(wb))
                    pp = pu[(gv * 2):(gv * 2 + 2)]
                    for s2 in range(2):
                        for k in range(8):
                            S.op("pe", lambda e, wb=wb, k=k, s2=s2, pp=pp: e.matmul(
                                pp[s2][:], lhsT=wb[:, k, :], rhs=hT[:, k, t0 + s2 * 512:t0 + (s2 + 1) * 512],
                                start=(k == 0), stop=(k == 7)),
                                reads=[_nm(wb)] + [("hT", (t0 + s2 * 512) // 128 + q) for q in range(4)],
                                writes=[_nm(pp[s2])])
                    u = ub[gv]
                    a = acc[gv]
                    dbg(S, "f_wb", wb[:].rearrange("p k n -> p (k n)"), [128, 1024], BF16, [_nm(wb)], ph)
                    if half == 0:
                        S.op("dve", lambda e, u=u: e.memset(u[:, 0:2], 0.0), writes=[_nm(u)])
                    else:
                        S.op("dve", lambda e, u=u, j=j: e.tensor_copy(out=u[:, 0:2], in_=halo[:, j, :]),
                             reads=["f_halo"], writes=[_nm(u)])
                    for s2 in range(2):
                        S.op("act", lambda e, u=u, s2=s2, pp=pp: e.copy(out=u[:, 2 + s2 * 512:2 + (s2 + 1) * 512],
                                                                        in_=pp[s2][:]),
                             reads=[_nm(pp[s2])], writes=[_nm(u)])
                    if half == 0:
                        S.op("dve", lambda e, u=u, j=j: e.tensor_copy(out=halo[:, j, :], in_=u[:, 1024:1026]),
                             reads=[_nm(u)], writes=["f_halo"])
                    dbg(S, "f_u", u[:], [128, 1026], F32, [_nm(u)], ph)
                    S.op("dve", lambda e, u=u, a=a, j=j: e.tensor_scalar(
                        out=a[:], in0=u[:, 2:1026], scalar1=cw[:, 2, j:j + 1], scalar2=cbt[:, j:j + 1],
                        op0=ALU.mult, op1=ALU.add), reads=[_nm(u), "f_cw", "f_cb"], writes=[_nm(a)])
                    S.op("dve", lambda e, u=u, a=a, j=j: e.scalar_tensor_tensor(
                        out=a[:], in0=u[:, 1:1025], scalar=cw[:, 1, j:j + 1], in1=a[:], op0=ALU.mult, op1=ALU.add),
                        reads=[_nm(u), "f_cw"], writes=[_nm(a)])
                    S.op("dve", lambda e, u=u, a=a, j=j: e.scalar_tensor_tensor(
                        out=a[:], in0=u[:, 0:1024], scalar=cw[:, 0, j:j + 1], in1=a[:], op0=ALU.mult, op1=ALU.add),
                        reads=[_nm(u), "f_cw"], writes=[_nm(a)])
                dbg(S, "f_acc0", acc[0][:], [128, 1024], F32, [_nm(acc[0])], ph)
                dbg(S, "f_acc1", acc[1][:], [128, 1024], F32, [_nm(acc[1])], ph)
                S.op("act", lambda e: e.activation(out=gl[:], in_=acc[0][:], func=AF.Gelu_apprx_tanh),
                     reads=[_nm(acc[0])], writes=["f_gl"])
                S.op("dve", lambda e, cc=cc: e.tensor_tensor(out=m[:, cc, :], in0=gl[:], in1=acc[1][:], op=ALU.mult),
                     reads=["f_gl", _nm(acc[1])], writes=[("f_m", cc)])
            for tt in range(8):
                t = half * 8 + tt
                pyy = py[(tt % 2) * 2:(tt % 2) * 2 + 2]
                for i in range(2):
                    for k in range(16):
                        S.op("pe", lambda e, i=i, k=k, tt=tt, pyy=pyy: e.matmul(
                            pyy[i][:], lhsT=m[:, k, tt * 128:(tt + 1) * 128], rhs=wd[:, k, i * 512:(i + 1) * 512],
                            start=(k == 0), stop=(k == 15)),
                            reads=[("f_m", k), ("f_wd", k)], writes=[_nm(pyy[i])])
                resid_update((ssy, junk, tmp), t, pyy)
        S.end_phase()


def ssm_tables(nc, S, dr, l, tab, tabb, sb, ps, identf):
    with ExitStack() as ph:
        are = sb(ph, "t_are", [128, 16], F32)
        aim = sb(ph, "t_aim", [128, 16], F32)
        stp = sb(ph, "t_stp", [128, 16], F32)
        th = sb(ph, "t_th", [128, 16], F32)
        r = sb(ph, "t_r", [128, 16], F32)
        sn = sb(ph, "t_sn", [128, 16], F32)
        cs = sb(ph, "t_cs", [128, 16], F32)
        t1 = sb(ph, "t_t1", [128, 16], F32)
        t2 = sb(ph, "t_t2", [128, 16], F32)
        lre = sb(ph, "t_lre", [128, 16], F32)
        lim = sb(ph, "t_lim", [128, 16], F32)
        ire = sb(ph, "t_ire", [128, 16], F32)
        iim = sb(ph, "t_iim", [128, 16], F32)
        pr = sb(ph, "t_pr", [128, 16], F32)
        pi = sb(ph, "t_pi", [128, 16], F32)
        cfr = sb(ph, "t_cfr", [128, 16], F32)
        cfi = sb(ph, "t_cfi", [128, 16], F32)
        Ere = sb(ph, "t_Ere", [128, 16, 256], F32)
        Eim = sb(ph, "t_Eim", [128, 16, 256], F32)
        ta = sb(ph, "t_ta", [128, 16, 128], F32)
        tb = sb(ph, "t_tb", [128, 16, 128], F32)
        sgn = sb(ph, "t_sgn", [128, 2], F32)
        outc = sb(ph, "t_outc", [128, 64], F32)
        halfpi = sb(ph, "t_hpi", [128, 1], F32)
        R = ["t"]

        def dv(fn, w=("t",)):
            S.op("dve", fn, reads=R, writes=list(w))

        S.dma(are[:], dr["are"][l], writes=R)
        S.dma(aim[:], dr["aim"][l], writes=R)
        S.dma(stp[:], dr["lstep"][l], writes=R)
        S.dma(sgn[:], dr["sgn"], writes=R)
        S.op("act", lambda e: e.activation(out=stp[:], in_=stp[:], func=AF.Exp), reads=R, writes=R)
        dv(lambda e: e.tensor_tensor(out=th[:], in0=aim[:], in1=stp[:], op=ALU.mult))
        dv(lambda e: e.tensor_tensor(out=t1[:], in0=are[:], in1=stp[:], op=ALU.mult))
        S.op("act", lambda e: e.activation(out=r[:], in_=t1[:], func=AF.Exp), reads=R, writes=R)
        dv(lambda e: e.memset(halfpi[:], float(np.pi / 2)))
        S.op("act", lambda e: e.activation(out=sn[:], in_=th[:], func=AF.Sin, scale=1.0 / 16), reads=R, writes=R)
        S.op("act", lambda e: e.activation(out=cs[:], in_=th[:], func=AF.Sin, scale=1.0 / 16, bias=halfpi[:]),
             reads=R, writes=R)
        for _ in range(4):
            dv(lambda e: e.tensor_tensor(out=t1[:], in0=sn[:], in1=cs[:], op=ALU.mult))
            dv(lambda e: e.tensor_tensor(out=t2[:], in0=sn[:], in1=sn[:], op=ALU.mult))
            dv(lambda e: e.tensor_scalar(out=sn[:], in0=t1[:], scalar1=2.0, scalar2=None, op0=ALU.mult))
            dv(lambda e: e.tensor_scalar(out=cs[:], in0=t2[:], scalar1=-2.0, scalar2=1.0, op0=ALU.mult, op1=ALU.add))
        dv(lambda e: e.tensor_tensor(out=lre[:], in0=r[:], in1=cs[:], op=ALU.mult))
        dv(lambda e: e.tensor_tensor(out=lim[:], in0=r[:], in1=sn[:], op=ALU.mult))
        dv(lambda e: e.reciprocal(out=t1[:], in_=r[:]))
        dv(lambda e: e.tensor_tensor(out=ire[:], in0=t1[:], in1=cs[:], op=ALU.mult))
        dv(lambda e: e.tensor_tensor(out=iim[:], in0=t1[:], in1=sn[:], op=ALU.mult))
        dv(lambda e: e.tensor_scalar(out=iim[:], in0=iim[:], scalar1=-1.0, scalar2=None, op0=ALU.mult))
        dv(lambda e: e.tensor_tensor(out=t1[:], in0=are[:], in1=are[:], op=ALU.mult))
        dv(lambda e: e.tensor_tensor(out=t2[:], in0=aim[:], in1=aim[:], op=ALU.mult))
        dv(lambda e: e.tensor_tensor(out=t1[:], in0=t1[:], in1=t2[:], op=ALU.add))
        dv(lambda e: e.reciprocal(out=t1[:], in_=t1[:]))
        dv(lambda e: e.tensor_scalar(out=t2[:], in0=lre[:], scalar1=-1.0, scalar2=None, op0=ALU.add))
        dv(lambda e: e.tensor_tensor(out=cfr[:], in0=t2[:], in1=are[:], op=ALU.mult))
        dv(lambda e: e.tensor_tensor(out=pr[:], in0=lim[:], in1=aim[:], op=ALU.mult))
        dv(lambda e: e.tensor_tensor(out=cfr[:], in0=cfr[:], in1=pr[:], op=ALU.add))
        dv(lambda e: e.tensor_tensor(out=cfr[:], in0=cfr[:], in1=t1[:], op=ALU.mult))
        dv(lambda e: e.tensor_tensor(out=cfi[:], in0=lim[:], in1=are[:], op=ALU.mult))
        dv(lambda e: e.tensor_tensor(out=pr[:], in0=t2[:], in1=aim[:], op=ALU.mult))
        dv(lambda e: e.tensor_tensor(out=cfi[:], in0=cfi[:], in1=pr[:], op=ALU.subtract))
        dv(lambda e: e.tensor_tensor(out=cfi[:], in0=cfi[:], in1=t1[:], op=ALU.mult))

        def power_table(bre_, bim_, nlev):
            dv(lambda e: e.memset(Ere[:, :, 0:1], 1.0))
            dv(lambda e: e.memset(Eim[:, :, 0:1], 0.0))
            dv(lambda e: e.tensor_copy(out=pr[:], in_=bre_[:]))
            dv(lambda e: e.tensor_copy(out=pi[:], in_=bim_[:]))
            for k in range(nlev):
                n = 1 << k
                prb = pr[:].unsqueeze(2).to_broadcast([128, 16, n])
                pib = pi[:].unsqueeze(2).to_broadcast([128, 16, n])
                dv(lambda e, n=n, prb=prb: e.tensor_tensor(out=ta[:, :, 0:n], in0=Ere[:, :, 0:n], in1=prb, op=ALU.mult))
                dv(lambda e, n=n, pib=pib: e.tensor_tensor(out=tb[:, :, 0:n], in0=Eim[:, :, 0:n], in1=pib, op=ALU.mult))
                dv(lambda e, n=n: e.tensor_tensor(out=Ere[:, :, n:2 * n], in0=ta[:, :, 0:n], in1=tb[:, :, 0:n],
                                                  op=ALU.subtract))
                dv(lambda e, n=n, pib=pib: e.tensor_tensor(out=ta[:, :, 0:n], in0=Ere[:, :, 0:n], in1=pib, op=ALU.mult))
                dv(lambda e, n=n, prb=prb: e.tensor_tensor(out=tb[:, :, 0:n], in0=Eim[:, :, 0:n], in1=prb, op=ALU.mult))
                dv(lambda e, n=n: e.tensor_tensor(out=Eim[:, :, n:2 * n], in0=ta[:, :, 0:n], in1=tb[:, :, 0:n],
                                                  op=ALU.add))
                if k < nlev - 1:
                    dv(lambda e: e.tensor_tensor(out=t1[:], in0=pr[:], in1=pr[:], op=ALU.mult))
                    dv(lambda e: e.tensor_tensor(out=t2[:], in0=pi[:], in1=pi[:], op=ALU.mult))
                    dv(lambda e: e.tensor_tensor(out=pi[:], in0=pr[:], in1=pi[:], op=ALU.mult))
                    dv(lambda e: e.tensor_scalar(out=pi[:], in0=pi[:], scalar1=2.0, scalar2=None, op0=ALU.mult))
                    dv(lambda e: e.tensor_tensor(out=pr[:], in0=t1[:], in1=t2[:], op=ALU.subtract))

        power_table(lre, lim, 8)
        o_t1 = 2048
        o_t2 = 2048 + 2048
        o_c = 2048 + 4096
        S.dma(tab[l][:, o_t1:o_t1 + 2048].rearrange("p (g t) -> p g t", t=128), Ere[:, :, 0:128], reads=R)
        dv(lambda e: e.tensor_scalar(out=ta[:], in0=Eim[:, :, 0:128], scalar1=sgn[:, 0:1], scalar2=None, op0=ALU.mult))
        S.dma(tab[l][:, o_t2:o_t2 + 2048].rearrange("p (g t) -> p g t", t=128), ta[:], reads=R)
        dv(lambda e: e.tensor_copy(out=outc[:, 0:16], in_=Ere[:, :, 128]))
        dv(lambda e: e.tensor_scalar(out=outc[:, 16:32], in0=Eim[:, :, 128], scalar1=sgn[:, 0:1], scalar2=None,
                                     op0=ALU.mult))
        dv(lambda e: e.tensor_scalar(out=outc[:, 32:48], in0=Eim[:, :, 128], scalar1=sgn[:, 1:2], scalar2=None,
                                     op0=ALU.mult))
        dv(lambda e: e.memset(outc[:, 48:64], 0.0))
        S.dma(tab[l][:, o_c:o_c + 64], outc[:], reads=R)
        S.end_phase()
        power_table(ire, iim, 7)
        ptr = [ps(ph, "t_ptr%d" % i, [128, 512], F32) for i in range(4)]
        arai = sb(ph, "t_arai", [128, 2, 16, 64], F32)
        for ri, E in ((0, Ere), (1, Eim)):
            for g in range(16):
                S.op("pe", lambda e, g=g, E=E, ri=ri: e.transpose(
                    out=ptr[ri * 2 + g // 8][:, (g % 8) * 64:(g % 8 + 1) * 64], in_=E[0:64, g, 0:128],
                    identity=identf[0:64, 0:64]), reads=R + ["identf"], writes=["t_ptr"])
            for hh in range(2):
                S.op("act", lambda e, hh=hh, ri=ri: e.copy(
                    out=arai[:, ri, hh * 8:(hh + 1) * 8, :].rearrange("p g n -> p (g n)"), in_=ptr[ri * 2 + hh][:]),
                    reads=["t_ptr"], writes=["t_arai"])
        S.dma(tab[l][:, 0:2048], arai[:].rearrange("p r g n -> p (r g n)"), reads=["t_arai"])
        bre = sb(ph, "t_bre", [64, 16, 16], F32)
        bim = sb(ph, "t_bim", [64, 16, 16], F32)
        Bre = sb(ph, "t_Bre", [64, 16, 16], F32)
        Bim = sb(ph, "t_Bim", [64, 16, 16], F32)
        tq = sb(ph, "t_tq", [64, 16, 16], F32)
        bmask = sb(ph, "t_bmask", [128, 8], F32)
        BT = sb(ph, "t_BT", [128, 2, 128], F32)
        Bblk = sb(ph, "t_Bblk", [128, 2, 8, 128], BF16)
        S.dma(bre[:], dr["bre"][l], writes=R)
        S.dma(bim[:], dr["bim"][l], writes=R)
        S.dma(bmask[:], dr["bmask"], writes=R)
        cfrb = cfr[0:64, :].unsqueeze(2).to_broadcast([64, 16, 16])
        cfib = cfi[0:64, :].unsqueeze(2).to_broadcast([64, 16, 16])
        dv(lambda e: e.tensor_tensor(out=Bre[:], in0=bre[:], in1=cfrb, op=ALU.mult))
        dv(lambda e: e.tensor_tensor(out=tq[:], in0=bim[:], in1=cfib, op=ALU.mult))
        dv(lambda e: e.tensor_tensor(out=Bre[:], in0=Bre[:], in1=tq[:], op=ALU.subtract))
        dv(lambda e: e.tensor_tensor(out=Bim[:], in0=bim[:], in1=cfrb, op=ALU.mult))
        dv(lambda e: e.tensor_tensor(out=tq[:], in0=bre[:], in1=cfib, op=ALU.mult))
        dv(lambda e: e.tensor_tensor(out=Bim[:], in0=Bim[:], in1=tq[:], op=ALU.add))
        for half in range(2):
            for ri, Bsrc in ((0, Bre), (1, Bim)):
                S.op("pe", lambda e, half=half, ri=ri, Bsrc=Bsrc: e.transpose(
                    out=ptr[0][:, (half * 2 + ri) * 64:(half * 2 + ri + 1) * 64],
                    in_=Bsrc[:, half * 8:(half + 1) * 8, :].rearrange("n g p -> n (g p)"),
                    identity=identf[0:64, 0:64]), reads=R + ["identf", "t_arai"], writes=["t_ptr0b"])
        S.op("act", lambda e: e.copy(out=BT[:].rearrange("p h c -> p (h c)"), in_=ptr[0][:, 0:256]),
             reads=["t_ptr0b"], writes=R)
        for half in range(2):
            S.op("dve", lambda e, half=half: e.tensor_tensor(
                out=Bblk[:, half], in0=BT[:, half, :].unsqueeze(1).to_broadcast([128, 8, 128]),
                in1=bmask[:].unsqueeze(2).to_broadcast([128, 8, 128]), op=ALU.mult), reads=R, writes=R)
        S.dma(tabb[l][:, 0:2048], Bblk[:].rearrange("p h g c -> p (h g c)"), reads=R)
        Cm = sb(ph, "t_Cm", [128, 16, 16], F32)
        mask2 = sb(ph, "t_mask2", [128, 16, 8], F32)
        Cpad = sb(ph, "t_Cpad", [128, 16, 8, 16], BF16)
        S.dma(Cm[0:64], dr["cre"][l], writes=R)
        S.dma(Cm[64:128], dr["cim"][l], writes=R)
        S.dma(mask2[:], dr["mask2"], writes=R)
        dv(lambda e: e.tensor_scalar(out=Cm[64:128], in0=Cm[64:128], scalar1=-1.0, scalar2=None, op0=ALU.mult))
        dv(lambda e: e.tensor_tensor(out=Cpad[:], in0=Cm[:].unsqueeze(2).to_broadcast([128, 16, 8, 16]),
                                     in1=mask2[:].unsqueeze(3).to_broadcast([128, 16, 8, 16]), op=ALU.mult))
        S.dma(tabb[l][:, 2048:4096], Cpad[:].rearrange("p g a b -> p (g a b)"), reads=R)
        S.end_phase()


def mixer(nc, S, dr, l, b, X, hT, GG, cols, tab, tabb, sb, ps, norm_to_hT, make_GG, resid_update, rstd_from_ss,
          identb, identf, onesb, att, ssm, pool):
    with ExitStack() as ph:
        norm_to_hT(ph, l, b, 0, 1)
        make_GG(ph, l, b, 2)
        S.end_phase()
    with ExitStack() as mx:
        yaT = sb(mx, "yaT", [128, 4, SEQ], BF16)
        ysT = sb(mx, "ysT", [128, 2, SEQ], BF16)
        ypT = sb(mx, "ypT", [128, 2, SEQ], BF16)
        if att:
            attention(nc, S, dr, l, b, hT, yaT, sb, ps, rstd_from_ss, identb, onesb)
        else:
            S.op("dve", lambda e: e.memset(yaT[:], 0.0), writes=["yaT"])
            S.end_phase()
        if ssm:
            ssm_branch(nc, S, dr, l, b, hT, ysT, tab, tabb, sb, ps)
        else:
            S.op("dve", lambda e: e.memset(ysT[:], 0.0), writes=["ysT"])
            S.end_phase()
        if pool:
            pool_branch(nc, S, dr, l, b, hT, ypT, sb, ps)
        else:
            S.op("dve", lambda e: e.memset(ypT[:], 0.0), writes=["ypT"])
            S.end_phase()
        merge(nc, S, dr, l, b, X, hT, GG, yaT, ysT, ypT, sb, ps, resid_update)


def merge(nc, S, dr, l, b, X, hT, GG, yaT, ysT, ypT, sb, ps, resid_update):
    with ExitStack() as ph:
        wg = [sb(ph, "m_wg%d" % i, [128, 8, 128], BF16) for i in range(4)]
        pa = sb(ph, "m_pa", [128, 4, D], BF16)
        pb = sb(ph, "m_pb", [128, 2, D], BF16)
        pc = sb(ph, "m_pc", [128, 2, D], BF16)
        wo = sb(ph, "m_wo", [128, 8, D], BF16)
        mT = sb(ph, "m_mT", [128, 8, 1024], BF16)
        sg = [sb(ph, "m_sg%d" % i, [128, 512], F32) for i in range(2)]
        tm = sb(ph, "m_tm", [128, 512], F32)
        ac = sb(ph, "m_ac", [128, 512], F32)
        ssy = sb(ph, "m_ssy", [128, 4], F32)
        junk = sb(ph, "m_junk", [128, 512], BF16)
        tmp = sb(ph, "m_tmp", [128, D], F32)
        pg = [ps(ph, "m_pg%d" % i, [128, 512], F32) for i in range(2)]
        pp = [ps(ph, "m_pp%d" % i, [128, 512], F32) for i in range(2)]
        py = [ps(ph, "m_py%d" % i, [128, 512], F32) for i in range(4)]
        for k in range(4):
            cast_load(S, pa[:, k, :], dr["pa"][l][:, k, :], "m_pa")
        cast_load(S, pb[:], dr["pb"][l], "m_pb")
        cast_load(S, pc[:], dr["pc"][l], "m_pc")
        for k in range(8):
            cast_load(S, wo[:, k, :], dr["wout"][l][:, k, :], "m_wo")
        brs = ((yaT, pa, 4, "yaT", "m_pa"), (ysT, pb, 2, "ysT", "m_pb"), (ypT, pc, 2, "ypT", "m_pc"))
        nl_ = 0
        ng = 0
        for half in range(2):
            for dc in range(8):
                wgs = []
                for br in range(3):
                    wb = wg[nl_ % 4]
                    nl_ += 1
                    cast_load(S, wb[:], dr["wg"][l, br * 8 + dc], _nm(wb))
                    wgs.append(wb)
                for s2 in range(2):
                    t0 = half * 1024 + s2 * 512
                    hreads = [("hT", t0 // 128 + q) for q in range(4)]
                    for br in range(3):
                        yT, pw, nk, yn, pn = brs[br]
                        g_ = pg[ng % 2]
                        p_ = pp[ng % 2]
                        s_ = sg[ng % 2]
                        ng += 1
                        for k in range(8):
                            S.op("pe", lambda e, k=k, g_=g_, wb=wgs[br], t0=t0: e.matmul(
                                g_[:], lhsT=wb[:, k, :], rhs=hT[:, k, t0:t0 + 512], start=(k == 0), stop=(k == 7)),
                                reads=[_nm(wgs[br])] + hreads, writes=[_nm(g_)])
                        for k in range(nk):
                            S.op("pe", lambda e, k=k, p_=p_, pw=pw, yT=yT, t0=t0, nk=nk, dc=dc: e.matmul(
                                p_[:], lhsT=pw[:, k, dc * 128:(dc + 1) * 128], rhs=yT[:, k, t0:t0 + 512],
                                start=(k == 0), stop=(k == nk - 1)), reads=[pn, yn], writes=[_nm(p_)])
                        S.op("act", lambda e, g_=g_, s_=s_: e.activation(out=s_[:], in_=g_[:], func=AF.Sigmoid),
                             reads=[_nm(g_)], writes=[_nm(s_)])
                        if br == 0:
                            S.op("dve", lambda e, p_=p_, s_=s_: e.tensor_tensor(out=ac[:], in0=p_[:], in1=s_[:], op=ALU.mult),
                                 reads=[_nm(p_), _nm(s_)], writes=["m_ac"])
                        elif br == 1:
                            S.op("dve", lambda e, p_=p_, s_=s_: e.tensor_tensor(out=tm[:], in0=p_[:], in1=s_[:], op=ALU.mult),
                                 reads=[_nm(p_), _nm(s_)], writes=["m_tm"])
                            S.op("dve", lambda e: e.tensor_tensor(out=ac[:], in0=ac[:], in1=tm[:], op=ALU.add),
                                 reads=["m_tm"], writes=["m_ac"])
                        else:
                            S.op("dve", lambda e, p_=p_, s_=s_: e.tensor_tensor(out=tm[:], in0=p_[:], in1=s_[:], op=ALU.mult),
                                 reads=[_nm(p_), _nm(s_)], writes=["m_tm"])
                            S.op("dve", lambda e, dc=dc, s2=s2: e.tensor_tensor(
                                out=mT[:, dc, s2 * 512:(s2 + 1) * 512], in0=ac[:], in1=tm[:], op=ALU.add),
                                reads=["m_tm", "m_ac"], writes=[("m_mT", dc)])
            for tt in range(8):
                t = half * 8 + tt
                pyy = py[(tt % 2) * 2:(tt % 2) * 2 + 2]
                for i in range(2):
                    for k in range(8):
                        S.op("pe", lambda e, i=i, k=k, tt=tt, pyy=pyy: e.matmul(
                            pyy[i][:], lhsT=mT[:, k, tt * 128:(tt + 1) * 128], rhs=wo[:, k, i * 512:(i + 1) * 512],
                            start=(k == 0), stop=(k == 7)), reads=[("m_mT", k), "m_wo"], writes=[_nm(pyy[i])])
                resid_update((ssy, junk, tmp), t, pyy)
        S.end_phase()


def pool_branch(nc, S, dr, l, b, hT, ypT, sb, ps):
    with ExitStack() as ph:
        w1 = sb(ph, "p_w1", [128, 8, 256], BF16)
        wp = sb(ph, "p_wp", [128, 2, 128], F32)
        wpb = sb(ph, "p_wpb", [128, 2, 128], BF16)
        psc = sb(ph, "p_psc", [128, 2], F32)
        invt = sb(ph, "p_invt", [128, 16], F32)
        U = sb(ph, "p_U", [128, 16 + SEQ], F32)
        s_a = sb(ph, "p_sa", [128, 16 + SEQ], F32)
        s_b = sb(ph, "p_sb", [128, 16 + SEQ], F32)
        pl = sb(ph, "p_pl", [128, SEQ], BF16)
        fx = sb(ph, "p_fx", [128, 16], F32)
        pu = [ps(ph, "p_pu%d" % i, [128, 512], F32) for i in range(4)]
        cast_load(S, w1[:], dr["w1"][l][:, :, 680:936], "p_w1")
        S.dma(psc[:], dr["pscale"][l], writes=["p_psc"])
        S.dma(invt[:], dr["invt"], writes=["p_invt"])
        S.op("dve", lambda e: e.memset(wp[:], 0.0), writes=["p_wp"])
        for g in range(4):
            S.dma(wp[(g % 2) * 64:(g % 2 + 1) * 64, g // 2, (g % 2) * 64:(g % 2 + 1) * 64], dr["wpool"][l, g],
                  reads=[], writes=["p_wp"])
        S.op("dve", lambda e: e.tensor_copy(out=wpb[:], in_=wp[:]), reads=["p_wp"], writes=["p_wpb"])
        for buf in (U, s_a, s_b):
            S.op("dve", lambda e, buf=buf: e.memset(buf[:, 0:16], 0.0), writes=[_nm(buf)])
        for ct in range(2):
            for tc in range(4):
                for k in range(8):
                    S.op("pe", lambda e, k=k, tc=tc, ct=ct: e.matmul(
                        pu[tc][:], lhsT=w1[:, k, ct * 128:(ct + 1) * 128], rhs=hT[:, k, tc * 512:(tc + 1) * 512],
                        start=(k == 0), stop=(k == 7)), reads=["p_w1"] + [("hT", tc * 4 + q) for q in range(4)],
                        writes=[_nm(pu[tc])])
                S.op("act", lambda e, tc=tc: e.copy(out=U[:, 16 + tc * 512:16 + (tc + 1) * 512], in_=pu[tc][:]),
                     reads=[_nm(pu[tc])], writes=["p_U"])
            def shadd(dst, src, sh):
                S.op("dve", lambda e: e.tensor_tensor(out=dst[:, 16:16 + SEQ], in0=src[:, 16:16 + SEQ],
                                                      in1=src[:, 16 - sh:16 - sh + SEQ], op=ALU.add),
                     reads=[_nm(src)], writes=[_nm(dst)])
            shadd(s_a, U, 1)
            shadd(s_b, s_a, 2)
            if ct == 0:
                halves = ((0, s_a, 2), (64, s_b, 4))
            else:
                shadd(s_a, s_b, 4)
                shadd(s_b, s_a, 8)
                halves = ((0, s_a, 8), (64, s_b, 16))
            for (p0, sbuf, win) in halves:
                S.op("dve", lambda e, p0=p0, sbuf=sbuf, win=win: e.scalar_tensor_tensor(
                    out=pl[p0:p0 + 64, :], in0=sbuf[p0:p0 + 64, 16:16 + SEQ], scalar=1.0 / win,
                    in1=U[p0:p0 + 64, 16:16 + SEQ], op0=ALU.mult, op1=ALU.subtract),
                    reads=[_nm(sbuf), "p_U"], writes=["p_pl"])
                S.op("dve", lambda e, p0=p0, sbuf=sbuf, win=win: e.tensor_tensor(
                    out=fx[p0:p0 + 64, 0:win - 1], in0=sbuf[p0:p0 + 64, 16:16 + win - 1],
                    in1=invt[p0:p0 + 64, 0:win - 1], op=ALU.mult), reads=[_nm(sbuf), "p_invt"], writes=["p_fx"])
                S.op("dve", lambda e, p0=p0, win=win: e.tensor_tensor(
                    out=pl[p0:p0 + 64, 0:win - 1], in0=fx[p0:p0 + 64, 0:win - 1], in1=U[p0:p0 + 64, 16:16 + win - 1],
                    op=ALU.subtract), reads=["p_fx", "p_U"], writes=["p_pl"])
            for tc in range(4):
                S.op("pe", lambda e, tc=tc, ct=ct: e.matmul(pu[tc][:], lhsT=wpb[:, ct, :],
                                                         rhs=pl[:, tc * 512:(tc + 1) * 512], start=True, stop=True),
                     reads=["p_wpb", "p_pl"], writes=[_nm(pu[tc])])
                S.op("act", lambda e, tc=tc, ct=ct: e.activation(
                    out=ypT[:, ct, tc * 512:(tc + 1) * 512], in_=pu[tc][:], func=AF.Copy, scale=psc[:, ct:ct + 1]),
                    reads=[_nm(pu[tc]), "p_psc"], writes=["ypT"])
        S.end_phase()


def ssm_branch(nc, S, dr, l, b, hT, ysT, tab, tabb, sb, ps):
    with ExitStack() as ph:
        w1 = sb(ph, "s_w1", [128, 8, 256], BF16)
        uT = sb(ph, "s_uT", [128, 2, SEQ], BF16)
        zT = sb(ph, "s_zT", [128, 2, SEQ], BF16)
        arai = sb(ph, "s_arai", [128, 2, 16, 64], F32)
        T1 = sb(ph, "s_T1", [128, 16, 128], F32)
        T2 = sb(ph, "s_T2", [128, 16, 128], F32)
        pcs = sb(ph, "s_pcs", [128, 64], F32)
        Bblk = sb(ph, "s_Bblk", [128, 2, 1024], BF16)
        Cpad = sb(ph, "s_Cpad", [128, 16, 128], BF16)
        triub = sb(ph, "s_triub", [128, 128], BF16)
        dsk = sb(ph, "s_dsk", [128, 2], F32)
        bgl = sb(ph, "s_bgl", [128, 2], F32)
        wgl = sb(ph, "s_wgl", [128, 2, 256], BF16)
        t1s = [sb(ph, "s_t1%d" % i, [128, 4, 2, 64], BF16) for i in range(2)]
        t2s = [sb(ph, "s_t2%d" % i, [128, 4, 2, 64], BF16) for i in range(2)]
        v = sb(ph, "s_v", [128, 16, 2, 64], BF16)
        xs = sb(ph, "s_xs", [128, 16, 128], BF16)
        xas = [sb(ph, "s_xa%d" % i, [128, 128], F32) for i in range(2)]
        xbs = [sb(ph, "s_xb%d" % i, [128, 128], F32) for i in range(2)]
        cA = [sb(ph, "s_cA%d" % i, [128, 16], F32) for i in range(2)]
        cB = [sb(ph, "s_cB%d" % i, [128, 16], F32) for i in range(2)]
        sA = sb(ph, "s_sA", [128, 16], F32)
        sB = sb(ph, "s_sB", [128, 16], F32)
        q1 = sb(ph, "s_q1", [128, 16], F32)
        yv = sb(ph, "s_yv", [128, 256], F32)
        sg = sb(ph, "s_sg", [128, 512], BF16)
        pbu = [ps(ph, "s_pbu%d" % i, [128, 512], F32) for i in range(2)]
        pA = [ps(ph, "s_pA%d" % i, [128, 512], F32) for i in range(2)]
        pB = [ps(ph, "s_pB%d" % i, [128, 512], F32) for i in range(2)]
        pyy = ps(ph, "s_py", [128, 512], F32)
        pg = ps(ph, "s_pg", [128, 512], F32)
        cast_load(S, w1[:], dr["w1"][l][:, :, 424:680], "s_w1")
        S.dma(arai[:].rearrange("p r g n -> p (r g n)"), tab[l][:, 0:2048], writes=["s_arai"])
        S.dma(T1[:].rearrange("p g t -> p (g t)"), tab[l][:, 2048:4096], writes=["s_T1"])
        S.dma(T2[:].rearrange("p g t -> p (g t)"), tab[l][:, 4096:6144], writes=["s_T2"])
        S.dma(pcs[:], tab[l][:, 6144:6208], writes=["s_pcs"])
        S.dma(Bblk[:].rearrange("p h c -> p (h c)"), tabb[l][:, 0:2048], writes=["s_Bblk"])
        S.dma(Cpad[:].rearrange("p g c -> p (g c)"), tabb[l][:, 2048:4096], writes=["s_Cpad"])
        S.dma(triub[:], dr["triub"], writes=["s_triub"])
        S.dma(dsk[:], dr["dskip"][l], writes=["s_dsk"])
        S.dma(bgl[:], dr["bglu"][l], writes=["s_bgl"])
        cast_load(S, wgl[:], dr["wglu"][l], "s_wgl")
        for ct in range(2):
            for tc in range(4):
                pp = pbu[tc % 2]
                for k in range(8):
                    S.op("pe", lambda e, k=k, tc=tc, ct=ct, pp=pp: e.matmul(
                        pp[:], lhsT=w1[:, k, ct * 128:(ct + 1) * 128], rhs=hT[:, k, tc * 512:(tc + 1) * 512],
                        start=(k == 0), stop=(k == 7)), reads=["s_w1"] + [("hT", tc * 4 + q) for q in range(4)],
                        writes=[_nm(pp)])
                S.op("act", lambda e, tc=tc, ct=ct, pp=pp: e.copy(out=uT[:, ct, tc * 512:(tc + 1) * 512], in_=pp[:]),
                     reads=[_nm(pp)], writes=["s_uT"])
        for i in range(2):
            S.op("dve", lambda e, i=i: e.memset(cA[i][:], 0.0), writes=[_nm(cA[i])])
            S.op("dve", lambda e, i=i: e.memset(cB[i][:], 0.0), writes=[_nm(cB[i])])
        Pr = pcs[:, 0:16]
        PiA = pcs[:, 16:32]
        PiB = pcs[:, 32:48]
        for t in range(NT):
            cAo, cBo = cA[t % 2], cB[t % 2]
            cAn, cBn = cA[(t + 1) % 2], cB[(t + 1) % 2]
            for half in range(2):
                for q2 in range(2):
                    S.op("pe", lambda e, half=half, q2=q2, t=t: e.matmul(
                        pbu[q2][:], lhsT=uT[:, half, t * 128:(t + 1) * 128], rhs=Bblk[:, half, q2 * 512:(q2 + 1) * 512],
                        start=True, stop=True), reads=["s_uT", "s_Bblk"], writes=[_nm(pbu[q2])])
                for q2 in range(2):
                    g0 = half * 8 + q2 * 4
                    t1 = t1s[q2]
                    t2 = t2s[q2]
                    bu = pbu[q2][:].rearrange("p (g r n) -> p g r n", g=4, r=2)
                    Ar = arai[:, 0, g0:g0 + 4, :]
                    Ai = arai[:, 1, g0:g0 + 4, :]
                    S.op("dve", lambda e, bu=bu, Ar=Ar: e.tensor_tensor(
                        out=t1[:], in0=bu, in1=Ar.unsqueeze(2).to_broadcast([128, 4, 2, 64]), op=ALU.mult),
                        reads=[_nm(pbu[q2]), "s_arai"], writes=[_nm(t1)])
                    S.op("dve", lambda e, bu=bu, Ai=Ai: e.tensor_tensor(
                        out=t2[:, :, 0, :], in0=bu[:, :, 1, :], in1=Ai, op=ALU.mult),
                        reads=[_nm(pbu[q2]), "s_arai"], writes=[_nm(t2)])
                    S.op("dve", lambda e, bu=bu, Ai=Ai: e.tensor_tensor(
                        out=t2[:, :, 1, :], in0=bu[:, :, 0, :], in1=Ai, op=ALU.mult),
                        reads=[_nm(pbu[q2]), "s_arai"], writes=[_nm(t2)])
                    S.op("pool", lambda e, g0=g0: e.tensor_tensor(
                        out=v[:, g0:g0 + 4, 0, :], in0=t1[:, :, 0, :], in1=t2[:, :, 0, :], op=ALU.subtract),
                        reads=[_nm(t1), _nm(t2)], writes=[("s_v", g0)])
                    S.op("pool", lambda e, g0=g0: e.tensor_tensor(
                        out=v[:, g0:g0 + 4, 1, :], in0=t1[:, :, 1, :], in1=t2[:, :, 1, :], op=ALU.add),
                        reads=[_nm(t1), _nm(t2)], writes=[("s_v", g0)])
                    pa_, pb_ = pA[q2], pB[q2]
                    for gg in range(4):
                        g = g0 + gg
                        S.op("pe", lambda e, g=g, gg=gg, pa_=pa_: e.matmul(
                            pa_[:, gg * 128:(gg + 1) * 128], lhsT=v[:, g].rearrange("p r n -> p (r n)"), rhs=triub[:],
                            start=True, stop=True), reads=[("s_v", g0), "s_triub"], writes=[_nm(pa_)])
                        S.op("pe", lambda e, g=g, gg=gg, pb_=pb_: e.matmul(
                            pb_[0:64, gg * 128:(gg + 1) * 128], lhsT=v[:, g, 1, :], rhs=triub[:],
                            start=True, stop=True), reads=[("s_v", g0), "s_triub"], writes=[_nm(pb_)])
                        S.op("pe", lambda e, g=g, gg=gg, pb_=pb_: e.matmul(
                            pb_[64:128, gg * 128:(gg + 1) * 128], lhsT=v[:, g, 0, :], rhs=triub[:],
                            start=True, stop=True), reads=[("s_v", g0), "s_triub"], writes=[_nm(pb_)])
                    A127 = pa_[:].rearrange("p (g t) -> p g t", t=128)[:, :, 127]
                    B127 = pb_[:].rearrange("p (g t) -> p g t", t=128)[:, :, 127]
                    gs = slice(g0, g0 + 4)
                    S.op("dve", lambda e, A127=A127, gs=gs: e.tensor_tensor(out=sA[:, gs], in0=A127, in1=cAo[:, gs], op=ALU.add),
                         reads=[_nm(pa_), _nm(cAo)], writes=["s_sA"])
                    S.op("dve", lambda e, B127=B127, gs=gs: e.tensor_tensor(out=sB[:, gs], in0=B127, in1=cBo[:, gs], op=ALU.add),
                         reads=[_nm(pb_), _nm(cBo)], writes=["s_sB"])
                    for gg in range(4):
                        g = g0 + gg
                        xa = xas[gg % 2]
                        xb = xbs[gg % 2]
                        S.op("dve", lambda e, g=g, gg=gg, pa_=pa_, xa=xa: e.scalar_tensor_tensor(
                            out=xa[:], in0=pa_[:, gg * 128:(gg + 1) * 128], scalar=cAo[:, g:g + 1], in1=T1[:, g, :],
                            op0=ALU.add, op1=ALU.mult), reads=[_nm(pa_), _nm(cAo), "s_T1"], writes=[_nm(xa)])
                        S.op("dve", lambda e, g=g, gg=gg, pb_=pb_, xb=xb: e.scalar_tensor_tensor(
                            out=xb[:], in0=pb_[:, gg * 128:(gg + 1) * 128], scalar=cBo[:, g:g + 1], in1=T2[:, g, :],
                            op0=ALU.add, op1=ALU.mult), reads=[_nm(pb_), _nm(cBo), "s_T2"], writes=[_nm(xb)])
                        S.op("pool", lambda e, g=g, xa=xa, xb=xb: e.tensor_tensor(out=xs[:, g, :], in0=xa[:], in1=xb[:], op=ALU.add),
                             reads=[_nm(xa), _nm(xb)], writes=[("s_xs", g)])
                    S.op("dve", lambda e, gs=gs: e.tensor_tensor(out=cAn[:, gs], in0=sA[:, gs], in1=Pr[:, gs], op=ALU.mult),
                         reads=["s_sA", "s_pcs"], writes=[_nm(cAn)])
                    S.op("dve", lambda e, gs=gs: e.tensor_tensor(out=q1[:, gs], in0=sB[:, gs], in1=PiA[:, gs], op=ALU.mult),
                         reads=["s_sB", "s_pcs"], writes=["s_q1"])
                    S.op("dve", lambda e, gs=gs: e.tensor_tensor(out=cAn[:, gs], in0=cAn[:, gs], in1=q1[:, gs], op=ALU.add),
                         reads=["s_q1"], writes=[_nm(cAn)])
                    S.op("dve", lambda e, gs=gs: e.tensor_tensor(out=cBn[:, gs], in0=sB[:, gs], in1=Pr[:, gs], op=ALU.mult),
                         reads=["s_sB", "s_pcs"], writes=[_nm(cBn)])
                    S.op("dve", lambda e, gs=gs: e.tensor_tensor(out=q1[:, gs], in0=sA[:, gs], in1=PiB[:, gs], op=ALU.mult),
                         reads=["s_sA", "s_pcs"], writes=["s_q1"])
                    S.op("dve", lambda e, gs=gs: e.tensor_tensor(out=cBn[:, gs], in0=cBn[:, gs], in1=q1[:, gs], op=ALU.add),
                         reads=["s_q1"], writes=[_nm(cBn)])
                for g8 in range(8):
                    g = half * 8 + g8
                    S.op("pe", lambda e, g=g, g8=g8, half=half: e.matmul(
                        pyy[:, half * 128:(half + 1) * 128], lhsT=Cpad[:, g, :], rhs=xs[:, g, :],
                        start=(g8 == 0), stop=(g8 == 7)), reads=[("s_xs", g), "s_Cpad"], writes=["s_py"])
                S.op("dve", lambda e, half=half, t=t: e.scalar_tensor_tensor(
                    out=yv[:, half * 128:(half + 1) * 128], in0=uT[:, half, t * 128:(t + 1) * 128],
                    scalar=dsk[:, half:half + 1], in1=pyy[:, half * 128:(half + 1) * 128], op0=ALU.mult, op1=ALU.add),
                    reads=["s_py", "s_uT", "s_dsk"], writes=["s_yv"])
                S.op("act", lambda e, half=half, t=t: e.activation(
                    out=zT[:, half, t * 128:(t + 1) * 128], in_=yv[:, half * 128:(half + 1) * 128],
                    func=AF.Gelu_apprx_tanh), reads=["s_yv"], writes=[("s_zT", t // 4)])
            if t % 4 == 3:
                tc = t // 4
                for ct in range(2):
                    for k in range(2):
                        S.op("pe", lambda e, k=k, ct=ct, tc=tc: e.matmul(
                            pg[:], lhsT=wgl[:, k, ct * 128:(ct + 1) * 128], rhs=zT[:, k, tc * 512:(tc + 1) * 512],
                            start=(k == 0), stop=(k == 1)), reads=["s_wgl", ("s_zT", tc)], writes=["s_pg"])
                    S.op("act", lambda e, ct=ct: e.activation(out=sg[:], in_=pg[:], func=AF.Sigmoid,
                                                              bias=bgl[:, ct:ct + 1]),
                         reads=["s_pg", "s_bgl"], writes=["s_sg"])
                    S.op("dve", lambda e, ct=ct, tc=tc: e.tensor_tensor(
                        out=ysT[:, ct, tc * 512:(tc + 1) * 512], in0=zT[:, ct, tc * 512:(tc + 1) * 512], in1=sg[:],
                        op=ALU.mult), reads=["s_sg", ("s_zT", tc)], writes=["ysT"])
        S.end_phase()


def attention(nc, S, dr, l, b, hT, yaT, sb, ps, rstd_from_ss, identb, onesb):
    with ExitStack() as at:
        cqnT = sb(at, "a_cqnT", [128, 2, SEQ], BF16)
        ckvT = sb(at, "a_ckvT", [128, SEQ], BF16)
        Vg = sb(at, "a_V", [128, NT, 128], BF16)
        kidT = sb(at, "a_kidT", [128, SEQ], BF16)
        Wsc = sb(at, "a_Wsc", [128, NT, 8], F32)
        with ExitStack() as ph:
            w1 = sb(ph, "a_w1", [128, 8, 424], BF16)
            gcq = sb(ph, "a_gcq", [128, 2], F32)
            gkv = sb(ph, "a_gkv", [128, 128], F32)
            ss = sb(ph, "a_ss", [128, 2], F32)
            junk = sb(ph, "a_junk", [128, 256], BF16)
            cqn = sb(ph, "a_cqn", [128, 256], BF16)
            kid4 = sb(ph, "a_kid4", [128, 4, 32], BF16)
            pz = [ps(ph, "a_pz%d" % i, [128, 512], F32) for i in range(2)]
            pt = [ps(ph, "a_pt%d" % i, [128, 1024], BF16) for i in range(2)]
            cast_load(S, w1[:], dr["w1"][l][:, :, 0:424], "a_w1")
            S.dma(gcq[:], dr["gcq"][l], writes=["a_gcq"])
            S.dma(gkv[:], dr["gckv"][l:l + 1, :].to_broadcast([128, 128]), writes=["a_gkv"])
            for t in range(NT):
                p = pz[t % 2]
                q = pt[t % 2]
                for k in range(8):
                    S.op("pe", lambda e, k=k, t=t, p=p: e.matmul(p[:, 0:424], lhsT=hT[:, k, t * 128:(t + 1) * 128],
                                                                 rhs=w1[:, k, :], start=(k == 0), stop=(k == 7)),
                         reads=["a_w1", ("hT", t)], writes=[_nm(p)])
                S.op("act", lambda e, p=p: e.activation(out=junk[:], in_=p[:, 0:256], func=AF.Square,
                                                        accum_out=ss[:, 0:1]), reads=[_nm(p)], writes=["a_junk", "a_ss"])
                S.op("act", lambda e, p=p: e.activation(out=junk[:, 0:128], in_=p[:, 256:384], func=AF.Square,
                                                        accum_out=ss[:, 1:2]), reads=[_nm(p)], writes=["a_junk", "a_ss"])
                S.op("dve", lambda e: e.tensor_scalar(out=ss[:, 0:1], in0=ss[:, 0:1], scalar1=1.0 / 256, scalar2=EPS,
                                                      op0=ALU.mult, op1=ALU.add), reads=["a_ss"], writes=["a_ss"])
                S.op("dve", lambda e: e.tensor_scalar(out=ss[:, 1:2], in0=ss[:, 1:2], scalar1=1.0 / 128, scalar2=EPS,
                                                      op0=ALU.mult, op1=ALU.add), reads=["a_ss"], writes=["a_ss"])
                S.op("act", lambda e: e.activation(out=ss[:], in_=ss[:], func=AF.Sqrt), reads=["a_ss"], writes=["a_ss"])
                S.op("dve", lambda e: e.reciprocal(out=ss[:], in_=ss[:]), reads=["a_ss"], writes=["a_ss"])
                S.op("dve", lambda e, p=p: e.tensor_scalar(out=cqn[:], in0=p[:, 0:256], scalar1=ss[:, 0:1], scalar2=None,
                                                           op0=ALU.mult), reads=[_nm(p), "a_ss"], writes=["a_cqn"])
                S.op("dve", lambda e, p=p, t=t: e.scalar_tensor_tensor(
                    out=Vg[:, t, :], in0=p[:, 256:384], scalar=ss[:, 1:2], in1=gkv[:], op0=ALU.mult, op1=ALU.mult),
                    reads=[_nm(p), "a_ss", "a_gkv"], writes=[("a_V", t)])
                S.op("dve", lambda e, p=p: e.tensor_copy(
                    out=kid4[:], in_=p[:, 384:416].unsqueeze(1).to_broadcast([128, 4, 32])),
                    reads=[_nm(p)], writes=["a_kid4"])
                S.op("dve", lambda e, p=p, t=t: e.tensor_scalar(out=Wsc[:, t, :], in0=p[:, 416:424], scalar1=IDXW,
                                                                scalar2=None, op0=ALU.mult),
                     reads=[_nm(p)], writes=["a_Wsc"])
                for c in range(2):
                    S.op("pe", lambda e, c=c, q=q: e.transpose(out=q[:, c * 128:(c + 1) * 128],
                                                              in_=cqn[:, c * 128:(c + 1) * 128], identity=identb[:]),
                         reads=["a_cqn", "identb"], writes=[_nm(q)])
                S.op("pe", lambda e, q=q, t=t: e.transpose(out=q[:, 256:384], in_=Vg[:, t, :], identity=identb[:]),
                     reads=[("a_V", t), "identb"], writes=[_nm(q)])
                S.op("pe", lambda e, q=q: e.transpose(out=q[:, 384:512], in_=kid4[:].rearrange("p a b -> p (a b)"),
                                                      identity=identb[:]), reads=["a_kid4", "identb"], writes=[_nm(q)])
                for c in range(2):
                    S.op("act", lambda e, c=c, q=q, t=t: e.activation(
                        out=cqnT[:, c, t * 128:(t + 1) * 128], in_=q[:, c * 128:(c + 1) * 128], func=AF.Copy,
                        scale=gcq[:, c:c + 1]), reads=[_nm(q), "a_gcq"], writes=[("a_cqnT", t)])
                S.op("dve", lambda e, q=q, t=t: e.tensor_copy(out=ckvT[:, t * 128:(t + 1) * 128], in_=q[:, 256:384]),
                     reads=[_nm(q)], writes=[("a_ckvT", t)])
                S.op("dve", lambda e, q=q, t=t: e.tensor_copy(out=kidT[:, t * 128:(t + 1) * 128], in_=q[:, 384:512]),
                     reads=[_nm(q)], writes=[("a_kidT", t)])
            S.end_phase()
        with ExitStack() as ph:
            wuq = sb(ph, "a_wuq", [128, 2, 1024], BF16)
            wqi = sb(ph, "a_wqi", [128, 2, 256], BF16)
            wuv = sb(ph, "a_wuv", [128, 8, 64], BF16)
            i4b = sb(ph, "a_i4b", [128, 512], BF16)
            altd = sb(ph, "a_altd", [128, 1024], BF16)
            at3 = sb(ph, "a_at3", [3, 2048], BF16)
            bt3 = sb(ph, "a_bt3", [3, 1024], BF16)
            m0 = sb(ph, "a_m0", [128, 128], F32)
            pow2 = sb(ph, "a_pow2", [128, NIT + 1], F32)
            qT = sb(ph, "a_qT", [128, 8, 128], BF16)
            qiT = sb(ph, "a_qiT", [128, 3, 128], BF16)
            acc = sb(ph, "a_acc", [128, SEQ], F32)
            jk = sb(ph, "a_jk", [128, SEQ], BF16)
            MBs = [sb(ph, "a_MB%d" % i, [128, SEQ], BF16) for i in range(2)]
            Rb = [sb(ph, "a_R%d" % i, [128, 512], BF16) for i in range(2)]
            wdg = sb(ph, "a_wdg", [128, 8, 128], BF16)
            PT = [sb(ph, "a_PT%d" % i, [128, 512], BF16) for i in range(2)]
            rec = sb(ph, "a_rec", [128, 512], F32)
            oT = sb(ph, "a_oT", [128, 1024], BF16)
            sm = sb(ph, "a_sm", [128, 8], F32)
            Wk = sb(ph, "a_Wk", [128, NIT + 1], F32)
            pS = [ps(ph, "a_pS%d" % i, [128, 512], F32) for i in range(2)]
            pL = [ps(ph, "a_pL%d" % i, [128, 512], F32) for i in range(2)]
            pQ = ps(ph, "a_pQ", [128, 512], F32)
            pACC = ps(ph, "a_pACC", [128, 512], F32)
            pO = ps(ph, "a_pO", [128, 512], F32)
            pD = ps(ph, "a_pD", [128, 512], F32)
            pY = pQ
            cast_load(S, wuq[:], dr["wuq"][l], "a_wuq")
            cast_load(S, wqi[:], dr["wqi"][l], "a_wqi")
            cast_load(S, wuv[:], dr["wuv"][l], "a_wuv")
            for nm, tl in (("i4b", i4b), ("altd", altd), ("at3", at3), ("bt3", bt3), ("m0", m0), ("pow2", pow2)):
                S.dma(tl[:], dr[nm], writes=["a_" + nm])
            hi, lo, w0, mid, cnt, tt, thr = (sm[:, i:i + 1] for i in range(7))
            ACCR = [("a_acc", c_) for c_ in range(4)]

            def stageA(i):
                steps = []
                N = 128 * (i + 1)
                qs = slice(i * 128, (i + 1) * 128)
                MB = MBs[i % 2]
                mbn = "a_MB%d" % (i % 2)

                def qi_proj(tl):
                    ncol = 96 if tl < 2 else 64
                    for k in range(2):
                        S.op("pe", lambda e: e.matmul(
                            pQ[0:ncol, tl * 128:(tl + 1) * 128], lhsT=wqi[:, k, tl * 96:tl * 96 + ncol],
                            rhs=cqnT[:, k, qs], start=(k == 0), stop=(k == 1)),
                            reads=["a_wqi", ("a_cqnT", i)], writes=["a_pQ"])
                    S.op("act", lambda e: e.copy(out=qiT[0:ncol, tl, :], in_=pQ[0:ncol, tl * 128:(tl + 1) * 128]),
                         reads=["a_pQ"], writes=["a_qiT"])
                for tl in range(3):
                    steps.append(partial(qi_proj, tl))

                nch = (N + 511) // 512

                def diag_build():
                    for h in range(8):
                        S.op("act", lambda e: e.activation(out=wdg[:, h, :], in_=identb[:], func=AF.Copy,
                                                           scale=Wsc[:, i, h:h + 1]),
                             reads=["identb", "a_Wsc"], writes=["a_wdg"])
                steps.append(diag_build)
                seq = [(cc, h) for cc in range(nch) for h in range(8)]

                def emitL(x):
                    cc, h = seq[x]
                    p0 = (h % 3) * 32
                    n = min(512, N - cc * 512)
                    pl_ = pL[x % 2]
                    S.op("pe", lambda e: e.matmul(
                        pl_[:, 0:n], lhsT=qiT[p0:p0 + 32, h // 3, :], rhs=kidT[p0:p0 + 32, cc * 512:cc * 512 + n],
                        start=True, stop=True), reads=["a_qiT"] + [("a_kidT", cc * 4 + q) for q in range((n + 127) // 128)],
                        writes=[_nm(pl_)])

                def idx_step(x):
                    cc, h = seq[x]
                    n = min(512, N - cc * 512)
                    if x == 0:
                        emitL(0)
                    if x + 1 < len(seq):
                        emitL(x + 1)
                    pl_ = pL[x % 2]
                    rb = Rb[x % 2]
                    S.op("act", lambda e: e.activation(out=rb[:, 0:n], in_=pl_[:, 0:n], func=AF.Relu),
                         reads=[_nm(pl_)], writes=[_nm(rb)])
                    S.op("pe", lambda e: e.matmul(pACC[:, 0:n], lhsT=wdg[:, h, :], rhs=rb[:, 0:n],
                                                  start=(h == 0), stop=(h == 7)),
                         reads=["a_wdg", _nm(rb)], writes=["a_pACC"])
                    if h == 7:
                        S.op("act", lambda e: e.copy(out=acc[:, cc * 512:cc * 512 + n], in_=pACC[:, 0:n]),
                             reads=["a_pACC"], writes=[("a_acc", cc)])
                for x in range(len(seq)):
                    steps.append(partial(idx_step, x))

                def bounds():
                    if i >= 2:
                        S.op("dve", lambda e: e.tensor_reduce(out=hi, in_=acc[:, 0:N], axis=AX.X, op=ALU.max),
                             reads=ACCR, writes=["a_sm"])
                        S.op("dve", lambda e: e.tensor_reduce(out=lo, in_=acc[:, 0:N], axis=AX.X, op=ALU.min),
                             reads=ACCR, writes=["a_sm"])
                    S.op("dve", lambda e: e.tensor_tensor(out=acc[:, N - 128:N], in0=acc[:, N - 128:N], in1=m0[:],
                                                          op=ALU.add), reads=["a_m0"] + ACCR, writes=ACCR)
                    if i >= 2:
                        S.op("dve", lambda e: e.tensor_tensor(out=w0, in0=hi, in1=lo, op=ALU.subtract),
                             reads=["a_sm"], writes=["a_sm"])
                        S.op("dve", lambda e: e.tensor_scalar(out=Wk[:], in0=pow2[:], scalar1=w0, scalar2=None,
                                                              op0=ALU.mult), reads=["a_sm", "a_pow2"], writes=["a_Wk"])
                        S.op("dve", lambda e: e.tensor_tensor(out=mid, in0=lo, in1=Wk[:, 0:1], op=ALU.add),
                             reads=["a_sm", "a_Wk"], writes=["a_sm"])
                    else:
                        S.op("dve", lambda e: e.memset(thr, -1.0e29), writes=["a_sm"])
                steps.append(bounds)

                def bis(k):
                    S.op("dve", lambda e: e.tensor_scalar(
                        out=jk[:, 0:N], in0=acc[:, 0:N], scalar1=mid, scalar2=0.0, op0=ALU.is_ge, op1=ALU.add,
                        accum_out=cnt), reads=ACCR + ["a_sm"], writes=["a_jk", "a_sm"])
                    S.op("dve", lambda e: e.tensor_scalar(out=tt, in0=cnt, scalar1=255.5, scalar2=0.5, op0=ALU.is_ge,
                                                          op1=ALU.subtract), reads=["a_sm"], writes=["a_sm"])
                    S.op("dve", lambda e: e.scalar_tensor_tensor(out=mid, in0=tt, scalar=Wk[:, k:k + 1], in1=mid,
                                                                 op0=ALU.mult, op1=ALU.add),
                         reads=["a_sm", "a_Wk"], writes=["a_sm"])
                if i >= 2:
                    for k in range(NIT):
                        steps.append(partial(bis, k))

                def fin():
                    if i >= 2:
                        S.op("dve", lambda e: e.tensor_tensor(out=thr, in0=mid, in1=Wk[:, NIT:NIT + 1], op=ALU.subtract),
                             reads=["a_sm", "a_Wk"], writes=["a_sm"])
                    S.op("dve", lambda e: e.tensor_scalar(out=MB[:, 0:N], in0=acc[:, 0:N], scalar1=thr, scalar2=-30000.0,
                                                          op0=ALU.is_lt, op1=ALU.mult),
                         reads=ACCR + ["a_sm"], writes=[mbn])
                steps.append(fin)
                return steps

            def stageB(i):
                steps = []
                qs = slice(i * 128, (i + 1) * 128)
                MB = MBs[i % 2]
                mbn = "a_MB%d" % (i % 2)

                def q_proj(g2):
                    for h in range(g2 * 4, g2 * 4 + 4):
                        for k in range(2):
                            S.op("pe", lambda e: e.matmul(
                                pS[g2][:, (h % 4) * 128:(h % 4 + 1) * 128], lhsT=wuq[:, k, h * 128:(h + 1) * 128],
                                rhs=cqnT[:, k, qs], start=(k == 0), stop=(k == 1)),
                                reads=["a_wuq", ("a_cqnT", i)], writes=[_nm(pS[g2])])
                    S.op("act", lambda e: e.activation(
                        out=qT[:, g2 * 4:(g2 + 1) * 4, :].rearrange("p h q -> p (h q)"), in_=pS[g2][:], func=AF.Copy,
                        scale=ATT_SC), reads=[_nm(pS[g2])], writes=["a_qT"])
                steps.append(partial(q_proj, 0))
                steps.append(partial(q_proj, 1))
                its = [(g, j) for g in range(2) for j in range(i + 1)]

                def emitS(n):
                    grp, j = its[n]
                    s_ = pS[n % 2]
                    S.op("pe", lambda e: e.matmul(
                        s_[:], lhsT=ckvT[:, j * 128:(j + 1) * 128],
                        rhs=qT[:, grp * 4:(grp + 1) * 4, :].rearrange("p h q -> p (h q)"), start=True, stop=False),
                        reads=[("a_ckvT", j), "a_qT"], writes=[_nm(s_)])
                    S.op("pe", lambda e: e.matmul(
                        s_[:], lhsT=MB[:, j * 128:(j + 1) * 128], rhs=i4b[:], start=False, stop=False),
                        reads=[mbn, "a_i4b"], writes=[_nm(s_)])
                    if j < i:
                        dd = i - j
                        S.op("pe", lambda e: e.matmul(
                            s_[:], lhsT=at3[0:3, dd * 128:(dd + 1) * 128], rhs=bt3[0:3, grp * 512:(grp + 1) * 512],
                            start=False, stop=True), reads=["a_at3", "a_bt3"], writes=[_nm(s_)])
                    else:
                        S.op("pe", lambda e: e.matmul(
                            s_[:], lhsT=identb[:], rhs=altd[:, grp * 512:(grp + 1) * 512], start=False, stop=True),
                            reads=["identb", "a_altd"], writes=[_nm(s_)])

                def it_step(n):
                    grp, j = its[n]
                    if n == 0:
                        emitS(0)
                    if n + 1 < len(its):
                        emitS(n + 1)
                    s_ = pS[n % 2]
                    ptb = PT[n % 2]
                    S.op("act", lambda e: e.activation(out=ptb[:], in_=s_[:], func=AF.Exp),
                         reads=[_nm(s_)], writes=[_nm(ptb)])
                    S.op("pe", lambda e: e.matmul(pO[:], lhsT=Vg[:, j, :], rhs=ptb[:], start=(j == 0), stop=(j == i)),
                         reads=[("a_V", j), _nm(ptb)], writes=["a_pO"])
                    S.op("pe", lambda e: e.matmul(pD[:], lhsT=onesb[:], rhs=ptb[:], start=(j == 0), stop=(j == i)),
                         reads=["onesb", _nm(ptb)], writes=["a_pD"])
                    if j == i:
                        S.op("dve", lambda e: e.reciprocal(out=rec[:], in_=pD[:]), reads=["a_pD"], writes=["a_rec"])
                        S.op("dve", lambda e: e.tensor_tensor(out=oT[:, grp * 512:(grp + 1) * 512], in0=pO[:],
                                                              in1=rec[:], op=ALU.mult),
                             reads=["a_pO", "a_rec"], writes=["a_oT"])
                for n in range(len(its)):
                    steps.append(partial(it_step, n))

                def y_proj():
                    for h in range(8):
                        S.op("pe", lambda e: e.matmul(
                            pY[(h % 2) * 64:(h % 2 + 1) * 64, (h // 2) * 128:(h // 2 + 1) * 128], lhsT=wuv[:, h, :],
                            rhs=oT[:, h * 128:(h + 1) * 128], start=True, stop=True),
                            reads=["a_wuv", "a_oT"], writes=["a_pQ"])
                    S.op("act", lambda e: e.copy(out=yaT[:, :, qs], in_=pY[:].rearrange("p (c q) -> p c q", q=128)),
                         reads=["a_pQ"], writes=["yaT"])
                steps.append(y_proj)
                return steps

            def run_merged(la, lb):
                na, nb_ = len(la), len(lb)
                ia = ib = 0
                while ia < na or ib < nb_:
                    if ib >= nb_ or (ia < na and ia * nb_ <= ib * na):
                        la[ia]()
                        ia += 1
                    else:
                        lb[ib]()
                        ib += 1

            run_merged(stageA(0), [])
            for i in range(NT):
                run_merged(stageA(i + 1) if i + 1 < NT else [], stageB(i))
            S.end_phase()


_BF = {"identb", "i4b", "onesb", "triub", "altd", "at3", "bt3"}


def kernel(**inputs):
    inp = {k: np.asarray(v) for k, v in inputs.items()}
    n_cores = 8
    shared = _layout_weights(inp)
    shared.update(_consts())
    x = np.ascontiguousarray(inp["x"], dtype=np.float32)
    c = np.asarray(inp["c"], dtype=np.float32)
    in_maps = []
    for ci in range(n_cores):
        m = dict(shared)
        m["x"] = x[ci * NB:(ci + 1) * NB]
        cc = c[ci * NB:(ci + 1) * NB]
        m["cT"] = np.ascontiguousarray(cc.reshape(NB, 8, 128).transpose(2, 1, 0))
        in_maps.append(m)
    shapes = {k: (v.shape, BF16 if k in _BF else F32) for k, v in in_maps[0].items()}
    nc = build_nc(shapes)
    in_maps = [{"d_" + k: v for k, v in m.items()} for m in in_maps]
    res = run_bass_kernel_spmd(nc, in_maps, core_ids=list(range(n_cores)))
    return np.concatenate([np.asarray(r["out"], dtype=np.float32) for r in res.results], axis=0)
```

```python
import types
from functools import partial
import numpy as np
import ml_dtypes
from contextlib import ExitStack
import concourse.bass as bass
import concourse.mybir as mybir
from concourse.bass_utils import run_bass_kernel_spmd

F32 = mybir.dt.float32
BF16 = mybir.dt.bfloat16
AF = mybir.ActivationFunctionType
ALU = mybir.AluOpType
AX = mybir.AxisListType

D = 1024
SEQ = 2048
NT = 16
NL = 2
NB = 4
EPS = 1e-6
ATT_SC = 128.0 ** -0.5
IDXW = (32.0 ** -0.5) * (8.0 ** -0.5)
NIT = 18
NEG = -1.0e30
POOL_WINS = (2, 4, 8, 16)


_BASE = {}
_UNIQ = [0]


def _nm(t):
    return _BASE[t.name]


def _freeze(fn):
    if fn.__closure__ is None:
        return fn
    cells = []
    for c in fn.__closure__:
        try:
            cells.append(types.CellType(c.cell_contents))
        except ValueError:
            cells.append(c)
    return types.FunctionType(fn.__code__, fn.__globals__, fn.__name__, fn.__defaults__, tuple(cells))


class Sched:
    ENG = ("pe", "act", "dve", "pool", "sp")

    def __init__(self, nc, es, n_dma_sems=12):
        self.nc = nc
        self.sems = {e: es.enter_context(nc.semaphore("s_" + e)) for e in self.ENG}
        self.cnt = {e: 0 for e in self.ENG}
        self.dma_sems = [es.enter_context(nc.semaphore("d%d" % i)) for i in range(n_dma_sems)]
        self.dma_cnt = [0] * n_dma_sems
        self.dma_rr = 0
        self.prog = {e: [] for e in self.ENG}
        self.seen = {e: {} for e in self.ENG}
        self.lastw = {}
        self.readers = {}
        self.ninst = 0
        self.excl = set()

    def _sem_of(self, key):
        if isinstance(key, tuple):
            return self.dma_sems[key[1]], 16
        return self.sems[key], 1

    def _needed(self, eng, deps):
        best = {}
        for (key, val) in deps:
            if key == eng and eng in ("pe", "sp"):
                continue
            if self.seen[eng].get(key, 0) >= val:
                continue
            if best.get(key, 0) < val:
                best[key] = val
        waits = []
        for key, val in best.items():
            self.seen[eng][key] = val
            sem, step = self._sem_of(key)
            waits.append((sem, val * step))
        return waits

    def _deps(self, reads, writes):
        deps = []
        for r in reads:
            t = self.lastw.get(r)
            if t is not None:
                deps.append(t)
        for w in writes:
            t = self.lastw.get(w)
            if t is not None:
                deps.append(t)
            rd = self.readers.get(w)
            if rd:
                deps.extend(rd.items())
        return deps

    def _commit(self, tok, reads, writes):
        for w in writes:
            self.lastw[w] = tok
            self.readers[w] = {}
        for r in reads:
            d = self.readers.setdefault(r, {})
            if d.get(tok[0], 0) < tok[1]:
                d[tok[0]] = tok[1]

    def op(self, eng, fn, reads=(), writes=()):
        fn = _freeze(fn)
        if self.excl:
            ex = [r for r in reads if r in self.excl]
            if ex:
                writes = list(writes) + [r for r in ex if r not in writes]
        waits = self._needed(eng, self._deps(reads, writes))
        self.cnt[eng] += 1
        tok = (eng, self.cnt[eng])
        sem = self.sems[eng]

        def emit(e, waits=waits, fn=fn, sem=sem):
            for (s, v) in waits:
                e.wait_ge(s, v)
            fn(e).then_inc(sem, 1)

        self.prog[eng].append(emit)
        self._commit(tok, reads, writes)
        self.ninst += 1
        return tok

    def dma(self, out, in_, reads=(), writes=(), q="sp", **kw):
        k = self.dma_rr
        self.dma_rr = (k + 1) % len(self.dma_sems)
        deps = self._deps(reads, writes)
        if self.dma_cnt[k] > 0:
            deps.append((("dma", k), self.dma_cnt[k]))
        waits = self._needed(q, deps)
        self.dma_cnt[k] += 1
        tok = (("dma", k), self.dma_cnt[k])
        sem = self.dma_sems[k]

        def emit(e, waits=waits, sem=sem, out=out, in_=in_, kw=kw):
            for (s, v) in waits:
                e.wait_ge(s, v)
            e.dma_start(out=out, in_=in_, **kw).then_inc(sem, 16)

        self.prog[q].append(emit)
        self._commit(tok, reads, writes)
        self.ninst += 1
        return tok

    def end_phase(self, q="sp"):
        waits = [(self.dma_sems[k], 16 * c) for k, c in enumerate(self.dma_cnt) if c > 0]
        waits += [(self.sems[e], self.cnt[e]) for e in self.ENG if self.cnt[e] > 0 and e != q]

        def emit(e, waits=waits):
            for (s, v) in waits:
                e.wait_ge(s, v)

        self.prog[q].append(emit)
        nc = self.nc
        prog = self.prog
        with nc.Block() as block:
            if prog["pe"]:
                @block.tensor
                def _(e):
                    for f in prog["pe"]:
                        f(e)
            if prog["act"]:
                @block.scalar
                def _(e):
                    for f in prog["act"]:
                        f(e)
            if prog["dve"]:
                @block.vector
                def _(e):
                    for f in prog["dve"]:
                        f(e)
            if prog["pool"]:
                @block.gpsimd
                def _(e):
                    for f in prog["pool"]:
                        f(e)
            if prog["sp"]:
                @block.sync
                def _(e):
                    for f in prog["sp"]:
                        f(e)
        self.prog = {e: [] for e in self.ENG}


def _consts():
    bf = ml_dtypes.bfloat16
    c = {}
    c["identf"] = np.eye(128, dtype=np.float32)
    c["identb"] = np.eye(128, dtype=np.float32).astype(bf)
    c["i4b"] = np.tile(np.eye(128, dtype=np.float32), (1, 4)).astype(bf)
    c["onesb"] = np.ones((128, 128), np.float32).astype(bf)
    c["onesf"] = np.ones((128, 128), np.float32)
    s = np.arange(128)
    c["triub"] = (s[:, None] <= s[None, :]).astype(np.float32).astype(bf)
    m0 = np.zeros((128, 128), np.float32)
    m0[:64, 64:] = NEG
    c["m0"] = m0
    slopes = 2.0 ** (-8.0 * np.arange(1, 9) / 8.0)
    altd = np.zeros((128, 8, 128), np.float32)
    for h in range(8):
        altd[:, h, :] = -slopes[h] * np.abs(s[None, :] - s[:, None])
    c["altd"] = altd.reshape(128, 1024).astype(bf)
    at3 = np.zeros((3, 16, 128), np.float32)
    at3[0] = 1.0
    at3[1] = (128.0 * np.arange(16))[:, None]
    at3[2] = s[None, :].astype(np.float32)
    at3p = np.zeros((128, 2048), np.float32)
    at3p[0:3] = at3.reshape(3, 2048)
    c["at3"] = at3p.astype(bf)
    bt3 = np.zeros((3, 8, 128), np.float32)
    for h in range(8):
        bt3[0, h] = -slopes[h] * s
        bt3[1, h] = -slopes[h]
        bt3[2, h] = slopes[h]
    bt3p = np.zeros((128, 1024), np.float32)
    bt3p[0:3] = bt3.reshape(3, 1024)
    c["bt3"] = bt3p.astype(bf)
    mk4 = np.zeros((128, 4), np.float32)
    for r in range(4):
        mk4[r * 32:(r + 1) * 32, r] = 1.0
    c["mk4"] = mk4
    sg = np.ones((128, 2), np.float32)
    sg[:64, 0] = -1.0
    sg[64:, 1] = -1.0
    c["sgn"] = sg
    bm = np.zeros((128, 8), np.float32)
    for p in range(128):
        bm[p, p // 16] = 1.0
    c["bmask"] = bm
    m2 = np.zeros((128, 16, 8), np.float32)
    for g in range(16):
        m2[:, g, g % 8] = 1.0
    c["mask2"] = m2
    c["pow2"] = np.tile((2.0 ** -(np.arange(NIT + 1) + 1.0))[None, :], (128, 1)).astype(np.float32)
    c["invt"] = np.tile((1.0 / (np.arange(16) + 1.0))[None, :], (128, 1)).astype(np.float32)
    return c


def _layout_weights(inp):
    f = lambda a: np.ascontiguousarray(a, dtype=np.float32)
    w = {}
    w["modw"] = f(inp["mod_w"].reshape(NL, 8, 128, 12, 512).transpose(0, 3, 2, 1, 4))
    w["modb"] = f(inp["mod_b"].reshape(NL, 48, 128).transpose(0, 2, 1))
    gv = np.stack([inp["mix_pre_g"], inp["mix_post_g"], inp["ffn_pre_g"], inp["ffn_post_g"]], axis=1)
    w["gvec"] = f(gv.reshape(NL, 4, 8, 128).transpose(0, 1, 3, 2))
    win = inp["w_in"].reshape(NL, 8, 128, 4008)
    w["w1"] = f(win[:, :, :, :936].transpose(0, 2, 1, 3))
    w["wg"] = f(win[:, :, :, 936:].reshape(NL, 8, 128, 24, 128).transpose(0, 3, 2, 1, 4))
    w["gcq"] = f(inp["g_cq"].reshape(NL, 2, 128).transpose(0, 2, 1))
    w["gckv"] = f(inp["g_ckv"])
    w["wuq"] = f(inp["w_uq"].reshape(NL, 2, 128, 1024).transpose(0, 2, 1, 3))
    w["wqi"] = f(inp["w_qi"].reshape(NL, 2, 128, 256).transpose(0, 2, 1, 3))
    w["wuv"] = f(inp["w_uv"].transpose(0, 2, 1, 3))
    dup = lambda a: np.concatenate([a, a], axis=1)
    w["are"] = f(dup(inp["a_re"].transpose(0, 2, 1)))
    w["aim"] = f(dup(inp["a_im"].transpose(0, 2, 1)))
    w["lstep"] = f(np.broadcast_to(inp["log_step"][:, None, :], (NL, 128, 16)))
    w["bre"] = f(inp["b_re"].transpose(0, 2, 1, 3))
    w["bim"] = f(inp["b_im"].transpose(0, 2, 1, 3))
    w["cre"] = f(inp["c_re"].transpose(0, 3, 1, 2))
    w["cim"] = f(inp["c_im"].transpose(0, 3, 1, 2))
    w["dskip"] = f(inp["d_skip"].reshape(NL, 2, 128).transpose(0, 2, 1))
    w["wglu"] = f(inp["w_glu"].reshape(NL, 2, 128, 256).transpose(0, 2, 1, 3))
    w["bglu"] = f(inp["b_glu"].reshape(NL, 2, 128).transpose(0, 2, 1))
    w["wpool"] = f(inp["w_pool"])
    w["pscale"] = f(inp["pool_scale"].reshape(NL, 2, 128).transpose(0, 2, 1))
    w["pa"] = f(inp["p_a"].reshape(NL, 4, 128, 1024).transpose(0, 2, 1, 3))
    w["pb"] = f(inp["p_b"].reshape(NL, 2, 128, 1024).transpose(0, 2, 1, 3))
    w["pc"] = f(inp["p_c"].reshape(NL, 2, 128, 1024).transpose(0, 2, 1, 3))
    w["wout"] = f(inp["w_out"].reshape(NL, 8, 128, 1024).transpose(0, 2, 1, 3))
    w["wup"] = f(inp["w_up"].reshape(NL, 8, 128, 32, 128).transpose(0, 3, 2, 1, 4))
    w["convw"] = f(inp["conv_w"].reshape(NL, 3, 32, 128).transpose(0, 3, 1, 2))
    w["convb"] = f(inp["conv_b"].reshape(NL, 32, 128).transpose(0, 2, 1))
    w["wdown"] = f(inp["w_down"].reshape(NL, 16, 128, 1024).transpose(0, 2, 1, 3))
    return w


def build_nc(shapes, nb=NB, nl=NL, do_mix=True, do_ffn=True, att=True, ssm=True, pool=True):
    nc = bass.Bass("TRN2", target_bir_lowering=False)
    DBG["nc"] = nc
    DBG["outs"] = {}
    dr = {}
    for name, (shape, dt) in shapes.items():
        dr[name] = nc.dram_tensor("d_" + name, list(shape), dt, kind="ExternalInput").ap()
    out = nc.dram_tensor("out", [nb, SEQ, D], F32, kind="ExternalOutput").ap()
    TABW = 2 * 1024 + 2 * 2048 + 64
    tab = nc.dram_tensor("tabscratch", [NL, 128, TABW], F32, kind="Internal").ap()
    tabb = nc.dram_tensor("tabscratchb", [NL, 128, 2048 + 2048], BF16, kind="Internal").ap()

    with ExitStack() as es:
        S = Sched(nc, es)

        def sb(st, name, shape, dt):
            _UNIQ[0] += 1
            actual = "%s__%d" % (name, _UNIQ[0])
            _BASE[actual] = name
            return st.enter_context(nc.sbuf_tensor(actual, shape, dt))

        def ps(st, name, shape, dt):
            S.excl.add(name)
            _UNIQ[0] += 1
            actual = "%s__%d" % (name, _UNIQ[0])
            _BASE[actual] = name
            return st.enter_context(nc.psum_tensor(actual, shape, dt))

        X = sb(es, "X", [128, NT, D], F32)
        hT = sb(es, "hT", [128, 8, SEQ], BF16)
        identf = sb(es, "identf", [128, 128], F32)
        identb = sb(es, "identb", [128, 128], BF16)
        onesb = sb(es, "onesb", [128, 128], BF16)
        onesf = sb(es, "onesf", [128, 128], F32)
        cols = sb(es, "cols", [128, NL, 6, 8, NB], F32)
        GG = sb(es, "GG", [128, D], F32)
        for nm, t in (("identf", identf), ("identb", identb), ("onesb", onesb), ("onesf", onesf)):
            S.dma(t[:], dr[nm], writes=[nm])

        with ExitStack() as ph:
            condT = sb(ph, "condT", [128, 8, NB], F32)
            modT = sb(ph, "modT", [128, 48, NB], F32)
            modb = sb(ph, "modb", [128, 48], F32)
            gvec = sb(ph, "gvec", [128, 4, 8], F32)
            mw = [sb(ph, "mw%d" % i, [128, 8, 512], F32) for i in range(2)]
            pm = ps(ph, "pm", [128, 512], F32)
            S.dma(condT[:], dr["cT"], writes=["condT"])
            S.op("act", lambda e: e.activation(out=condT[:], in_=condT[:], func=AF.Silu),
                 reads=["condT"], writes=["condT"])
            for l in range(nl):
                S.dma(modb[:], dr["modb"][l], writes=["modb"])
                S.dma(gvec[:], dr["gvec"][l].rearrange("w p c -> p w c"), writes=["gvec"])
                for j in range(12):
                    buf = mw[j % 2]
                    S.dma(buf[:], dr["modw"][l, j], writes=["mw%d" % (j % 2)])
                    for f4 in range(4):
                        fc = j * 4 + f4
                        for k in range(8):
                            S.op("pe", lambda e, buf=buf, f4=f4, k=k, fc=fc: e.matmul(
                                pm[:, fc * NB:(fc + 1) * NB], lhsT=buf[:, k, f4 * 128:(f4 + 1) * 128],
                                rhs=condT[:, k, :], start=(k == 0), stop=(k == 7)),
                                reads=["mw%d" % (j % 2), "condT"], writes=["pm"])
                S.op("dve", lambda e: e.tensor_tensor(
                    out=modT[:], in0=pm[:, 0:48 * NB].rearrange("p (f b) -> p f b", b=NB),
                    in1=modb[:].unsqueeze(2).to_broadcast([128, 48, NB]), op=ALU.add),
                    reads=["pm", "modb"], writes=["modT"])
                def gb(w):
                    return gvec[:, w, :].unsqueeze(2).to_broadcast([128, 8, NB])
                for (dst, sc0, gw) in ((0, 8, 0), (3, 32, 2)):
                    S.op("dve", lambda e, dst=dst, sc0=sc0, gw=gw: e.scalar_tensor_tensor(
                        out=cols[:, l, dst], in0=modT[:, sc0:sc0 + 8, :], scalar=1.0, in1=gb(gw),
                        op0=ALU.add, op1=ALU.mult), reads=["modT", "gvec"], writes=["cols"])
                for (dst, sh0) in ((1, 0), (4, 24)):
                    S.op("dve", lambda e, dst=dst, sh0=sh0: e.tensor_copy(
                        out=cols[:, l, dst], in_=modT[:, sh0:sh0 + 8, :]), reads=["modT"], writes=["cols"])
                for (dst, g0, gw) in ((2, 16, 1), (5, 40, 3)):
                    S.op("dve", lambda e, dst=dst, g0=g0, gw=gw: e.tensor_tensor(
                        out=cols[:, l, dst], in0=modT[:, g0:g0 + 8, :], in1=gb(gw), op=ALU.mult),
                        reads=["modT", "gvec"], writes=["cols"])
            dbg(S, "cols", cols[:].rearrange("p l s c b -> p (l s c b)"), [128, NL * 6 * 8 * NB], F32, ["cols"])
            S.end_phase()

        def rstd_from_ss(ss, n, width):
            S.op("dve", lambda e: e.tensor_scalar(out=ss[:, 0:width], in0=ss[:, 0:width], scalar1=1.0 / n,
                                                  scalar2=EPS, op0=ALU.mult, op1=ALU.add),
                 reads=[_nm(ss)], writes=[_nm(ss)])
            S.op("act", lambda e: e.activation(out=ss[:, 0:width], in_=ss[:, 0:width], func=AF.Sqrt),
                 reads=[_nm(ss)], writes=[_nm(ss)])
            S.op("dve", lambda e: e.reciprocal(out=ss[:, 0:width], in_=ss[:, 0:width]),
                 reads=[_nm(ss)], writes=[_nm(ss)])

        def norm_to_hT(ph, l, b, ca, cb):
            ss = sb(ph, "n_ss", [128, NT], F32)
            junk = sb(ph, "n_junk", [128, D], BF16)
            xn = [sb(ph, "n_xn%d" % i, [128, D], BF16) for i in range(2)]
            pt = [ps(ph, "n_pt%d" % i, [128, 1024], BF16) for i in range(2)]
            for t in range(NT):
                S.op("act", lambda e, t=t: e.activation(out=junk[:], in_=X[:, t, :], func=AF.Square,
                                                        accum_out=ss[:, t:t + 1]),
                     reads=[("X", t)], writes=["n_junk", "n_ss"])
            rstd_from_ss(ss, float(D), NT)
            for t in range(NT):
                xb = xn[t % 2]
                pp = pt[t % 2]
                S.op("dve", lambda e, t=t, xb=xb: e.tensor_scalar(out=xb[:], in0=X[:, t, :],
                                                               scalar1=ss[:, t:t + 1], scalar2=None, op0=ALU.mult),
                     reads=[("X", t), "n_ss"], writes=[_nm(xb)])
                for c in range(8):
                    S.op("pe", lambda e, c=c, xb=xb, pp=pp: e.transpose(
                        out=pp[:, c * 128:(c + 1) * 128], in_=xb[:, c * 128:(c + 1) * 128], identity=identb[:]),
                        reads=[_nm(xb), "identb"], writes=[_nm(pp)])
                for c in range(8):
                    eng = "act" if c % 2 else "dve"
                    if eng == "dve":
                        S.op("dve", lambda e, c=c, t=t, pp=pp: e.tensor_scalar(
                            out=hT[:, c, t * 128:(t + 1) * 128], in0=pp[:, c * 128:(c + 1) * 128],
                            scalar1=cols[:, l, ca, c, b:b + 1], scalar2=cols[:, l, cb, c, b:b + 1],
                            op0=ALU.mult, op1=ALU.add), reads=[_nm(pp), "cols"], writes=[("hT", t)])
                    else:
                        S.op("act", lambda e, c=c, t=t, pp=pp: e.activation(
                            out=hT[:, c, t * 128:(t + 1) * 128], in_=pp[:, c * 128:(c + 1) * 128],
                            func=AF.Identity, scale=cols[:, l, ca, c, b:b + 1], bias=cols[:, l, cb, c, b:b + 1]),
                            reads=[_nm(pp), "cols"], writes=[("hT", t)])

        def make_GG(ph, l, b, cg):
            dg = sb(ph, "gg_diag", [128, 128], F32)
            pg = [ps(ph, "gg_ps%d" % i, [128, 512], F32) for i in range(2)]
            for c in range(8):
                S.op("dve", lambda e, c=c: e.tensor_scalar(out=dg[:], in0=identf[:], scalar1=cols[:, l, cg, c, b:b + 1],
                                                        scalar2=None, op0=ALU.mult),
                     reads=["identf", "cols"], writes=["gg_diag"])
                S.op("pe", lambda e, c=c: e.matmul(pg[c // 4][:, (c % 4) * 128:(c % 4 + 1) * 128], lhsT=onesf[:],
                                                   rhs=dg[:], start=True, stop=True),
                     reads=["gg_diag", "onesf"], writes=[_nm(pg[c // 4])])
            for i in range(2):
                S.op("act", lambda e, i=i: e.copy(out=GG[:, i * 512:(i + 1) * 512], in_=pg[i][:]),
                     reads=[_nm(pg[i])], writes=["GG"])

        def resid_update(ph_tiles, t, py):
            ssy, junk, tmp = ph_tiles
            for i in range(2):
                S.op("act", lambda e, i=i: e.activation(out=junk[:, 0:512], in_=py[i][:], func=AF.Square,
                                                        accum_out=ssy[:, i:i + 1]),
                     reads=[_nm(py[i])], writes=[_nm(junk), _nm(ssy)])
            S.op("dve", lambda e: e.tensor_tensor(out=ssy[:, 2:3], in0=ssy[:, 0:1], in1=ssy[:, 1:2], op=ALU.add),
                 reads=[_nm(ssy)], writes=[_nm(ssy)])
            S.op("dve", lambda e: e.tensor_scalar(out=ssy[:, 2:3], in0=ssy[:, 2:3], scalar1=1.0 / D, scalar2=EPS,
                                                  op0=ALU.mult, op1=ALU.add), reads=[_nm(ssy)], writes=[_nm(ssy)])
            S.op("act", lambda e: e.activation(out=ssy[:, 2:3], in_=ssy[:, 2:3], func=AF.Sqrt),
                 reads=[_nm(ssy)], writes=[_nm(ssy)])
            S.op("dve", lambda e: e.reciprocal(out=ssy[:, 2:3], in_=ssy[:, 2:3]), reads=[_nm(ssy)], writes=[_nm(ssy)])
            for i in range(2):
                S.op("dve", lambda e, i=i: e.scalar_tensor_tensor(
                    out=tmp[:, i * 512:(i + 1) * 512], in0=py[i][:], scalar=ssy[:, 2:3],
                    in1=GG[:, i * 512:(i + 1) * 512], op0=ALU.mult, op1=ALU.mult),
                    reads=[_nm(py[i]), _nm(ssy), "GG"], writes=[_nm(tmp)])
            if t == 8:
                dbg(S, "r_ssy", ssy[:], [128, 4], F32, [_nm(ssy)])
                dbg(S, "r_tmp", tmp[:], [128, D], F32, [_nm(tmp)])
            S.op("dve", lambda e: e.tensor_tensor(out=X[:, t, :], in0=X[:, t, :], in1=tmp[:], op=ALU.add),
                 reads=[_nm(tmp), ("X", t)], writes=[("X", t)])

        if do_mix and ssm:
            for l in range(nl):
                ssm_tables(nc, S, dr, l, tab, tabb, sb, ps, identf)

        for b in range(nb):
            with ExitStack() as ph:
                for t in range(NT):
                    S.dma(X[:, t, :], dr["x"][b, t * 128:(t + 1) * 128, :], writes=[("X", t)])
                S.end_phase()
            for l in range(nl):
                if do_mix:
                    mixer(nc, S, dr, l, b, X, hT, GG, cols, tab, tabb, sb, ps, norm_to_hT, make_GG, resid_update,
                          rstd_from_ss, identb, identf, onesb, att, ssm, pool)
                if do_ffn:
                    ffn(nc, S, dr, l, b, X, hT, GG, sb, ps, norm_to_hT, make_GG, resid_update)
            with ExitStack() as ph:
                for t in range(NT):
                    S.dma(out[b, t * 128:(t + 1) * 128, :], X[:, t, :], reads=[("X", t)])
                S.end_phase()
        print("instructions:", S.ninst)
    return nc


DBG = {"on": False, "nc": None, "outs": {}}


def dbg(S, name, ap, shape, dt, reads, st=None):
    if not DBG["on"] or name in DBG["outs"]:
        return
    nc = DBG["nc"]
    d = nc.dram_tensor("dbg_" + name, list(shape), F32, kind="ExternalOutput").ap()
    DBG["outs"][name] = d
    if dt != F32:
        tmpf = st.enter_context(nc.sbuf_tensor("dbgt_" + name, list(shape), F32))
        S.op("dve", lambda e: e.tensor_copy(out=tmpf[:], in_=ap), reads=reads, writes=["dbgt_" + name])
        S.dma(d, tmpf[:], reads=["dbgt_" + name])
    else:
        S.dma(d, ap, reads=reads)


def cast_load(S, dst, src, wname):
    S.dma(dst, src, writes=[wname], q="pool")


def ffn(nc, S, dr, l, b, X, hT, GG, sb, ps, norm_to_hT, make_GG, resid_update):
    with ExitStack() as ph:
        norm_to_hT(ph, l, b, 3, 4)
        make_GG(ph, l, b, 5)
        dbg(S, "f_hT", hT[:].rearrange("p c t -> p (c t)"), [128, 8 * SEQ], BF16, [("hT", t) for t in range(NT)], ph)
        dbg(S, "f_GG", GG[:], [128, D], F32, ["GG"])
        S.end_phase()
    with ExitStack() as ph:
        wd = sb(ph, "f_wd", [128, 16, D], BF16)
        m = sb(ph, "f_m", [128, 16, 1024], BF16)
        cw = sb(ph, "f_cw", [128, 3, 32], F32)
        cbt = sb(ph, "f_cb", [128, 32], F32)
        halo = sb(ph, "f_halo", [128, 32, 2], F32)
        wu = [sb(ph, "f_wu%d" % i, [128, 8, 128], BF16) for i in range(3)]
        ub = [sb(ph, "f_ub%d" % i, [128, 1026], F32) for i in range(2)]
        acc = [sb(ph, "f_acc%d" % i, [128, 1024], F32) for i in range(2)]
        gl = sb(ph, "f_gl", [128, 1024], F32)
        ssy = sb(ph, "f_ssy", [128, 4], F32)
        junk = sb(ph, "f_junk", [128, 512], BF16)
        tmp = sb(ph, "f_tmp", [128, D], F32)
        pu = [ps(ph, "f_pu%d" % i, [128, 512], F32) for i in range(4)]
        py = [ps(ph, "f_py%d" % i, [128, 512], F32) for i in range(4)]
        S.dma(cw[:], dr["convw"][l], writes=["f_cw"])
        S.dma(cbt[:], dr["convb"][l], writes=["f_cb"])
        for k in range(16):
            cast_load(S, wd[:, k, :], dr["wdown"][l][:, k, :], ("f_wd", k))
        nload = 0
        for half in range(2):
            t0 = half * 1024
            for cc in range(16):
                for gv in range(2):
                    j = gv * 16 + cc
                    wb = wu[nload % 3]
                    nload += 1
                    cast_load(S, wb[:], dr["wup"][l, j], _nm(wb))
                    pp = pu[(gv * 2):(gv * 2 + 2)]
                    for s2 in range(2):
                        for k in range(8):
                            S.op("pe", lambda e, wb=wb, k=k, s2=s2, pp=pp: e.matmul(
                                pp[s2][:], lhsT=wb[:, k, :], rhs=hT[:, k, t0 + s2 * 512:t0 + (s2 + 1) * 512],
                                start=(k == 0), stop=(k == 7)),
                                reads=[_nm(wb)] + [("hT", (t0 + s2 * 512) // 128 + q) for q in range(4)],
                                writes=[_nm(pp[s2])])
                    u = ub[gv]
                    a = acc[gv]
                    dbg(S, "f_wb", wb[:].rearrange("p k n -> p (k n)"), [128, 1024], BF16, [_nm(wb)], ph)
                    if half == 0:
                        S.op("dve", lambda e, u=u: e.memset(u[:, 0:2], 0.0), writes=[_nm(u)])
                    else:
                        S.op("dve", lambda e, u=u, j=j: e.tensor_copy(out=u[:, 0:2], in_=halo[:, j, :]),
                             reads=["f_halo"], writes=[_nm(u)])
                    for s2 in range(2):
                        S.op("act", lambda e, u=u, s2=s2, pp=pp: e.copy(out=u[:, 2 + s2 * 512:2 + (s2 + 1) * 512],
                                                                        in_=pp[s2][:]),
                             reads=[_nm(pp[s2])], writes=[_nm(u)])
                    if half == 0:
                        S.op("dve", lambda e, u=u, j=j: e.tensor_copy(out=halo[:, j, :], in_=u[:, 1024:1026]),
                             reads=[_nm(u)], writes=["f_halo"])
                    dbg(S, "f_u", u[:], [128, 1026], F32, [_nm(u)], ph)
                    S.op("dve", lambda e, u=u, a=a, j=j: e.tensor_scalar(
                        out=a[:], in0=u[:, 2:1026], scalar1=cw[:, 2, j:j + 1], scalar2=cbt[:, j:j + 1],
                        op0=ALU.mult, op1=ALU.add), reads=[_nm(u), "f_cw", "f_cb"], writes=[_nm(a)])
                    S.op("dve", lambda e, u=u, a=a, j=j: e.scalar_tensor_tensor(
                        out=a[:], in0=u[:, 1:1025], scalar=cw[:, 1, j:j + 1], in1=a[:], op0=ALU.mult, op1=ALU.add),
                        reads=[_nm(u), "f_cw"], writes=[_nm(a)])
                    S.op("dve", lambda e, u=u, a=a, j=j: e.scalar_tensor_tensor(
                        out=a[:], in0=u[:, 0:1024], scalar=cw[:, 0, j:j + 1], in1=a[:], op0=ALU.mult, op1=ALU.add),
                        reads=[_nm(u), "f_cw"], writes=[_nm(a)])
                dbg(S, "f_acc0", acc[0][:], [128, 1024], F32, [_nm(acc[0])], ph)
                dbg(S, "f_acc1", acc[1][:], [128, 1024], F32, [_nm(acc[1])], ph)
                S.op("act", lambda e: e.activation(out=gl[:], in_=acc[0][:], func=AF.Gelu_apprx_tanh),
                     reads=[_nm(acc[0])], writes=["f_gl"])
                S.op("dve", lambda e, cc=cc: e.tensor_tensor(out=m[:, cc, :], in0=gl[:], in1=acc[1][:], op=ALU.mult),
                     reads=["f_gl", _nm(acc[1])], writes=[("f_m", cc)])
            for tt in range(8):
                t = half * 8 + tt
                pyy = py[(tt % 2) * 2:(tt % 2) * 2 + 2]
                for i in range(2):
                    for k in range(16):
                        S.op("pe", lambda e, i=i, k=k, tt=tt, pyy=pyy: e.matmul(
                            pyy[i][:], lhsT=m[:, k, tt * 128:(tt + 1) * 128], rhs=wd[:, k, i * 512:(i + 1) * 512],
                            start=(k == 0), stop=(k == 15)),
                            reads=[("f_m", k), ("f_wd", k)], writes=[_nm(pyy[i])])
                resid_update((ssy, junk, tmp), t, pyy)
        S.end_phase()


def ssm_tables(nc, S, dr, l, tab, tabb, sb, ps, identf):
    with ExitStack() as ph:
        are = sb(ph, "t_are", [128, 16], F32)
        aim = sb(ph, "t_aim", [128, 16], F32)
        stp = sb(ph, "t_stp", [128, 16], F32)
        th = sb(ph, "t_th", [128, 16], F32)
        r = sb(ph, "t_r", [128, 16], F32)
        sn = sb(ph, "t_sn", [128, 16], F32)
        cs = sb(ph, "t_cs", [128, 16], F32)
        t1 = sb(ph, "t_t1", [128, 16], F32)
        t2 = sb(ph, "t_t2", [128, 16], F32)
        lre = sb(ph, "t_lre", [128, 16], F32)
        lim = sb(ph, "t_lim", [128, 16], F32)
        ire = sb(ph, "t_ire", [128, 16], F32)
        iim = sb(ph, "t_iim", [128, 16], F32)
        pr = sb(ph, "t_pr", [128, 16], F32)
        pi = sb(ph, "t_pi", [128, 16], F32)
        cfr = sb(ph, "t_cfr", [128, 16], F32)
        cfi = sb(ph, "t_cfi", [128, 16], F32)
        Ere = sb(ph, "t_Ere", [128, 16, 256], F32)
        Eim = sb(ph, "t_Eim", [128, 16, 256], F32)
        ta = sb(ph, "t_ta", [128, 16, 128], F32)
        tb = sb(ph, "t_tb", [128, 16, 128], F32)
        sgn = sb(ph, "t_sgn", [128, 2], F32)
        outc = sb(ph, "t_outc", [128, 64], F32)
        halfpi = sb(ph, "t_hpi", [128, 1], F32)
        R = ["t"]

        def dv(fn, w=("t",)):
            S.op("dve", fn, reads=R, writes=list(w))

        S.dma(are[:], dr["are"][l], writes=R)
        S.dma(aim[:], dr["aim"][l], writes=R)
        S.dma(stp[:], dr["lstep"][l], writes=R)
        S.dma(sgn[:], dr["sgn"], writes=R)
        S.op("act", lambda e: e.activation(out=stp[:], in_=stp[:], func=AF.Exp), reads=R, writes=R)
        dv(lambda e: e.tensor_tensor(out=th[:], in0=aim[:], in1=stp[:], op=ALU.mult))
        dv(lambda e: e.tensor_tensor(out=t1[:], in0=are[:], in1=stp[:], op=ALU.mult))
        S.op("act", lambda e: e.activation(out=r[:], in_=t1[:], func=AF.Exp), reads=R, writes=R)
        dv(lambda e: e.memset(halfpi[:], float(np.pi / 2)))
        S.op("act", lambda e: e.activation(out=sn[:], in_=th[:], func=AF.Sin, scale=1.0 / 16), reads=R, writes=R)
        S.op("act", lambda e: e.activation(out=cs[:], in_=th[:], func=AF.Sin, scale=1.0 / 16, bias=halfpi[:]),
             reads=R, writes=R)
        for _ in range(4):
            dv(lambda e: e.tensor_tensor(out=t1[:], in0=sn[:], in1=cs[:], op=ALU.mult))
            dv(lambda e: e.tensor_tensor(out=t2[:], in0=sn[:], in1=sn[:], op=ALU.mult))
            dv(lambda e: e.tensor_scalar(out=sn[:], in0=t1[:], scalar1=2.0, scalar2=None, op0=ALU.mult))
            dv(lambda e: e.tensor_scalar(out=cs[:], in0=t2[:], scalar1=-2.0, scalar2=1.0, op0=ALU.mult, op1=ALU.add))
        dv(lambda e: e.tensor_tensor(out=lre[:], in0=r[:], in1=cs[:], op=ALU.mult))
        dv(lambda e: e.tensor_tensor(out=lim[:], in0=r[:], in1=sn[:], op=ALU.mult))
        dv(lambda e: e.reciprocal(out=t1[:], in_=r[:]))
        dv(lambda e: e.tensor_tensor(out=ire[:], in0=t1[:], in1=cs[:], op=ALU.mult))
        dv(lambda e: e.tensor_tensor(out=iim[:], in0=t1[:], in1=sn[:], op=ALU.mult))
        dv(lambda e: e.tensor_scalar(out=iim[:], in0=iim[:], scalar1=-1.0, scalar2=None, op0=ALU.mult))
        dv(lambda e: e.tensor_tensor(out=t1[:], in0=are[:], in1=are[:], op=ALU.mult))
        dv(lambda e: e.tensor_tensor(out=t2[:], in0=aim[:], in1=aim[:], op=ALU.mult))
        dv(lambda e: e.tensor_tensor(out=t1[:], in0=t1[:], in1=t2[:], op=ALU.add))
        dv(lambda e: e.reciprocal(out=t1[:], in_=t1[:]))
        dv(lambda e: e.tensor_scalar(out=t2[:], in0=lre[:], scalar1=-1.0, scalar2=None, op0=ALU.add))
        dv(lambda e: e.tensor_tensor(out=cfr[:], in0=t2[:], in1=are[:], op=ALU.mult))
        dv(lambda e: e.tensor_tensor(out=pr[:], in0=lim[:], in1=aim[:], op=ALU.mult))
        dv(lambda e: e.tensor_tensor(out=cfr[:], in0=cfr[:], in1=pr[:], op=ALU.add))
        dv(lambda e: e.tensor_tensor(out=cfr[:], in0=cfr[:], in1=t1[:], op=ALU.mult))
        dv(lambda e: e.tensor_tensor(out=cfi[:], in0=lim[:], in1=are[:], op=ALU.mult))
        dv(lambda e: e.tensor_tensor(out=pr[:], in0=t2[:], in1=aim[:], op=ALU.mult))
        dv(lambda e: e.tensor_tensor(out=cfi[:], in0=cfi[:], in1=pr[:], op=ALU.subtract))
        dv(lambda e: e.tensor_tensor(out=cfi[:], in0=cfi[:], in1=t1[:], op=ALU.mult))

        def power_table(bre_, bim_, nlev):
            dv(lambda e: e.memset(Ere[:, :, 0:1], 1.0))
            dv(lambda e: e.memset(Eim[:, :, 0:1], 0.0))
            dv(lambda e: e.tensor_copy(out=pr[:], in_=bre_[:]))
            dv(lambda e: e.tensor_copy(out=pi[:], in_=bim_[:]))
            for k in range(nlev):
                n = 1 << k
                prb = pr[:].unsqueeze(2).to_broadcast([128, 16, n])
                pib = pi[:].unsqueeze(2).to_broadcast([128, 16, n])
                dv(lambda e, n=n, prb=prb: e.tensor_tensor(out=ta[:, :, 0:n], in0=Ere[:, :, 0:n], in1=prb, op=ALU.mult))
                dv(lambda e, n=n, pib=pib: e.tensor_tensor(out=tb[:, :, 0:n], in0=Eim[:, :, 0:n], in1=pib, op=ALU.mult))
                dv(lambda e, n=n: e.tensor_tensor(out=Ere[:, :, n:2 * n], in0=ta[:, :, 0:n], in1=tb[:, :, 0:n],
                                                  op=ALU.subtract))
                dv(lambda e, n=n, pib=pib: e.tensor_tensor(out=ta[:, :, 0:n], in0=Ere[:, :, 0:n], in1=pib, op=ALU.mult))
                dv(lambda e, n=n, prb=prb: e.tensor_tensor(out=tb[:, :, 0:n], in0=Eim[:, :, 0:n], in1=prb, op=ALU.mult))
                dv(lambda e, n=n: e.tensor_tensor(out=Eim[:, :, n:2 * n], in0=ta[:, :, 0:n], in1=tb[:, :, 0:n],
                                                  op=ALU.add))
                if k < nlev - 1:
                    dv(lambda e: e.tensor_tensor(out=t1[:], in0=pr[:], in1=pr[:], op=ALU.mult))
                    dv(lambda e: e.tensor_tensor(out=t2[:], in0=pi[:], in1=pi[:], op=ALU.mult))
                    dv(lambda e: e.tensor_tensor(out=pi[:], in0=pr[:], in1=pi[:], op=ALU.mult))
                    dv(lambda e: e.tensor_scalar(out=pi[:], in0=pi[:], scalar1=2.0, scalar2=None, op0=ALU.mult))
                    dv(lambda e: e.tensor_tensor(out=pr[:], in0=t1[:], in1=t2[:], op=ALU.subtract))

        power_table(lre, lim, 8)
        o_t1 = 2048
        o_t2 = 2048 + 2048
        o_c = 2048 + 4096
        S.dma(tab[l][:, o_t1:o_t1 + 2048].rearrange("p (g t) -> p g t", t=128), Ere[:, :, 0:128], reads=R)
        dv(lambda e: e.tensor_scalar(out=ta[:], in0=Eim[:, :, 0:128], scalar1=sgn[:, 0:1], scalar2=None, op0=ALU.mult))
        S.dma(tab[l][:, o_t2:o_t2 + 2048].rearrange("p (g t) -> p g t", t=128), ta[:], reads=R)
        dv(lambda e: e.tensor_copy(out=outc[:, 0:16], in_=Ere[:, :, 128]))
        dv(lambda e: e.tensor_scalar(out=outc[:, 16:32], in0=Eim[:, :, 128], scalar1=sgn[:, 0:1], scalar2=None,
                                     op0=ALU.mult))
        dv(lambda e: e.tensor_scalar(out=outc[:, 32:48], in0=Eim[:, :, 128], scalar1=sgn[:, 1:2], scalar2=None,
                                     op0=ALU.mult))
        dv(lambda e: e.memset(outc[:, 48:64], 0.0))
        S.dma(tab[l][:, o_c:o_c + 64], outc[:], reads=R)
        S.end_phase()
        power_table(ire, iim, 7)
        ptr = [ps(ph, "t_ptr%d" % i, [128, 512], F32) for i in range(4)]
        arai = sb(ph, "t_arai", [128, 2, 16, 64], F32)
        for ri, E in ((0, Ere), (1, Eim)):
            for g in range(16):
                S.op("pe", lambda e, g=g, E=E, ri=ri: e.transpose(
                    out=ptr[ri * 2 + g // 8][:, (g % 8) * 64:(g % 8 + 1) * 64], in_=E[0:64, g, 0:128],
                    identity=identf[0:64, 0:64]), reads=R + ["identf"], writes=["t_ptr"])
            for hh in range(2):
                S.op("act", lambda e, hh=hh, ri=ri: e.copy(
                    out=arai[:, ri, hh * 8:(hh + 1) * 8, :].rearrange("p g n -> p (g n)"), in_=ptr[ri * 2 + hh][:]),
                    reads=["t_ptr"], writes=["t_arai"])
        S.dma(tab[l][:, 0:2048], arai[:].rearrange("p r g n -> p (r g n)"), reads=["t_arai"])
        bre = sb(ph, "t_bre", [64, 16, 16], F32)
        bim = sb(ph, "t_bim", [64, 16, 16], F32)
        Bre = sb(ph, "t_Bre", [64, 16, 16], F32)
        Bim = sb(ph, "t_Bim", [64, 16, 16], F32)
        tq = sb(ph, "t_tq", [64, 16, 16], F32)
        bmask = sb(ph, "t_bmask", [128, 8], F32)
        BT = sb(ph, "t_BT", [128, 2, 128], F32)
        Bblk = sb(ph, "t_Bblk", [128, 2, 8, 128], BF16)
        S.dma(bre[:], dr["bre"][l], writes=R)
        S.dma(bim[:], dr["bim"][l], writes=R)
        S.dma(bmask[:], dr["bmask"], writes=R)
        cfrb = cfr[0:64, :].unsqueeze(2).to_broadcast([64, 16, 16])
        cfib = cfi[0:64, :].unsqueeze(2).to_broadcast([64, 16, 16])
        dv(lambda e: e.tensor_tensor(out=Bre[:], in0=bre[:], in1=cfrb, op=ALU.mult))
        dv(lambda e: e.tensor_tensor(out=tq[:], in0=bim[:], in1=cfib, op=ALU.mult))
        dv(lambda e: e.tensor_tensor(out=Bre[:], in0=Bre[:], in1=tq[:], op=ALU.subtract))
        dv(lambda e: e.tensor_tensor(out=Bim[:], in0=bim[:], in1=cfrb, op=ALU.mult))
        dv(lambda e: e.tensor_tensor(out=tq[:], in0=bre[:], in1=cfib, op=ALU.mult))
        dv(lambda e: e.tensor_tensor(out=Bim[:], in0=Bim[:], in1=tq[:], op=ALU.add))
        for half in range(2):
            for ri, Bsrc in ((0, Bre), (1, Bim)):
                S.op("pe", lambda e, half=half, ri=ri, Bsrc=Bsrc: e.transpose(
                    out=ptr[0][:, (half * 2 + ri) * 64:(half * 2 + ri + 1) * 64],
                    in_=Bsrc[:, half * 8:(half + 1) * 8, :].rearrange("n g p -> n (g p)"),
                    identity=identf[0:64, 0:64]), reads=R + ["identf", "t_arai"], writes=["t_ptr0b"])
        S.op("act", lambda e: e.copy(out=BT[:].rearrange("p h c -> p (h c)"), in_=ptr[0][:, 0:256]),
             reads=["t_ptr0b"], writes=R)
        for half in range(2):
            S.op("dve", lambda e, half=half: e.tensor_tensor(
                out=Bblk[:, half], in0=BT[:, half, :].unsqueeze(1).to_broadcast([128, 8, 128]),
                in1=bmask[:].unsqueeze(2).to_broadcast([128, 8, 128]), op=ALU.mult), reads=R, writes=R)
        S.dma(tabb[l][:, 0:2048], Bblk[:].rearrange("p h g c -> p (h g c)"), reads=R)
        Cm = sb(ph, "t_Cm", [128, 16, 16], F32)
        mask2 = sb(ph, "t_mask2", [128, 16, 8], F32)
        Cpad = sb(ph, "t_Cpad", [128, 16, 8, 16], BF16)
        S.dma(Cm[0:64], dr["cre"][l], writes=R)
        S.dma(Cm[64:128], dr["cim"][l], writes=R)
        S.dma(mask2[:], dr["mask2"], writes=R)
        dv(lambda e: e.tensor_scalar(out=Cm[64:128], in0=Cm[64:128], scalar1=-1.0, scalar2=None, op0=ALU.mult))
        dv(lambda e: e.tensor_tensor(out=Cpad[:], in0=Cm[:].unsqueeze(2).to_broadcast([128, 16, 8, 16]),
                                     in1=mask2[:].unsqueeze(3).to_broadcast([128, 16, 8, 16]), op=ALU.mult))
        S.dma(tabb[l][:, 2048:4096], Cpad[:].rearrange("p g a b -> p (g a b)"), reads=R)
        S.end_phase()


def mixer(nc, S, dr, l, b, X, hT, GG, cols, tab, tabb, sb, ps, norm_to_hT, make_GG, resid_update, rstd_from_ss,
          identb, identf, onesb, att, ssm, pool):
    with ExitStack() as ph:
        norm_to_hT(ph, l, b, 0, 1)
        make_GG(ph, l, b, 2)
        S.end_phase()
    with ExitStack() as mx:
        yaT = sb(mx, "yaT", [128, 4, SEQ], BF16)
        ysT = sb(mx, "ysT", [128, 2, SEQ], BF16)
        ypT = sb(mx, "ypT", [128, 2, SEQ], BF16)
        if att:
            attention(nc, S, dr, l, b, hT, yaT, sb, ps, rstd_from_ss, identb, onesb)
        else:
            S.op("dve", lambda e: e.memset(yaT[:], 0.0), writes=["yaT"])
            S.end_phase()
        if ssm:
            ssm_branch(nc, S, dr, l, b, hT, ysT, tab, tabb, sb, ps)
        else:
            S.op("dve", lambda e: e.memset(ysT[:], 0.0), writes=["ysT"])
            S.end_phase()
        if pool:
            pool_branch(nc, S, dr, l, b, hT, ypT, sb, ps)
        else:
            S.op("dve", lambda e: e.memset(ypT[:], 0.0), writes=["ypT"])
            S.end_phase()
        merge(nc, S, dr, l, b, X, hT, GG, yaT, ysT, ypT, sb, ps, resid_update)


def merge(nc, S, dr, l, b, X, hT, GG, yaT, ysT, ypT, sb, ps, resid_update):
    with ExitStack() as ph:
        wg = [sb(ph, "m_wg%d" % i, [128, 8, 128], BF16) for i in range(4)]
        pa = sb(ph, "m_pa", [128, 4, D], BF16)
        pb = sb(ph, "m_pb", [128, 2, D], BF16)
        pc = sb(ph, "m_pc", [128, 2, D], BF16)
        wo = sb(ph, "m_wo", [128, 8, D], BF16)
        mT = sb(ph, "m_mT", [128, 8, 1024], BF16)
        sg = [sb(ph, "m_sg%d" % i, [128, 512], F32) for i in range(2)]
        tm = sb(ph, "m_tm", [128, 512], F32)
        ac = sb(ph, "m_ac", [128, 512], F32)
        ssy = sb(ph, "m_ssy", [128, 4], F32)
        junk = sb(ph, "m_junk", [128, 512], BF16)
        tmp = sb(ph, "m_tmp", [128, D], F32)
        pg = [ps(ph, "m_pg%d" % i, [128, 512], F32) for i in range(2)]
        pp = [ps(ph, "m_pp%d" % i, [128, 512], F32) for i in range(2)]
        py = [ps(ph, "m_py%d" % i, [128, 512], F32) for i in range(4)]
        for k in range(4):
            cast_load(S, pa[:, k, :], dr["pa"][l][:, k, :], "m_pa")
        cast_load(S, pb[:], dr["pb"][l], "m_pb")
        cast_load(S, pc[:], dr["pc"][l], "m_pc")
        for k in range(8):
            cast_load(S, wo[:, k, :], dr["wout"][l][:, k, :], "m_wo")
        brs = ((yaT, pa, 4, "yaT", "m_pa"), (ysT, pb, 2, "ysT", "m_pb"), (ypT, pc, 2, "ypT", "m_pc"))
        nl_ = 0
        ng = 0
        for half in range(2):
            for dc in range(8):
                wgs = []
                for br in range(3):
                    wb = wg[nl_ % 4]
                    nl_ += 1
                    cast_load(S, wb[:], dr["wg"][l, br * 8 + dc], _nm(wb))
                    wgs.append(wb)
                for s2 in range(2):
                    t0 = half * 1024 + s2 * 512
                    hreads = [("hT", t0 // 128 + q) for q in range(4)]
                    for br in range(3):
                        yT, pw, nk, yn, pn = brs[br]
                        g_ = pg[ng % 2]
                        p_ = pp[ng % 2]
                        s_ = sg[ng % 2]
                        ng += 1
                        for k in range(8):
                            S.op("pe", lambda e, k=k, g_=g_, wb=wgs[br], t0=t0: e.matmul(
                                g_[:], lhsT=wb[:, k, :], rhs=hT[:, k, t0:t0 + 512], start=(k == 0), stop=(k == 7)),
                                reads=[_nm(wgs[br])] + hreads, writes=[_nm(g_)])
                        for k in range(nk):
                            S.op("pe", lambda e, k=k, p_=p_, pw=pw, yT=yT, t0=t0, nk=nk, dc=dc: e.matmul(
                                p_[:], lhsT=pw[:, k, dc * 128:(dc + 1) * 128], rhs=yT[:, k, t0:t0 + 512],
                                start=(k == 0), stop=(k == nk - 1)), reads=[pn, yn], writes=[_nm(p_)])
                        S.op("act", lambda e, g_=g_, s_=s_: e.activation(out=s_[:], in_=g_[:], func=AF.Sigmoid),
                             reads=[_nm(g_)], writes=[_nm(s_)])
                        if br == 0:
                            S.op("dve", lambda e, p_=p_, s_=s_: e.tensor_tensor(out=ac[:], in0=p_[:], in1=s_[:], op=ALU.mult),
                                 reads=[_nm(p_), _nm(s_)], writes=["m_ac"])
                        elif br == 1:
                            S.op("dve", lambda e, p_=p_, s_=s_: e.tensor_tensor(out=tm[:], in0=p_[:], in1=s_[:], op=ALU.mult),
                                 reads=[_nm(p_), _nm(s_)], writes=["m_tm"])
                            S.op("dve", lambda e: e.tensor_tensor(out=ac[:], in0=ac[:], in1=tm[:], op=ALU.add),
                                 reads=["m_tm"], writes=["m_ac"])
                        else:
                            S.op("dve", lambda e, p_=p_, s_=s_: e.tensor_tensor(out=tm[:], in0=p_[:], in1=s_[:], op=ALU.mult),
                                 reads=[_nm(p_), _nm(s_)], writes=["m_tm"])
                            S.op("dve", lambda e, dc=dc, s2=s2: e.tensor_tensor(
                                out=mT[:, dc, s2 * 512:(s2 + 1) * 512], in0=ac[:], in1=tm[:], op=ALU.add),
                                reads=["m_tm", "m_ac"], writes=[("m_mT", dc)])
            for tt in range(8):
                t = half * 8 + tt
                pyy = py[(tt % 2) * 2:(tt % 2) * 2 + 2]
                for i in range(2):
                    for k in range(8):
                        S.op("pe", lambda e, i=i, k=k, tt=tt, pyy=pyy: e.matmul(
                            pyy[i][:], lhsT=mT[:, k, tt * 128:(tt + 1) * 128], rhs=wo[:, k, i * 512:(i + 1) * 512],
                            start=(k == 0), stop=(k == 7)), reads=[("m_mT", k), "m_wo"], writes=[_nm(pyy[i])])
                resid_update((ssy, junk, tmp), t, pyy)
        S.end_phase()


def pool_branch(nc, S, dr, l, b, hT, ypT, sb, ps):
    with ExitStack() as ph:
        w1 = sb(ph, "p_w1", [128, 8, 256], BF16)
        wp = sb(ph, "p_wp", [128, 2, 128], F32)
        wpb = sb(ph, "p_wpb", [128, 2, 128], BF16)
        psc = sb(ph, "p_psc", [128, 2], F32)
        invt = sb(ph, "p_invt", [128, 16], F32)
        U = sb(ph, "p_U", [128, 16 + SEQ], F32)
        s_a = sb(ph, "p_sa", [128, 16 + SEQ], F32)
        s_b = sb(ph, "p_sb", [128, 16 + SEQ], F32)
        pl = sb(ph, "p_pl", [128, SEQ], BF16)
        fx = sb(ph, "p_fx", [128, 16], F32)
        pu = [ps(ph, "p_pu%d" % i, [128, 512], F32) for i in range(4)]
        cast_load(S, w1[:], dr["w1"][l][:, :, 680:936], "p_w1")
        S.dma(psc[:], dr["pscale"][l], writes=["p_psc"])
        S.dma(invt[:], dr["invt"], writes=["p_invt"])
        S.op("dve", lambda e: e.memset(wp[:], 0.0), writes=["p_wp"])
        for g in range(4):
            S.dma(wp[(g % 2) * 64:(g % 2 + 1) * 64, g // 2, (g % 2) * 64:(g % 2 + 1) * 64], dr["wpool"][l, g],
                  reads=[], writes=["p_wp"])
        S.op("dve", lambda e: e.tensor_copy(out=wpb[:], in_=wp[:]), reads=["p_wp"], writes=["p_wpb"])
        for buf in (U, s_a, s_b):
            S.op("dve", lambda e, buf=buf: e.memset(buf[:, 0:16], 0.0), writes=[_nm(buf)])
        for ct in range(2):
            for tc in range(4):
                for k in range(8):
                    S.op("pe", lambda e, k=k, tc=tc, ct=ct: e.matmul(
                        pu[tc][:], lhsT=w1[:, k, ct * 128:(ct + 1) * 128], rhs=hT[:, k, tc * 512:(tc + 1) * 512],
                        start=(k == 0), stop=(k == 7)), reads=["p_w1"] + [("hT", tc * 4 + q) for q in range(4)],
                        writes=[_nm(pu[tc])])
                S.op("act", lambda e, tc=tc: e.copy(out=U[:, 16 + tc * 512:16 + (tc + 1) * 512], in_=pu[tc][:]),
                     reads=[_nm(pu[tc])], writes=["p_U"])
            def shadd(dst, src, sh):
                S.op("dve", lambda e: e.tensor_tensor(out=dst[:, 16:16 + SEQ], in0=src[:, 16:16 + SEQ],
                                                      in1=src[:, 16 - sh:16 - sh + SEQ], op=ALU.add),
                     reads=[_nm(src)], writes=[_nm(dst)])
            shadd(s_a, U, 1)
            shadd(s_b, s_a, 2)
            if ct == 0:
                halves = ((0, s_a, 2), (64, s_b, 4))
            else:
                shadd(s_a, s_b, 4)
                shadd(s_b, s_a, 8)
                halves = ((0, s_a, 8), (64, s_b, 16))
            for (p0, sbuf, win) in halves:
                S.op("dve", lambda e, p0=p0, sbuf=sbuf, win=win: e.scalar_tensor_tensor(
                    out=pl[p0:p0 + 64, :], in0=sbuf[p0:p0 + 64, 16:16 + SEQ], scalar=1.0 / win,
                    in1=U[p0:p0 + 64, 16:16 + SEQ], op0=ALU.mult, op1=ALU.subtract),
                    reads=[_nm(sbuf), "p_U"], writes=["p_pl"])
                S.op("dve", lambda e, p0=p0, sbuf=sbuf, win=win: e.tensor_tensor(
                    out=fx[p0:p0 + 64, 0:win - 1], in0=sbuf[p0:p0 + 64, 16:16 + win - 1],
                    in1=invt[p0:p0 + 64, 0:win - 1], op=ALU.mult), reads=[_nm(sbuf), "p_invt"], writes=["p_fx"])
                S.op("dve", lambda e, p0=p0, win=win: e.tensor_tensor(
                    out=pl[p0:p0 + 64, 0:win - 1], in0=fx[p0:p0 + 64, 0:win - 1], in1=U[p0:p0 + 64, 16:16 + win - 1],
                    op=ALU.subtract), reads=["p_fx", "p_U"], writes=["p_pl"])
            for tc in range(4):
                S.op("pe", lambda e, tc=tc, ct=ct: e.matmul(pu[tc][:], lhsT=wpb[:, ct, :],
                                                         rhs=pl[:, tc * 512:(tc + 1) * 512], start=True, stop=True),
                     reads=["p_wpb", "p_pl"], writes=[_nm(pu[tc])])
                S.op("act", lambda e, tc=tc, ct=ct: e.activation(
                    out=ypT[:, ct, tc * 512:(tc + 1) * 512], in_=pu[tc][:], func=AF.Copy, scale=psc[:, ct:ct + 1]),
                    reads=[_nm(pu[tc]), "p_psc"], writes=["ypT"])
        S.end_phase()


def ssm_branch(nc, S, dr, l, b, hT, ysT, tab, tabb, sb, ps):
    with ExitStack() as ph:
        uT = sb(ph, "s_uT", [128, 2, SEQ], BF16)
        with ExitStack() as p1:
            w1 = sb(p1, "s_w1", [128, 8, 256], BF16)
            pj = [ps(p1, "s_pj%d" % i, [128, 512], F32) for i in range(2)]
            cast_load(S, w1[:], dr["w1"][l][:, :, 424:680], "s_w1")
            for ct in range(2):
                for tc in range(4):
                    pp = pj[tc % 2]
                    for k in range(8):
                        S.op("pe", lambda e, k=k, tc=tc, ct=ct, pp=pp: e.matmul(
                            pp[:], lhsT=w1[:, k, ct * 128:(ct + 1) * 128], rhs=hT[:, k, tc * 512:(tc + 1) * 512],
                            start=(k == 0), stop=(k == 7)), reads=["s_w1"] + [("hT", tc * 4 + q) for q in range(4)],
                            writes=[_nm(pp)])
                    S.op("act", lambda e, tc=tc, ct=ct, pp=pp: e.copy(out=uT[:, ct, tc * 512:(tc + 1) * 512], in_=pp[:]),
                         reads=[_nm(pp)], writes=["s_uT"])
            S.end_phase()
        zT = sb(ph, "s_zT", [128, 2, SEQ], BF16)
        arai = sb(ph, "s_arai", [128, 2, 16, 64], F32)
        T1 = sb(ph, "s_T1", [128, 16, 128], F32)
        T2 = sb(ph, "s_T2", [128, 16, 128], F32)
        pcs = sb(ph, "s_pcs", [128, 64], F32)
        Bblk = sb(ph, "s_Bblk", [128, 2, 1024], BF16)
        Cpad = sb(ph, "s_Cpad", [128, 16, 128], BF16)
        triub = sb(ph, "s_triub", [128, 128], BF16)
        dsk = sb(ph, "s_dsk", [128, 2], F32)
        bgl = sb(ph, "s_bgl", [128, 2], F32)
        wgl = sb(ph, "s_wgl", [128, 2, 256], BF16)
        t1s = [sb(ph, "s_t1%d" % i, [128, 4, 2, 64], BF16) for i in range(2)]
        t2s = [sb(ph, "s_t2%d" % i, [128, 4, 2, 64], BF16) for i in range(2)]
        v = sb(ph, "s_v", [128, 16, 2, 64], BF16)
        vsw = sb(ph, "s_vsw", [128, 16, 2, 64], BF16)
        xs = sb(ph, "s_xs", [128, 16, 128], BF16)
        xas = [sb(ph, "s_xa%d" % i, [128, 128], F32) for i in range(2)]
        xbs = [sb(ph, "s_xb%d" % i, [128, 128], F32) for i in range(2)]
        cA = [sb(ph, "s_cA%d" % i, [128, 16], F32) for i in range(2)]
        cB = [sb(ph, "s_cB%d" % i, [128, 16], F32) for i in range(2)]
        sA = sb(ph, "s_sA", [128, 16], F32)
        sB = sb(ph, "s_sB", [128, 16], F32)
        q1 = sb(ph, "s_q1", [128, 16], F32)
        yv = sb(ph, "s_yv", [128, 256], F32)
        sg = sb(ph, "s_sg", [128, 512], BF16)
        pbu = [ps(ph, "s_pbu%d" % i, [128, 512], F32) for i in range(2)]
        pA = [ps(ph, "s_pA%d" % i, [128, 512], F32) for i in range(2)]
        pB = [ps(ph, "s_pB%d" % i, [128, 512], F32) for i in range(2)]
        pyy = ps(ph, "s_py", [128, 512], F32)
        pg = ps(ph, "s_pg", [128, 512], F32)
        S.dma(arai[:].rearrange("p r g n -> p (r g n)"), tab[l][:, 0:2048], writes=["s_arai"])
        S.dma(T1[:].rearrange("p g t -> p (g t)"), tab[l][:, 2048:4096], writes=["s_T1"])
        S.dma(T2[:].rearrange("p g t -> p (g t)"), tab[l][:, 4096:6144], writes=["s_T2"])
        S.dma(pcs[:], tab[l][:, 6144:6208], writes=["s_pcs"])
        S.dma(Bblk[:].rearrange("p h c -> p (h c)"), tabb[l][:, 0:2048], writes=["s_Bblk"])
        S.dma(Cpad[:].rearrange("p g c -> p (g c)"), tabb[l][:, 2048:4096], writes=["s_Cpad"])
        S.dma(triub[:], dr["triub"], writes=["s_triub"])
        S.dma(dsk[:], dr["dskip"][l], writes=["s_dsk"])
        S.dma(bgl[:], dr["bglu"][l], writes=["s_bgl"])
        cast_load(S, wgl[:], dr["wglu"][l], "s_wgl")
        for i in range(2):
            S.op("dve", lambda e, i=i: e.memset(cA[i][:], 0.0), writes=[_nm(cA[i])])
            S.op("dve", lambda e, i=i: e.memset(cB[i][:], 0.0), writes=[_nm(cB[i])])
        Pr = pcs[:, 0:16]
        PiA = pcs[:, 16:32]
        PiB = pcs[:, 32:48]
        for t in range(NT):
            cAo, cBo = cA[t % 2], cB[t % 2]
            cAn, cBn = cA[(t + 1) % 2], cB[(t + 1) % 2]
            for half in range(2):
                for q2 in range(2):
                    S.op("pe", lambda e, half=half, q2=q2, t=t: e.matmul(
                        pbu[q2][:], lhsT=uT[:, half, t * 128:(t + 1) * 128], rhs=Bblk[:, half, q2 * 512:(q2 + 1) * 512],
                        start=True, stop=True), reads=["s_uT", "s_Bblk"], writes=[_nm(pbu[q2])])
                for q2 in range(2):
                    g0 = half * 8 + q2 * 4
                    t1 = t1s[q2]
                    t2 = t2s[q2]
                    bu = pbu[q2][:].rearrange("p (g r n) -> p g r n", g=4, r=2)
                    Ar = arai[:, 0, g0:g0 + 4, :]
                    Ai = arai[:, 1, g0:g0 + 4, :]
                    S.op("dve", lambda e, bu=bu, Ar=Ar: e.tensor_tensor(
                        out=t1[:], in0=bu, in1=Ar.unsqueeze(2).to_broadcast([128, 4, 2, 64]), op=ALU.mult),
                        reads=[_nm(pbu[q2]), "s_arai"], writes=[_nm(t1)])
                    S.op("dve", lambda e, bu=bu, Ai=Ai: e.tensor_tensor(
                        out=t2[:, :, 0, :], in0=bu[:, :, 1, :], in1=Ai, op=ALU.mult),
                        reads=[_nm(pbu[q2]), "s_arai"], writes=[_nm(t2)])
                    S.op("dve", lambda e, bu=bu, Ai=Ai: e.tensor_tensor(
                        out=t2[:, :, 1, :], in0=bu[:, :, 0, :], in1=Ai, op=ALU.mult),
                        reads=[_nm(pbu[q2]), "s_arai"], writes=[_nm(t2)])
                    S.op("pool", lambda e, g0=g0: e.tensor_tensor(
                        out=v[:, g0:g0 + 4, 0, :], in0=t1[:, :, 0, :], in1=t2[:, :, 0, :], op=ALU.subtract),
                        reads=[_nm(t1), _nm(t2)], writes=[("s_v", g0)])
                    S.op("pool", lambda e, g0=g0: e.tensor_tensor(
                        out=v[:, g0:g0 + 4, 1, :], in0=t1[:, :, 1, :], in1=t2[:, :, 1, :], op=ALU.add),
                        reads=[_nm(t1), _nm(t2)], writes=[("s_v", g0)])
                    S.op("pool", lambda e, g0=g0: e.tensor_tensor(
                        out=vsw[:, g0:g0 + 4, 1, :], in0=t1[:, :, 0, :], in1=t2[:, :, 0, :], op=ALU.subtract),
                        reads=[_nm(t1), _nm(t2)], writes=[("s_vsw", g0)])
                    S.op("pool", lambda e, g0=g0: e.tensor_tensor(
                        out=vsw[:, g0:g0 + 4, 0, :], in0=t1[:, :, 1, :], in1=t2[:, :, 1, :], op=ALU.add),
                        reads=[_nm(t1), _nm(t2)], writes=[("s_vsw", g0)])
                    pa_, pb_ = pA[q2], pB[q2]
                    for gg in range(4):
                        g = g0 + gg
                        S.op("pe", lambda e, g=g, gg=gg, pa_=pa_: e.matmul(
                            pa_[:, gg * 128:(gg + 1) * 128], lhsT=v[:, g].rearrange("p r n -> p (r n)"), rhs=triub[:],
                            start=True, stop=True), reads=[("s_v", g0), "s_triub"], writes=[_nm(pa_)])
                        S.op("pe", lambda e, g=g, gg=gg, pb_=pb_: e.matmul(
                            pb_[:, gg * 128:(gg + 1) * 128], lhsT=vsw[:, g].rearrange("p r n -> p (r n)"), rhs=triub[:],
                            start=True, stop=True), reads=[("s_vsw", g0), "s_triub"], writes=[_nm(pb_)])
                    A127 = pa_[:].rearrange("p (g t) -> p g t", t=128)[:, :, 127]
                    B127 = pb_[:].rearrange("p (g t) -> p g t", t=128)[:, :, 127]
                    gs = slice(g0, g0 + 4)
                    S.op("dve", lambda e, A127=A127, gs=gs: e.tensor_tensor(out=sA[:, gs], in0=A127, in1=cAo[:, gs], op=ALU.add),
                         reads=[_nm(pa_), _nm(cAo)], writes=["s_sA"])
                    S.op("dve", lambda e, B127=B127, gs=gs: e.tensor_tensor(out=sB[:, gs], in0=B127, in1=cBo[:, gs], op=ALU.add),
                         reads=[_nm(pb_), _nm(cBo)], writes=["s_sB"])
                    for gg in range(4):
                        g = g0 + gg
                        xa = xas[gg % 2]
                        xb = xbs[gg % 2]
                        S.op("dve", lambda e, g=g, gg=gg, pa_=pa_, xa=xa: e.scalar_tensor_tensor(
                            out=xa[:], in0=pa_[:, gg * 128:(gg + 1) * 128], scalar=cAo[:, g:g + 1], in1=T1[:, g, :],
                            op0=ALU.add, op1=ALU.mult), reads=[_nm(pa_), _nm(cAo), "s_T1"], writes=[_nm(xa)])
                        S.op("dve", lambda e, g=g, gg=gg, pb_=pb_, xb=xb: e.scalar_tensor_tensor(
                            out=xb[:], in0=pb_[:, gg * 128:(gg + 1) * 128], scalar=cBo[:, g:g + 1], in1=T2[:, g, :],
                            op0=ALU.add, op1=ALU.mult), reads=[_nm(pb_), _nm(cBo), "s_T2"], writes=[_nm(xb)])
                        S.op("pool", lambda e, g=g, xa=xa, xb=xb: e.tensor_tensor(out=xs[:, g, :], in0=xa[:], in1=xb[:], op=ALU.add),
                             reads=[_nm(xa), _nm(xb)], writes=[("s_xs", g)])
                    S.op("dve", lambda e, gs=gs: e.tensor_tensor(out=cAn[:, gs], in0=sA[:, gs], in1=Pr[:, gs], op=ALU.mult),
                         reads=["s_sA", "s_pcs"], writes=[_nm(cAn)])
                    S.op("dve", lambda e, gs=gs: e.tensor_tensor(out=q1[:, gs], in0=sB[:, gs], in1=PiA[:, gs], op=ALU.mult),
                         reads=["s_sB", "s_pcs"], writes=["s_q1"])
                    S.op("dve", lambda e, gs=gs: e.tensor_tensor(out=cAn[:, gs], in0=cAn[:, gs], in1=q1[:, gs], op=ALU.add),
                         reads=["s_q1"], writes=[_nm(cAn)])
                    S.op("dve", lambda e, gs=gs: e.tensor_tensor(out=cBn[:, gs], in0=sB[:, gs], in1=Pr[:, gs], op=ALU.mult),
                         reads=["s_sB", "s_pcs"], writes=[_nm(cBn)])
                    S.op("dve", lambda e, gs=gs: e.tensor_tensor(out=q1[:, gs], in0=sA[:, gs], in1=PiB[:, gs], op=ALU.mult),
                         reads=["s_sA", "s_pcs"], writes=["s_q1"])
                    S.op("dve", lambda e, gs=gs: e.tensor_tensor(out=cBn[:, gs], in0=cBn[:, gs], in1=q1[:, gs], op=ALU.add),
                         reads=["s_q1"], writes=[_nm(cBn)])
                for g8 in range(8):
                    g = half * 8 + g8
                    S.op("pe", lambda e, g=g, g8=g8, half=half: e.matmul(
                        pyy[:, half * 128:(half + 1) * 128], lhsT=Cpad[:, g, :], rhs=xs[:, g, :],
                        start=(g8 == 0), stop=(g8 == 7)), reads=[("s_xs", g), "s_Cpad"], writes=["s_py"])
                S.op("dve", lambda e, half=half, t=t: e.scalar_tensor_tensor(
                    out=yv[:, half * 128:(half + 1) * 128], in0=uT[:, half, t * 128:(t + 1) * 128],
                    scalar=dsk[:, half:half + 1], in1=pyy[:, half * 128:(half + 1) * 128], op0=ALU.mult, op1=ALU.add),
                    reads=["s_py", "s_uT", "s_dsk"], writes=["s_yv"])
                S.op("act", lambda e, half=half, t=t: e.activation(
                    out=zT[:, half, t * 128:(t + 1) * 128], in_=yv[:, half * 128:(half + 1) * 128],
                    func=AF.Gelu_apprx_tanh), reads=["s_yv"], writes=[("s_zT", t // 4)])
            if t % 4 == 3:
                tc = t // 4
                for ct in range(2):
                    for k in range(2):
                        S.op("pe", lambda e, k=k, ct=ct, tc=tc: e.matmul(
                            pg[:], lhsT=wgl[:, k, ct * 128:(ct + 1) * 128], rhs=zT[:, k, tc * 512:(tc + 1) * 512],
                            start=(k == 0), stop=(k == 1)), reads=["s_wgl", ("s_zT", tc)], writes=["s_pg"])
                    S.op("act", lambda e, ct=ct: e.activation(out=sg[:], in_=pg[:], func=AF.Sigmoid,
                                                              bias=bgl[:, ct:ct + 1]),
                         reads=["s_pg", "s_bgl"], writes=["s_sg"])
                    S.op("dve", lambda e, ct=ct, tc=tc: e.tensor_tensor(
                        out=ysT[:, ct, tc * 512:(tc + 1) * 512], in0=zT[:, ct, tc * 512:(tc + 1) * 512], in1=sg[:],
                        op=ALU.mult), reads=["s_sg", ("s_zT", tc)], writes=["ysT"])
        S.end_phase()


def attention(nc, S, dr, l, b, hT, yaT, sb, ps, rstd_from_ss, identb, onesb):
    with ExitStack() as at:
        cqnT = sb(at, "a_cqnT", [128, 2, SEQ], BF16)
        ckvT = sb(at, "a_ckvT", [128, SEQ], BF16)
        Vg = sb(at, "a_V", [128, NT, 128], BF16)
        kidT = sb(at, "a_kidT", [128, SEQ], BF16)
        Wsc = sb(at, "a_Wsc", [128, NT, 8], F32)
        with ExitStack() as ph:
            w1 = sb(ph, "a_w1", [128, 8, 424], BF16)
            gcq = sb(ph, "a_gcq", [128, 2], F32)
            gkv = sb(ph, "a_gkv", [128, 128], F32)
            ss = sb(ph, "a_ss", [128, 2], F32)
            junk = sb(ph, "a_junk", [128, 256], BF16)
            cqn = sb(ph, "a_cqn", [128, 256], BF16)
            kid4 = sb(ph, "a_kid4", [128, 4, 32], BF16)
            pz = [ps(ph, "a_pz%d" % i, [128, 512], F32) for i in range(2)]
            pt = [ps(ph, "a_pt%d" % i, [128, 1024], BF16) for i in range(2)]
            cast_load(S, w1[:], dr["w1"][l][:, :, 0:424], "a_w1")
            S.dma(gcq[:], dr["gcq"][l], writes=["a_gcq"])
            S.dma(gkv[:], dr["gckv"][l:l + 1, :].to_broadcast([128, 128]), writes=["a_gkv"])
            for t in range(NT):
                p = pz[t % 2]
                q = pt[t % 2]
                for k in range(8):
                    S.op("pe", lambda e, k=k, t=t, p=p: e.matmul(p[:, 0:424], lhsT=hT[:, k, t * 128:(t + 1) * 128],
                                                                 rhs=w1[:, k, :], start=(k == 0), stop=(k == 7)),
                         reads=["a_w1", ("hT", t)], writes=[_nm(p)])
                S.op("act", lambda e, p=p: e.activation(out=junk[:], in_=p[:, 0:256], func=AF.Square,
                                                        accum_out=ss[:, 0:1]), reads=[_nm(p)], writes=["a_junk", "a_ss"])
                S.op("act", lambda e, p=p: e.activation(out=junk[:, 0:128], in_=p[:, 256:384], func=AF.Square,
                                                        accum_out=ss[:, 1:2]), reads=[_nm(p)], writes=["a_junk", "a_ss"])
                S.op("dve", lambda e: e.tensor_scalar(out=ss[:, 0:1], in0=ss[:, 0:1], scalar1=1.0 / 256, scalar2=EPS,
                                                      op0=ALU.mult, op1=ALU.add), reads=["a_ss"], writes=["a_ss"])
                S.op("dve", lambda e: e.tensor_scalar(out=ss[:, 1:2], in0=ss[:, 1:2], scalar1=1.0 / 128, scalar2=EPS,
                                                      op0=ALU.mult, op1=ALU.add), reads=["a_ss"], writes=["a_ss"])
                S.op("act", lambda e: e.activation(out=ss[:], in_=ss[:], func=AF.Sqrt), reads=["a_ss"], writes=["a_ss"])
                S.op("dve", lambda e: e.reciprocal(out=ss[:], in_=ss[:]), reads=["a_ss"], writes=["a_ss"])
                S.op("dve", lambda e, p=p: e.tensor_scalar(out=cqn[:], in0=p[:, 0:256], scalar1=ss[:, 0:1], scalar2=None,
                                                           op0=ALU.mult), reads=[_nm(p), "a_ss"], writes=["a_cqn"])
                S.op("dve", lambda e, p=p, t=t: e.scalar_tensor_tensor(
                    out=Vg[:, t, :], in0=p[:, 256:384], scalar=ss[:, 1:2], in1=gkv[:], op0=ALU.mult, op1=ALU.mult),
                    reads=[_nm(p), "a_ss", "a_gkv"], writes=[("a_V", t)])
                S.op("dve", lambda e, p=p: e.tensor_copy(
                    out=kid4[:], in_=p[:, 384:416].unsqueeze(1).to_broadcast([128, 4, 32])),
                    reads=[_nm(p)], writes=["a_kid4"])
                S.op("dve", lambda e, p=p, t=t: e.tensor_scalar(out=Wsc[:, t, :], in0=p[:, 416:424], scalar1=IDXW,
                                                                scalar2=None, op0=ALU.mult),
                     reads=[_nm(p)], writes=["a_Wsc"])
                for c in range(2):
                    S.op("pe", lambda e, c=c, q=q: e.transpose(out=q[:, c * 128:(c + 1) * 128],
                                                              in_=cqn[:, c * 128:(c + 1) * 128], identity=identb[:]),
                         reads=["a_cqn", "identb"], writes=[_nm(q)])
                S.op("pe", lambda e, q=q, t=t: e.transpose(out=q[:, 256:384], in_=Vg[:, t, :], identity=identb[:]),
                     reads=[("a_V", t), "identb"], writes=[_nm(q)])
                S.op("pe", lambda e, q=q: e.transpose(out=q[:, 384:512], in_=kid4[:].rearrange("p a b -> p (a b)"),
                                                      identity=identb[:]), reads=["a_kid4", "identb"], writes=[_nm(q)])
                for c in range(2):
                    S.op("act", lambda e, c=c, q=q, t=t: e.activation(
                        out=cqnT[:, c, t * 128:(t + 1) * 128], in_=q[:, c * 128:(c + 1) * 128], func=AF.Copy,
                        scale=gcq[:, c:c + 1]), reads=[_nm(q), "a_gcq"], writes=[("a_cqnT", t)])
                S.op("dve", lambda e, q=q, t=t: e.tensor_copy(out=ckvT[:, t * 128:(t + 1) * 128], in_=q[:, 256:384]),
                     reads=[_nm(q)], writes=[("a_ckvT", t)])
                S.op("dve", lambda e, q=q, t=t: e.tensor_copy(out=kidT[:, t * 128:(t + 1) * 128], in_=q[:, 384:512]),
                     reads=[_nm(q)], writes=[("a_kidT", t)])
            S.end_phase()
        with ExitStack() as ph:
            wuq = sb(ph, "a_wuq", [128, 2, 1024], BF16)
            wqi = sb(ph, "a_wqi", [128, 2, 256], BF16)
            wuv = sb(ph, "a_wuv", [128, 8, 64], BF16)
            i4b = sb(ph, "a_i4b", [128, 512], BF16)
            altd = sb(ph, "a_altd", [128, 1024], BF16)
            at3 = sb(ph, "a_at3", [128, 2048], BF16)
            bt3 = sb(ph, "a_bt3", [128, 1024], BF16)
            mk4 = sb(ph, "a_mk4", [128, 4], F32)
            wuvp = sb(ph, "a_wuvp", [128, 8, 128], BF16)
            m0 = sb(ph, "a_m0", [128, 128], F32)
            pow2 = sb(ph, "a_pow2", [128, NIT + 1], F32)
            qT = sb(ph, "a_qT", [128, 8, 128], BF16)
            qiT = sb(ph, "a_qiT", [128, 8, 128], BF16)
            acc = sb(ph, "a_acc", [128, SEQ], F32)
            jk = sb(ph, "a_jk", [128, SEQ], BF16)
            MBs = [sb(ph, "a_MB%d" % i, [128, SEQ], BF16) for i in range(2)]
            Rb = [sb(ph, "a_R%d" % i, [128, 512], BF16) for i in range(2)]
            wdg = sb(ph, "a_wdg", [128, 8, 128], BF16)
            PT = [sb(ph, "a_PT%d" % i, [128, 512], BF16) for i in range(2)]
            rec = sb(ph, "a_rec", [128, 512], F32)
            oT = sb(ph, "a_oT", [128, 1024], BF16)
            sm = sb(ph, "a_sm", [128, 8], F32)
            Wk = sb(ph, "a_Wk", [128, NIT + 1], F32)
            pS = [ps(ph, "a_pS%d" % i, [128, 512], F32) for i in range(2)]
            pL = [ps(ph, "a_pL%d" % i, [128, 512], F32) for i in range(2)]
            pQ = ps(ph, "a_pQ", [128, 512], F32)
            pACC = ps(ph, "a_pACC", [128, 512], F32)
            pO = ps(ph, "a_pO", [128, 512], F32)
            pD = ps(ph, "a_pD", [128, 512], F32)
            pY = pQ
            cast_load(S, wuq[:], dr["wuq"][l], "a_wuq")
            cast_load(S, wqi[:], dr["wqi"][l], "a_wqi")
            cast_load(S, wuv[:], dr["wuv"][l], "a_wuv")
            for nm, tl in (("i4b", i4b), ("altd", altd), ("at3", at3), ("bt3", bt3), ("m0", m0), ("pow2", pow2),
                           ("mk4", mk4)):
                S.dma(tl[:], dr[nm], writes=["a_" + nm])
            S.op("dve", lambda e: e.memset(wuvp[:], 0.0), writes=["a_wuvp"])
            for h in range(8):
                S.op("dve", lambda e, h=h: e.tensor_copy(out=wuvp[:, h, (h % 2) * 64:(h % 2 + 1) * 64], in_=wuv[:, h, :]),
                     reads=["a_wuv"], writes=["a_wuvp"])
            hi, lo, w0, mid, cnt, tt, thr = (sm[:, i:i + 1] for i in range(7))
            ACCR = [("a_acc", c_) for c_ in range(4)]

            def stageA(i):
                steps = []
                N = 128 * (i + 1)
                qs = slice(i * 128, (i + 1) * 128)
                MB = MBs[i % 2]
                mbn = "a_MB%d" % (i % 2)

                def qi_proj(tl):
                    for k in range(2):
                        S.op("pe", lambda e: e.matmul(
                            pQ[:, tl * 128:(tl + 1) * 128], lhsT=wqi[:, k, tl * 128:(tl + 1) * 128],
                            rhs=cqnT[:, k, qs], start=(k == 0), stop=(k == 1)),
                            reads=["a_wqi", ("a_cqnT", i)], writes=["a_pQ"])
                    for h in range(tl * 4, tl * 4 + 4):
                        S.op("act", lambda e: e.activation(out=qiT[:, h, :], in_=pQ[:, tl * 128:(tl + 1) * 128],
                                                           func=AF.Copy, scale=mk4[:, h % 4:h % 4 + 1]),
                             reads=["a_pQ", "a_mk4"], writes=["a_qiT"])
                for tl in range(2):
                    steps.append(partial(qi_proj, tl))

                nch = (N + 511) // 512

                def diag_build():
                    for h in range(8):
                        S.op("act", lambda e: e.activation(out=wdg[:, h, :], in_=identb[:], func=AF.Copy,
                                                           scale=Wsc[:, i, h:h + 1]),
                             reads=["identb", "a_Wsc"], writes=["a_wdg"])
                steps.append(diag_build)
                seq = [(cc, h) for cc in range(nch) for h in range(8)]

                def emitL(x):
                    cc, h = seq[x]
                    n = min(512, N - cc * 512)
                    pl_ = pL[x % 2]
                    S.op("pe", lambda e: e.matmul(
                        pl_[:, 0:n], lhsT=qiT[:, h, :], rhs=kidT[:, cc * 512:cc * 512 + n],
                        start=True, stop=True), reads=["a_qiT"] + [("a_kidT", cc * 4 + q) for q in range((n + 127) // 128)],
                        writes=[_nm(pl_)])

                def idx_step(x):
                    cc, h = seq[x]
                    n = min(512, N - cc * 512)
                    if x == 0:
                        emitL(0)
                    if x + 1 < len(seq):
                        emitL(x + 1)
                    pl_ = pL[x % 2]
                    rb = Rb[x % 2]
                    S.op("act", lambda e: e.activation(out=rb[:, 0:n], in_=pl_[:, 0:n], func=AF.Relu),
                         reads=[_nm(pl_)], writes=[_nm(rb)])
                    S.op("pe", lambda e: e.matmul(pACC[:, 0:n], lhsT=wdg[:, h, :], rhs=rb[:, 0:n],
                                                  start=(h == 0), stop=(h == 7)),
                         reads=["a_wdg", _nm(rb)], writes=["a_pACC"])
                    if h == 7:
                        S.op("act", lambda e: e.copy(out=acc[:, cc * 512:cc * 512 + n], in_=pACC[:, 0:n]),
                             reads=["a_pACC"], writes=[("a_acc", cc)])
                for x in range(len(seq)):
                    steps.append(partial(idx_step, x))

                def bounds():
                    if i >= 2:
                        S.op("dve", lambda e: e.tensor_reduce(out=hi, in_=acc[:, 0:N], axis=AX.X, op=ALU.max),
                             reads=ACCR, writes=["a_sm"])
                        S.op("dve", lambda e: e.tensor_reduce(out=lo, in_=acc[:, 0:N], axis=AX.X, op=ALU.min),
                             reads=ACCR, writes=["a_sm"])
                    S.op("dve", lambda e: e.tensor_tensor(out=acc[:, N - 128:N], in0=acc[:, N - 128:N], in1=m0[:],
                                                          op=ALU.add), reads=["a_m0"] + ACCR, writes=ACCR)
                    if i >= 2:
                        S.op("dve", lambda e: e.tensor_tensor(out=w0, in0=hi, in1=lo, op=ALU.subtract),
                             reads=["a_sm"], writes=["a_sm"])
                        S.op("dve", lambda e: e.tensor_scalar(out=Wk[:], in0=pow2[:], scalar1=w0, scalar2=None,
                                                              op0=ALU.mult), reads=["a_sm", "a_pow2"], writes=["a_Wk"])
                        S.op("dve", lambda e: e.tensor_tensor(out=mid, in0=lo, in1=Wk[:, 0:1], op=ALU.add),
                             reads=["a_sm", "a_Wk"], writes=["a_sm"])
                    else:
                        S.op("dve", lambda e: e.memset(thr, -1.0e29), writes=["a_sm"])
                steps.append(bounds)

                def bis(k):
                    S.op("dve", lambda e: e.tensor_scalar(
                        out=jk[:, 0:N], in0=acc[:, 0:N], scalar1=mid, scalar2=0.0, op0=ALU.is_ge, op1=ALU.add,
                        accum_out=cnt), reads=ACCR + ["a_sm"], writes=["a_jk", "a_sm"])
                    S.op("dve", lambda e: e.tensor_scalar(out=tt, in0=cnt, scalar1=255.5, scalar2=0.5, op0=ALU.is_ge,
                                                          op1=ALU.subtract), reads=["a_sm"], writes=["a_sm"])
                    S.op("dve", lambda e: e.scalar_tensor_tensor(out=mid, in0=tt, scalar=Wk[:, k:k + 1], in1=mid,
                                                                 op0=ALU.mult, op1=ALU.add),
                         reads=["a_sm", "a_Wk"], writes=["a_sm"])
                if i >= 2:
                    for k in range(NIT):
                        steps.append(partial(bis, k))

                def fin():
                    if i >= 2:
                        S.op("dve", lambda e: e.tensor_tensor(out=thr, in0=mid, in1=Wk[:, NIT:NIT + 1], op=ALU.subtract),
                             reads=["a_sm", "a_Wk"], writes=["a_sm"])
                    S.op("dve", lambda e: e.tensor_scalar(out=MB[:, 0:N], in0=acc[:, 0:N], scalar1=thr, scalar2=-30000.0,
                                                          op0=ALU.is_lt, op1=ALU.mult),
                         reads=ACCR + ["a_sm"], writes=[mbn])
                steps.append(fin)
                return steps

            def stageB(i):
                steps = []
                qs = slice(i * 128, (i + 1) * 128)
                MB = MBs[i % 2]
                mbn = "a_MB%d" % (i % 2)

                def q_proj(g2):
                    for h in range(g2 * 4, g2 * 4 + 4):
                        for k in range(2):
                            S.op("pe", lambda e: e.matmul(
                                pS[g2][:, (h % 4) * 128:(h % 4 + 1) * 128], lhsT=wuq[:, k, h * 128:(h + 1) * 128],
                                rhs=cqnT[:, k, qs], start=(k == 0), stop=(k == 1)),
                                reads=["a_wuq", ("a_cqnT", i)], writes=[_nm(pS[g2])])
                    S.op("act", lambda e: e.activation(
                        out=qT[:, g2 * 4:(g2 + 1) * 4, :].rearrange("p h q -> p (h q)"), in_=pS[g2][:], func=AF.Copy,
                        scale=ATT_SC), reads=[_nm(pS[g2])], writes=["a_qT"])
                steps.append(partial(q_proj, 0))
                steps.append(partial(q_proj, 1))
                its = [(g, j) for g in range(2) for j in range(i + 1)]

                def emitS(n):
                    grp, j = its[n]
                    s_ = pS[n % 2]
                    S.op("pe", lambda e: e.matmul(
                        s_[:], lhsT=ckvT[:, j * 128:(j + 1) * 128],
                        rhs=qT[:, grp * 4:(grp + 1) * 4, :].rearrange("p h q -> p (h q)"), start=True, stop=False),
                        reads=[("a_ckvT", j), "a_qT"], writes=[_nm(s_)])
                    S.op("pe", lambda e: e.matmul(
                        s_[:], lhsT=MB[:, j * 128:(j + 1) * 128], rhs=i4b[:], start=False, stop=False),
                        reads=[mbn, "a_i4b"], writes=[_nm(s_)])
                    if j < i:
                        dd = i - j
                        S.op("pe", lambda e: e.matmul(
                            s_[:], lhsT=at3[:, dd * 128:(dd + 1) * 128], rhs=bt3[:, grp * 512:(grp + 1) * 512],
                            start=False, stop=True), reads=["a_at3", "a_bt3"], writes=[_nm(s_)])
                    else:
                        S.op("pe", lambda e: e.matmul(
                            s_[:], lhsT=identb[:], rhs=altd[:, grp * 512:(grp + 1) * 512], start=False, stop=True),
                            reads=["identb", "a_altd"], writes=[_nm(s_)])

                def it_step(n):
                    grp, j = its[n]
                    if n == 0:
                        emitS(0)
                    if n + 1 < len(its):
                        emitS(n + 1)
                    s_ = pS[n % 2]
                    ptb = PT[n % 2]
                    S.op("act", lambda e: e.activation(out=ptb[:], in_=s_[:], func=AF.Exp),
                         reads=[_nm(s_)], writes=[_nm(ptb)])
                    S.op("pe", lambda e: e.matmul(pO[:], lhsT=Vg[:, j, :], rhs=ptb[:], start=(j == 0), stop=(j == i)),
                         reads=[("a_V", j), _nm(ptb)], writes=["a_pO"])
                    S.op("pe", lambda e: e.matmul(pD[:], lhsT=onesb[:], rhs=ptb[:], start=(j == 0), stop=(j == i)),
                         reads=["onesb", _nm(ptb)], writes=["a_pD"])
                    if j == i:
                        S.op("dve", lambda e: e.reciprocal(out=rec[:], in_=pD[:]), reads=["a_pD"], writes=["a_rec"])
                        S.op("dve", lambda e: e.tensor_tensor(out=oT[:, grp * 512:(grp + 1) * 512], in0=pO[:],
                                                              in1=rec[:], op=ALU.mult),
                             reads=["a_pO", "a_rec"], writes=["a_oT"])
                for n in range(len(its)):
                    steps.append(partial(it_step, n))

                def y_proj():
                    for h in range(8):
                        S.op("pe", lambda e: e.matmul(
                            pY[:, (h // 2) * 128:(h // 2 + 1) * 128], lhsT=wuvp[:, h, :],
                            rhs=oT[:, h * 128:(h + 1) * 128], start=(h % 2 == 0), stop=(h % 2 == 1)),
                            reads=["a_wuvp", "a_oT"], writes=["a_pQ"])
                    S.op("act", lambda e: e.copy(out=yaT[:, :, qs], in_=pY[:].rearrange("p (c q) -> p c q", q=128)),
                         reads=["a_pQ"], writes=["yaT"])
                steps.append(y_proj)
                return steps

            def run_merged(la, lb):
                na, nb_ = len(la), len(lb)
                ia = ib = 0
                while ia < na or ib < nb_:
                    if ib >= nb_ or (ia < na and ia * nb_ <= ib * na):
                        la[ia]()
                        ia += 1
                    else:
                        lb[ib]()
                        ib += 1

            run_merged(stageA(0), [])
            for i in range(NT):
                run_merged(stageA(i + 1) if i + 1 < NT else [], stageB(i))
            S.end_phase()


_BF = {"identb", "i4b", "onesb", "triub", "altd", "at3", "bt3"}


def kernel(**inputs):
    inp = {k: np.asarray(v) for k, v in inputs.items()}
    n_cores = 8
    shared = _layout_weights(inp)
    shared.update(_consts())
    x = np.ascontiguousarray(inp["x"], dtype=np.float32)
    c = np.asarray(inp["c"], dtype=np.float32)
    in_maps = []
    for ci in range(n_cores):
        m = dict(shared)
        m["x"] = x[ci * NB:(ci + 1) * NB]
        cc = c[ci * NB:(ci + 1) * NB]
        m["cT"] = np.ascontiguousarray(cc.reshape(NB, 8, 128).transpose(2, 1, 0))
        in_maps.append(m)
    shapes = {k: (v.shape, BF16 if k in _BF else F32) for k, v in in_maps[0].items()}
    nc = build_nc(shapes)
    in_maps = [{"d_" + k: v for k, v in m.items()} for m in in_maps]
    res = run_bass_kernel_spmd(nc, in_maps, core_ids=list(range(n_cores)))
    return np.concatenate([np.asarray(r["out"], dtype=np.float32) for r in res.results], axis=0)
```

```python
import types
from functools import partial
import numpy as np
import ml_dtypes
from contextlib import ExitStack
import concourse.bass as bass
import concourse.mybir as mybir
from concourse.bass_utils import run_bass_kernel_spmd

F32 = mybir.dt.float32
BF16 = mybir.dt.bfloat16
AF = mybir.ActivationFunctionType
ALU = mybir.AluOpType
AX = mybir.AxisListType

D = 1024
SEQ = 2048
NT = 16
NL = 2
NB = 4
EPS = 1e-6
ATT_SC = 128.0 ** -0.5
IDXW = (32.0 ** -0.5) * (8.0 ** -0.5)
NIT = 18
NEG = -1.0e30
POOL_WINS = (2, 4, 8, 16)


_BASE = {}
_UNIQ = [0]


def _nm(t):
    return _BASE[t.name]


def _freeze(fn):
    if fn.__closure__ is None:
        return fn
    cells = []
    for c in fn.__closure__:
        try:
            cells.append(types.CellType(c.cell_contents))
        except ValueError:
            cells.append(c)
    return types.FunctionType(fn.__code__, fn.__globals__, fn.__name__, fn.__defaults__, tuple(cells))


class Sched:
    ENG = ("pe", "act", "dve", "pool", "sp")

    def __init__(self, nc, es, n_dma_sems=12):
        self.nc = nc
        self.sems = {e: es.enter_context(nc.semaphore("s_" + e)) for e in self.ENG}
        self.cnt = {e: 0 for e in self.ENG}
        self.dma_sems = [es.enter_context(nc.semaphore("d%d" % i)) for i in range(n_dma_sems)]
        self.dma_cnt = [0] * n_dma_sems
        self.dma_rr = 0
        self.prog = {e: [] for e in self.ENG}
        self.seen = {e: {} for e in self.ENG}
        self.lastw = {}
        self.readers = {}
        self.ninst = 0
        self.excl = set()

    def _sem_of(self, key):
        if isinstance(key, tuple):
            return self.dma_sems[key[1]], 16
        return self.sems[key], 1

    def _needed(self, eng, deps):
        best = {}
        for (key, val) in deps:
            if key == eng and eng in ("pe", "sp"):
                continue
            if self.seen[eng].get(key, 0) >= val:
                continue
            if best.get(key, 0) < val:
                best[key] = val
        waits = []
        for key, val in best.items():
            self.seen[eng][key] = val
            sem, step = self._sem_of(key)
            waits.append((sem, val * step))
        return waits

    def _deps(self, reads, writes):
        deps = []
        for r in reads:
            t = self.lastw.get(r)
            if t is not None:
                deps.append(t)
        for w in writes:
            t = self.lastw.get(w)
            if t is not None:
                deps.append(t)
            rd = self.readers.get(w)
            if rd:
                deps.extend(rd.items())
        return deps

    def _commit(self, tok, reads, writes):
        for w in writes:
            self.lastw[w] = tok
            self.readers[w] = {}
        for r in reads:
            d = self.readers.setdefault(r, {})
            if d.get(tok[0], 0) < tok[1]:
                d[tok[0]] = tok[1]

    def op(self, eng, fn, reads=(), writes=()):
        fn = _freeze(fn)
        if self.excl:
            ex = [r for r in reads if r in self.excl]
            if ex:
                writes = list(writes) + [r for r in ex if r not in writes]
        waits = self._needed(eng, self._deps(reads, writes))
        self.cnt[eng] += 1
        tok = (eng, self.cnt[eng])
        sem = self.sems[eng]

        def emit(e, waits=waits, fn=fn, sem=sem):
            for (s, v) in waits:
                e.wait_ge(s, v)
            fn(e).then_inc(sem, 1)

        self.prog[eng].append(emit)
        self._commit(tok, reads, writes)
        self.ninst += 1
        return tok

    def dma(self, out, in_, reads=(), writes=(), q="sp", **kw):
        k = self.dma_rr
        self.dma_rr = (k + 1) % len(self.dma_sems)
        deps = self._deps(reads, writes)
        if self.dma_cnt[k] > 0:
            deps.append((("dma", k), self.dma_cnt[k]))
        waits = self._needed(q, deps)
        self.dma_cnt[k] += 1
        tok = (("dma", k), self.dma_cnt[k])
        sem = self.dma_sems[k]

        def emit(e, waits=waits, sem=sem, out=out, in_=in_, kw=kw):
            for (s, v) in waits:
                e.wait_ge(s, v)
            e.dma_start(out=out, in_=in_, **kw).then_inc(sem, 16)

        self.prog[q].append(emit)
        self._commit(tok, reads, writes)
        self.ninst += 1
        return tok

    def end_phase(self, q="sp"):
        waits = [(self.dma_sems[k], 16 * c) for k, c in enumerate(self.dma_cnt) if c > 0]
        waits += [(self.sems[e], self.cnt[e]) for e in self.ENG if self.cnt[e] > 0 and e != q]

        def emit(e, waits=waits):
            for (s, v) in waits:
                e.wait_ge(s, v)

        self.prog[q].append(emit)
        nc = self.nc
        prog = self.prog
        with nc.Block() as block:
            if prog["pe"]:
                @block.tensor
                def _(e):
                    for f in prog["pe"]:
                        f(e)
            if prog["act"]:
                @block.scalar
                def _(e):
                    for f in prog["act"]:
                        f(e)
            if prog["dve"]:
                @block.vector
                def _(e):
                    for f in prog["dve"]:
                        f(e)
            if prog["pool"]:
                @block.gpsimd
                def _(e):
                    for f in prog["pool"]:
                        f(e)
            if prog["sp"]:
                @block.sync
                def _(e):
                    for f in prog["sp"]:
                        f(e)
        self.prog = {e: [] for e in self.ENG}


def _consts():
    bf = ml_dtypes.bfloat16
    c = {}
    c["identf"] = np.eye(128, dtype=np.float32)
    c["identb"] = np.eye(128, dtype=np.float32).astype(bf)
    c["i4b"] = np.tile(np.eye(128, dtype=np.float32), (1, 4)).astype(bf)
    c["onesb"] = np.ones((128, 128), np.float32).astype(bf)
    c["onesf"] = np.ones((128, 128), np.float32)
    s = np.arange(128)
    c["triub"] = (s[:, None] <= s[None, :]).astype(np.float32).astype(bf)
    m0 = np.zeros((128, 128), np.float32)
    m0[:64, 64:] = NEG
    c["m0"] = m0
    slopes = 2.0 ** (-8.0 * np.arange(1, 9) / 8.0)
    altd = np.zeros((128, 8, 128), np.float32)
    for h in range(8):
        altd[:, h, :] = -slopes[h] * np.abs(s[None, :] - s[:, None])
    c["altd"] = altd.reshape(128, 1024).astype(bf)
    at3 = np.zeros((3, 16, 128), np.float32)
    at3[0] = 1.0
    at3[1] = (128.0 * np.arange(16))[:, None]
    at3[2] = s[None, :].astype(np.float32)
    at3p = np.zeros((128, 2048), np.float32)
    at3p[0:3] = at3.reshape(3, 2048)
    c["at3"] = at3p.astype(bf)
    bt3 = np.zeros((3, 8, 128), np.float32)
    for h in range(8):
        bt3[0, h] = -slopes[h] * s
        bt3[1, h] = -slopes[h]
        bt3[2, h] = slopes[h]
    bt3p = np.zeros((128, 1024), np.float32)
    bt3p[0:3] = bt3.reshape(3, 1024)
    c["bt3"] = bt3p.astype(bf)
    mk4 = np.zeros((128, 4), np.float32)
    for r in range(4):
        mk4[r * 32:(r + 1) * 32, r] = 1.0
    c["mk4"] = mk4
    sg = np.ones((128, 2), np.float32)
    sg[:64, 0] = -1.0
    sg[64:, 1] = -1.0
    c["sgn"] = sg
    bm = np.zeros((128, 8), np.float32)
    for p in range(128):
        bm[p, p // 16] = 1.0
    c["bmask"] = bm
    m2 = np.zeros((128, 16, 8), np.float32)
    for g in range(16):
        m2[:, g, g % 8] = 1.0
    c["mask2"] = m2
    c["pow2"] = np.tile((2.0 ** -(np.arange(NIT + 1) + 1.0))[None, :], (128, 1)).astype(np.float32)
    c["invt"] = np.tile((1.0 / (np.arange(16) + 1.0))[None, :], (128, 1)).astype(np.float32)
    return c


def _layout_weights(inp):
    f = lambda a: np.ascontiguousarray(a, dtype=np.float32)
    w = {}
    w["modw"] = f(inp["mod_w"].reshape(NL, 8, 128, 12, 512).transpose(0, 3, 2, 1, 4))
    w["modb"] = f(inp["mod_b"].reshape(NL, 48, 128).transpose(0, 2, 1))
    gv = np.stack([inp["mix_pre_g"], inp["mix_post_g"], inp["ffn_pre_g"], inp["ffn_post_g"]], axis=1)
    w["gvec"] = f(gv.reshape(NL, 4, 8, 128).transpose(0, 1, 3, 2))
    win = inp["w_in"].reshape(NL, 8, 128, 4008)
    w["w1"] = f(win[:, :, :, :936].transpose(0, 2, 1, 3))
    w["wg"] = f(win[:, :, :, 936:].reshape(NL, 8, 128, 24, 128).transpose(0, 3, 2, 1, 4))
    w["gcq"] = f(inp["g_cq"].reshape(NL, 2, 128).transpose(0, 2, 1))
    w["gckv"] = f(inp["g_ckv"])
    w["wuq"] = f(inp["w_uq"].reshape(NL, 2, 128, 1024).transpose(0, 2, 1, 3))
    w["wqi"] = f(inp["w_qi"].reshape(NL, 2, 128, 256).transpose(0, 2, 1, 3))
    w["wuv"] = f(inp["w_uv"].transpose(0, 2, 1, 3))
    dup = lambda a: np.concatenate([a, a], axis=1)
    w["are"] = f(dup(inp["a_re"].transpose(0, 2, 1)))
    w["aim"] = f(dup(inp["a_im"].transpose(0, 2, 1)))
    w["lstep"] = f(np.broadcast_to(inp["log_step"][:, None, :], (NL, 128, 16)))
    w["bre"] = f(inp["b_re"].transpose(0, 2, 1, 3))
    w["bim"] = f(inp["b_im"].transpose(0, 2, 1, 3))
    w["cre"] = f(inp["c_re"].transpose(0, 3, 1, 2))
    w["cim"] = f(inp["c_im"].transpose(0, 3, 1, 2))
    w["dskip"] = f(inp["d_skip"].reshape(NL, 2, 128).transpose(0, 2, 1))
    w["wglu"] = f(inp["w_glu"].reshape(NL, 2, 128, 256).transpose(0, 2, 1, 3))
    w["bglu"] = f(inp["b_glu"].reshape(NL, 2, 128).transpose(0, 2, 1))
    w["wpool"] = f(inp["w_pool"])
    w["pscale"] = f(inp["pool_scale"].reshape(NL, 2, 128).transpose(0, 2, 1))
    w["pa"] = f(inp["p_a"].reshape(NL, 4, 128, 1024).transpose(0, 2, 1, 3))
    w["pb"] = f(inp["p_b"].reshape(NL, 2, 128, 1024).transpose(0, 2, 1, 3))
    w["pc"] = f(inp["p_c"].reshape(NL, 2, 128, 1024).transpose(0, 2, 1, 3))
    w["wout"] = f(inp["w_out"].reshape(NL, 8, 128, 1024).transpose(0, 2, 1, 3))
    w["wup"] = f(inp["w_up"].reshape(NL, 8, 128, 32, 128).transpose(0, 3, 2, 1, 4))
    w["convw"] = f(inp["conv_w"].reshape(NL, 3, 32, 128).transpose(0, 3, 1, 2))
    w["convb"] = f(inp["conv_b"].reshape(NL, 32, 128).transpose(0, 2, 1))
    w["wdown"] = f(inp["w_down"].reshape(NL, 16, 128, 1024).transpose(0, 2, 1, 3))
    return w


def build_nc(shapes, nb=NB, nl=NL, do_mix=True, do_ffn=True, att=True, ssm=True, pool=True):
    nc = bass.Bass("TRN2", target_bir_lowering=False)
    DBG["nc"] = nc
    DBG["outs"] = {}
    dr = {}
    for name, (shape, dt) in shapes.items():
        dr[name] = nc.dram_tensor("d_" + name, list(shape), dt, kind="ExternalInput").ap()
    out = nc.dram_tensor("out", [nb, SEQ, D], F32, kind="ExternalOutput").ap()
    TABW = 2 * 1024 + 2 * 2048 + 64
    tab = nc.dram_tensor("tabscratch", [NL, 128, TABW], F32, kind="Internal").ap()
    tabb = nc.dram_tensor("tabscratchb", [NL, 128, 2048 + 2048], BF16, kind="Internal").ap()

    with ExitStack() as es:
        S = Sched(nc, es)

        def sb(st, name, shape, dt):
            _UNIQ[0] += 1
            actual = "%s__%d" % (name, _UNIQ[0])
            _BASE[actual] = name
            return st.enter_context(nc.sbuf_tensor(actual, shape, dt))

        def ps(st, name, shape, dt):
            S.excl.add(name)
            _UNIQ[0] += 1
            actual = "%s__%d" % (name, _UNIQ[0])
            _BASE[actual] = name
            return st.enter_context(nc.psum_tensor(actual, shape, dt))

        X = sb(es, "X", [128, NT, D], F32)
        hT = sb(es, "hT", [128, 8, SEQ], BF16)
        identf = sb(es, "identf", [128, 128], F32)
        identb = sb(es, "identb", [128, 128], BF16)
        onesb = sb(es, "onesb", [128, 128], BF16)
        onesf = sb(es, "onesf", [128, 128], F32)
        cols = sb(es, "cols", [128, NL, 6, 8, NB], F32)
        GG = sb(es, "GG", [128, D], F32)
        for nm, t in (("identf", identf), ("identb", identb), ("onesb", onesb), ("onesf", onesf)):
            S.dma(t[:], dr[nm], writes=[nm])

        with ExitStack() as ph:
            condT = sb(ph, "condT", [128, 8, NB], F32)
            modT = sb(ph, "modT", [128, 48, NB], F32)
            modb = sb(ph, "modb", [128, 48], F32)
            gvec = sb(ph, "gvec", [128, 4, 8], F32)
            mw = [sb(ph, "mw%d" % i, [128, 8, 512], F32) for i in range(2)]
            pm = ps(ph, "pm", [128, 512], F32)
            S.dma(condT[:], dr["cT"], writes=["condT"])
            S.op("act", lambda e: e.activation(out=condT[:], in_=condT[:], func=AF.Silu),
                 reads=["condT"], writes=["condT"])
            for l in range(nl):
                S.dma(modb[:], dr["modb"][l], writes=["modb"])
                S.dma(gvec[:], dr["gvec"][l].rearrange("w p c -> p w c"), writes=["gvec"])
                for j in range(12):
                    buf = mw[j % 2]
                    S.dma(buf[:], dr["modw"][l, j], writes=["mw%d" % (j % 2)])
                    for f4 in range(4):
                        fc = j * 4 + f4
                        for k in range(8):
                            S.op("pe", lambda e, buf=buf, f4=f4, k=k, fc=fc: e.matmul(
                                pm[:, fc * NB:(fc + 1) * NB], lhsT=buf[:, k, f4 * 128:(f4 + 1) * 128],
                                rhs=condT[:, k, :], start=(k == 0), stop=(k == 7)),
                                reads=["mw%d" % (j % 2), "condT"], writes=["pm"])
                S.op("dve", lambda e: e.tensor_tensor(
                    out=modT[:], in0=pm[:, 0:48 * NB].rearrange("p (f b) -> p f b", b=NB),
                    in1=modb[:].unsqueeze(2).to_broadcast([128, 48, NB]), op=ALU.add),
                    reads=["pm", "modb"], writes=["modT"])
                def gb(w):
                    return gvec[:, w, :].unsqueeze(2).to_broadcast([128, 8, NB])
                for (dst, sc0, gw) in ((0, 8, 0), (3, 32, 2)):
                    S.op("dve", lambda e, dst=dst, sc0=sc0, gw=gw: e.scalar_tensor_tensor(
                        out=cols[:, l, dst], in0=modT[:, sc0:sc0 + 8, :], scalar=1.0, in1=gb(gw),
                        op0=ALU.add, op1=ALU.mult), reads=["modT", "gvec"], writes=["cols"])
                for (dst, sh0) in ((1, 0), (4, 24)):
                    S.op("dve", lambda e, dst=dst, sh0=sh0: e.tensor_copy(
                        out=cols[:, l, dst], in_=modT[:, sh0:sh0 + 8, :]), reads=["modT"], writes=["cols"])
                for (dst, g0, gw) in ((2, 16, 1), (5, 40, 3)):
                    S.op("dve", lambda e, dst=dst, g0=g0, gw=gw: e.tensor_tensor(
                        out=cols[:, l, dst], in0=modT[:, g0:g0 + 8, :], in1=gb(gw), op=ALU.mult),
                        reads=["modT", "gvec"], writes=["cols"])
            dbg(S, "cols", cols[:].rearrange("p l s c b -> p (l s c b)"), [128, NL * 6 * 8 * NB], F32, ["cols"])
            S.end_phase()

        def rstd_from_ss(ss, n, width):
            S.op("dve", lambda e: e.tensor_scalar(out=ss[:, 0:width], in0=ss[:, 0:width], scalar1=1.0 / n,
                                                  scalar2=EPS, op0=ALU.mult, op1=ALU.add),
                 reads=[_nm(ss)], writes=[_nm(ss)])
            S.op("act", lambda e: e.activation(out=ss[:, 0:width], in_=ss[:, 0:width], func=AF.Sqrt),
                 reads=[_nm(ss)], writes=[_nm(ss)])
            S.op("dve", lambda e: e.reciprocal(out=ss[:, 0:width], in_=ss[:, 0:width]),
                 reads=[_nm(ss)], writes=[_nm(ss)])

        def norm_to_hT(ph, l, b, ca, cb):
            ss = sb(ph, "n_ss", [128, NT], F32)
            junk = sb(ph, "n_junk", [128, D], BF16)
            xn = [sb(ph, "n_xn%d" % i, [128, D], BF16) for i in range(2)]
            pt = [ps(ph, "n_pt%d" % i, [128, 1024], BF16) for i in range(2)]
            for t in range(NT):
                S.op("act", lambda e, t=t: e.activation(out=junk[:], in_=X[:, t, :], func=AF.Square,
                                                        accum_out=ss[:, t:t + 1]),
                     reads=[("X", t)], writes=["n_junk", "n_ss"])
            rstd_from_ss(ss, float(D), NT)
            for t in range(NT):
                xb = xn[t % 2]
                pp = pt[t % 2]
                S.op("dve", lambda e, t=t, xb=xb: e.tensor_scalar(out=xb[:], in0=X[:, t, :],
                                                               scalar1=ss[:, t:t + 1], scalar2=None, op0=ALU.mult),
                     reads=[("X", t), "n_ss"], writes=[_nm(xb)])
                for c in range(8):
                    S.op("pe", lambda e, c=c, xb=xb, pp=pp: e.transpose(
                        out=pp[:, c * 128:(c + 1) * 128], in_=xb[:, c * 128:(c + 1) * 128], identity=identb[:]),
                        reads=[_nm(xb), "identb"], writes=[_nm(pp)])
                for c in range(8):
                    eng = "act" if c % 2 else "dve"
                    if eng == "dve":
                        S.op("dve", lambda e, c=c, t=t, pp=pp: e.tensor_scalar(
                            out=hT[:, c, t * 128:(t + 1) * 128], in0=pp[:, c * 128:(c + 1) * 128],
                            scalar1=cols[:, l, ca, c, b:b + 1], scalar2=cols[:, l, cb, c, b:b + 1],
                            op0=ALU.mult, op1=ALU.add), reads=[_nm(pp), "cols"], writes=[("hT", t)])
                    else:
                        S.op("act", lambda e, c=c, t=t, pp=pp: e.activation(
                            out=hT[:, c, t * 128:(t + 1) * 128], in_=pp[:, c * 128:(c + 1) * 128],
                            func=AF.Identity, scale=cols[:, l, ca, c, b:b + 1], bias=cols[:, l, cb, c, b:b + 1]),
                            reads=[_nm(pp), "cols"], writes=[("hT", t)])

        def make_GG(ph, l, b, cg):
            dg = sb(ph, "gg_diag", [128, 128], F32)
            pg = [ps(ph, "gg_ps%d" % i, [128, 512], F32) for i in range(2)]
            for c in range(8):
                S.op("dve", lambda e, c=c: e.tensor_scalar(out=dg[:], in0=identf[:], scalar1=cols[:, l, cg, c, b:b + 1],
                                                        scalar2=None, op0=ALU.mult),
                     reads=["identf", "cols"], writes=["gg_diag"])
                S.op("pe", lambda e, c=c: e.matmul(pg[c // 4][:, (c % 4) * 128:(c % 4 + 1) * 128], lhsT=onesf[:],
                                                   rhs=dg[:], start=True, stop=True),
                     reads=["gg_diag", "onesf"], writes=[_nm(pg[c // 4])])
            for i in range(2):
                S.op("act", lambda e, i=i: e.copy(out=GG[:, i * 512:(i + 1) * 512], in_=pg[i][:]),
                     reads=[_nm(pg[i])], writes=["GG"])

        def resid_update(ph_tiles, t, py):
            ssy, junk, tmp = ph_tiles
            for i in range(2):
                S.op("act", lambda e, i=i: e.activation(out=junk[:, 0:512], in_=py[i][:], func=AF.Square,
                                                        accum_out=ssy[:, i:i + 1]),
                     reads=[_nm(py[i])], writes=[_nm(junk), _nm(ssy)])
            S.op("dve", lambda e: e.tensor_tensor(out=ssy[:, 2:3], in0=ssy[:, 0:1], in1=ssy[:, 1:2], op=ALU.add),
                 reads=[_nm(ssy)], writes=[_nm(ssy)])
            S.op("dve", lambda e: e.tensor_scalar(out=ssy[:, 2:3], in0=ssy[:, 2:3], scalar1=1.0 / D, scalar2=EPS,
                                                  op0=ALU.mult, op1=ALU.add), reads=[_nm(ssy)], writes=[_nm(ssy)])
            S.op("act", lambda e: e.activation(out=ssy[:, 2:3], in_=ssy[:, 2:3], func=AF.Sqrt),
                 reads=[_nm(ssy)], writes=[_nm(ssy)])
            S.op("dve", lambda e: e.reciprocal(out=ssy[:, 2:3], in_=ssy[:, 2:3]), reads=[_nm(ssy)], writes=[_nm(ssy)])
            for i in range(2):
                S.op("dve", lambda e, i=i: e.scalar_tensor_tensor(
                    out=tmp[:, i * 512:(i + 1) * 512], in0=py[i][:], scalar=ssy[:, 2:3],
                    in1=GG[:, i * 512:(i + 1) * 512], op0=ALU.mult, op1=ALU.mult),
                    reads=[_nm(py[i]), _nm(ssy), "GG"], writes=[_nm(tmp)])
            if t == 8:
                dbg(S, "r_ssy", ssy[:], [128, 4], F32, [_nm(ssy)])
                dbg(S, "r_tmp", tmp[:], [128, D], F32, [_nm(tmp)])
            S.op("dve", lambda e: e.tensor_tensor(out=X[:, t, :], in0=X[:, t, :], in1=tmp[:], op=ALU.add),
                 reads=[_nm(tmp), ("X", t)], writes=[("X", t)])

        if do_mix and ssm:
            for l in range(nl):
                ssm_tables(nc, S, dr, l, tab, tabb, sb, ps, identf)

        for b in range(nb):
            with ExitStack() as ph:
                for t in range(NT):
                    S.dma(X[:, t, :], dr["x"][b, t * 128:(t + 1) * 128, :], writes=[("X", t)])
                S.end_phase()
            for l in range(nl):
                if do_mix:
                    mixer(nc, S, dr, l, b, X, hT, GG, cols, tab, tabb, sb, ps, norm_to_hT, make_GG, resid_update,
                          rstd_from_ss, identb, identf, onesb, att, ssm, pool)
                if do_ffn:
                    ffn(nc, S, dr, l, b, X, hT, GG, sb, ps, norm_to_hT, make_GG, resid_update)
            with ExitStack() as ph:
                for t in range(NT):
                    S.dma(out[b, t * 128:(t + 1) * 128, :], X[:, t, :], reads=[("X", t)])
                S.end_phase()
        print("instructions:", S.ninst)
    return nc


DBG = {"on": False, "nc": None, "outs": {}}


def dbg(S, name, ap, shape, dt, reads, st=None):
    if not DBG["on"] or name in DBG["outs"]:
        return
    nc = DBG["nc"]
    d = nc.dram_tensor("dbg_" + name, list(shape), F32, kind="ExternalOutput").ap()
    DBG["outs"][name] = d
    if dt != F32:
        tmpf = st.enter_context(nc.sbuf_tensor("dbgt_" + name, list(shape), F32))
        S.op("dve", lambda e: e.tensor_copy(out=tmpf[:], in_=ap), reads=reads, writes=["dbgt_" + name])
        S.dma(d, tmpf[:], reads=["dbgt_" + name])
    else:
        S.dma(d, ap, reads=reads)


def cast_load(S, dst, src, wname):
    S.dma(dst, src, writes=[wname], q="pool")


def ffn(nc, S, dr, l, b, X, hT, GG, sb, ps, norm_to_hT, make_GG, resid_update):
    with ExitStack() as ph:
        norm_to_hT(ph, l, b, 3, 4)
        make_GG(ph, l, b, 5)
        dbg(S, "f_hT", hT[:].rearrange("p c t -> p (c t)"), [128, 8 * SEQ], BF16, [("hT", t) for t in range(NT)], ph)
        dbg(S, "f_GG", GG[:], [128, D], F32, ["GG"])
        S.end_phase()
    with ExitStack() as ph:
        wd = sb(ph, "f_wd", [128, 16, D], BF16)
        m = sb(ph, "f_m", [128, 16, 1024], BF16)
        cw = sb(ph, "f_cw", [128, 3, 32], F32)
        cbt = sb(ph, "f_cb", [128, 32], F32)
        halo = sb(ph, "f_halo", [128, 32, 2], F32)
        wu = [sb(ph, "f_wu%d" % i, [128, 8, 128], BF16) for i in range(3)]
        ub = [sb(ph, "f_ub%d" % i, [128, 1026], F32) for i in range(2)]
        acc = [sb(ph, "f_acc%d" % i, [128, 1024], F32) for i in range(2)]
        gl = sb(ph, "f_gl", [128, 1024], F32)
        ssy = sb(ph, "f_ssy", [128, 4], F32)
        junk = sb(ph, "f_junk", [128, 512], BF16)
        tmp = sb(ph, "f_tmp", [128, D], F32)
        pu = [ps(ph, "f_pu%d" % i, [128, 512], F32) for i in range(4)]
        py = [ps(ph, "f_py%d" % i, [128, 512], F32) for i in range(4)]
        S.dma(cw[:], dr["convw"][l], writes=["f_cw"])
        S.dma(cbt[:], dr["convb"][l], writes=["f_cb"])
        for k in range(16):
            cast_load(S, wd[:, k, :], dr["wdown"][l][:, k, :], ("f_wd", k))
        nload = 0
        for half in range(2):
            t0 = half * 1024
            for cc in range(16):
                for gv in range(2):
                    j = gv * 16 + cc
                    wb = wu[nload % 3]
                    nload += 1
                    cast_load(S, wb[:], dr["wup"][l, j], _nm(wb))
                    pp = pu[(gv * 2):(gv * 2 + 2)]
                    for s2 in range(2):
                        for k in range(8):
                            S.op("pe", lambda e, wb=wb, k=k, s2=s2, pp=pp: e.matmul(
                                pp[s2][:], lhsT=wb[:, k, :], rhs=hT[:, k, t0 + s2 * 512:t0 + (s2 + 1) * 512],
                                start=(k == 0), stop=(k == 7)),
                                reads=[_nm(wb)] + [("hT", (t0 + s2 * 512) // 128 + q) for q in range(4)],
                                writes=[_nm(pp[s2])])
                    u = ub[gv]
                    a = acc[gv]
                    dbg(S, "f_wb", wb[:].rearrange("p k n -> p (k n)"), [128, 1024], BF16, [_nm(wb)], ph)
                    if half == 0:
                        S.op("dve", lambda e, u=u: e.memset(u[:, 0:2], 0.0), writes=[_nm(u)])
                    else:
                        S.op("dve", lambda e, u=u, j=j: e.tensor_copy(out=u[:, 0:2], in_=halo[:, j, :]),
                             reads=["f_halo"], writes=[_nm(u)])
                    for s2 in range(2):
                        S.op("act", lambda e, u=u, s2=s2, pp=pp: e.copy(out=u[:, 2 + s2 * 512:2 + (s2 + 1) * 512],
                                                                        in_=pp[s2][:]),
                             reads=[_nm(pp[s2])], writes=[_nm(u)])
                    if half == 0:
                        S.op("dve", lambda e, u=u, j=j: e.tensor_copy(out=halo[:, j, :], in_=u[:, 1024:1026]),
                             reads=[_nm(u)], writes=["f_halo"])
                    dbg(S, "f_u", u[:], [128, 1026], F32, [_nm(u)], ph)
                    S.op("dve", lambda e, u=u, a=a, j=j: e.tensor_scalar(
                        out=a[:], in0=u[:, 2:1026], scalar1=cw[:, 2, j:j + 1], scalar2=cbt[:, j:j + 1],
                        op0=ALU.mult, op1=ALU.add), reads=[_nm(u), "f_cw", "f_cb"], writes=[_nm(a)])
                    S.op("dve", lambda e, u=u, a=a, j=j: e.scalar_tensor_tensor(
                        out=a[:], in0=u[:, 1:1025], scalar=cw[:, 1, j:j + 1], in1=a[:], op0=ALU.mult, op1=ALU.add),
                        reads=[_nm(u), "f_cw"], writes=[_nm(a)])
                    S.op("dve", lambda e, u=u, a=a, j=j: e.scalar_tensor_tensor(
                        out=a[:], in0=u[:, 0:1024], scalar=cw[:, 0, j:j + 1], in1=a[:], op0=ALU.mult, op1=ALU.add),
                        reads=[_nm(u), "f_cw"], writes=[_nm(a)])
                dbg(S, "f_acc0", acc[0][:], [128, 1024], F32, [_nm(acc[0])], ph)
                dbg(S, "f_acc1", acc[1][:], [128, 1024], F32, [_nm(acc[1])], ph)
                S.op("act", lambda e: e.activation(out=gl[:], in_=acc[0][:], func=AF.Gelu_apprx_tanh),
                     reads=[_nm(acc[0])], writes=["f_gl"])
                S.op("dve", lambda e, cc=cc: e.tensor_tensor(out=m[:, cc, :], in0=gl[:], in1=acc[1][:], op=ALU.mult),
                     reads=["f_gl", _nm(acc[1])], writes=[("f_m", cc)])
            for tt in range(8):
                t = half * 8 + tt
                pyy = py[(tt % 2) * 2:(tt % 2) * 2 + 2]
                for i in range(2):
                    for k in range(16):
                        S.op("pe", lambda e, i=i, k=k, tt=tt, pyy=pyy: e.matmul(
                            pyy[i][:], lhsT=m[:, k, tt * 128:(tt + 1) * 128], rhs=wd[:, k, i * 512:(i + 1) * 512],
                            start=(k == 0), stop=(k == 15)),
                            reads=[("f_m", k), ("f_wd", k)], writes=[_nm(pyy[i])])
                resid_update((ssy, junk, tmp), t, pyy)
        S.end_phase()


def ssm_tables(nc, S, dr, l, tab, tabb, sb, ps, identf):
    with ExitStack() as ph:
        are = sb(ph, "t_are", [128, 16], F32)
        aim = sb(ph, "t_aim", [128, 16], F32)
        stp = sb(ph, "t_stp", [128, 16], F32)
        th = sb(ph, "t_th", [128, 16], F32)
        r = sb(ph, "t_r", [128, 16], F32)
        sn = sb(ph, "t_sn", [128, 16], F32)
        cs = sb(ph, "t_cs", [128, 16], F32)
        t1 = sb(ph, "t_t1", [128, 16], F32)
        t2 = sb(ph, "t_t2", [128, 16], F32)
        lre = sb(ph, "t_lre", [128, 16], F32)
        lim = sb(ph, "t_lim", [128, 16], F32)
        ire = sb(ph, "t_ire", [128, 16], F32)
        iim = sb(ph, "t_iim", [128, 16], F32)
        pr = sb(ph, "t_pr", [128, 16], F32)
        pi = sb(ph, "t_pi", [128, 16], F32)
        cfr = sb(ph, "t_cfr", [128, 16], F32)
        cfi = sb(ph, "t_cfi", [128, 16], F32)
        Ere = sb(ph, "t_Ere", [128, 16, 256], F32)
        Eim = sb(ph, "t_Eim", [128, 16, 256], F32)
        ta = sb(ph, "t_ta", [128, 16, 128], F32)
        tb = sb(ph, "t_tb", [128, 16, 128], F32)
        sgn = sb(ph, "t_sgn", [128, 2], F32)
        outc = sb(ph, "t_outc", [128, 64], F32)
        halfpi = sb(ph, "t_hpi", [128, 1], F32)
        R = ["t"]

        def dv(fn, w=("t",)):
            S.op("dve", fn, reads=R, writes=list(w))

        S.dma(are[:], dr["are"][l], writes=R)
        S.dma(aim[:], dr["aim"][l], writes=R)
        S.dma(stp[:], dr["lstep"][l], writes=R)
        S.dma(sgn[:], dr["sgn"], writes=R)
        S.op("act", lambda e: e.activation(out=stp[:], in_=stp[:], func=AF.Exp), reads=R, writes=R)
        dv(lambda e: e.tensor_tensor(out=th[:], in0=aim[:], in1=stp[:], op=ALU.mult))
        dv(lambda e: e.tensor_tensor(out=t1[:], in0=are[:], in1=stp[:], op=ALU.mult))
        S.op("act", lambda e: e.activation(out=r[:], in_=t1[:], func=AF.Exp), reads=R, writes=R)
        dv(lambda e: e.memset(halfpi[:], float(np.pi / 2)))
        S.op("act", lambda e: e.activation(out=sn[:], in_=th[:], func=AF.Sin, scale=1.0 / 16), reads=R, writes=R)
        S.op("act", lambda e: e.activation(out=cs[:], in_=th[:], func=AF.Sin, scale=1.0 / 16, bias=halfpi[:]),
             reads=R, writes=R)
        for _ in range(4):
            dv(lambda e: e.tensor_tensor(out=t1[:], in0=sn[:], in1=cs[:], op=ALU.mult))
            dv(lambda e: e.tensor_tensor(out=t2[:], in0=sn[:], in1=sn[:], op=ALU.mult))
            dv(lambda e: e.tensor_scalar(out=sn[:], in0=t1[:], scalar1=2.0, scalar2=None, op0=ALU.mult))
            dv(lambda e: e.tensor_scalar(out=cs[:], in0=t2[:], scalar1=-2.0, scalar2=1.0, op0=ALU.mult, op1=ALU.add))
        dv(lambda e: e.tensor_tensor(out=lre[:], in0=r[:], in1=cs[:], op=ALU.mult))
        dv(lambda e: e.tensor_tensor(out=lim[:], in0=r[:], in1=sn[:], op=ALU.mult))
        dv(lambda e: e.reciprocal(out=t1[:], in_=r[:]))
        dv(lambda e: e.tensor_tensor(out=ire[:], in0=t1[:], in1=cs[:], op=ALU.mult))
        dv(lambda e: e.tensor_tensor(out=iim[:], in0=t1[:], in1=sn[:], op=ALU.mult))
        dv(lambda e: e.tensor_scalar(out=iim[:], in0=iim[:], scalar1=-1.0, scalar2=None, op0=ALU.mult))
        dv(lambda e: e.tensor_tensor(out=t1[:], in0=are[:], in1=are[:], op=ALU.mult))
        dv(lambda e: e.tensor_tensor(out=t2[:], in0=aim[:], in1=aim[:], op=ALU.mult))
        dv(lambda e: e.tensor_tensor(out=t1[:], in0=t1[:], in1=t2[:], op=ALU.add))
        dv(lambda e: e.reciprocal(out=t1[:], in_=t1[:]))
        dv(lambda e: e.tensor_scalar(out=t2[:], in0=lre[:], scalar1=-1.0, scalar2=None, op0=ALU.add))
        dv(lambda e: e.tensor_tensor(out=cfr[:], in0=t2[:], in1=are[:], op=ALU.mult))
        dv(lambda e: e.tensor_tensor(out=pr[:], in0=lim[:], in1=aim[:], op=ALU.mult))
        dv(lambda e: e.tensor_tensor(out=cfr[:], in0=cfr[:], in1=pr[:], op=ALU.add))
        dv(lambda e: e.tensor_tensor(out=cfr[:], in0=cfr[:], in1=t1[:], op=ALU.mult))
        dv(lambda e: e.tensor_tensor(out=cfi[:], in0=lim[:], in1=are[:], op=ALU.mult))
        dv(lambda e: e.tensor_tensor(out=pr[:], in0=t2[:], in1=aim[:], op=ALU.mult))
        dv(lambda e: e.tensor_tensor(out=cfi[:], in0=cfi[:], in1=pr[:], op=ALU.subtract))
        dv(lambda e: e.tensor_tensor(out=cfi[:], in0=cfi[:], in1=t1[:], op=ALU.mult))

        def power_table(bre_, bim_, nlev):
            dv(lambda e: e.memset(Ere[:, :, 0:1], 1.0))
            dv(lambda e: e.memset(Eim[:, :, 0:1], 0.0))
            dv(lambda e: e.tensor_copy(out=pr[:], in_=bre_[:]))
            dv(lambda e: e.tensor_copy(out=pi[:], in_=bim_[:]))
            for k in range(nlev):
                n = 1 << k
                prb = pr[:].unsqueeze(2).to_broadcast([128, 16, n])
                pib = pi[:].unsqueeze(2).to_broadcast([128, 16, n])
                dv(lambda e, n=n, prb=prb: e.tensor_tensor(out=ta[:, :, 0:n], in0=Ere[:, :, 0:n], in1=prb, op=ALU.mult))
                dv(lambda e, n=n, pib=pib: e.tensor_tensor(out=tb[:, :, 0:n], in0=Eim[:, :, 0:n], in1=pib, op=ALU.mult))
                dv(lambda e, n=n: e.tensor_tensor(out=Ere[:, :, n:2 * n], in0=ta[:, :, 0:n], in1=tb[:, :, 0:n],
                                                  op=ALU.subtract))
                dv(lambda e, n=n, pib=pib: e.tensor_tensor(out=ta[:, :, 0:n], in0=Ere[:, :, 0:n], in1=pib, op=ALU.mult))
                dv(lambda e, n=n, prb=prb: e.tensor_tensor(out=tb[:, :, 0:n], in0=Eim[:, :, 0:n], in1=prb, op=ALU.mult))
                dv(lambda e, n=n: e.tensor_tensor(out=Eim[:, :, n:2 * n], in0=ta[:, :, 0:n], in1=tb[:, :, 0:n],
                                                  op=ALU.add))
                if k < nlev - 1:
                    dv(lambda e: e.tensor_tensor(out=t1[:], in0=pr[:], in1=pr[:], op=ALU.mult))
                    dv(lambda e: e.tensor_tensor(out=t2[:], in0=pi[:], in1=pi[:], op=ALU.mult))
                    dv(lambda e: e.tensor_tensor(out=pi[:], in0=pr[:], in1=pi[:], op=ALU.mult))
                    dv(lambda e: e.tensor_scalar(out=pi[:], in0=pi[:], scalar1=2.0, scalar2=None, op0=ALU.mult))
                    dv(lambda e: e.tensor_tensor(out=pr[:], in0=t1[:], in1=t2[:], op=ALU.subtract))

        power_table(lre, lim, 8)
        o_t1 = 2048
        o_t2 = 2048 + 2048
        o_c = 2048 + 4096
        S.dma(tab[l][:, o_t1:o_t1 + 2048].rearrange("p (g t) -> p g t", t=128), Ere[:, :, 0:128], reads=R)
        dv(lambda e: e.tensor_scalar(out=ta[:], in0=Eim[:, :, 0:128], scalar1=sgn[:, 0:1], scalar2=None, op0=ALU.mult))
        S.dma(tab[l][:, o_t2:o_t2 + 2048].rearrange("p (g t) -> p g t", t=128), ta[:], reads=R)
        dv(lambda e: e.tensor_copy(out=outc[:, 0:16], in_=Ere[:, :, 128]))
        dv(lambda e: e.tensor_scalar(out=outc[:, 16:32], in0=Eim[:, :, 128], scalar1=sgn[:, 0:1], scalar2=None,
                                     op0=ALU.mult))
        dv(lambda e: e.tensor_scalar(out=outc[:, 32:48], in0=Eim[:, :, 128], scalar1=sgn[:, 1:2], scalar2=None,
                                     op0=ALU.mult))
        dv(lambda e: e.memset(outc[:, 48:64], 0.0))
        S.dma(tab[l][:, o_c:o_c + 64], outc[:], reads=R)
        S.end_phase()
        power_table(ire, iim, 7)
        ptr = [ps(ph, "t_ptr%d" % i, [128, 512], F32) for i in range(4)]
        arai = sb(ph, "t_arai", [128, 2, 16, 64], F32)
        for ri, E in ((0, Ere), (1, Eim)):
            for g in range(16):
                S.op("pe", lambda e, g=g, E=E, ri=ri: e.transpose(
                    out=ptr[ri * 2 + g // 8][:, (g % 8) * 64:(g % 8 + 1) * 64], in_=E[0:64, g, 0:128],
                    identity=identf[0:64, 0:64]), reads=R + ["identf"], writes=["t_ptr"])
            for hh in range(2):
                S.op("act", lambda e, hh=hh, ri=ri: e.copy(
                    out=arai[:, ri, hh * 8:(hh + 1) * 8, :].rearrange("p g n -> p (g n)"), in_=ptr[ri * 2 + hh][:]),
                    reads=["t_ptr"], writes=["t_arai"])
        S.dma(tab[l][:, 0:2048], arai[:].rearrange("p r g n -> p (r g n)"), reads=["t_arai"])
        bre = sb(ph, "t_bre", [64, 16, 16], F32)
        bim = sb(ph, "t_bim", [64, 16, 16], F32)
        Bre = sb(ph, "t_Bre", [64, 16, 16], F32)
        Bim = sb(ph, "t_Bim", [64, 16, 16], F32)
        tq = sb(ph, "t_tq", [64, 16, 16], F32)
        bmask = sb(ph, "t_bmask", [128, 8], F32)
        BT = sb(ph, "t_BT", [128, 2, 128], F32)
        Bblk = sb(ph, "t_Bblk", [128, 2, 8, 128], BF16)
        S.dma(bre[:], dr["bre"][l], writes=R)
        S.dma(bim[:], dr["bim"][l], writes=R)
        S.dma(bmask[:], dr["bmask"], writes=R)
        cfrb = cfr[0:64, :].unsqueeze(2).to_broadcast([64, 16, 16])
        cfib = cfi[0:64, :].unsqueeze(2).to_broadcast([64, 16, 16])
        dv(lambda e: e.tensor_tensor(out=Bre[:], in0=bre[:], in1=cfrb, op=ALU.mult))
        dv(lambda e: e.tensor_tensor(out=tq[:], in0=bim[:], in1=cfib, op=ALU.mult))
        dv(lambda e: e.tensor_tensor(out=Bre[:], in0=Bre[:], in1=tq[:], op=ALU.subtract))
        dv(lambda e: e.tensor_tensor(out=Bim[:], in0=bim[:], in1=cfrb, op=ALU.mult))
        dv(lambda e: e.tensor_tensor(out=tq[:], in0=bre[:], in1=cfib, op=ALU.mult))
        dv(lambda e: e.tensor_tensor(out=Bim[:], in0=Bim[:], in1=tq[:], op=ALU.add))
        for half in range(2):
            for ri, Bsrc in ((0, Bre), (1, Bim)):
                S.op("pe", lambda e, half=half, ri=ri, Bsrc=Bsrc: e.transpose(
                    out=ptr[0][:, (half * 2 + ri) * 64:(half * 2 + ri + 1) * 64],
                    in_=Bsrc[:, half * 8:(half + 1) * 8, :].rearrange("n g p -> n (g p)"),
                    identity=identf[0:64, 0:64]), reads=R + ["identf", "t_arai"], writes=["t_ptr0b"])
        S.op("act", lambda e: e.copy(out=BT[:].rearrange("p h c -> p (h c)"), in_=ptr[0][:, 0:256]),
             reads=["t_ptr0b"], writes=R)
        for half in range(2):
            S.op("dve", lambda e, half=half: e.tensor_tensor(
                out=Bblk[:, half], in0=BT[:, half, :].unsqueeze(1).to_broadcast([128, 8, 128]),
                in1=bmask[:].unsqueeze(2).to_broadcast([128, 8, 128]), op=ALU.mult), reads=R, writes=R)
        S.dma(tabb[l][:, 0:2048], Bblk[:].rearrange("p h g c -> p (h g c)"), reads=R)
        Cm = sb(ph, "t_Cm", [128, 16, 16], F32)
        mask2 = sb(ph, "t_mask2", [128, 16, 8], F32)
        Cpad = sb(ph, "t_Cpad", [128, 16, 8, 16], BF16)
        S.dma(Cm[0:64], dr["cre"][l], writes=R)
        S.dma(Cm[64:128], dr["cim"][l], writes=R)
        S.dma(mask2[:], dr["mask2"], writes=R)
        dv(lambda e: e.tensor_scalar(out=Cm[64:128], in0=Cm[64:128], scalar1=-1.0, scalar2=None, op0=ALU.mult))
        dv(lambda e: e.tensor_tensor(out=Cpad[:], in0=Cm[:].unsqueeze(2).to_broadcast([128, 16, 8, 16]),
                                     in1=mask2[:].unsqueeze(3).to_broadcast([128, 16, 8, 16]), op=ALU.mult))
        S.dma(tabb[l][:, 2048:4096], Cpad[:].rearrange("p g a b -> p (g a b)"), reads=R)
        S.end_phase()


def mixer(nc, S, dr, l, b, X, hT, GG, cols, tab, tabb, sb, ps, norm_to_hT, make_GG, resid_update, rstd_from_ss,
          identb, identf, onesb, att, ssm, pool):
    with ExitStack() as ph:
        norm_to_hT(ph, l, b, 0, 1)
        make_GG(ph, l, b, 2)
        S.end_phase()
    with ExitStack() as mx:
        yaT = sb(mx, "yaT", [128, 4, SEQ], BF16)
        if att:
            attention(nc, S, dr, l, b, hT, yaT, sb, ps, rstd_from_ss, identb, onesb)
        else:
            S.op("dve", lambda e: e.memset(yaT[:], 0.0), writes=["yaT"])
            S.end_phase()
        ysT = sb(mx, "ysT", [128, 2, SEQ], BF16)
        ypT = sb(mx, "ypT", [128, 2, SEQ], BF16)
        if ssm:
            ssm_branch(nc, S, dr, l, b, hT, ysT, tab, tabb, sb, ps)
        else:
            S.op("dve", lambda e: e.memset(ysT[:], 0.0), writes=["ysT"])
            S.end_phase()
        if pool:
            pool_branch(nc, S, dr, l, b, hT, ypT, sb, ps)
        else:
            S.op("dve", lambda e: e.memset(ypT[:], 0.0), writes=["ypT"])
            S.end_phase()
        merge(nc, S, dr, l, b, X, hT, GG, yaT, ysT, ypT, sb, ps, resid_update)


def merge(nc, S, dr, l, b, X, hT, GG, yaT, ysT, ypT, sb, ps, resid_update):
    with ExitStack() as ph:
        wg = [sb(ph, "m_wg%d" % i, [128, 8, 128], BF16) for i in range(4)]
        pa = sb(ph, "m_pa", [128, 4, D], BF16)
        pb = sb(ph, "m_pb", [128, 2, D], BF16)
        pc = sb(ph, "m_pc", [128, 2, D], BF16)
        wo = sb(ph, "m_wo", [128, 8, D], BF16)
        mT = sb(ph, "m_mT", [128, 8, 1024], BF16)
        sg = [sb(ph, "m_sg%d" % i, [128, 512], F32) for i in range(2)]
        tm = sb(ph, "m_tm", [128, 512], F32)
        ac = sb(ph, "m_ac", [128, 512], F32)
        ssy = sb(ph, "m_ssy", [128, 4], F32)
        junk = sb(ph, "m_junk", [128, 512], BF16)
        tmp = sb(ph, "m_tmp", [128, D], F32)
        pg = [ps(ph, "m_pg%d" % i, [128, 512], F32) for i in range(2)]
        pp = [ps(ph, "m_pp%d" % i, [128, 512], F32) for i in range(2)]
        py = [ps(ph, "m_py%d" % i, [128, 512], F32) for i in range(4)]
        for k in range(4):
            cast_load(S, pa[:, k, :], dr["pa"][l][:, k, :], "m_pa")
        cast_load(S, pb[:], dr["pb"][l], "m_pb")
        cast_load(S, pc[:], dr["pc"][l], "m_pc")
        for k in range(8):
            cast_load(S, wo[:, k, :], dr["wout"][l][:, k, :], "m_wo")
        brs = ((yaT, pa, 4, "yaT", "m_pa"), (ysT, pb, 2, "ysT", "m_pb"), (ypT, pc, 2, "ypT", "m_pc"))
        nl_ = 0
        ng = 0
        for half in range(2):
            for dc in range(8):
                wgs = []
                for br in range(3):
                    wb = wg[nl_ % 4]
                    nl_ += 1
                    cast_load(S, wb[:], dr["wg"][l, br * 8 + dc], _nm(wb))
                    wgs.append(wb)
                for s2 in range(2):
                    t0 = half * 1024 + s2 * 512
                    hreads = [("hT", t0 // 128 + q) for q in range(4)]
                    for br in range(3):
                        yT, pw, nk, yn, pn = brs[br]
                        g_ = pg[ng % 2]
                        p_ = pp[ng % 2]
                        s_ = sg[ng % 2]
                        ng += 1
                        for k in range(8):
                            S.op("pe", lambda e, k=k, g_=g_, wb=wgs[br], t0=t0: e.matmul(
                                g_[:], lhsT=wb[:, k, :], rhs=hT[:, k, t0:t0 + 512], start=(k == 0), stop=(k == 7)),
                                reads=[_nm(wgs[br])] + hreads, writes=[_nm(g_)])
                        for k in range(nk):
                            S.op("pe", lambda e, k=k, p_=p_, pw=pw, yT=yT, t0=t0, nk=nk, dc=dc: e.matmul(
                                p_[:], lhsT=pw[:, k, dc * 128:(dc + 1) * 128], rhs=yT[:, k, t0:t0 + 512],
                                start=(k == 0), stop=(k == nk - 1)), reads=[pn, yn], writes=[_nm(p_)])
                        S.op("act", lambda e, g_=g_, s_=s_: e.activation(out=s_[:], in_=g_[:], func=AF.Sigmoid),
                             reads=[_nm(g_)], writes=[_nm(s_)])
                        if br == 0:
                            S.op("dve", lambda e, p_=p_, s_=s_: e.tensor_tensor(out=ac[:], in0=p_[:], in1=s_[:], op=ALU.mult),
                                 reads=[_nm(p_), _nm(s_)], writes=["m_ac"])
                        elif br == 1:
                            S.op("dve", lambda e, p_=p_, s_=s_: e.tensor_tensor(out=tm[:], in0=p_[:], in1=s_[:], op=ALU.mult),
                                 reads=[_nm(p_), _nm(s_)], writes=["m_tm"])
                            S.op("dve", lambda e: e.tensor_tensor(out=ac[:], in0=ac[:], in1=tm[:], op=ALU.add),
                                 reads=["m_tm"], writes=["m_ac"])
                        else:
                            S.op("dve", lambda e, p_=p_, s_=s_: e.tensor_tensor(out=tm[:], in0=p_[:], in1=s_[:], op=ALU.mult),
                                 reads=[_nm(p_), _nm(s_)], writes=["m_tm"])
                            S.op("dve", lambda e, dc=dc, s2=s2: e.tensor_tensor(
                                out=mT[:, dc, s2 * 512:(s2 + 1) * 512], in0=ac[:], in1=tm[:], op=ALU.add),
                                reads=["m_tm", "m_ac"], writes=[("m_mT", dc)])
            for tt in range(8):
                t = half * 8 + tt
                pyy = py[(tt % 2) * 2:(tt % 2) * 2 + 2]
                for i in range(2):
                    for k in range(8):
                        S.op("pe", lambda e, i=i, k=k, tt=tt, pyy=pyy: e.matmul(
                            pyy[i][:], lhsT=mT[:, k, tt * 128:(tt + 1) * 128], rhs=wo[:, k, i * 512:(i + 1) * 512],
                            start=(k == 0), stop=(k == 7)), reads=[("m_mT", k), "m_wo"], writes=[_nm(pyy[i])])
                resid_update((ssy, junk, tmp), t, pyy)
        S.end_phase()


def pool_branch(nc, S, dr, l, b, hT, ypT, sb, ps):
    with ExitStack() as ph:
        w1 = sb(ph, "p_w1", [128, 8, 256], BF16)
        wp = sb(ph, "p_wp", [128, 2, 128], F32)
        wpb = sb(ph, "p_wpb", [128, 2, 128], BF16)
        psc = sb(ph, "p_psc", [128, 2], F32)
        invt = sb(ph, "p_invt", [128, 16], F32)
        U = sb(ph, "p_U", [128, 16 + SEQ], F32)
        s_a = sb(ph, "p_sa", [128, 16 + SEQ], F32)
        s_b = sb(ph, "p_sb", [128, 16 + SEQ], F32)
        pl = sb(ph, "p_pl", [128, SEQ], BF16)
        fx = sb(ph, "p_fx", [128, 16], F32)
        pu = [ps(ph, "p_pu%d" % i, [128, 512], F32) for i in range(4)]
        cast_load(S, w1[:], dr["w1"][l][:, :, 680:936], "p_w1")
        S.dma(psc[:], dr["pscale"][l], writes=["p_psc"])
        S.dma(invt[:], dr["invt"], writes=["p_invt"])
        S.op("dve", lambda e: e.memset(wp[:], 0.0), writes=["p_wp"])
        for g in range(4):
            S.dma(wp[(g % 2) * 64:(g % 2 + 1) * 64, g // 2, (g % 2) * 64:(g % 2 + 1) * 64], dr["wpool"][l, g],
                  reads=[], writes=["p_wp"])
        S.op("dve", lambda e: e.tensor_copy(out=wpb[:], in_=wp[:]), reads=["p_wp"], writes=["p_wpb"])
        for buf in (U, s_a, s_b):
            S.op("dve", lambda e, buf=buf: e.memset(buf[:, 0:16], 0.0), writes=[_nm(buf)])
        for ct in range(2):
            for tc in range(4):
                for k in range(8):
                    S.op("pe", lambda e, k=k, tc=tc, ct=ct: e.matmul(
                        pu[tc][:], lhsT=w1[:, k, ct * 128:(ct + 1) * 128], rhs=hT[:, k, tc * 512:(tc + 1) * 512],
                        start=(k == 0), stop=(k == 7)), reads=["p_w1"] + [("hT", tc * 4 + q) for q in range(4)],
                        writes=[_nm(pu[tc])])
                S.op("act", lambda e, tc=tc: e.copy(out=U[:, 16 + tc * 512:16 + (tc + 1) * 512], in_=pu[tc][:]),
                     reads=[_nm(pu[tc])], writes=["p_U"])
            def shadd(dst, src, sh):
                S.op("dve", lambda e: e.tensor_tensor(out=dst[:, 16:16 + SEQ], in0=src[:, 16:16 + SEQ],
                                                      in1=src[:, 16 - sh:16 - sh + SEQ], op=ALU.add),
                     reads=[_nm(src)], writes=[_nm(dst)])
            shadd(s_a, U, 1)
            shadd(s_b, s_a, 2)
            if ct == 0:
                halves = ((0, s_a, 2), (64, s_b, 4))
            else:
                shadd(s_a, s_b, 4)
                shadd(s_b, s_a, 8)
                halves = ((0, s_a, 8), (64, s_b, 16))
            for (p0, sbuf, win) in halves:
                S.op("dve", lambda e, p0=p0, sbuf=sbuf, win=win: e.scalar_tensor_tensor(
                    out=pl[p0:p0 + 64, :], in0=sbuf[p0:p0 + 64, 16:16 + SEQ], scalar=1.0 / win,
                    in1=U[p0:p0 + 64, 16:16 + SEQ], op0=ALU.mult, op1=ALU.subtract),
                    reads=[_nm(sbuf), "p_U"], writes=["p_pl"])
                S.op("dve", lambda e, p0=p0, sbuf=sbuf, win=win: e.tensor_tensor(
                    out=fx[p0:p0 + 64, 0:win - 1], in0=sbuf[p0:p0 + 64, 16:16 + win - 1],
                    in1=invt[p0:p0 + 64, 0:win - 1], op=ALU.mult), reads=[_nm(sbuf), "p_invt"], writes=["p_fx"])
                S.op("dve", lambda e, p0=p0, win=win: e.tensor_tensor(
                    out=pl[p0:p0 + 64, 0:win - 1], in0=fx[p0:p0 + 64, 0:win - 1], in1=U[p0:p0 + 64, 16:16 + win - 1],
                    op=ALU.subtract), reads=["p_fx", "p_U"], writes=["p_pl"])
            for tc in range(4):
                S.op("pe", lambda e, tc=tc, ct=ct: e.matmul(pu[tc][:], lhsT=wpb[:, ct, :],
                                                         rhs=pl[:, tc * 512:(tc + 1) * 512], start=True, stop=True),
                     reads=["p_wpb", "p_pl"], writes=[_nm(pu[tc])])
                S.op("act", lambda e, tc=tc, ct=ct: e.activation(
                    out=ypT[:, ct, tc * 512:(tc + 1) * 512], in_=pu[tc][:], func=AF.Copy, scale=psc[:, ct:ct + 1]),
                    reads=[_nm(pu[tc]), "p_psc"], writes=["ypT"])
        S.end_phase()


def ssm_branch(nc, S, dr, l, b, hT, ysT, tab, tabb, sb, ps):
    with ExitStack() as ph:
        uT = sb(ph, "s_uT", [128, 2, SEQ], BF16)
        with ExitStack() as p1:
            w1 = sb(p1, "s_w1", [128, 8, 256], BF16)
            pj = [ps(p1, "s_pj%d" % i, [128, 512], F32) for i in range(2)]
            cast_load(S, w1[:], dr["w1"][l][:, :, 424:680], "s_w1")
            for ct in range(2):
                for tc in range(4):
                    pp = pj[tc % 2]
                    for k in range(8):
                        S.op("pe", lambda e, k=k, tc=tc, ct=ct, pp=pp: e.matmul(
                            pp[:], lhsT=w1[:, k, ct * 128:(ct + 1) * 128], rhs=hT[:, k, tc * 512:(tc + 1) * 512],
                            start=(k == 0), stop=(k == 7)), reads=["s_w1"] + [("hT", tc * 4 + q) for q in range(4)],
                            writes=[_nm(pp)])
                    S.op("act", lambda e, tc=tc, ct=ct, pp=pp: e.copy(out=uT[:, ct, tc * 512:(tc + 1) * 512], in_=pp[:]),
                         reads=[_nm(pp)], writes=["s_uT"])
            S.end_phase()
        zT = sb(ph, "s_zT", [128, 2, SEQ], BF16)
        arai = sb(ph, "s_arai", [128, 2, 16, 64], F32)
        T1 = sb(ph, "s_T1", [128, 16, 128], F32)
        T2 = sb(ph, "s_T2", [128, 16, 128], F32)
        pcs = sb(ph, "s_pcs", [128, 64], F32)
        Bblk = sb(ph, "s_Bblk", [128, 2, 1024], BF16)
        Cpad = sb(ph, "s_Cpad", [128, 16, 128], BF16)
        triub = sb(ph, "s_triub", [128, 128], BF16)
        dsk = sb(ph, "s_dsk", [128, 2], F32)
        bgl = sb(ph, "s_bgl", [128, 2], F32)
        wgl = sb(ph, "s_wgl", [128, 2, 256], BF16)
        t1s = [sb(ph, "s_t1%d" % i, [128, 4, 2, 64], BF16) for i in range(2)]
        t2s = [sb(ph, "s_t2%d" % i, [128, 4, 2, 64], BF16) for i in range(2)]
        v = sb(ph, "s_v", [128, 16, 2, 64], BF16)
        vsw = sb(ph, "s_vsw", [128, 16, 2, 64], BF16)
        xs = sb(ph, "s_xs", [128, 16, 128], BF16)
        xas = [sb(ph, "s_xa%d" % i, [128, 128], F32) for i in range(2)]
        xbs = [sb(ph, "s_xb%d" % i, [128, 128], F32) for i in range(2)]
        cA = [sb(ph, "s_cA%d" % i, [128, 16], F32) for i in range(2)]
        cB = [sb(ph, "s_cB%d" % i, [128, 16], F32) for i in range(2)]
        sA = sb(ph, "s_sA", [128, 16], F32)
        sB = sb(ph, "s_sB", [128, 16], F32)
        q1 = sb(ph, "s_q1", [128, 16], F32)
        yv = sb(ph, "s_yv", [128, 256], F32)
        sg = sb(ph, "s_sg", [128, 512], BF16)
        pbu = [ps(ph, "s_pbu%d" % i, [128, 512], F32) for i in range(2)]
        pA = [ps(ph, "s_pA%d" % i, [128, 512], F32) for i in range(2)]
        pB = [ps(ph, "s_pB%d" % i, [128, 512], F32) for i in range(2)]
        pyy = ps(ph, "s_py", [128, 512], F32)
        pg = ps(ph, "s_pg", [128, 512], F32)
        S.dma(arai[:].rearrange("p r g n -> p (r g n)"), tab[l][:, 0:2048], writes=["s_arai"])
        S.dma(T1[:].rearrange("p g t -> p (g t)"), tab[l][:, 2048:4096], writes=["s_T1"])
        S.dma(T2[:].rearrange("p g t -> p (g t)"), tab[l][:, 4096:6144], writes=["s_T2"])
        S.dma(pcs[:], tab[l][:, 6144:6208], writes=["s_pcs"])
        S.dma(Bblk[:].rearrange("p h c -> p (h c)"), tabb[l][:, 0:2048], writes=["s_Bblk"])
        S.dma(Cpad[:].rearrange("p g c -> p (g c)"), tabb[l][:, 2048:4096], writes=["s_Cpad"])
        S.dma(triub[:], dr["triub"], writes=["s_triub"])
        S.dma(dsk[:], dr["dskip"][l], writes=["s_dsk"])
        S.dma(bgl[:], dr["bglu"][l], writes=["s_bgl"])
        cast_load(S, wgl[:], dr["wglu"][l], "s_wgl")
        for i in range(2):
            S.op("dve", lambda e, i=i: e.memset(cA[i][:], 0.0), writes=[_nm(cA[i])])
            S.op("dve", lambda e, i=i: e.memset(cB[i][:], 0.0), writes=[_nm(cB[i])])
        Pr = pcs[:, 0:16]
        PiA = pcs[:, 16:32]
        PiB = pcs[:, 32:48]
        for t in range(NT):
            cAo, cBo = cA[t % 2], cB[t % 2]
            cAn, cBn = cA[(t + 1) % 2], cB[(t + 1) % 2]
            for half in range(2):
                for q2 in range(2):
                    S.op("pe", lambda e, half=half, q2=q2, t=t: e.matmul(
                        pbu[q2][:], lhsT=uT[:, half, t * 128:(t + 1) * 128], rhs=Bblk[:, half, q2 * 512:(q2 + 1) * 512],
                        start=True, stop=True), reads=["s_uT", "s_Bblk"], writes=[_nm(pbu[q2])])
                for q2 in range(2):
                    g0 = half * 8 + q2 * 4
                    t1 = t1s[q2]
                    t2 = t2s[q2]
                    bu = pbu[q2][:].rearrange("p (g r n) -> p g r n", g=4, r=2)
                    Ar = arai[:, 0, g0:g0 + 4, :]
                    Ai = arai[:, 1, g0:g0 + 4, :]
                    S.op("dve", lambda e, bu=bu, Ar=Ar: e.tensor_tensor(
                        out=t1[:], in0=bu, in1=Ar.unsqueeze(2).to_broadcast([128, 4, 2, 64]), op=ALU.mult),
                        reads=[_nm(pbu[q2]), "s_arai"], writes=[_nm(t1)])
                    S.op("dve", lambda e, bu=bu, Ai=Ai: e.tensor_tensor(
                        out=t2[:, :, 0, :], in0=bu[:, :, 1, :], in1=Ai, op=ALU.mult),
                        reads=[_nm(pbu[q2]), "s_arai"], writes=[_nm(t2)])
                    S.op("dve", lambda e, bu=bu, Ai=Ai: e.tensor_tensor(
                        out=t2[:, :, 1, :], in0=bu[:, :, 0, :], in1=Ai, op=ALU.mult),
                        reads=[_nm(pbu[q2]), "s_arai"], writes=[_nm(t2)])
                    S.op("pool", lambda e, g0=g0: e.tensor_tensor(
                        out=v[:, g0:g0 + 4, 0, :], in0=t1[:, :, 0, :], in1=t2[:, :, 0, :], op=ALU.subtract),
                        reads=[_nm(t1), _nm(t2)], writes=[("s_v", g0)])
                    S.op("pool", lambda e, g0=g0: e.tensor_tensor(
                        out=v[:, g0:g0 + 4, 1, :], in0=t1[:, :, 1, :], in1=t2[:, :, 1, :], op=ALU.add),
                        reads=[_nm(t1), _nm(t2)], writes=[("s_v", g0)])
                    S.op("pool", lambda e, g0=g0: e.tensor_tensor(
                        out=vsw[:, g0:g0 + 4, 1, :], in0=t1[:, :, 0, :], in1=t2[:, :, 0, :], op=ALU.subtract),
                        reads=[_nm(t1), _nm(t2)], writes=[("s_vsw", g0)])
                    S.op("pool", lambda e, g0=g0: e.tensor_tensor(
                        out=vsw[:, g0:g0 + 4, 0, :], in0=t1[:, :, 1, :], in1=t2[:, :, 1, :], op=ALU.add),
                        reads=[_nm(t1), _nm(t2)], writes=[("s_vsw", g0)])
                    pa_, pb_ = pA[q2], pB[q2]
                    for gg in range(4):
                        g = g0 + gg
                        S.op("pe", lambda e, g=g, gg=gg, pa_=pa_: e.matmul(
                            pa_[:, gg * 128:(gg + 1) * 128], lhsT=v[:, g].rearrange("p r n -> p (r n)"), rhs=triub[:],
                            start=True, stop=True), reads=[("s_v", g0), "s_triub"], writes=[_nm(pa_)])
                        S.op("pe", lambda e, g=g, gg=gg, pb_=pb_: e.matmul(
                            pb_[:, gg * 128:(gg + 1) * 128], lhsT=vsw[:, g].rearrange("p r n -> p (r n)"), rhs=triub[:],
                            start=True, stop=True), reads=[("s_vsw", g0), "s_triub"], writes=[_nm(pb_)])
                    A127 = pa_[:].rearrange("p (g t) -> p g t", t=128)[:, :, 127]
                    B127 = pb_[:].rearrange("p (g t) -> p g t", t=128)[:, :, 127]
                    gs = slice(g0, g0 + 4)
                    S.op("dve", lambda e, A127=A127, gs=gs: e.tensor_tensor(out=sA[:, gs], in0=A127, in1=cAo[:, gs], op=ALU.add),
                         reads=[_nm(pa_), _nm(cAo)], writes=["s_sA"])
                    S.op("dve", lambda e, B127=B127, gs=gs: e.tensor_tensor(out=sB[:, gs], in0=B127, in1=cBo[:, gs], op=ALU.add),
                         reads=[_nm(pb_), _nm(cBo)], writes=["s_sB"])
                    for gg in range(4):
                        g = g0 + gg
                        xa = xas[gg % 2]
                        xb = xbs[gg % 2]
                        S.op("dve", lambda e, g=g, gg=gg, pa_=pa_, xa=xa: e.scalar_tensor_tensor(
                            out=xa[:], in0=pa_[:, gg * 128:(gg + 1) * 128], scalar=cAo[:, g:g + 1], in1=T1[:, g, :],
                            op0=ALU.add, op1=ALU.mult), reads=[_nm(pa_), _nm(cAo), "s_T1"], writes=[_nm(xa)])
                        S.op("dve", lambda e, g=g, gg=gg, pb_=pb_, xb=xb: e.scalar_tensor_tensor(
                            out=xb[:], in0=pb_[:, gg * 128:(gg + 1) * 128], scalar=cBo[:, g:g + 1], in1=T2[:, g, :],
                            op0=ALU.add, op1=ALU.mult), reads=[_nm(pb_), _nm(cBo), "s_T2"], writes=[_nm(xb)])
                        S.op("pool", lambda e, g=g, xa=xa, xb=xb: e.tensor_tensor(out=xs[:, g, :], in0=xa[:], in1=xb[:], op=ALU.add),
                             reads=[_nm(xa), _nm(xb)], writes=[("s_xs", g)])
                    S.op("dve", lambda e, gs=gs: e.tensor_tensor(out=cAn[:, gs], in0=sA[:, gs], in1=Pr[:, gs], op=ALU.mult),
                         reads=["s_sA", "s_pcs"], writes=[_nm(cAn)])
                    S.op("dve", lambda e, gs=gs: e.tensor_tensor(out=q1[:, gs], in0=sB[:, gs], in1=PiA[:, gs], op=ALU.mult),
                         reads=["s_sB", "s_pcs"], writes=["s_q1"])
                    S.op("dve", lambda e, gs=gs: e.tensor_tensor(out=cAn[:, gs], in0=cAn[:, gs], in1=q1[:, gs], op=ALU.add),
                         reads=["s_q1"], writes=[_nm(cAn)])
                    S.op("dve", lambda e, gs=gs: e.tensor_tensor(out=cBn[:, gs], in0=sB[:, gs], in1=Pr[:, gs], op=ALU.mult),
                         reads=["s_sB", "s_pcs"], writes=[_nm(cBn)])
                    S.op("dve", lambda e, gs=gs: e.tensor_tensor(out=q1[:, gs], in0=sA[:, gs], in1=PiB[:, gs], op=ALU.mult),
                         reads=["s_sA", "s_pcs"], writes=["s_q1"])
                    S.op("dve", lambda e, gs=gs: e.tensor_tensor(out=cBn[:, gs], in0=cBn[:, gs], in1=q1[:, gs], op=ALU.add),
                         reads=["s_q1"], writes=[_nm(cBn)])
                for g8 in range(8):
                    g = half * 8 + g8
                    S.op("pe", lambda e, g=g, g8=g8, half=half: e.matmul(
                        pyy[:, half * 128:(half + 1) * 128], lhsT=Cpad[:, g, :], rhs=xs[:, g, :],
                        start=(g8 == 0), stop=(g8 == 7)), reads=[("s_xs", g), "s_Cpad"], writes=["s_py"])
                S.op("dve", lambda e, half=half, t=t: e.scalar_tensor_tensor(
                    out=yv[:, half * 128:(half + 1) * 128], in0=uT[:, half, t * 128:(t + 1) * 128],
                    scalar=dsk[:, half:half + 1], in1=pyy[:, half * 128:(half + 1) * 128], op0=ALU.mult, op1=ALU.add),
                    reads=["s_py", "s_uT", "s_dsk"], writes=["s_yv"])
                S.op("act", lambda e, half=half, t=t: e.activation(
                    out=zT[:, half, t * 128:(t + 1) * 128], in_=yv[:, half * 128:(half + 1) * 128],
                    func=AF.Gelu_apprx_tanh), reads=["s_yv"], writes=[("s_zT", t // 4)])
            if t % 4 == 3:
                tc = t // 4
                for ct in range(2):
                    for k in range(2):
                        S.op("pe", lambda e, k=k, ct=ct, tc=tc: e.matmul(
                            pg[:], lhsT=wgl[:, k, ct * 128:(ct + 1) * 128], rhs=zT[:, k, tc * 512:(tc + 1) * 512],
                            start=(k == 0), stop=(k == 1)), reads=["s_wgl", ("s_zT", tc)], writes=["s_pg"])
                    S.op("act", lambda e, ct=ct: e.activation(out=sg[:], in_=pg[:], func=AF.Sigmoid,
                                                              bias=bgl[:, ct:ct + 1]),
                         reads=["s_pg", "s_bgl"], writes=["s_sg"])
                    S.op("dve", lambda e, ct=ct, tc=tc: e.tensor_tensor(
                        out=ysT[:, ct, tc * 512:(tc + 1) * 512], in0=zT[:, ct, tc * 512:(tc + 1) * 512], in1=sg[:],
                        op=ALU.mult), reads=["s_sg", ("s_zT", tc)], writes=["ysT"])
        S.end_phase()


def attention(nc, S, dr, l, b, hT, yaT, sb, ps, rstd_from_ss, identb, onesb):
    with ExitStack() as at:
        cqnT = sb(at, "a_cqnT", [128, 2, SEQ], BF16)
        ckvT = sb(at, "a_ckvT", [128, SEQ], BF16)
        Vg = sb(at, "a_V", [128, NT, 128], BF16)
        kidT = sb(at, "a_kidT", [128, SEQ], BF16)
        Wsc = sb(at, "a_Wsc", [128, NT, 8], F32)
        with ExitStack() as ph:
            w1 = sb(ph, "a_w1", [128, 8, 424], BF16)
            gcq = sb(ph, "a_gcq", [128, 2], F32)
            gkv = sb(ph, "a_gkv", [128, 128], F32)
            ss = sb(ph, "a_ss", [128, 2], F32)
            junk = sb(ph, "a_junk", [128, 256], BF16)
            cqn = sb(ph, "a_cqn", [128, 256], BF16)
            kid4 = sb(ph, "a_kid4", [128, 4, 32], BF16)
            pz = [ps(ph, "a_pz%d" % i, [128, 512], F32) for i in range(2)]
            pt = [ps(ph, "a_pt%d" % i, [128, 1024], BF16) for i in range(2)]
            cast_load(S, w1[:], dr["w1"][l][:, :, 0:424], "a_w1")
            S.dma(gcq[:], dr["gcq"][l], writes=["a_gcq"])
            S.dma(gkv[:], dr["gckv"][l:l + 1, :].to_broadcast([128, 128]), writes=["a_gkv"])
            for t in range(NT):
                p = pz[t % 2]
                q = pt[t % 2]
                for k in range(8):
                    S.op("pe", lambda e, k=k, t=t, p=p: e.matmul(p[:, 0:424], lhsT=hT[:, k, t * 128:(t + 1) * 128],
                                                                 rhs=w1[:, k, :], start=(k == 0), stop=(k == 7)),
                         reads=["a_w1", ("hT", t)], writes=[_nm(p)])
                S.op("act", lambda e, p=p: e.activation(out=junk[:], in_=p[:, 0:256], func=AF.Square,
                                                        accum_out=ss[:, 0:1]), reads=[_nm(p)], writes=["a_junk", "a_ss"])
                S.op("act", lambda e, p=p: e.activation(out=junk[:, 0:128], in_=p[:, 256:384], func=AF.Square,
                                                        accum_out=ss[:, 1:2]), reads=[_nm(p)], writes=["a_junk", "a_ss"])
                S.op("dve", lambda e: e.tensor_scalar(out=ss[:, 0:1], in0=ss[:, 0:1], scalar1=1.0 / 256, scalar2=EPS,
                                                      op0=ALU.mult, op1=ALU.add), reads=["a_ss"], writes=["a_ss"])
                S.op("dve", lambda e: e.tensor_scalar(out=ss[:, 1:2], in0=ss[:, 1:2], scalar1=1.0 / 128, scalar2=EPS,
                                                      op0=ALU.mult, op1=ALU.add), reads=["a_ss"], writes=["a_ss"])
                S.op("act", lambda e: e.activation(out=ss[:], in_=ss[:], func=AF.Sqrt), reads=["a_ss"], writes=["a_ss"])
                S.op("dve", lambda e: e.reciprocal(out=ss[:], in_=ss[:]), reads=["a_ss"], writes=["a_ss"])
                S.op("dve", lambda e, p=p: e.tensor_scalar(out=cqn[:], in0=p[:, 0:256], scalar1=ss[:, 0:1], scalar2=None,
                                                           op0=ALU.mult), reads=[_nm(p), "a_ss"], writes=["a_cqn"])
                S.op("dve", lambda e, p=p, t=t: e.scalar_tensor_tensor(
                    out=Vg[:, t, :], in0=p[:, 256:384], scalar=ss[:, 1:2], in1=gkv[:], op0=ALU.mult, op1=ALU.mult),
                    reads=[_nm(p), "a_ss", "a_gkv"], writes=[("a_V", t)])
                S.op("dve", lambda e, p=p: e.tensor_copy(
                    out=kid4[:], in_=p[:, 384:416].unsqueeze(1).to_broadcast([128, 4, 32])),
                    reads=[_nm(p)], writes=["a_kid4"])
                S.op("dve", lambda e, p=p, t=t: e.tensor_scalar(out=Wsc[:, t, :], in0=p[:, 416:424], scalar1=IDXW,
                                                                scalar2=None, op0=ALU.mult),
                     reads=[_nm(p)], writes=["a_Wsc"])
                for c in range(2):
                    S.op("pe", lambda e, c=c, q=q: e.transpose(out=q[:, c * 128:(c + 1) * 128],
                                                              in_=cqn[:, c * 128:(c + 1) * 128], identity=identb[:]),
                         reads=["a_cqn", "identb"], writes=[_nm(q)])
                S.op("pe", lambda e, q=q, t=t: e.transpose(out=q[:, 256:384], in_=Vg[:, t, :], identity=identb[:]),
                     reads=[("a_V", t), "identb"], writes=[_nm(q)])
                S.op("pe", lambda e, q=q: e.transpose(out=q[:, 384:512], in_=kid4[:].rearrange("p a b -> p (a b)"),
                                                      identity=identb[:]), reads=["a_kid4", "identb"], writes=[_nm(q)])
                for c in range(2):
                    S.op("act", lambda e, c=c, q=q, t=t: e.activation(
                        out=cqnT[:, c, t * 128:(t + 1) * 128], in_=q[:, c * 128:(c + 1) * 128], func=AF.Copy,
                        scale=gcq[:, c:c + 1]), reads=[_nm(q), "a_gcq"], writes=[("a_cqnT", t)])
                S.op("dve", lambda e, q=q, t=t: e.tensor_copy(out=ckvT[:, t * 128:(t + 1) * 128], in_=q[:, 256:384]),
                     reads=[_nm(q)], writes=[("a_ckvT", t)])
                S.op("dve", lambda e, q=q, t=t: e.tensor_copy(out=kidT[:, t * 128:(t + 1) * 128], in_=q[:, 384:512]),
                     reads=[_nm(q)], writes=[("a_kidT", t)])
            S.end_phase()
        with ExitStack() as ph:
            wuq = sb(ph, "a_wuq", [128, 2, 1024], BF16)
            wqi = sb(ph, "a_wqi", [128, 2, 256], BF16)
            wuv = sb(ph, "a_wuv", [128, 8, 64], BF16)
            i4b = sb(ph, "a_i4b", [128, 512], BF16)
            altd = sb(ph, "a_altd", [128, 1024], BF16)
            at3 = sb(ph, "a_at3", [128, 2048], BF16)
            bt3 = sb(ph, "a_bt3", [128, 1024], BF16)
            mk4 = sb(ph, "a_mk4", [128, 4], F32)
            wuvp = sb(ph, "a_wuvp", [128, 8, 128], BF16)
            m0 = sb(ph, "a_m0", [128, 128], F32)
            pow2 = sb(ph, "a_pow2", [128, NIT + 1], F32)
            qT = sb(ph, "a_qT", [128, 8, 128], BF16)
            qiT = sb(ph, "a_qiT", [128, 8, 128], BF16)
            accs = [sb(ph, "a_acc%d" % i, [128, SEQ], F32) for i in range(2)]
            jk = sb(ph, "a_jk", [128, SEQ], mybir.dt.uint8)
            MBs = [sb(ph, "a_MB%d" % i, [128, SEQ], BF16) for i in range(2)]
            Rb = [sb(ph, "a_R%d" % i, [128, 512], BF16) for i in range(2)]
            wdg = sb(ph, "a_wdg", [128, 8, 128], BF16)
            PT = [sb(ph, "a_PT%d" % i, [128, 512], BF16) for i in range(2)]
            rec = sb(ph, "a_rec", [128, 512], F32)
            oT = sb(ph, "a_oT", [128, 1024], BF16)
            sm = sb(ph, "a_sm", [128, 8], F32)
            Wk = sb(ph, "a_Wk", [128, NIT + 1], F32)
            pS = [ps(ph, "a_pS%d" % i, [128, 512], F32) for i in range(2)]
            pL = [ps(ph, "a_pL%d" % i, [128, 512], F32) for i in range(2)]
            pQ = ps(ph, "a_pQ", [128, 512], F32)
            pACC = ps(ph, "a_pACC", [128, 512], F32)
            pO = ps(ph, "a_pO", [128, 512], F32)
            pD = ps(ph, "a_pD", [128, 512], F32)
            pY = pQ
            cast_load(S, wuq[:], dr["wuq"][l], "a_wuq")
            cast_load(S, wqi[:], dr["wqi"][l], "a_wqi")
            cast_load(S, wuv[:], dr["wuv"][l], "a_wuv")
            for nm, tl in (("i4b", i4b), ("altd", altd), ("at3", at3), ("bt3", bt3), ("m0", m0), ("pow2", pow2),
                           ("mk4", mk4)):
                S.dma(tl[:], dr[nm], writes=["a_" + nm])
            S.op("dve", lambda e: e.memset(wuvp[:], 0.0), writes=["a_wuvp"])
            for h in range(8):
                S.op("dve", lambda e, h=h: e.tensor_copy(out=wuvp[:, h, (h % 2) * 64:(h % 2 + 1) * 64], in_=wuv[:, h, :]),
                     reads=["a_wuv"], writes=["a_wuvp"])
            hi, lo, w0, mid, cnt, tt, thr = (sm[:, i:i + 1] for i in range(7))

            def stageA(i, part):
                steps = []
                N = 128 * (i + 1)
                qs = slice(i * 128, (i + 1) * 128)
                MB = MBs[i % 2]
                mbn = "a_MB%d" % (i % 2)
                acc = accs[i % 2]
                ACCR = [("a_acc%d" % (i % 2), c_) for c_ in range(4)]

                def qi_proj(tl):
                    for k in range(2):
                        S.op("pe", lambda e: e.matmul(
                            pQ[:, tl * 128:(tl + 1) * 128], lhsT=wqi[:, k, tl * 128:(tl + 1) * 128],
                            rhs=cqnT[:, k, qs], start=(k == 0), stop=(k == 1)),
                            reads=["a_wqi", ("a_cqnT", i)], writes=["a_pQ"])
                    for h in range(tl * 4, tl * 4 + 4):
                        S.op("act", lambda e: e.activation(out=qiT[:, h, :], in_=pQ[:, tl * 128:(tl + 1) * 128],
                                                           func=AF.Copy, scale=mk4[:, h % 4:h % 4 + 1]),
                             reads=["a_pQ", "a_mk4"], writes=["a_qiT"])
                for tl in range(2):
                    steps.append(partial(qi_proj, tl))

                nch = (N + 511) // 512

                def diag_build():
                    for h in range(8):
                        S.op("act", lambda e: e.activation(out=wdg[:, h, :], in_=identb[:], func=AF.Copy,
                                                           scale=Wsc[:, i, h:h + 1]),
                             reads=["identb", "a_Wsc"], writes=["a_wdg"])
                steps.append(diag_build)
                seq = [(cc, h) for cc in range(nch) for h in range(8)]

                def emitL(x):
                    cc, h = seq[x]
                    n = min(512, N - cc * 512)
                    pl_ = pL[x % 2]
                    S.op("pe", lambda e: e.matmul(
                        pl_[:, 0:n], lhsT=qiT[:, h, :], rhs=kidT[:, cc * 512:cc * 512 + n],
                        start=True, stop=True), reads=["a_qiT"] + [("a_kidT", cc * 4 + q) for q in range((n + 127) // 128)],
                        writes=[_nm(pl_)])

                def idx_step(x):
                    cc, h = seq[x]
                    n = min(512, N - cc * 512)
                    if x == 0:
                        emitL(0)
                    if x + 1 < len(seq):
                        emitL(x + 1)
                    pl_ = pL[x % 2]
                    rb = Rb[x % 2]
                    S.op("act", lambda e: e.activation(out=rb[:, 0:n], in_=pl_[:, 0:n], func=AF.Relu),
                         reads=[_nm(pl_)], writes=[_nm(rb)])
                    S.op("pe", lambda e: e.matmul(pACC[:, 0:n], lhsT=wdg[:, h, :], rhs=rb[:, 0:n],
                                                  start=(h == 0), stop=(h == 7)),
                         reads=["a_wdg", _nm(rb)], writes=["a_pACC"])
                    if h == 7:
                        S.op("act", lambda e: e.copy(out=acc[:, cc * 512:cc * 512 + n], in_=pACC[:, 0:n]),
                             reads=["a_pACC"], writes=[("a_acc%d" % (i % 2), cc)])
                for x in range(len(seq)):
                    steps.append(partial(idx_step, x))
                if part == 1:
                    return steps
                steps = []

                def bounds():
                    if i >= 2:
                        S.op("dve", lambda e: e.tensor_reduce(out=hi, in_=acc[:, 0:N], axis=AX.X, op=ALU.max),
                             reads=ACCR, writes=["a_sm"])
                        S.op("dve", lambda e: e.tensor_reduce(out=lo, in_=acc[:, 0:N], axis=AX.X, op=ALU.min),
                             reads=ACCR, writes=["a_sm"])
                    S.op("dve", lambda e: e.tensor_tensor(out=acc[:, N - 128:N], in0=acc[:, N - 128:N], in1=m0[:],
                                                          op=ALU.add), reads=["a_m0"] + ACCR, writes=ACCR)
                    if i >= 2:
                        S.op("dve", lambda e: e.tensor_tensor(out=w0, in0=hi, in1=lo, op=ALU.subtract),
                             reads=["a_sm"], writes=["a_sm"])
                        S.op("dve", lambda e: e.tensor_scalar(out=Wk[:], in0=pow2[:], scalar1=w0, scalar2=None,
                                                              op0=ALU.mult), reads=["a_sm", "a_pow2"], writes=["a_Wk"])
                        S.op("dve", lambda e: e.tensor_tensor(out=mid, in0=lo, in1=Wk[:, 0:1], op=ALU.add),
                             reads=["a_sm", "a_Wk"], writes=["a_sm"])
                    else:
                        S.op("dve", lambda e: e.memset(thr, -1.0e29), writes=["a_sm"])
                steps.append(bounds)

                def bis(k):
                    S.op("dve", lambda e: e.tensor_scalar(
                        out=jk[:, 0:N], in0=acc[:, 0:N], scalar1=mid, scalar2=0.0, op0=ALU.is_ge, op1=ALU.add,
                        accum_out=cnt), reads=ACCR + ["a_sm"], writes=["a_jk", "a_sm"])
                    S.op("dve", lambda e: e.tensor_scalar(out=tt, in0=cnt, scalar1=255.5, scalar2=0.5, op0=ALU.is_ge,
                                                          op1=ALU.subtract), reads=["a_sm"], writes=["a_sm"])
                    S.op("dve", lambda e: e.scalar_tensor_tensor(out=mid, in0=tt, scalar=Wk[:, k:k + 1], in1=mid,
                                                                 op0=ALU.mult, op1=ALU.add),
                         reads=["a_sm", "a_Wk"], writes=["a_sm"])
                if i >= 2:
                    for k in range(NIT):
                        steps.append(partial(bis, k))

                def fin():
                    if i >= 2:
                        S.op("dve", lambda e: e.tensor_tensor(out=thr, in0=mid, in1=Wk[:, NIT:NIT + 1], op=ALU.subtract),
                             reads=["a_sm", "a_Wk"], writes=["a_sm"])
                    S.op("dve", lambda e: e.tensor_scalar(out=MB[:, 0:N], in0=acc[:, 0:N], scalar1=thr, scalar2=-30000.0,
                                                          op0=ALU.is_lt, op1=ALU.mult),
                         reads=ACCR + ["a_sm"], writes=[mbn])
                steps.append(fin)
                return steps

            def stageB(i):
                steps = []
                qs = slice(i * 128, (i + 1) * 128)
                MB = MBs[i % 2]
                mbn = "a_MB%d" % (i % 2)

                def q_proj(g2):
                    for h in range(g2 * 4, g2 * 4 + 4):
                        for k in range(2):
                            S.op("pe", lambda e: e.matmul(
                                pS[g2][:, (h % 4) * 128:(h % 4 + 1) * 128], lhsT=wuq[:, k, h * 128:(h + 1) * 128],
                                rhs=cqnT[:, k, qs], start=(k == 0), stop=(k == 1)),
                                reads=["a_wuq", ("a_cqnT", i)], writes=[_nm(pS[g2])])
                    S.op("act", lambda e: e.activation(
                        out=qT[:, g2 * 4:(g2 + 1) * 4, :].rearrange("p h q -> p (h q)"), in_=pS[g2][:], func=AF.Copy,
                        scale=ATT_SC), reads=[_nm(pS[g2])], writes=["a_qT"])
                steps.append(partial(q_proj, 0))
                steps.append(partial(q_proj, 1))
                its = [(g, j) for g in range(2) for j in range(i + 1)]

                def emitS(n):
                    grp, j = its[n]
                    s_ = pS[n % 2]
                    S.op("pe", lambda e: e.matmul(
                        s_[:], lhsT=ckvT[:, j * 128:(j + 1) * 128],
                        rhs=qT[:, grp * 4:(grp + 1) * 4, :].rearrange("p h q -> p (h q)"), start=True, stop=False),
                        reads=[("a_ckvT", j), "a_qT"], writes=[_nm(s_)])
                    S.op("pe", lambda e: e.matmul(
                        s_[:], lhsT=MB[:, j * 128:(j + 1) * 128], rhs=i4b[:], start=False, stop=False),
                        reads=[mbn, "a_i4b"], writes=[_nm(s_)])
                    if j < i:
                        dd = i - j
                        S.op("pe", lambda e: e.matmul(
                            s_[:], lhsT=at3[:, dd * 128:(dd + 1) * 128], rhs=bt3[:, grp * 512:(grp + 1) * 512],
                            start=False, stop=True), reads=["a_at3", "a_bt3"], writes=[_nm(s_)])
                    else:
                        S.op("pe", lambda e: e.matmul(
                            s_[:], lhsT=identb[:], rhs=altd[:, grp * 512:(grp + 1) * 512], start=False, stop=True),
                            reads=["identb", "a_altd"], writes=[_nm(s_)])

                def it_step(n):
                    grp, j = its[n]
                    if n == 0:
                        emitS(0)
                    if n + 1 < len(its):
                        emitS(n + 1)
                    s_ = pS[n % 2]
                    ptb = PT[n % 2]
                    S.op("act", lambda e: e.activation(out=ptb[:], in_=s_[:], func=AF.Exp),
                         reads=[_nm(s_)], writes=[_nm(ptb)])
                    S.op("pe", lambda e: e.matmul(pO[:], lhsT=Vg[:, j, :], rhs=ptb[:], start=(j == 0), stop=(j == i)),
                         reads=[("a_V", j), _nm(ptb)], writes=["a_pO"])
                    S.op("pe", lambda e: e.matmul(pD[:], lhsT=onesb[:], rhs=ptb[:], start=(j == 0), stop=(j == i)),
                         reads=["onesb", _nm(ptb)], writes=["a_pD"])
                    if j == i:
                        S.op("dve", lambda e: e.reciprocal(out=rec[:], in_=pD[:]), reads=["a_pD"], writes=["a_rec"])
                        S.op("dve", lambda e: e.tensor_tensor(out=oT[:, grp * 512:(grp + 1) * 512], in0=pO[:],
                                                              in1=rec[:], op=ALU.mult),
                             reads=["a_pO", "a_rec"], writes=["a_oT"])
                for n in range(len(its)):
                    steps.append(partial(it_step, n))

                def y_proj():
                    for h in range(8):
                        S.op("pe", lambda e: e.matmul(
                            pY[:, (h // 2) * 128:(h // 2 + 1) * 128], lhsT=wuvp[:, h, :],
                            rhs=oT[:, h * 128:(h + 1) * 128], start=(h % 2 == 0), stop=(h % 2 == 1)),
                            reads=["a_wuvp", "a_oT"], writes=["a_pQ"])
                    S.op("act", lambda e: e.copy(out=yaT[:, :, qs], in_=pY[:].rearrange("p (c q) -> p c q", q=128)),
                         reads=["a_pQ"], writes=["yaT"])
                steps.append(y_proj)
                return steps

            def run_merged(*lists):
                pos = [0] * len(lists)
                total = sum(len(x) for x in lists)
                for _ in range(total):
                    best, bf = None, None
                    for q, lst in enumerate(lists):
                        if pos[q] < len(lst):
                            frac = pos[q] / len(lst)
                            if bf is None or frac < bf:
                                best, bf = q, frac
                    lists[best][pos[best]]()
                    pos[best] += 1

            run_merged(stageA(0, 1))
            run_merged(stageA(1, 1), stageA(0, 2))
            for i in range(NT):
                run_merged(stageA(i + 2, 1) if i + 2 < NT else [], stageA(i + 1, 2) if i + 1 < NT else [], stageB(i))
            S.end_phase()


_BF = {"identb", "i4b", "onesb", "triub", "altd", "at3", "bt3"}


def kernel(**inputs):
    inp = {k: np.asarray(v) for k, v in inputs.items()}
    n_cores = 8
    shared = _layout_weights(inp)
    shared.update(_consts())
    x = np.ascontiguousarray(inp["x"], dtype=np.float32)
    c = np.asarray(inp["c"], dtype=np.float32)
    in_maps = []
    for ci in range(n_cores):
        m = dict(shared)
        m["x"] = x[ci * NB:(ci + 1) * NB]
        cc = c[ci * NB:(ci + 1) * NB]
        m["cT"] = np.ascontiguousarray(cc.reshape(NB, 8, 128).transpose(2, 1, 0))
        in_maps.append(m)
    shapes = {k: (v.shape, BF16 if k in _BF else F32) for k, v in in_maps[0].items()}
    nc = build_nc(shapes)
    in_maps = [{"d_" + k: v for k, v in m.items()} for m in in_maps]
    res = run_bass_kernel_spmd(nc, in_maps, core_ids=list(range(n_cores)))
    return np.concatenate([np.asarray(r["out"], dtype=np.float32) for r in res.results], axis=0)
```

```python
import types
from functools import partial
import numpy as np
import ml_dtypes
from contextlib import ExitStack
import concourse.bass as bass
import concourse.mybir as mybir
from concourse.bass_utils import run_bass_kernel_spmd

F32 = mybir.dt.float32
BF16 = mybir.dt.bfloat16
AF = mybir.ActivationFunctionType
ALU = mybir.AluOpType
AX = mybir.AxisListType

D = 1024
SEQ = 2048
NT = 16
NL = 2
NB = 4
EPS = 1e-6
ATT_SC = 128.0 ** -0.5
IDXW = (32.0 ** -0.5) * (8.0 ** -0.5)
NIT = 18
NEG = -1.0e30
POOL_WINS = (2, 4, 8, 16)


_BASE = {}
_UNIQ = [0]


def _nm(t):
    return _BASE[t.name]


def _freeze(fn):
    if fn.__closure__ is None:
        return fn
    cells = []
    for c in fn.__closure__:
        try:
            cells.append(types.CellType(c.cell_contents))
        except ValueError:
            cells.append(c)
    return types.FunctionType(fn.__code__, fn.__globals__, fn.__name__, fn.__defaults__, tuple(cells))


class Sched:
    ENG = ("pe", "act", "dve", "pool", "sp")

    def __init__(self, nc, es, n_dma_sems=12):
        self.nc = nc
        self.sems = {e: es.enter_context(nc.semaphore("s_" + e)) for e in self.ENG}
        self.cnt = {e: 0 for e in self.ENG}
        self.dma_sems = [es.enter_context(nc.semaphore("d%d" % i)) for i in range(n_dma_sems)]
        self.dma_cnt = [0] * n_dma_sems
        self.dma_rr = 0
        self.prog = {e: [] for e in self.ENG}
        self.seen = {e: {} for e in self.ENG}
        self.lastw = {}
        self.readers = {}
        self.ninst = 0
        self.excl = set()

    def _sem_of(self, key):
        if isinstance(key, tuple):
            return self.dma_sems[key[1]], 16
        return self.sems[key], 1

    def _needed(self, eng, deps):
        best = {}
        for (key, val) in deps:
            if key == eng and eng in ("pe", "sp"):
                continue
            if self.seen[eng].get(key, 0) >= val:
                continue
            if best.get(key, 0) < val:
                best[key] = val
        waits = []
        for key, val in best.items():
            self.seen[eng][key] = val
            sem, step = self._sem_of(key)
            waits.append((sem, val * step))
        return waits

    def _deps(self, reads, writes):
        deps = []
        for r in reads:
            t = self.lastw.get(r)
            if t is not None:
                deps.append(t)
        for w in writes:
            t = self.lastw.get(w)
            if t is not None:
                deps.append(t)
            rd = self.readers.get(w)
            if rd:
                deps.extend(rd.items())
        return deps

    def _commit(self, tok, reads, writes):
        for w in writes:
            self.lastw[w] = tok
            self.readers[w] = {}
        for r in reads:
            d = self.readers.setdefault(r, {})
            if d.get(tok[0], 0) < tok[1]:
                d[tok[0]] = tok[1]

    def op(self, eng, fn, reads=(), writes=()):
        fn = _freeze(fn)
        if self.excl:
            ex = [r for r in reads if r in self.excl]
            if ex:
                writes = list(writes) + [r for r in ex if r not in writes]
        waits = self._needed(eng, self._deps(reads, writes))
        self.cnt[eng] += 1
        tok = (eng, self.cnt[eng])
        sem = self.sems[eng]

        def emit(e, waits=waits, fn=fn, sem=sem):
            for (s, v) in waits:
                e.wait_ge(s, v)
            fn(e).then_inc(sem, 1)

        self.prog[eng].append(emit)
        self._commit(tok, reads, writes)
        self.ninst += 1
        return tok

    def dma(self, out, in_, reads=(), writes=(), q="sp", **kw):
        k = self.dma_rr
        self.dma_rr = (k + 1) % len(self.dma_sems)
        deps = self._deps(reads, writes)
        if self.dma_cnt[k] > 0:
            deps.append((("dma", k), self.dma_cnt[k]))
        waits = self._needed(q, deps)
        self.dma_cnt[k] += 1
        tok = (("dma", k), self.dma_cnt[k])
        sem = self.dma_sems[k]

        def emit(e, waits=waits, sem=sem, out=out, in_=in_, kw=kw):
            for (s, v) in waits:
                e.wait_ge(s, v)
            e.dma_start(out=out, in_=in_, **kw).then_inc(sem, 16)

        self.prog[q].append(emit)
        self._commit(tok, reads, writes)
        self.ninst += 1
        return tok

    def end_phase(self, q="sp"):
        waits = [(self.dma_sems[k], 16 * c) for k, c in enumerate(self.dma_cnt) if c > 0]
        waits += [(self.sems[e], self.cnt[e]) for e in self.ENG if self.cnt[e] > 0 and e != q]

        def emit(e, waits=waits):
            for (s, v) in waits:
                e.wait_ge(s, v)

        self.prog[q].append(emit)
        nc = self.nc
        prog = self.prog
        with nc.Block() as block:
            if prog["pe"]:
                @block.tensor
                def _(e):
                    for f in prog["pe"]:
                        f(e)
            if prog["act"]:
                @block.scalar
                def _(e):
                    for f in prog["act"]:
                        f(e)
            if prog["dve"]:
                @block.vector
                def _(e):
                    for f in prog["dve"]:
                        f(e)
            if prog["pool"]:
                @block.gpsimd
                def _(e):
                    for f in prog["pool"]:
                        f(e)
            if prog["sp"]:
                @block.sync
                def _(e):
                    for f in prog["sp"]:
                        f(e)
        self.prog = {e: [] for e in self.ENG}


def _consts():
    bf = ml_dtypes.bfloat16
    c = {}
    c["identf"] = np.eye(128, dtype=np.float32)
    c["identb"] = np.eye(128, dtype=np.float32).astype(bf)
    c["i4b"] = np.tile(np.eye(128, dtype=np.float32), (1, 4)).astype(bf)
    c["onesb"] = np.ones((128, 128), np.float32).astype(bf)
    c["onesf"] = np.ones((128, 128), np.float32)
    s = np.arange(128)
    c["triub"] = (s[:, None] <= s[None, :]).astype(np.float32).astype(bf)
    m0 = np.zeros((128, 128), np.float32)
    m0[:64, 64:] = NEG
    c["m0"] = m0
    slopes = 2.0 ** (-8.0 * np.arange(1, 9) / 8.0)
    altd = np.zeros((128, 8, 128), np.float32)
    for h in range(8):
        altd[:, h, :] = -slopes[h] * np.abs(s[None, :] - s[:, None])
    c["altd"] = altd.reshape(128, 1024).astype(bf)
    at3 = np.zeros((3, 16, 128), np.float32)
    at3[0] = 1.0
    at3[1] = (128.0 * np.arange(16))[:, None]
    at3[2] = s[None, :].astype(np.float32)
    at3p = np.zeros((128, 2048), np.float32)
    at3p[0:3] = at3.reshape(3, 2048)
    c["at3"] = at3p.astype(bf)
    bt3 = np.zeros((3, 8, 128), np.float32)
    for h in range(8):
        bt3[0, h] = -slopes[h] * s
        bt3[1, h] = -slopes[h]
        bt3[2, h] = slopes[h]
    bt3p = np.zeros((128, 1024), np.float32)
    bt3p[0:3] = bt3.reshape(3, 1024)
    c["bt3"] = bt3p.astype(bf)
    mk4 = np.zeros((128, 4), np.float32)
    for r in range(4):
        mk4[r * 32:(r + 1) * 32, r] = 1.0
    c["mk4"] = mk4
    sg = np.ones((128, 2), np.float32)
    sg[:64, 0] = -1.0
    sg[64:, 1] = -1.0
    c["sgn"] = sg
    bm = np.zeros((128, 8), np.float32)
    for p in range(128):
        bm[p, p // 16] = 1.0
    c["bmask"] = bm
    m2 = np.zeros((128, 16, 8), np.float32)
    for g in range(16):
        m2[:, g, g % 8] = 1.0
    c["mask2"] = m2
    c["pow2"] = np.tile((2.0 ** -(np.arange(NIT + 1) + 1.0))[None, :], (128, 1)).astype(np.float32)
    c["invt"] = np.tile((1.0 / (np.arange(16) + 1.0))[None, :], (128, 1)).astype(np.float32)
    return c


def _layout_weights(inp):
    f = lambda a: np.ascontiguousarray(a, dtype=np.float32)
    w = {}
    w["modw"] = f(inp["mod_w"].reshape(NL, 8, 128, 12, 512).transpose(0, 3, 2, 1, 4))
    w["modb"] = f(inp["mod_b"].reshape(NL, 48, 128).transpose(0, 2, 1))
    gv = np.stack([inp["mix_pre_g"], inp["mix_post_g"], inp["ffn_pre_g"], inp["ffn_post_g"]], axis=1)
    w["gvec"] = f(gv.reshape(NL, 4, 8, 128).transpose(0, 1, 3, 2))
    win = inp["w_in"].reshape(NL, 8, 128, 4008)
    w["w1"] = f(win[:, :, :, :936].transpose(0, 2, 1, 3))
    w["wg"] = f(win[:, :, :, 936:].reshape(NL, 8, 128, 24, 128).transpose(0, 3, 2, 1, 4))
    w["gcq"] = f(inp["g_cq"].reshape(NL, 2, 128).transpose(0, 2, 1))
    w["gckv"] = f(inp["g_ckv"])
    w["wuq"] = f(inp["w_uq"].reshape(NL, 2, 128, 1024).transpose(0, 2, 1, 3))
    w["wqi"] = f(inp["w_qi"].reshape(NL, 2, 128, 256).transpose(0, 2, 1, 3))
    w["wuv"] = f(inp["w_uv"].transpose(0, 2, 1, 3))
    dup = lambda a: np.concatenate([a, a], axis=1)
    w["are"] = f(dup(inp["a_re"].transpose(0, 2, 1)))
    w["aim"] = f(dup(inp["a_im"].transpose(0, 2, 1)))
    w["lstep"] = f(np.broadcast_to(inp["log_step"][:, None, :], (NL, 128, 16)))
    w["bre"] = f(inp["b_re"].transpose(0, 2, 1, 3))
    w["bim"] = f(inp["b_im"].transpose(0, 2, 1, 3))
    w["cre"] = f(inp["c_re"].transpose(0, 3, 1, 2))
    w["cim"] = f(inp["c_im"].transpose(0, 3, 1, 2))
    w["dskip"] = f(inp["d_skip"].reshape(NL, 2, 128).transpose(0, 2, 1))
    w["wglu"] = f(inp["w_glu"].reshape(NL, 2, 128, 256).transpose(0, 2, 1, 3))
    w["bglu"] = f(inp["b_glu"].reshape(NL, 2, 128).transpose(0, 2, 1))
    w["wpool"] = f(inp["w_pool"])
    w["pscale"] = f(inp["pool_scale"].reshape(NL, 2, 128).transpose(0, 2, 1))
    w["pa"] = f(inp["p_a"].reshape(NL, 4, 128, 1024).transpose(0, 2, 1, 3))
    w["pb"] = f(inp["p_b"].reshape(NL, 2, 128, 1024).transpose(0, 2, 1, 3))
    w["pc"] = f(inp["p_c"].reshape(NL, 2, 128, 1024).transpose(0, 2, 1, 3))
    w["wout"] = f(inp["w_out"].reshape(NL, 8, 128, 1024).transpose(0, 2, 1, 3))
    w["wup"] = f(inp["w_up"].reshape(NL, 8, 128, 32, 128).transpose(0, 3, 2, 1, 4))
    w["convw"] = f(inp["conv_w"].reshape(NL, 3, 32, 128).transpose(0, 3, 1, 2))
    w["convb"] = f(inp["conv_b"].reshape(NL, 32, 128).transpose(0, 2, 1))
    w["wdown"] = f(inp["w_down"].reshape(NL, 16, 128, 1024).transpose(0, 2, 1, 3))
    return w


def build_nc(shapes, nb=NB, nl=NL, do_mix=True, do_ffn=True, att=True, ssm=True, pool=True):
    nc = bass.Bass("TRN2", target_bir_lowering=False)
    DBG["nc"] = nc
    DBG["outs"] = {}
    dr = {}
    for name, (shape, dt) in shapes.items():
        dr[name] = nc.dram_tensor("d_" + name, list(shape), dt, kind="ExternalInput").ap()
    out = nc.dram_tensor("out", [nb, SEQ, D], F32, kind="ExternalOutput").ap()
    TABW = 2 * 1024 + 2 * 2048 + 64
    tab = nc.dram_tensor("tabscratch", [NL, 128, TABW], F32, kind="Internal").ap()
    tabb = nc.dram_tensor("tabscratchb", [NL, 128, 2048 + 2048], BF16, kind="Internal").ap()

    with ExitStack() as es:
        S = Sched(nc, es)

        def sb(st, name, shape, dt):
            _UNIQ[0] += 1
            actual = "%s__%d" % (name, _UNIQ[0])
            _BASE[actual] = name
            return st.enter_context(nc.sbuf_tensor(actual, shape, dt))

        def ps(st, name, shape, dt):
            S.excl.add(name)
            _UNIQ[0] += 1
            actual = "%s__%d" % (name, _UNIQ[0])
            _BASE[actual] = name
            return st.enter_context(nc.psum_tensor(actual, shape, dt))

        X = sb(es, "X", [128, NT, D], F32)
        hT = sb(es, "hT", [128, 8, SEQ], BF16)
        identf = sb(es, "identf", [128, 128], F32)
        identb = sb(es, "identb", [128, 128], BF16)
        onesb = sb(es, "onesb", [128, 128], BF16)
        onesf = sb(es, "onesf", [128, 128], F32)
        cols = sb(es, "cols", [128, NL, 6, 8, NB], F32)
        GG = sb(es, "GG", [128, D], F32)
        for nm, t in (("identf", identf), ("identb", identb), ("onesb", onesb), ("onesf", onesf)):
            S.dma(t[:], dr[nm], writes=[nm])

        with ExitStack() as ph:
            condT = sb(ph, "condT", [128, 8, NB], F32)
            modT = sb(ph, "modT", [128, 48, NB], F32)
            modb = sb(ph, "modb", [128, 48], F32)
            gvec = sb(ph, "gvec", [128, 4, 8], F32)
            mw = [sb(ph, "mw%d" % i, [128, 8, 512], F32) for i in range(2)]
            pm = ps(ph, "pm", [128, 512], F32)
            S.dma(condT[:], dr["cT"], writes=["condT"])
            S.op("act", lambda e: e.activation(out=condT[:], in_=condT[:], func=AF.Silu),
                 reads=["condT"], writes=["condT"])
            for l in range(nl):
                S.dma(modb[:], dr["modb"][l], writes=["modb"])
                S.dma(gvec[:], dr["gvec"][l].rearrange("w p c -> p w c"), writes=["gvec"])
                for j in range(12):
                    buf = mw[j % 2]
                    S.dma(buf[:], dr["modw"][l, j], writes=["mw%d" % (j % 2)])
                    for f4 in range(4):
                        fc = j * 4 + f4
                        for k in range(8):
                            S.op("pe", lambda e, buf=buf, f4=f4, k=k, fc=fc: e.matmul(
                                pm[:, fc * NB:(fc + 1) * NB], lhsT=buf[:, k, f4 * 128:(f4 + 1) * 128],
                                rhs=condT[:, k, :], start=(k == 0), stop=(k == 7)),
                                reads=["mw%d" % (j % 2), "condT"], writes=["pm"])
                S.op("dve", lambda e: e.tensor_tensor(
                    out=modT[:], in0=pm[:, 0:48 * NB].rearrange("p (f b) -> p f b", b=NB),
                    in1=modb[:].unsqueeze(2).to_broadcast([128, 48, NB]), op=ALU.add),
                    reads=["pm", "modb"], writes=["modT"])
                def gb(w):
                    return gvec[:, w, :].unsqueeze(2).to_broadcast([128, 8, NB])
                for (dst, sc0, gw) in ((0, 8, 0), (3, 32, 2)):
                    S.op("dve", lambda e, dst=dst, sc0=sc0, gw=gw: e.scalar_tensor_tensor(
                        out=cols[:, l, dst], in0=modT[:, sc0:sc0 + 8, :], scalar=1.0, in1=gb(gw),
                        op0=ALU.add, op1=ALU.mult), reads=["modT", "gvec"], writes=["cols"])
                for (dst, sh0) in ((1, 0), (4, 24)):
                    S.op("dve", lambda e, dst=dst, sh0=sh0: e.tensor_copy(
                        out=cols[:, l, dst], in_=modT[:, sh0:sh0 + 8, :]), reads=["modT"], writes=["cols"])
                for (dst, g0, gw) in ((2, 16, 1), (5, 40, 3)):
                    S.op("dve", lambda e, dst=dst, g0=g0, gw=gw: e.tensor_tensor(
                        out=cols[:, l, dst], in0=modT[:, g0:g0 + 8, :], in1=gb(gw), op=ALU.mult),
                        reads=["modT", "gvec"], writes=["cols"])
            dbg(S, "cols", cols[:].rearrange("p l s c b -> p (l s c b)"), [128, NL * 6 * 8 * NB], F32, ["cols"])
            S.end_phase()

        def rstd_from_ss(ss, n, width):
            S.op("dve", lambda e: e.tensor_scalar(out=ss[:, 0:width], in0=ss[:, 0:width], scalar1=1.0 / n,
                                                  scalar2=EPS, op0=ALU.mult, op1=ALU.add),
                 reads=[_nm(ss)], writes=[_nm(ss)])
            S.op("act", lambda e: e.activation(out=ss[:, 0:width], in_=ss[:, 0:width], func=AF.Sqrt),
                 reads=[_nm(ss)], writes=[_nm(ss)])
            S.op("dve", lambda e: e.reciprocal(out=ss[:, 0:width], in_=ss[:, 0:width]),
                 reads=[_nm(ss)], writes=[_nm(ss)])

        def norm_to_hT(ph, l, b, ca, cb):
            ss = sb(ph, "n_ss", [128, NT], F32)
            junk = sb(ph, "n_junk", [128, D], BF16)
            xn = [sb(ph, "n_xn%d" % i, [128, D], BF16) for i in range(4)]
            pt = [ps(ph, "n_pt%d" % i, [128, 1024], BF16) for i in range(4)]
            for t in range(NT):
                S.op("act", lambda e, t=t: e.activation(out=junk[:], in_=X[:, t, :], func=AF.Square,
                                                        accum_out=ss[:, t:t + 1]),
                     reads=[("X", t)], writes=["n_junk", "n_ss"])
            rstd_from_ss(ss, float(D), NT)
            for t in range(NT):
                xb = xn[t % 4]
                pp = pt[t % 4]
                S.op("dve", lambda e, t=t, xb=xb: e.tensor_scalar(out=xb[:], in0=X[:, t, :],
                                                               scalar1=ss[:, t:t + 1], scalar2=None, op0=ALU.mult),
                     reads=[("X", t), "n_ss"], writes=[_nm(xb)])
                for c in range(8):
                    S.op("pe", lambda e, c=c, xb=xb, pp=pp: e.transpose(
                        out=pp[:, c * 128:(c + 1) * 128], in_=xb[:, c * 128:(c + 1) * 128], identity=identb[:]),
                        reads=[_nm(xb), "identb"], writes=[_nm(pp)])
                for c in range(8):
                    eng = "act" if c % 2 else "dve"
                    if eng == "dve":
                        S.op("dve", lambda e, c=c, t=t, pp=pp: e.tensor_scalar(
                            out=hT[:, c, t * 128:(t + 1) * 128], in0=pp[:, c * 128:(c + 1) * 128],
                            scalar1=cols[:, l, ca, c, b:b + 1], scalar2=cols[:, l, cb, c, b:b + 1],
                            op0=ALU.mult, op1=ALU.add), reads=[_nm(pp), "cols"], writes=[("hTc", t, c)])
                    else:
                        S.op("act", lambda e, c=c, t=t, pp=pp: e.activation(
                            out=hT[:, c, t * 128:(t + 1) * 128], in_=pp[:, c * 128:(c + 1) * 128],
                            func=AF.Identity, scale=cols[:, l, ca, c, b:b + 1], bias=cols[:, l, cb, c, b:b + 1]),
                            reads=[_nm(pp), "cols"], writes=[("hTc", t, c)])

        def make_GG(ph, l, b, cg):
            dg = sb(ph, "gg_diag", [128, 128], F32)
            pg = [ps(ph, "gg_ps%d" % i, [128, 512], F32) for i in range(2)]
            for c in range(8):
                S.op("dve", lambda e, c=c: e.tensor_scalar(out=dg[:], in0=identf[:], scalar1=cols[:, l, cg, c, b:b + 1],
                                                        scalar2=None, op0=ALU.mult),
                     reads=["identf", "cols"], writes=["gg_diag"])
                S.op("pe", lambda e, c=c: e.matmul(pg[c // 4][:, (c % 4) * 128:(c % 4 + 1) * 128], lhsT=onesf[:],
                                                   rhs=dg[:], start=True, stop=True),
                     reads=["gg_diag", "onesf"], writes=[_nm(pg[c // 4])])
            for i in range(2):
                S.op("act", lambda e, i=i: e.copy(out=GG[:, i * 512:(i + 1) * 512], in_=pg[i][:]),
                     reads=[_nm(pg[i])], writes=["GG"])

        def resid_update(ph_tiles, t, py):
            ssy, junk, tmp = ph_tiles
            for i in range(2):
                S.op("act", lambda e, i=i: e.activation(out=junk[:, 0:512], in_=py[i][:], func=AF.Square,
                                                        accum_out=ssy[:, i:i + 1]),
                     reads=[_nm(py[i])], writes=[_nm(junk), _nm(ssy)])
            S.op("dve", lambda e: e.tensor_tensor(out=ssy[:, 2:3], in0=ssy[:, 0:1], in1=ssy[:, 1:2], op=ALU.add),
                 reads=[_nm(ssy)], writes=[_nm(ssy)])
            S.op("dve", lambda e: e.tensor_scalar(out=ssy[:, 2:3], in0=ssy[:, 2:3], scalar1=1.0 / D, scalar2=EPS,
                                                  op0=ALU.mult, op1=ALU.add), reads=[_nm(ssy)], writes=[_nm(ssy)])
            S.op("act", lambda e: e.activation(out=ssy[:, 2:3], in_=ssy[:, 2:3], func=AF.Sqrt),
                 reads=[_nm(ssy)], writes=[_nm(ssy)])
            S.op("dve", lambda e: e.reciprocal(out=ssy[:, 2:3], in_=ssy[:, 2:3]), reads=[_nm(ssy)], writes=[_nm(ssy)])
            for i in range(2):
                S.op("dve", lambda e, i=i: e.scalar_tensor_tensor(
                    out=tmp[:, i * 512:(i + 1) * 512], in0=py[i][:], scalar=ssy[:, 2:3],
                    in1=GG[:, i * 512:(i + 1) * 512], op0=ALU.mult, op1=ALU.mult),
                    reads=[_nm(py[i]), _nm(ssy), "GG"], writes=[_nm(tmp)])
            if t == 8:
                dbg(S, "r_ssy", ssy[:], [128, 4], F32, [_nm(ssy)])
                dbg(S, "r_tmp", tmp[:], [128, D], F32, [_nm(tmp)])
            S.op("dve", lambda e: e.tensor_tensor(out=X[:, t, :], in0=X[:, t, :], in1=tmp[:], op=ALU.add),
                 reads=[_nm(tmp), ("X", t)], writes=[("X", t)])

        if do_mix and ssm:
            for l in range(nl):
                ssm_tables(nc, S, dr, l, tab, tabb, sb, ps, identf)

        for b in range(nb):
            with ExitStack() as ph:
                for t in range(NT):
                    S.dma(X[:, t, :], dr["x"][b, t * 128:(t + 1) * 128, :], writes=[("X", t)])
                S.end_phase()
            for l in range(nl):
                if do_mix:
                    mixer(nc, S, dr, l, b, X, hT, GG, cols, tab, tabb, sb, ps, norm_to_hT, make_GG, resid_update,
                          rstd_from_ss, identb, identf, onesb, att, ssm, pool)
                if do_ffn:
                    ffn(nc, S, dr, l, b, X, hT, GG, sb, ps, norm_to_hT, make_GG, resid_update)
            with ExitStack() as ph:
                for t in range(NT):
                    S.dma(out[b, t * 128:(t + 1) * 128, :], X[:, t, :], reads=[("X", t)])
                S.end_phase()
        print("instructions:", S.ninst)
    return nc


DBG = {"on": False, "nc": None, "outs": {}}


def dbg(S, name, ap, shape, dt, reads, st=None):
    if not DBG["on"] or name in DBG["outs"]:
        return
    nc = DBG["nc"]
    d = nc.dram_tensor("dbg_" + name, list(shape), F32, kind="ExternalOutput").ap()
    DBG["outs"][name] = d
    if dt != F32:
        tmpf = st.enter_context(nc.sbuf_tensor("dbgt_" + name, list(shape), F32))
        S.op("dve", lambda e: e.tensor_copy(out=tmpf[:], in_=ap), reads=reads, writes=["dbgt_" + name])
        S.dma(d, tmpf[:], reads=["dbgt_" + name])
    else:
        S.dma(d, ap, reads=reads)


def cast_load(S, dst, src, wname):
    S.dma(dst, src, writes=[wname], q="pool")


def ffn(nc, S, dr, l, b, X, hT, GG, sb, ps, norm_to_hT, make_GG, resid_update):
    with ExitStack() as ph:
        norm_to_hT(ph, l, b, 3, 4)
        make_GG(ph, l, b, 5)
        dbg(S, "f_hT", hT[:].rearrange("p c t -> p (c t)"), [128, 8 * SEQ], BF16, [("hT", t) for t in range(NT)], ph)
        dbg(S, "f_GG", GG[:], [128, D], F32, ["GG"])
        S.end_phase()
    with ExitStack() as ph:
        wd = sb(ph, "f_wd", [128, 16, D], BF16)
        m = sb(ph, "f_m", [128, 16, 1024], BF16)
        cw = sb(ph, "f_cw", [128, 3, 32], F32)
        cbt = sb(ph, "f_cb", [128, 32], F32)
        halo = sb(ph, "f_halo", [128, 32, 2], F32)
        wu = [sb(ph, "f_wu%d" % i, [128, 8, 128], BF16) for i in range(3)]
        ub = [sb(ph, "f_ub%d" % i, [128, 1026], F32) for i in range(2)]
        acc = [sb(ph, "f_acc%d" % i, [128, 1024], F32) for i in range(2)]
        gl = sb(ph, "f_gl", [128, 1024], F32)
        ssy = sb(ph, "f_ssy", [128, 4], F32)
        junk = sb(ph, "f_junk", [128, 512], BF16)
        tmp = sb(ph, "f_tmp", [128, D], F32)
        pu = [ps(ph, "f_pu%d" % i, [128, 512], F32) for i in range(4)]
        py = [ps(ph, "f_py%d" % i, [128, 512], F32) for i in range(4)]
        S.dma(cw[:], dr["convw"][l], writes=["f_cw"])
        S.dma(cbt[:], dr["convb"][l], writes=["f_cb"])
        for k in range(16):
            cast_load(S, wd[:, k, :], dr["wdown"][l][:, k, :], ("f_wd", k))
        nload = 0
        for half in range(2):
            t0 = half * 1024
            for cc in range(16):
                for gv in range(2):
                    j = gv * 16 + cc
                    wb = wu[nload % 3]
                    nload += 1
                    cast_load(S, wb[:], dr["wup"][l, j], _nm(wb))
                    pp = pu[(gv * 2):(gv * 2 + 2)]
                    for s2 in range(2):
                        for k in range(8):
                            S.op("pe", lambda e, wb=wb, k=k, s2=s2, pp=pp: e.matmul(
                                pp[s2][:], lhsT=wb[:, k, :], rhs=hT[:, k, t0 + s2 * 512:t0 + (s2 + 1) * 512],
                                start=(k == 0), stop=(k == 7)),
                                reads=[_nm(wb)] + [("hT", (t0 + s2 * 512) // 128 + q) for q in range(4)],
                                writes=[_nm(pp[s2])])
                    u = ub[gv]
                    a = acc[gv]
                    dbg(S, "f_wb", wb[:].rearrange("p k n -> p (k n)"), [128, 1024], BF16, [_nm(wb)], ph)
                    if half == 0:
                        S.op("dve", lambda e, u=u: e.memset(u[:, 0:2], 0.0), writes=[_nm(u)])
                    else:
                        S.op("dve", lambda e, u=u, j=j: e.tensor_copy(out=u[:, 0:2], in_=halo[:, j, :]),
                             reads=["f_halo"], writes=[_nm(u)])
                    for s2 in range(2):
                        S.op("act", lambda e, u=u, s2=s2, pp=pp: e.copy(out=u[:, 2 + s2 * 512:2 + (s2 + 1) * 512],
                                                                        in_=pp[s2][:]),
                             reads=[_nm(pp[s2])], writes=[_nm(u)])
                    if half == 0:
                        S.op("dve", lambda e, u=u, j=j: e.tensor_copy(out=halo[:, j, :], in_=u[:, 1024:1026]),
                             reads=[_nm(u)], writes=["f_halo"])
                    dbg(S, "f_u", u[:], [128, 1026], F32, [_nm(u)], ph)
                    S.op("dve", lambda e, u=u, a=a, j=j: e.tensor_scalar(
                        out=a[:], in0=u[:, 2:1026], scalar1=cw[:, 2, j:j + 1], scalar2=cbt[:, j:j + 1],
                        op0=ALU.mult, op1=ALU.add), reads=[_nm(u), "f_cw", "f_cb"], writes=[_nm(a)])
                    S.op("dve", lambda e, u=u, a=a, j=j: e.scalar_tensor_tensor(
                        out=a[:], in0=u[:, 1:1025], scalar=cw[:, 1, j:j + 1], in1=a[:], op0=ALU.mult, op1=ALU.add),
                        reads=[_nm(u), "f_cw"], writes=[_nm(a)])
                    S.op("dve", lambda e, u=u, a=a, j=j: e.scalar_tensor_tensor(
                        out=a[:], in0=u[:, 0:1024], scalar=cw[:, 0, j:j + 1], in1=a[:], op0=ALU.mult, op1=ALU.add),
                        reads=[_nm(u), "f_cw"], writes=[_nm(a)])
                dbg(S, "f_acc0", acc[0][:], [128, 1024], F32, [_nm(acc[0])], ph)
                dbg(S, "f_acc1", acc[1][:], [128, 1024], F32, [_nm(acc[1])], ph)
                S.op("act", lambda e: e.activation(out=gl[:], in_=acc[0][:], func=AF.Gelu_apprx_tanh),
                     reads=[_nm(acc[0])], writes=["f_gl"])
                S.op("dve", lambda e, cc=cc: e.tensor_tensor(out=m[:, cc, :], in0=gl[:], in1=acc[1][:], op=ALU.mult),
                     reads=["f_gl", _nm(acc[1])], writes=[("f_m", cc)])
            for tt in range(8):
                t = half * 8 + tt
                pyy = py[(tt % 2) * 2:(tt % 2) * 2 + 2]
                for i in range(2):
                    for k in range(16):
                        S.op("pe", lambda e, i=i, k=k, tt=tt, pyy=pyy: e.matmul(
                            pyy[i][:], lhsT=m[:, k, tt * 128:(tt + 1) * 128], rhs=wd[:, k, i * 512:(i + 1) * 512],
                            start=(k == 0), stop=(k == 15)),
                            reads=[("f_m", k), ("f_wd", k)], writes=[_nm(pyy[i])])
                resid_update((ssy, junk, tmp), t, pyy)
        S.end_phase()


def ssm_tables(nc, S, dr, l, tab, tabb, sb, ps, identf):
    with ExitStack() as ph:
        are = sb(ph, "t_are", [128, 16], F32)
        aim = sb(ph, "t_aim", [128, 16], F32)
        stp = sb(ph, "t_stp", [128, 16], F32)
        th = sb(ph, "t_th", [128, 16], F32)
        r = sb(ph, "t_r", [128, 16], F32)
        sn = sb(ph, "t_sn", [128, 16], F32)
        cs = sb(ph, "t_cs", [128, 16], F32)
        t1 = sb(ph, "t_t1", [128, 16], F32)
        t2 = sb(ph, "t_t2", [128, 16], F32)
        lre = sb(ph, "t_lre", [128, 16], F32)
        lim = sb(ph, "t_lim", [128, 16], F32)
        ire = sb(ph, "t_ire", [128, 16], F32)
        iim = sb(ph, "t_iim", [128, 16], F32)
        pr = sb(ph, "t_pr", [128, 16], F32)
        pi = sb(ph, "t_pi", [128, 16], F32)
        cfr = sb(ph, "t_cfr", [128, 16], F32)
        cfi = sb(ph, "t_cfi", [128, 16], F32)
        Ere = sb(ph, "t_Ere", [128, 16, 256], F32)
        Eim = sb(ph, "t_Eim", [128, 16, 256], F32)
        ta = sb(ph, "t_ta", [128, 16, 128], F32)
        tb = sb(ph, "t_tb", [128, 16, 128], F32)
        sgn = sb(ph, "t_sgn", [128, 2], F32)
        outc = sb(ph, "t_outc", [128, 64], F32)
        halfpi = sb(ph, "t_hpi", [128, 1], F32)
        R = ["t"]

        def dv(fn, w=("t",)):
            S.op("dve", fn, reads=R, writes=list(w))

        S.dma(are[:], dr["are"][l], writes=R)
        S.dma(aim[:], dr["aim"][l], writes=R)
        S.dma(stp[:], dr["lstep"][l], writes=R)
        S.dma(sgn[:], dr["sgn"], writes=R)
        S.op("act", lambda e: e.activation(out=stp[:], in_=stp[:], func=AF.Exp), reads=R, writes=R)
        dv(lambda e: e.tensor_tensor(out=th[:], in0=aim[:], in1=stp[:], op=ALU.mult))
        dv(lambda e: e.tensor_tensor(out=t1[:], in0=are[:], in1=stp[:], op=ALU.mult))
        S.op("act", lambda e: e.activation(out=r[:], in_=t1[:], func=AF.Exp), reads=R, writes=R)
        dv(lambda e: e.memset(halfpi[:], float(np.pi / 2)))
        S.op("act", lambda e: e.activation(out=sn[:], in_=th[:], func=AF.Sin, scale=1.0 / 16), reads=R, writes=R)
        S.op("act", lambda e: e.activation(out=cs[:], in_=th[:], func=AF.Sin, scale=1.0 / 16, bias=halfpi[:]),
             reads=R, writes=R)
        for _ in range(4):
            dv(lambda e: e.tensor_tensor(out=t1[:], in0=sn[:], in1=cs[:], op=ALU.mult))
            dv(lambda e: e.tensor_tensor(out=t2[:], in0=sn[:], in1=sn[:], op=ALU.mult))
            dv(lambda e: e.tensor_scalar(out=sn[:], in0=t1[:], scalar1=2.0, scalar2=None, op0=ALU.mult))
            dv(lambda e: e.tensor_scalar(out=cs[:], in0=t2[:], scalar1=-2.0, scalar2=1.0, op0=ALU.mult, op1=ALU.add))
        dv(lambda e: e.tensor_tensor(out=lre[:], in0=r[:], in1=cs[:], op=ALU.mult))
        dv(lambda e: e.tensor_tensor(out=lim[:], in0=r[:], in1=sn[:], op=ALU.mult))
        dv(lambda e: e.reciprocal(out=t1[:], in_=r[:]))
        dv(lambda e: e.tensor_tensor(out=ire[:], in0=t1[:], in1=cs[:], op=ALU.mult))
        dv(lambda e: e.tensor_tensor(out=iim[:], in0=t1[:], in1=sn[:], op=ALU.mult))
        dv(lambda e: e.tensor_scalar(out=iim[:], in0=iim[:], scalar1=-1.0, scalar2=None, op0=ALU.mult))
        dv(lambda e: e.tensor_tensor(out=t1[:], in0=are[:], in1=are[:], op=ALU.mult))
        dv(lambda e: e.tensor_tensor(out=t2[:], in0=aim[:], in1=aim[:], op=ALU.mult))
        dv(lambda e: e.tensor_tensor(out=t1[:], in0=t1[:], in1=t2[:], op=ALU.add))
        dv(lambda e: e.reciprocal(out=t1[:], in_=t1[:]))
        dv(lambda e: e.tensor_scalar(out=t2[:], in0=lre[:], scalar1=-1.0, scalar2=None, op0=ALU.add))
        dv(lambda e: e.tensor_tensor(out=cfr[:], in0=t2[:], in1=are[:], op=ALU.mult))
        dv(lambda e: e.tensor_tensor(out=pr[:], in0=lim[:], in1=aim[:], op=ALU.mult))
        dv(lambda e: e.tensor_tensor(out=cfr[:], in0=cfr[:], in1=pr[:], op=ALU.add))
        dv(lambda e: e.tensor_tensor(out=cfr[:], in0=cfr[:], in1=t1[:], op=ALU.mult))
        dv(lambda e: e.tensor_tensor(out=cfi[:], in0=lim[:], in1=are[:], op=ALU.mult))
        dv(lambda e: e.tensor_tensor(out=pr[:], in0=t2[:], in1=aim[:], op=ALU.mult))
        dv(lambda e: e.tensor_tensor(out=cfi[:], in0=cfi[:], in1=pr[:], op=ALU.subtract))
        dv(lambda e: e.tensor_tensor(out=cfi[:], in0=cfi[:], in1=t1[:], op=ALU.mult))

        def power_table(bre_, bim_, nlev):
            dv(lambda e: e.memset(Ere[:, :, 0:1], 1.0))
            dv(lambda e: e.memset(Eim[:, :, 0:1], 0.0))
            dv(lambda e: e.tensor_copy(out=pr[:], in_=bre_[:]))
            dv(lambda e: e.tensor_copy(out=pi[:], in_=bim_[:]))
            for k in range(nlev):
                n = 1 << k
                prb = pr[:].unsqueeze(2).to_broadcast([128, 16, n])
                pib = pi[:].unsqueeze(2).to_broadcast([128, 16, n])
                dv(lambda e, n=n, prb=prb: e.tensor_tensor(out=ta[:, :, 0:n], in0=Ere[:, :, 0:n], in1=prb, op=ALU.mult))
                dv(lambda e, n=n, pib=pib: e.tensor_tensor(out=tb[:, :, 0:n], in0=Eim[:, :, 0:n], in1=pib, op=ALU.mult))
                dv(lambda e, n=n: e.tensor_tensor(out=Ere[:, :, n:2 * n], in0=ta[:, :, 0:n], in1=tb[:, :, 0:n],
                                                  op=ALU.subtract))
                dv(lambda e, n=n, pib=pib: e.tensor_tensor(out=ta[:, :, 0:n], in0=Ere[:, :, 0:n], in1=pib, op=ALU.mult))
                dv(lambda e, n=n, prb=prb: e.tensor_tensor(out=tb[:, :, 0:n], in0=Eim[:, :, 0:n], in1=prb, op=ALU.mult))
                dv(lambda e, n=n: e.tensor_tensor(out=Eim[:, :, n:2 * n], in0=ta[:, :, 0:n], in1=tb[:, :, 0:n],
                                                  op=ALU.add))
                if k < nlev - 1:
                    dv(lambda e: e.tensor_tensor(out=t1[:], in0=pr[:], in1=pr[:], op=ALU.mult))
                    dv(lambda e: e.tensor_tensor(out=t2[:], in0=pi[:], in1=pi[:], op=ALU.mult))
                    dv(lambda e: e.tensor_tensor(out=pi[:], in0=pr[:], in1=pi[:], op=ALU.mult))
                    dv(lambda e: e.tensor_scalar(out=pi[:], in0=pi[:], scalar1=2.0, scalar2=None, op0=ALU.mult))
                    dv(lambda e: e.tensor_tensor(out=pr[:], in0=t1[:], in1=t2[:], op=ALU.subtract))

        power_table(lre, lim, 8)
        o_t1 = 2048
        o_t2 = 2048 + 2048
        o_c = 2048 + 4096
        S.dma(tab[l][:, o_t1:o_t1 + 2048].rearrange("p (g t) -> p g t", t=128), Ere[:, :, 0:128], reads=R)
        dv(lambda e: e.tensor_scalar(out=ta[:], in0=Eim[:, :, 0:128], scalar1=sgn[:, 0:1], scalar2=None, op0=ALU.mult))
        S.dma(tab[l][:, o_t2:o_t2 + 2048].rearrange("p (g t) -> p g t", t=128), ta[:], reads=R)
        dv(lambda e: e.tensor_copy(out=outc[:, 0:16], in_=Ere[:, :, 128]))
        dv(lambda e: e.tensor_scalar(out=outc[:, 16:32], in0=Eim[:, :, 128], scalar1=sgn[:, 0:1], scalar2=None,
                                     op0=ALU.mult))
        dv(lambda e: e.tensor_scalar(out=outc[:, 32:48], in0=Eim[:, :, 128], scalar1=sgn[:, 1:2], scalar2=None,
                                     op0=ALU.mult))
        dv(lambda e: e.memset(outc[:, 48:64], 0.0))
        S.dma(tab[l][:, o_c:o_c + 64], outc[:], reads=R)
        S.end_phase()
        power_table(ire, iim, 7)
        ptr = [ps(ph, "t_ptr%d" % i, [128, 512], F32) for i in range(4)]
        arai = sb(ph, "t_arai", [128, 2, 16, 64], F32)
        for ri, E in ((0, Ere), (1, Eim)):
            for g in range(16):
                S.op("pe", lambda e, g=g, E=E, ri=ri: e.transpose(
                    out=ptr[ri * 2 + g // 8][:, (g % 8) * 64:(g % 8 + 1) * 64], in_=E[0:64, g, 0:128],
                    identity=identf[0:64, 0:64]), reads=R + ["identf"], writes=["t_ptr"])
            for hh in range(2):
                S.op("act", lambda e, hh=hh, ri=ri: e.copy(
                    out=arai[:, ri, hh * 8:(hh + 1) * 8, :].rearrange("p g n -> p (g n)"), in_=ptr[ri * 2 + hh][:]),
                    reads=["t_ptr"], writes=["t_arai"])
        S.dma(tab[l][:, 0:2048], arai[:].rearrange("p r g n -> p (r g n)"), reads=["t_arai"])
        bre = sb(ph, "t_bre", [64, 16, 16], F32)
        bim = sb(ph, "t_bim", [64, 16, 16], F32)
        Bre = sb(ph, "t_Bre", [64, 16, 16], F32)
        Bim = sb(ph, "t_Bim", [64, 16, 16], F32)
        tq = sb(ph, "t_tq", [64, 16, 16], F32)
        bmask = sb(ph, "t_bmask", [128, 8], F32)
        BT = sb(ph, "t_BT", [128, 2, 128], F32)
        Bblk = sb(ph, "t_Bblk", [128, 2, 8, 128], BF16)
        S.dma(bre[:], dr["bre"][l], writes=R)
        S.dma(bim[:], dr["bim"][l], writes=R)
        S.dma(bmask[:], dr["bmask"], writes=R)
        cfrb = cfr[0:64, :].unsqueeze(2).to_broadcast([64, 16, 16])
        cfib = cfi[0:64, :].unsqueeze(2).to_broadcast([64, 16, 16])
        dv(lambda e: e.tensor_tensor(out=Bre[:], in0=bre[:], in1=cfrb, op=ALU.mult))
        dv(lambda e: e.tensor_tensor(out=tq[:], in0=bim[:], in1=cfib, op=ALU.mult))
        dv(lambda e: e.tensor_tensor(out=Bre[:], in0=Bre[:], in1=tq[:], op=ALU.subtract))
        dv(lambda e: e.tensor_tensor(out=Bim[:], in0=bim[:], in1=cfrb, op=ALU.mult))
        dv(lambda e: e.tensor_tensor(out=tq[:], in0=bre[:], in1=cfib, op=ALU.mult))
        dv(lambda e: e.tensor_tensor(out=Bim[:], in0=Bim[:], in1=tq[:], op=ALU.add))
        for half in range(2):
            for ri, Bsrc in ((0, Bre), (1, Bim)):
                S.op("pe", lambda e, half=half, ri=ri, Bsrc=Bsrc: e.transpose(
                    out=ptr[0][:, (half * 2 + ri) * 64:(half * 2 + ri + 1) * 64],
                    in_=Bsrc[:, half * 8:(half + 1) * 8, :].rearrange("n g p -> n (g p)"),
                    identity=identf[0:64, 0:64]), reads=R + ["identf", "t_arai"], writes=["t_ptr0b"])
        S.op("act", lambda e: e.copy(out=BT[:].rearrange("p h c -> p (h c)"), in_=ptr[0][:, 0:256]),
             reads=["t_ptr0b"], writes=R)
        for half in range(2):
            S.op("dve", lambda e, half=half: e.tensor_tensor(
                out=Bblk[:, half], in0=BT[:, half, :].unsqueeze(1).to_broadcast([128, 8, 128]),
                in1=bmask[:].unsqueeze(2).to_broadcast([128, 8, 128]), op=ALU.mult), reads=R, writes=R)
        S.dma(tabb[l][:, 0:2048], Bblk[:].rearrange("p h g c -> p (h g c)"), reads=R)
        Cm = sb(ph, "t_Cm", [128, 16, 16], F32)
        mask2 = sb(ph, "t_mask2", [128, 16, 8], F32)
        Cpad = sb(ph, "t_Cpad", [128, 16, 8, 16], BF16)
        S.dma(Cm[0:64], dr["cre"][l], writes=R)
        S.dma(Cm[64:128], dr["cim"][l], writes=R)
        S.dma(mask2[:], dr["mask2"], writes=R)
        dv(lambda e: e.tensor_scalar(out=Cm[64:128], in0=Cm[64:128], scalar1=-1.0, scalar2=None, op0=ALU.mult))
        dv(lambda e: e.tensor_tensor(out=Cpad[:], in0=Cm[:].unsqueeze(2).to_broadcast([128, 16, 8, 16]),
                                     in1=mask2[:].unsqueeze(3).to_broadcast([128, 16, 8, 16]), op=ALU.mult))
        S.dma(tabb[l][:, 2048:4096], Cpad[:].rearrange("p g a b -> p (g a b)"), reads=R)
        S.end_phase()


def mixer(nc, S, dr, l, b, X, hT, GG, cols, tab, tabb, sb, ps, norm_to_hT, make_GG, resid_update, rstd_from_ss,
          identb, identf, onesb, att, ssm, pool):
    with ExitStack() as ph:
        norm_to_hT(ph, l, b, 0, 1)
        make_GG(ph, l, b, 2)
        S.end_phase()
    with ExitStack() as mx:
        yaT = sb(mx, "yaT", [128, 4, SEQ], BF16)
        if att:
            attention(nc, S, dr, l, b, hT, yaT, sb, ps, rstd_from_ss, identb, onesb)
        else:
            S.op("dve", lambda e: e.memset(yaT[:], 0.0), writes=["yaT"])
            S.end_phase()
        ysT = sb(mx, "ysT", [128, 2, SEQ], BF16)
        ypT = sb(mx, "ypT", [128, 2, SEQ], BF16)
        if ssm:
            ssm_branch(nc, S, dr, l, b, hT, ysT, tab, tabb, sb, ps)
        else:
            S.op("dve", lambda e: e.memset(ysT[:], 0.0), writes=["ysT"])
            S.end_phase()
        if pool:
            pool_branch(nc, S, dr, l, b, hT, ypT, sb, ps)
        else:
            S.op("dve", lambda e: e.memset(ypT[:], 0.0), writes=["ypT"])
            S.end_phase()
        merge(nc, S, dr, l, b, X, hT, GG, yaT, ysT, ypT, sb, ps, resid_update)


def merge(nc, S, dr, l, b, X, hT, GG, yaT, ysT, ypT, sb, ps, resid_update):
    with ExitStack() as ph:
        wg = [sb(ph, "m_wg%d" % i, [128, 8, 128], BF16) for i in range(4)]
        pa = sb(ph, "m_pa", [128, 4, D], BF16)
        pb = sb(ph, "m_pb", [128, 2, D], BF16)
        pc = sb(ph, "m_pc", [128, 2, D], BF16)
        wo = sb(ph, "m_wo", [128, 8, D], BF16)
        mT = sb(ph, "m_mT", [128, 8, 1024], BF16)
        sg = [sb(ph, "m_sg%d" % i, [128, 512], F32) for i in range(2)]
        tm = sb(ph, "m_tm", [128, 512], F32)
        ac = sb(ph, "m_ac", [128, 512], F32)
        ssy = sb(ph, "m_ssy", [128, 4], F32)
        junk = sb(ph, "m_junk", [128, 512], BF16)
        tmp = sb(ph, "m_tmp", [128, D], F32)
        pg = [ps(ph, "m_pg%d" % i, [128, 512], F32) for i in range(2)]
        pp = [ps(ph, "m_pp%d" % i, [128, 512], F32) for i in range(2)]
        py = [ps(ph, "m_py%d" % i, [128, 512], F32) for i in range(4)]
        for k in range(4):
            cast_load(S, pa[:, k, :], dr["pa"][l][:, k, :], "m_pa")
        cast_load(S, pb[:], dr["pb"][l], "m_pb")
        cast_load(S, pc[:], dr["pc"][l], "m_pc")
        for k in range(8):
            cast_load(S, wo[:, k, :], dr["wout"][l][:, k, :], "m_wo")
        brs = ((yaT, pa, 4, "yaT", "m_pa"), (ysT, pb, 2, "ysT", "m_pb"), (ypT, pc, 2, "ypT", "m_pc"))
        nl_ = 0
        ng = 0
        for half in range(2):
            for dc in range(8):
                wgs = []
                for br in range(3):
                    wb = wg[nl_ % 4]
                    nl_ += 1
                    cast_load(S, wb[:], dr["wg"][l, br * 8 + dc], _nm(wb))
                    wgs.append(wb)
                for s2 in range(2):
                    t0 = half * 1024 + s2 * 512
                    hreads = [("hT", t0 // 128 + q) for q in range(4)]
                    for br in range(3):
                        yT, pw, nk, yn, pn = brs[br]
                        g_ = pg[ng % 2]
                        p_ = pp[ng % 2]
                        s_ = sg[ng % 2]
                        ng += 1
                        for k in range(8):
                            S.op("pe", lambda e, k=k, g_=g_, wb=wgs[br], t0=t0: e.matmul(
                                g_[:], lhsT=wb[:, k, :], rhs=hT[:, k, t0:t0 + 512], start=(k == 0), stop=(k == 7)),
                                reads=[_nm(wgs[br])] + hreads, writes=[_nm(g_)])
                        for k in range(nk):
                            S.op("pe", lambda e, k=k, p_=p_, pw=pw, yT=yT, t0=t0, nk=nk, dc=dc: e.matmul(
                                p_[:], lhsT=pw[:, k, dc * 128:(dc + 1) * 128], rhs=yT[:, k, t0:t0 + 512],
                                start=(k == 0), stop=(k == nk - 1)), reads=[pn, yn], writes=[_nm(p_)])
                        S.op("act", lambda e, g_=g_, s_=s_: e.activation(out=s_[:], in_=g_[:], func=AF.Sigmoid),
                             reads=[_nm(g_)], writes=[_nm(s_)])
                        if br == 0:
                            S.op("dve", lambda e, p_=p_, s_=s_: e.tensor_tensor(out=ac[:], in0=p_[:], in1=s_[:], op=ALU.mult),
                                 reads=[_nm(p_), _nm(s_)], writes=["m_ac"])
                        elif br == 1:
                            S.op("dve", lambda e, p_=p_, s_=s_: e.tensor_tensor(out=tm[:], in0=p_[:], in1=s_[:], op=ALU.mult),
                                 reads=[_nm(p_), _nm(s_)], writes=["m_tm"])
                            S.op("dve", lambda e: e.tensor_tensor(out=ac[:], in0=ac[:], in1=tm[:], op=ALU.add),
                                 reads=["m_tm"], writes=["m_ac"])
                        else:
                            S.op("dve", lambda e, p_=p_, s_=s_: e.tensor_tensor(out=tm[:], in0=p_[:], in1=s_[:], op=ALU.mult),
                                 reads=[_nm(p_), _nm(s_)], writes=["m_tm"])
                            S.op("dve", lambda e, dc=dc, s2=s2: e.tensor_tensor(
                                out=mT[:, dc, s2 * 512:(s2 + 1) * 512], in0=ac[:], in1=tm[:], op=ALU.add),
                                reads=["m_tm", "m_ac"], writes=[("m_mT", dc)])
            for tt in range(8):
                t = half * 8 + tt
                pyy = py[(tt % 2) * 2:(tt % 2) * 2 + 2]
                for i in range(2):
                    for k in range(8):
                        S.op("pe", lambda e, i=i, k=k, tt=tt, pyy=pyy: e.matmul(
                            pyy[i][:], lhsT=mT[:, k, tt * 128:(tt + 1) * 128], rhs=wo[:, k, i * 512:(i + 1) * 512],
                            start=(k == 0), stop=(k == 7)), reads=[("m_mT", k), "m_wo"], writes=[_nm(pyy[i])])
                resid_update((ssy, junk, tmp), t, pyy)
        S.end_phase()


def pool_branch(nc, S, dr, l, b, hT, ypT, sb, ps):
    with ExitStack() as ph:
        w1 = sb(ph, "p_w1", [128, 8, 256], BF16)
        wp = sb(ph, "p_wp", [128, 2, 128], F32)
        wpb = sb(ph, "p_wpb", [128, 2, 128], BF16)
        psc = sb(ph, "p_psc", [128, 2], F32)
        invt = sb(ph, "p_invt", [128, 16], F32)
        U = sb(ph, "p_U", [128, 16 + SEQ], F32)
        s_a = sb(ph, "p_sa", [128, 16 + SEQ], F32)
        s_b = sb(ph, "p_sb", [128, 16 + SEQ], F32)
        pl = sb(ph, "p_pl", [128, SEQ], BF16)
        fx = sb(ph, "p_fx", [128, 16], F32)
        pu = [ps(ph, "p_pu%d" % i, [128, 512], F32) for i in range(4)]
        cast_load(S, w1[:], dr["w1"][l][:, :, 680:936], "p_w1")
        S.dma(psc[:], dr["pscale"][l], writes=["p_psc"])
        S.dma(invt[:], dr["invt"], writes=["p_invt"])
        S.op("dve", lambda e: e.memset(wp[:], 0.0), writes=["p_wp"])
        for g in range(4):
            S.dma(wp[(g % 2) * 64:(g % 2 + 1) * 64, g // 2, (g % 2) * 64:(g % 2 + 1) * 64], dr["wpool"][l, g],
                  reads=[], writes=["p_wp"])
        S.op("dve", lambda e: e.tensor_copy(out=wpb[:], in_=wp[:]), reads=["p_wp"], writes=["p_wpb"])
        for buf in (U, s_a, s_b):
            S.op("dve", lambda e, buf=buf: e.memset(buf[:, 0:16], 0.0), writes=[_nm(buf)])
        for ct in range(2):
            for tc in range(4):
                for k in range(8):
                    S.op("pe", lambda e, k=k, tc=tc, ct=ct: e.matmul(
                        pu[tc][:], lhsT=w1[:, k, ct * 128:(ct + 1) * 128], rhs=hT[:, k, tc * 512:(tc + 1) * 512],
                        start=(k == 0), stop=(k == 7)), reads=["p_w1"] + [("hT", tc * 4 + q) for q in range(4)],
                        writes=[_nm(pu[tc])])
                S.op("act", lambda e, tc=tc: e.copy(out=U[:, 16 + tc * 512:16 + (tc + 1) * 512], in_=pu[tc][:]),
                     reads=[_nm(pu[tc])], writes=["p_U"])
            def shadd(dst, src, sh):
                S.op("dve", lambda e: e.tensor_tensor(out=dst[:, 16:16 + SEQ], in0=src[:, 16:16 + SEQ],
                                                      in1=src[:, 16 - sh:16 - sh + SEQ], op=ALU.add),
                     reads=[_nm(src)], writes=[_nm(dst)])
            shadd(s_a, U, 1)
            shadd(s_b, s_a, 2)
            if ct == 0:
                halves = ((0, s_a, 2), (64, s_b, 4))
            else:
                shadd(s_a, s_b, 4)
                shadd(s_b, s_a, 8)
                halves = ((0, s_a, 8), (64, s_b, 16))
            for (p0, sbuf, win) in halves:
                S.op("dve", lambda e, p0=p0, sbuf=sbuf, win=win: e.scalar_tensor_tensor(
                    out=pl[p0:p0 + 64, :], in0=sbuf[p0:p0 + 64, 16:16 + SEQ], scalar=1.0 / win,
                    in1=U[p0:p0 + 64, 16:16 + SEQ], op0=ALU.mult, op1=ALU.subtract),
                    reads=[_nm(sbuf), "p_U"], writes=["p_pl"])
                S.op("dve", lambda e, p0=p0, sbuf=sbuf, win=win: e.tensor_tensor(
                    out=fx[p0:p0 + 64, 0:win - 1], in0=sbuf[p0:p0 + 64, 16:16 + win - 1],
                    in1=invt[p0:p0 + 64, 0:win - 1], op=ALU.mult), reads=[_nm(sbuf), "p_invt"], writes=["p_fx"])
                S.op("dve", lambda e, p0=p0, win=win: e.tensor_tensor(
                    out=pl[p0:p0 + 64, 0:win - 1], in0=fx[p0:p0 + 64, 0:win - 1], in1=U[p0:p0 + 64, 16:16 + win - 1],
                    op=ALU.subtract), reads=["p_fx", "p_U"], writes=["p_pl"])
            for tc in range(4):
                S.op("pe", lambda e, tc=tc, ct=ct: e.matmul(pu[tc][:], lhsT=wpb[:, ct, :],
                                                         rhs=pl[:, tc * 512:(tc + 1) * 512], start=True, stop=True),
                     reads=["p_wpb", "p_pl"], writes=[_nm(pu[tc])])
                S.op("act", lambda e, tc=tc, ct=ct: e.activation(
                    out=ypT[:, ct, tc * 512:(tc + 1) * 512], in_=pu[tc][:], func=AF.Copy, scale=psc[:, ct:ct + 1]),
                    reads=[_nm(pu[tc]), "p_psc"], writes=["ypT"])
        S.end_phase()


def ssm_branch(nc, S, dr, l, b, hT, ysT, tab, tabb, sb, ps):
    with ExitStack() as ph:
        uT = sb(ph, "s_uT", [128, 2, SEQ], BF16)
        with ExitStack() as p1:
            w1 = sb(p1, "s_w1", [128, 8, 256], BF16)
            pj = [ps(p1, "s_pj%d" % i, [128, 512], F32) for i in range(2)]
            cast_load(S, w1[:], dr["w1"][l][:, :, 424:680], "s_w1")
            for ct in range(2):
                for tc in range(4):
                    pp = pj[tc % 2]
                    for k in range(8):
                        S.op("pe", lambda e, k=k, tc=tc, ct=ct, pp=pp: e.matmul(
                            pp[:], lhsT=w1[:, k, ct * 128:(ct + 1) * 128], rhs=hT[:, k, tc * 512:(tc + 1) * 512],
                            start=(k == 0), stop=(k == 7)), reads=["s_w1"] + [("hT", tc * 4 + q) for q in range(4)],
                            writes=[_nm(pp)])
                    S.op("act", lambda e, tc=tc, ct=ct, pp=pp: e.copy(out=uT[:, ct, tc * 512:(tc + 1) * 512], in_=pp[:]),
                         reads=[_nm(pp)], writes=["s_uT"])
            S.end_phase()
        zT = sb(ph, "s_zT", [128, 2, SEQ], BF16)
        arai = sb(ph, "s_arai", [128, 2, 16, 64], F32)
        T1 = sb(ph, "s_T1", [128, 16, 128], F32)
        T2 = sb(ph, "s_T2", [128, 16, 128], F32)
        pcs = sb(ph, "s_pcs", [128, 64], F32)
        Bblk = sb(ph, "s_Bblk", [128, 2, 1024], BF16)
        Cpad = sb(ph, "s_Cpad", [128, 16, 128], BF16)
        triub = sb(ph, "s_triub", [128, 128], BF16)
        dsk = sb(ph, "s_dsk", [128, 2], F32)
        bgl = sb(ph, "s_bgl", [128, 2], F32)
        wgl = sb(ph, "s_wgl", [128, 2, 256], BF16)
        t1s = [sb(ph, "s_t1%d" % i, [128, 4, 2, 64], BF16) for i in range(2)]
        t2s = [sb(ph, "s_t2%d" % i, [128, 4, 2, 64], BF16) for i in range(2)]
        v = sb(ph, "s_v", [128, 16, 2, 64], BF16)
        vsw = sb(ph, "s_vsw", [128, 16, 2, 64], BF16)
        xs = sb(ph, "s_xs", [128, 16, 128], BF16)
        xas = [sb(ph, "s_xa%d" % i, [128, 128], F32) for i in range(2)]
        xbs = [sb(ph, "s_xb%d" % i, [128, 128], F32) for i in range(2)]
        cA = [sb(ph, "s_cA%d" % i, [128, 16], F32) for i in range(2)]
        cB = [sb(ph, "s_cB%d" % i, [128, 16], F32) for i in range(2)]
        sA = sb(ph, "s_sA", [128, 16], F32)
        sB = sb(ph, "s_sB", [128, 16], F32)
        q1 = sb(ph, "s_q1", [128, 16], F32)
        yv = sb(ph, "s_yv", [128, 256], F32)
        sg = sb(ph, "s_sg", [128, 512], BF16)
        pbu = [ps(ph, "s_pbu%d" % i, [128, 512], F32) for i in range(2)]
        pA = [ps(ph, "s_pA%d" % i, [128, 512], F32) for i in range(2)]
        pB = [ps(ph, "s_pB%d" % i, [128, 512], F32) for i in range(2)]
        pyy = ps(ph, "s_py", [128, 512], F32)
        pg = ps(ph, "s_pg", [128, 512], F32)
        S.dma(arai[:].rearrange("p r g n -> p (r g n)"), tab[l][:, 0:2048], writes=["s_arai"])
        S.dma(T1[:].rearrange("p g t -> p (g t)"), tab[l][:, 2048:4096], writes=["s_T1"])
        S.dma(T2[:].rearrange("p g t -> p (g t)"), tab[l][:, 4096:6144], writes=["s_T2"])
        S.dma(pcs[:], tab[l][:, 6144:6208], writes=["s_pcs"])
        S.dma(Bblk[:].rearrange("p h c -> p (h c)"), tabb[l][:, 0:2048], writes=["s_Bblk"])
        S.dma(Cpad[:].rearrange("p g c -> p (g c)"), tabb[l][:, 2048:4096], writes=["s_Cpad"])
        S.dma(triub[:], dr["triub"], writes=["s_triub"])
        S.dma(dsk[:], dr["dskip"][l], writes=["s_dsk"])
        S.dma(bgl[:], dr["bglu"][l], writes=["s_bgl"])
        cast_load(S, wgl[:], dr["wglu"][l], "s_wgl")
        for i in range(2):
            S.op("dve", lambda e, i=i: e.memset(cA[i][:], 0.0), writes=[_nm(cA[i])])
            S.op("dve", lambda e, i=i: e.memset(cB[i][:], 0.0), writes=[_nm(cB[i])])
        Pr = pcs[:, 0:16]
        PiA = pcs[:, 16:32]
        PiB = pcs[:, 32:48]
        def stage1(t, half, q2):
            g0 = half * 8 + q2 * 4
            t1 = t1s[q2]
            t2 = t2s[q2]
            S.op("pe", lambda e: e.matmul(
                pbu[q2][:], lhsT=uT[:, half, t * 128:(t + 1) * 128], rhs=Bblk[:, half, q2 * 512:(q2 + 1) * 512],
                start=True, stop=True), reads=["s_uT", "s_Bblk"], writes=[_nm(pbu[q2])])
            bu = pbu[q2][:].rearrange("p (g r n) -> p g r n", g=4, r=2)
            Ar = arai[:, 0, g0:g0 + 4, :]
            Ai = arai[:, 1, g0:g0 + 4, :]
            S.op("dve", lambda e: e.tensor_tensor(
                out=t1[:], in0=bu, in1=Ar.unsqueeze(2).to_broadcast([128, 4, 2, 64]), op=ALU.mult),
                reads=[_nm(pbu[q2]), "s_arai"], writes=[_nm(t1)])
            S.op("dve", lambda e: e.tensor_tensor(
                out=t2[:, :, 0, :], in0=bu[:, :, 1, :], in1=Ai, op=ALU.mult),
                reads=[_nm(pbu[q2]), "s_arai"], writes=[_nm(t2)])
            S.op("dve", lambda e: e.tensor_tensor(
                out=t2[:, :, 1, :], in0=bu[:, :, 0, :], in1=Ai, op=ALU.mult),
                reads=[_nm(pbu[q2]), "s_arai"], writes=[_nm(t2)])
            S.op("pool", lambda e: e.tensor_tensor(
                out=v[:, g0:g0 + 4, 0, :], in0=t1[:, :, 0, :], in1=t2[:, :, 0, :], op=ALU.subtract),
                reads=[_nm(t1), _nm(t2)], writes=[("s_v", g0)])
            S.op("pool", lambda e: e.tensor_tensor(
                out=v[:, g0:g0 + 4, 1, :], in0=t1[:, :, 1, :], in1=t2[:, :, 1, :], op=ALU.add),
                reads=[_nm(t1), _nm(t2)], writes=[("s_v", g0)])
            S.op("pool", lambda e: e.tensor_tensor(
                out=vsw[:, g0:g0 + 4, 1, :], in0=t1[:, :, 0, :], in1=t2[:, :, 0, :], op=ALU.subtract),
                reads=[_nm(t1), _nm(t2)], writes=[("s_vsw", g0)])
            S.op("pool", lambda e: e.tensor_tensor(
                out=vsw[:, g0:g0 + 4, 0, :], in0=t1[:, :, 1, :], in1=t2[:, :, 1, :], op=ALU.add),
                reads=[_nm(t1), _nm(t2)], writes=[("s_vsw", g0)])
            pa_, pb_ = pA[q2], pB[q2]
            for gg in range(4):
                g = g0 + gg
                S.op("pe", lambda e: e.matmul(
                    pa_[:, gg * 128:(gg + 1) * 128], lhsT=v[:, g].rearrange("p r n -> p (r n)"), rhs=triub[:],
                    start=True, stop=True), reads=[("s_v", g0), "s_triub"], writes=[_nm(pa_)])
                S.op("pe", lambda e: e.matmul(
                    pb_[:, gg * 128:(gg + 1) * 128], lhsT=vsw[:, g].rearrange("p r n -> p (r n)"), rhs=triub[:],
                    start=True, stop=True), reads=[("s_vsw", g0), "s_triub"], writes=[_nm(pb_)])

        def stage2(t, half, q2):
            cAo, cBo = cA[t % 2], cB[t % 2]
            cAn, cBn = cA[(t + 1) % 2], cB[(t + 1) % 2]
            g0 = half * 8 + q2 * 4
            pa_, pb_ = pA[q2], pB[q2]
            A127 = pa_[:].rearrange("p (g t) -> p g t", t=128)[:, :, 127]
            B127 = pb_[:].rearrange("p (g t) -> p g t", t=128)[:, :, 127]
            gs = slice(g0, g0 + 4)
            S.op("dve", lambda e: e.tensor_tensor(out=sA[:, gs], in0=A127, in1=cAo[:, gs], op=ALU.add),
                 reads=[_nm(pa_), _nm(cAo)], writes=["s_sA"])
            S.op("dve", lambda e: e.tensor_tensor(out=sB[:, gs], in0=B127, in1=cBo[:, gs], op=ALU.add),
                 reads=[_nm(pb_), _nm(cBo)], writes=["s_sB"])
            for gg in range(4):
                g = g0 + gg
                xa = xas[gg % 2]
                xb = xbs[gg % 2]
                S.op("dve", lambda e: e.scalar_tensor_tensor(
                    out=xa[:], in0=pa_[:, gg * 128:(gg + 1) * 128], scalar=cAo[:, g:g + 1], in1=T1[:, g, :],
                    op0=ALU.add, op1=ALU.mult), reads=[_nm(pa_), _nm(cAo), "s_T1"], writes=[_nm(xa)])
                S.op("dve", lambda e: e.scalar_tensor_tensor(
                    out=xb[:], in0=pb_[:, gg * 128:(gg + 1) * 128], scalar=cBo[:, g:g + 1], in1=T2[:, g, :],
                    op0=ALU.add, op1=ALU.mult), reads=[_nm(pb_), _nm(cBo), "s_T2"], writes=[_nm(xb)])
                S.op("pool", lambda e: e.tensor_tensor(out=xs[:, g, :], in0=xa[:], in1=xb[:], op=ALU.add),
                     reads=[_nm(xa), _nm(xb)], writes=[("s_xs", g)])
            S.op("dve", lambda e: e.tensor_tensor(out=cAn[:, gs], in0=sA[:, gs], in1=Pr[:, gs], op=ALU.mult),
                 reads=["s_sA", "s_pcs"], writes=[_nm(cAn)])
            S.op("dve", lambda e: e.tensor_tensor(out=q1[:, gs], in0=sB[:, gs], in1=PiA[:, gs], op=ALU.mult),
                 reads=["s_sB", "s_pcs"], writes=["s_q1"])
            S.op("dve", lambda e: e.tensor_tensor(out=cAn[:, gs], in0=cAn[:, gs], in1=q1[:, gs], op=ALU.add),
                 reads=["s_q1"], writes=[_nm(cAn)])
            S.op("dve", lambda e: e.tensor_tensor(out=cBn[:, gs], in0=sB[:, gs], in1=Pr[:, gs], op=ALU.mult),
                 reads=["s_sB", "s_pcs"], writes=[_nm(cBn)])
            S.op("dve", lambda e: e.tensor_tensor(out=q1[:, gs], in0=sA[:, gs], in1=PiB[:, gs], op=ALU.mult),
                 reads=["s_sA", "s_pcs"], writes=["s_q1"])
            S.op("dve", lambda e: e.tensor_tensor(out=cBn[:, gs], in0=cBn[:, gs], in1=q1[:, gs], op=ALU.add),
                 reads=["s_q1"], writes=[_nm(cBn)])
            if q2 == 1:
                for g8 in range(8):
                    g = half * 8 + g8
                    S.op("pe", lambda e: e.matmul(
                        pyy[:, half * 128:(half + 1) * 128], lhsT=Cpad[:, g, :], rhs=xs[:, g, :],
                        start=(g8 == 0), stop=(g8 == 7)), reads=[("s_xs", g), "s_Cpad"], writes=["s_py"])
                S.op("dve", lambda e: e.scalar_tensor_tensor(
                    out=yv[:, half * 128:(half + 1) * 128], in0=uT[:, half, t * 128:(t + 1) * 128],
                    scalar=dsk[:, half:half + 1], in1=pyy[:, half * 128:(half + 1) * 128], op0=ALU.mult, op1=ALU.add),
                    reads=["s_py", "s_uT", "s_dsk"], writes=["s_yv"])
                S.op("act", lambda e: e.activation(
                    out=zT[:, half, t * 128:(t + 1) * 128], in_=yv[:, half * 128:(half + 1) * 128],
                    func=AF.Gelu_apprx_tanh), reads=["s_yv"], writes=[("s_zT", t // 4)])
            if q2 == 1 and half == 1 and t % 4 == 3:
                tc = t // 4
                for ct in range(2):
                    for k in range(2):
                        S.op("pe", lambda e: e.matmul(
                            pg[:], lhsT=wgl[:, k, ct * 128:(ct + 1) * 128], rhs=zT[:, k, tc * 512:(tc + 1) * 512],
                            start=(k == 0), stop=(k == 1)), reads=["s_wgl", ("s_zT", tc)], writes=["s_pg"])
                    S.op("act", lambda e: e.activation(out=sg[:], in_=pg[:], func=AF.Sigmoid,
                                                       bias=bgl[:, ct:ct + 1]),
                         reads=["s_pg", "s_bgl"], writes=["s_sg"])
                    S.op("dve", lambda e: e.tensor_tensor(
                        out=ysT[:, ct, tc * 512:(tc + 1) * 512], in0=zT[:, ct, tc * 512:(tc + 1) * 512], in1=sg[:],
                        op=ALU.mult), reads=["s_sg", ("s_zT", tc)], writes=["ysT"])

        units = [(t, half, q2) for t in range(NT) for half in range(2) for q2 in range(2)]
        stage1(*units[0])
        for ui, u in enumerate(units):
            if ui + 1 < len(units):
                stage1(*units[ui + 1])
            stage2(*u)
        S.end_phase()


def attention(nc, S, dr, l, b, hT, yaT, sb, ps, rstd_from_ss, identb, onesb):
    with ExitStack() as at:
        cqnT = sb(at, "a_cqnT", [128, 2, SEQ], BF16)
        ckvT = sb(at, "a_ckvT", [128, SEQ], BF16)
        Vg = sb(at, "a_V", [128, NT, 128], BF16)
        kidT = sb(at, "a_kidT", [128, SEQ], BF16)
        Wsc = sb(at, "a_Wsc", [128, NT, 8], F32)
        with ExitStack() as ph:
            w1 = sb(ph, "a_w1", [128, 8, 424], BF16)
            gcq = sb(ph, "a_gcq", [128, 2], F32)
            gkv = sb(ph, "a_gkv", [128, 128], F32)
            ss = sb(ph, "a_ss", [128, 2], F32)
            junk = sb(ph, "a_junk", [128, 256], BF16)
            cqn = sb(ph, "a_cqn", [128, 256], BF16)
            kid4 = sb(ph, "a_kid4", [128, 4, 32], BF16)
            pz = [ps(ph, "a_pz%d" % i, [128, 512], F32) for i in range(2)]
            pt = [ps(ph, "a_pt%d" % i, [128, 1024], BF16) for i in range(2)]
            cast_load(S, w1[:], dr["w1"][l][:, :, 0:424], "a_w1")
            S.dma(gcq[:], dr["gcq"][l], writes=["a_gcq"])
            S.dma(gkv[:], dr["gckv"][l:l + 1, :].to_broadcast([128, 128]), writes=["a_gkv"])
            for t in range(NT):
                p = pz[t % 2]
                q = pt[t % 2]
                for k in range(8):
                    S.op("pe", lambda e, k=k, t=t, p=p: e.matmul(p[:, 0:424], lhsT=hT[:, k, t * 128:(t + 1) * 128],
                                                                 rhs=w1[:, k, :], start=(k == 0), stop=(k == 7)),
                         reads=["a_w1", ("hT", t)], writes=[_nm(p)])
                S.op("act", lambda e, p=p: e.activation(out=junk[:], in_=p[:, 0:256], func=AF.Square,
                                                        accum_out=ss[:, 0:1]), reads=[_nm(p)], writes=["a_junk", "a_ss"])
                S.op("act", lambda e, p=p: e.activation(out=junk[:, 0:128], in_=p[:, 256:384], func=AF.Square,
                                                        accum_out=ss[:, 1:2]), reads=[_nm(p)], writes=["a_junk", "a_ss"])
                S.op("dve", lambda e: e.tensor_scalar(out=ss[:, 0:1], in0=ss[:, 0:1], scalar1=1.0 / 256, scalar2=EPS,
                                                      op0=ALU.mult, op1=ALU.add), reads=["a_ss"], writes=["a_ss"])
                S.op("dve", lambda e: e.tensor_scalar(out=ss[:, 1:2], in0=ss[:, 1:2], scalar1=1.0 / 128, scalar2=EPS,
                                                      op0=ALU.mult, op1=ALU.add), reads=["a_ss"], writes=["a_ss"])
                S.op("act", lambda e: e.activation(out=ss[:], in_=ss[:], func=AF.Sqrt), reads=["a_ss"], writes=["a_ss"])
                S.op("dve", lambda e: e.reciprocal(out=ss[:], in_=ss[:]), reads=["a_ss"], writes=["a_ss"])
                S.op("dve", lambda e, p=p: e.tensor_scalar(out=cqn[:], in0=p[:, 0:256], scalar1=ss[:, 0:1], scalar2=None,
                                                           op0=ALU.mult), reads=[_nm(p), "a_ss"], writes=["a_cqn"])
                S.op("dve", lambda e, p=p, t=t: e.scalar_tensor_tensor(
                    out=Vg[:, t, :], in0=p[:, 256:384], scalar=ss[:, 1:2], in1=gkv[:], op0=ALU.mult, op1=ALU.mult),
                    reads=[_nm(p), "a_ss", "a_gkv"], writes=[("a_V", t)])
                S.op("dve", lambda e, p=p: e.tensor_copy(
                    out=kid4[:], in_=p[:, 384:416].unsqueeze(1).to_broadcast([128, 4, 32])),
                    reads=[_nm(p)], writes=["a_kid4"])
                S.op("dve", lambda e, p=p, t=t: e.tensor_scalar(out=Wsc[:, t, :], in0=p[:, 416:424], scalar1=IDXW,
                                                                scalar2=None, op0=ALU.mult),
                     reads=[_nm(p)], writes=["a_Wsc"])
                for c in range(2):
                    S.op("pe", lambda e, c=c, q=q: e.transpose(out=q[:, c * 128:(c + 1) * 128],
                                                              in_=cqn[:, c * 128:(c + 1) * 128], identity=identb[:]),
                         reads=["a_cqn", "identb"], writes=[_nm(q)])
                S.op("pe", lambda e, q=q, t=t: e.transpose(out=q[:, 256:384], in_=Vg[:, t, :], identity=identb[:]),
                     reads=[("a_V", t), "identb"], writes=[_nm(q)])
                S.op("pe", lambda e, q=q: e.transpose(out=q[:, 384:512], in_=kid4[:].rearrange("p a b -> p (a b)"),
                                                      identity=identb[:]), reads=["a_kid4", "identb"], writes=[_nm(q)])
                for c in range(2):
                    S.op("act", lambda e, c=c, q=q, t=t: e.activation(
                        out=cqnT[:, c, t * 128:(t + 1) * 128], in_=q[:, c * 128:(c + 1) * 128], func=AF.Copy,
                        scale=gcq[:, c:c + 1]), reads=[_nm(q), "a_gcq"], writes=[("a_cqnT", t)])
                S.op("dve", lambda e, q=q, t=t: e.tensor_copy(out=ckvT[:, t * 128:(t + 1) * 128], in_=q[:, 256:384]),
                     reads=[_nm(q)], writes=[("a_ckvT", t)])
                S.op("dve", lambda e, q=q, t=t: e.tensor_copy(out=kidT[:, t * 128:(t + 1) * 128], in_=q[:, 384:512]),
                     reads=[_nm(q)], writes=[("a_kidT", t)])
            S.end_phase()
        with ExitStack() as ph:
            wuq = sb(ph, "a_wuq", [128, 2, 1024], BF16)
            wqi = sb(ph, "a_wqi", [128, 2, 256], BF16)
            wuv = sb(ph, "a_wuv", [128, 8, 64], BF16)
            i4b = sb(ph, "a_i4b", [128, 512], BF16)
            altd = sb(ph, "a_altd", [128, 1024], BF16)
            at3 = sb(ph, "a_at3", [128, 2048], BF16)
            bt3 = sb(ph, "a_bt3", [128, 1024], BF16)
            mk4 = sb(ph, "a_mk4", [128, 4], F32)
            wuvp = sb(ph, "a_wuvp", [128, 8, 128], BF16)
            m0 = sb(ph, "a_m0", [128, 128], F32)
            pow2 = sb(ph, "a_pow2", [128, NIT + 1], F32)
            qT = sb(ph, "a_qT", [128, 8, 128], BF16)
            qiT = sb(ph, "a_qiT", [128, 8, 128], BF16)
            accs = [sb(ph, "a_acc%d" % i, [128, SEQ], F32) for i in range(2)]
            jk = sb(ph, "a_jk", [128, SEQ], mybir.dt.uint8)
            MBs = [sb(ph, "a_MB%d" % i, [128, SEQ], BF16) for i in range(2)]
            Rb = [sb(ph, "a_R%d" % i, [128, 512], BF16) for i in range(2)]
            wdg = sb(ph, "a_wdg", [128, 8, 128], BF16)
            PT = [sb(ph, "a_PT%d" % i, [128, 512], BF16) for i in range(2)]
            rec = sb(ph, "a_rec", [128, 512], F32)
            oT = sb(ph, "a_oT", [128, 1024], BF16)
            sm = sb(ph, "a_sm", [128, 8], F32)
            Wk = sb(ph, "a_Wk", [128, NIT + 1], F32)
            pS = [ps(ph, "a_pS%d" % i, [128, 512], F32) for i in range(2)]
            pL = [ps(ph, "a_pL%d" % i, [128, 512], F32) for i in range(2)]
            pQ = ps(ph, "a_pQ", [128, 512], F32)
            pACC = ps(ph, "a_pACC", [128, 512], F32)
            pO = ps(ph, "a_pO", [128, 512], F32)
            pD = ps(ph, "a_pD", [128, 512], F32)
            pY = pQ
            cast_load(S, wuq[:], dr["wuq"][l], "a_wuq")
            cast_load(S, wqi[:], dr["wqi"][l], "a_wqi")
            cast_load(S, wuv[:], dr["wuv"][l], "a_wuv")
            for nm, tl in (("i4b", i4b), ("altd", altd), ("at3", at3), ("bt3", bt3), ("m0", m0), ("pow2", pow2),
                           ("mk4", mk4)):
                S.dma(tl[:], dr[nm], writes=["a_" + nm])
            S.op("dve", lambda e: e.memset(wuvp[:], 0.0), writes=["a_wuvp"])
            for h in range(8):
                S.op("dve", lambda e, h=h: e.tensor_copy(out=wuvp[:, h, (h % 2) * 64:(h % 2 + 1) * 64], in_=wuv[:, h, :]),
                     reads=["a_wuv"], writes=["a_wuvp"])
            hi, lo, w0, mid, cnt, tt, thr = (sm[:, i:i + 1] for i in range(7))

            def stageA(i, part):
                steps = []
                N = 128 * (i + 1)
                qs = slice(i * 128, (i + 1) * 128)
                MB = MBs[i % 2]
                mbn = "a_MB%d" % (i % 2)
                acc = accs[i % 2]
                ACCR = [("a_acc%d" % (i % 2), c_) for c_ in range(4)]

                def qi_proj(tl):
                    for k in range(2):
                        S.op("pe", lambda e: e.matmul(
                            pQ[:, tl * 128:(tl + 1) * 128], lhsT=wqi[:, k, tl * 128:(tl + 1) * 128],
                            rhs=cqnT[:, k, qs], start=(k == 0), stop=(k == 1)),
                            reads=["a_wqi", ("a_cqnT", i)], writes=["a_pQ"])
                    for h in range(tl * 4, tl * 4 + 4):
                        S.op("act", lambda e: e.activation(out=qiT[:, h, :], in_=pQ[:, tl * 128:(tl + 1) * 128],
                                                           func=AF.Copy, scale=mk4[:, h % 4:h % 4 + 1]),
                             reads=["a_pQ", "a_mk4"], writes=["a_qiT"])
                for tl in range(2):
                    steps.append(partial(qi_proj, tl))

                nch = (N + 511) // 512

                def diag_build():
                    for h in range(8):
                        S.op("act", lambda e: e.activation(out=wdg[:, h, :], in_=identb[:], func=AF.Copy,
                                                           scale=Wsc[:, i, h:h + 1]),
                             reads=["identb", "a_Wsc"], writes=["a_wdg"])
                steps.append(diag_build)
                seq = [(cc, h) for cc in range(nch) for h in range(8)]

                def emitL(x):
                    cc, h = seq[x]
                    n = min(512, N - cc * 512)
                    pl_ = pL[x % 2]
                    S.op("pe", lambda e: e.matmul(
                        pl_[:, 0:n], lhsT=qiT[:, h, :], rhs=kidT[:, cc * 512:cc * 512 + n],
                        start=True, stop=True), reads=["a_qiT"] + [("a_kidT", cc * 4 + q) for q in range((n + 127) // 128)],
                        writes=[_nm(pl_)])

                def idx_step(x):
                    cc, h = seq[x]
                    n = min(512, N - cc * 512)
                    if x == 0:
                        emitL(0)
                    if x + 1 < len(seq):
                        emitL(x + 1)
                    pl_ = pL[x % 2]
                    rb = Rb[x % 2]
                    S.op("act", lambda e: e.activation(out=rb[:, 0:n], in_=pl_[:, 0:n], func=AF.Relu),
                         reads=[_nm(pl_)], writes=[_nm(rb)])
                    S.op("pe", lambda e: e.matmul(pACC[:, 0:n], lhsT=wdg[:, h, :], rhs=rb[:, 0:n],
                                                  start=(h == 0), stop=(h == 7)),
                         reads=["a_wdg", _nm(rb)], writes=["a_pACC"])
                    if h == 7:
                        S.op("act", lambda e: e.copy(out=acc[:, cc * 512:cc * 512 + n], in_=pACC[:, 0:n]),
                             reads=["a_pACC"], writes=[("a_acc%d" % (i % 2), cc)])
                for x in range(len(seq)):
                    steps.append(partial(idx_step, x))
                if part == 1:
                    return steps
                steps = []

                def bounds():
                    if i >= 2:
                        S.op("dve", lambda e: e.tensor_reduce(out=hi, in_=acc[:, 0:N], axis=AX.X, op=ALU.max),
                             reads=ACCR, writes=["a_sm"])
                        S.op("dve", lambda e: e.tensor_reduce(out=lo, in_=acc[:, 0:N], axis=AX.X, op=ALU.min),
                             reads=ACCR, writes=["a_sm"])
                    S.op("dve", lambda e: e.tensor_tensor(out=acc[:, N - 128:N], in0=acc[:, N - 128:N], in1=m0[:],
                                                          op=ALU.add), reads=["a_m0"] + ACCR, writes=ACCR)
                    if i >= 2:
                        S.op("dve", lambda e: e.tensor_tensor(out=w0, in0=hi, in1=lo, op=ALU.subtract),
                             reads=["a_sm"], writes=["a_sm"])
                        S.op("dve", lambda e: e.tensor_scalar(out=Wk[:], in0=pow2[:], scalar1=w0, scalar2=None,
                                                              op0=ALU.mult), reads=["a_sm", "a_pow2"], writes=["a_Wk"])
                        S.op("dve", lambda e: e.tensor_tensor(out=mid, in0=lo, in1=Wk[:, 0:1], op=ALU.add),
                             reads=["a_sm", "a_Wk"], writes=["a_sm"])
                    else:
                        S.op("dve", lambda e: e.memset(thr, -1.0e29), writes=["a_sm"])
                steps.append(bounds)

                def bis(k):
                    S.op("dve", lambda e: e.tensor_scalar(
                        out=jk[:, 0:N], in0=acc[:, 0:N], scalar1=mid, scalar2=0.0, op0=ALU.is_ge, op1=ALU.add,
                        accum_out=cnt), reads=ACCR + ["a_sm"], writes=["a_jk", "a_sm"])
                    S.op("dve", lambda e: e.tensor_scalar(out=tt, in0=cnt, scalar1=255.5, scalar2=0.5, op0=ALU.is_ge,
                                                          op1=ALU.subtract), reads=["a_sm"], writes=["a_sm"])
                    S.op("dve", lambda e: e.scalar_tensor_tensor(out=mid, in0=tt, scalar=Wk[:, k:k + 1], in1=mid,
                                                                 op0=ALU.mult, op1=ALU.add),
                         reads=["a_sm", "a_Wk"], writes=["a_sm"])
                if i >= 2:
                    for k in range(NIT):
                        steps.append(partial(bis, k))

                def fin():
                    if i >= 2:
                        S.op("dve", lambda e: e.tensor_tensor(out=thr, in0=mid, in1=Wk[:, NIT:NIT + 1], op=ALU.subtract),
                             reads=["a_sm", "a_Wk"], writes=["a_sm"])
                    S.op("dve", lambda e: e.tensor_scalar(out=MB[:, 0:N], in0=acc[:, 0:N], scalar1=thr, scalar2=-30000.0,
                                                          op0=ALU.is_lt, op1=ALU.mult),
                         reads=ACCR + ["a_sm"], writes=[mbn])
                steps.append(fin)
                return steps

            def stageB(i):
                steps = []
                qs = slice(i * 128, (i + 1) * 128)
                MB = MBs[i % 2]
                mbn = "a_MB%d" % (i % 2)

                def q_proj(g2):
                    for h in range(g2 * 4, g2 * 4 + 4):
                        for k in range(2):
                            S.op("pe", lambda e: e.matmul(
                                pS[g2][:, (h % 4) * 128:(h % 4 + 1) * 128], lhsT=wuq[:, k, h * 128:(h + 1) * 128],
                                rhs=cqnT[:, k, qs], start=(k == 0), stop=(k == 1)),
                                reads=["a_wuq", ("a_cqnT", i)], writes=[_nm(pS[g2])])
                    S.op("act", lambda e: e.activation(
                        out=qT[:, g2 * 4:(g2 + 1) * 4, :].rearrange("p h q -> p (h q)"), in_=pS[g2][:], func=AF.Copy,
                        scale=ATT_SC), reads=[_nm(pS[g2])], writes=["a_qT"])
                steps.append(partial(q_proj, 0))
                steps.append(partial(q_proj, 1))
                its = [(g, j) for g in range(2) for j in range(i + 1)]

                def emitS(n):
                    grp, j = its[n]
                    s_ = pS[n % 2]
                    S.op("pe", lambda e: e.matmul(
                        s_[:], lhsT=ckvT[:, j * 128:(j + 1) * 128],
                        rhs=qT[:, grp * 4:(grp + 1) * 4, :].rearrange("p h q -> p (h q)"), start=True, stop=False),
                        reads=[("a_ckvT", j), "a_qT"], writes=[_nm(s_)])
                    S.op("pe", lambda e: e.matmul(
                        s_[:], lhsT=MB[:, j * 128:(j + 1) * 128], rhs=i4b[:], start=False, stop=False),
                        reads=[mbn, "a_i4b"], writes=[_nm(s_)])
                    if j < i:
                        dd = i - j
                        S.op("pe", lambda e: e.matmul(
                            s_[:], lhsT=at3[:, dd * 128:(dd + 1) * 128], rhs=bt3[:, grp * 512:(grp + 1) * 512],
                            start=False, stop=True), reads=["a_at3", "a_bt3"], writes=[_nm(s_)])
                    else:
                        S.op("pe", lambda e: e.matmul(
                            s_[:], lhsT=identb[:], rhs=altd[:, grp * 512:(grp + 1) * 512], start=False, stop=True),
                            reads=["identb", "a_altd"], writes=[_nm(s_)])

                def it_step(n):
                    grp, j = its[n]
                    if n == 0:
                        emitS(0)
                    if n + 1 < len(its):
                        emitS(n + 1)
                    s_ = pS[n % 2]
                    ptb = PT[n % 2]
                    S.op("act", lambda e: e.activation(out=ptb[:], in_=s_[:], func=AF.Exp),
                         reads=[_nm(s_)], writes=[_nm(ptb)])
                    S.op("pe", lambda e: e.matmul(pO[:], lhsT=Vg[:, j, :], rhs=ptb[:], start=(j == 0), stop=(j == i)),
                         reads=[("a_V", j), _nm(ptb)], writes=["a_pO"])
                    S.op("pe", lambda e: e.matmul(pD[:], lhsT=onesb[:], rhs=ptb[:], start=(j == 0), stop=(j == i)),
                         reads=["onesb", _nm(ptb)], writes=["a_pD"])
                    if j == i:
                        S.op("dve", lambda e: e.reciprocal(out=rec[:], in_=pD[:]), reads=["a_pD"], writes=["a_rec"])
                        S.op("dve", lambda e: e.tensor_tensor(out=oT[:, grp * 512:(grp + 1) * 512], in0=pO[:],
                                                              in1=rec[:], op=ALU.mult),
                             reads=["a_pO", "a_rec"], writes=["a_oT"])
                for n in range(len(its)):
                    steps.append(partial(it_step, n))

                def y_proj():
                    for h in range(8):
                        S.op("pe", lambda e: e.matmul(
                            pY[:, (h // 2) * 128:(h // 2 + 1) * 128], lhsT=wuvp[:, h, :],
                            rhs=oT[:, h * 128:(h + 1) * 128], start=(h % 2 == 0), stop=(h % 2 == 1)),
                            reads=["a_wuvp", "a_oT"], writes=["a_pQ"])
                    S.op("act", lambda e: e.copy(out=yaT[:, :, qs], in_=pY[:].rearrange("p (c q) -> p c q", q=128)),
                         reads=["a_pQ"], writes=["yaT"])
                steps.append(y_proj)
                return steps

            def run_merged(*lists):
                pos = [0] * len(lists)
                total = sum(len(x) for x in lists)
                for _ in range(total):
                    best, bf = None, None
                    for q, lst in enumerate(lists):
                        if pos[q] < len(lst):
                            frac = pos[q] / len(lst)
                            if bf is None or frac < bf:
                                best, bf = q, frac
                    lists[best][pos[best]]()
                    pos[best] += 1

            run_merged(stageA(0, 1))
            run_merged(stageA(1, 1), stageA(0, 2))
            for i in range(NT):
                run_merged(stageA(i + 2, 1) if i + 2 < NT else [], stageA(i + 1, 2) if i + 1 < NT else [], stageB(i))
            S.end_phase()


_BF = {"identb", "i4b", "onesb", "triub", "altd", "at3", "bt3"}


def kernel(**inputs):
    inp = {k: np.asarray(v) for k, v in inputs.items()}
    n_cores = 8
    shared = _layout_weights(inp)
    shared.update(_consts())
    x = np.ascontiguousarray(inp["x"], dtype=np.float32)
    c = np.asarray(inp["c"], dtype=np.float32)
    in_maps = []
    for ci in range(n_cores):
        m = dict(shared)
        m["x"] = x[ci * NB:(ci + 1) * NB]
        cc = c[ci * NB:(ci + 1) * NB]
        m["cT"] = np.ascontiguousarray(cc.reshape(NB, 8, 128).transpose(2, 1, 0))
        in_maps.append(m)
    shapes = {k: (v.shape, BF16 if k in _BF else F32) for k, v in in_maps[0].items()}
    nc = build_nc(shapes)
    in_maps = [{"d_" + k: v for k, v in m.items()} for m in in_maps]
    res = run_bass_kernel_spmd(nc, in_maps, core_ids=list(range(n_cores)))
    return np.concatenate([np.asarray(r["out"], dtype=np.float32) for r in res.results], axis=0)
```

```python
import types
from functools import partial
import numpy as np
import ml_dtypes
from contextlib import ExitStack
import concourse.bass as bass
import concourse.mybir as mybir
from concourse.bass_utils import run_bass_kernel_spmd

F32 = mybir.dt.float32
BF16 = mybir.dt.bfloat16
AF = mybir.ActivationFunctionType
ALU = mybir.AluOpType
AX = mybir.AxisListType

D = 1024
SEQ = 2048
NT = 16
NL = 2
NB = 4
EPS = 1e-6
ATT_SC = 128.0 ** -0.5
IDXW = (32.0 ** -0.5) * (8.0 ** -0.5)
NIT = 18
NEG = -1.0e30
POOL_WINS = (2, 4, 8, 16)


_BASE = {}
_UNIQ = [0]


def _nm(t):
    return _BASE[t.name]


def _freeze(fn):
    if fn.__closure__ is None:
        return fn
    cells = []
    for c in fn.__closure__:
        try:
            cells.append(types.CellType(c.cell_contents))
        except ValueError:
            cells.append(c)
    return types.FunctionType(fn.__code__, fn.__globals__, fn.__name__, fn.__defaults__, tuple(cells))


class Sched:
    ENG = ("pe", "act", "dve", "pool", "sp")

    def __init__(self, nc, es, n_dma_sems=12):
        self.nc = nc
        self.sems = {e: es.enter_context(nc.semaphore("s_" + e)) for e in self.ENG}
        self.cnt = {e: 0 for e in self.ENG}
        self.dma_sems = [es.enter_context(nc.semaphore("d%d" % i)) for i in range(n_dma_sems)]
        self.dma_cnt = [0] * n_dma_sems
        self.dma_rr = 0
        self.prog = {e: [] for e in self.ENG}
        self.seen = {e: {} for e in self.ENG}
        self.lastw = {}
        self.readers = {}
        self.ninst = 0
        self.excl = set()

    def _sem_of(self, key):
        if isinstance(key, tuple):
            return self.dma_sems[key[1]], 16
        return self.sems[key], 1

    def _needed(self, eng, deps):
        best = {}
        for (key, val) in deps:
            if key == eng and eng in ("pe", "sp"):
                continue
            if self.seen[eng].get(key, 0) >= val:
                continue
            if best.get(key, 0) < val:
                best[key] = val
        waits = []
        for key, val in best.items():
            self.seen[eng][key] = val
            sem, step = self._sem_of(key)
            waits.append((sem, val * step))
        return waits

    def _deps(self, reads, writes):
        deps = []
        for r in reads:
            t = self.lastw.get(r)
            if t is not None:
                deps.append(t)
        for w in writes:
            t = self.lastw.get(w)
            if t is not None:
                deps.append(t)
            rd = self.readers.get(w)
            if rd:
                deps.extend(rd.items())
        return deps

    def _commit(self, tok, reads, writes):
        for w in writes:
            self.lastw[w] = tok
            self.readers[w] = {}
        for r in reads:
            d = self.readers.setdefault(r, {})
            if d.get(tok[0], 0) < tok[1]:
                d[tok[0]] = tok[1]

    def op(self, eng, fn, reads=(), writes=()):
        fn = _freeze(fn)
        if self.excl:
            ex = [r for r in reads if r in self.excl]
            if ex:
                writes = list(writes) + [r for r in ex if r not in writes]
        waits = self._needed(eng, self._deps(reads, writes))
        self.cnt[eng] += 1
        tok = (eng, self.cnt[eng])
        sem = self.sems[eng]

        def emit(e, waits=waits, fn=fn, sem=sem):
            for (s, v) in waits:
                e.wait_ge(s, v)
            fn(e).then_inc(sem, 1)

        self.prog[eng].append(emit)
        self._commit(tok, reads, writes)
        self.ninst += 1
        return tok

    def dma(self, out, in_, reads=(), writes=(), q="sp", **kw):
        k = self.dma_rr
        self.dma_rr = (k + 1) % len(self.dma_sems)
        deps = self._deps(reads, writes)
        if self.dma_cnt[k] > 0:
            deps.append((("dma", k), self.dma_cnt[k]))
        waits = self._needed(q, deps)
        self.dma_cnt[k] += 1
        tok = (("dma", k), self.dma_cnt[k])
        sem = self.dma_sems[k]

        def emit(e, waits=waits, sem=sem, out=out, in_=in_, kw=kw):
            for (s, v) in waits:
                e.wait_ge(s, v)
            e.dma_start(out=out, in_=in_, **kw).then_inc(sem, 16)

        self.prog[q].append(emit)
        self._commit(tok, reads, writes)
        self.ninst += 1
        return tok

    def end_phase(self, q="sp"):
        waits = [(self.dma_sems[k], 16 * c) for k, c in enumerate(self.dma_cnt) if c > 0]
        waits += [(self.sems[e], self.cnt[e]) for e in self.ENG if self.cnt[e] > 0 and e != q]

        def emit(e, waits=waits):
            for (s, v) in waits:
                e.wait_ge(s, v)

        self.prog[q].append(emit)
        nc = self.nc
        prog = self.prog
        with nc.Block() as block:
            if prog["pe"]:
                @block.tensor
                def _(e):
                    for f in prog["pe"]:
                        f(e)
            if prog["act"]:
                @block.scalar
                def _(e):
                    for f in prog["act"]:
                        f(e)
            if prog["dve"]:
                @block.vector
                def _(e):
                    for f in prog["dve"]:
                        f(e)
            if prog["pool"]:
                @block.gpsimd
                def _(e):
                    for f in prog["pool"]:
                        f(e)
            if prog["sp"]:
                @block.sync
                def _(e):
                    for f in prog["sp"]:
                        f(e)
        self.prog = {e: [] for e in self.ENG}


def _consts():
    bf = ml_dtypes.bfloat16
    c = {}
    c["identf"] = np.eye(128, dtype=np.float32)
    c["identb"] = np.eye(128, dtype=np.float32).astype(bf)
    c["i4b"] = np.tile(np.eye(128, dtype=np.float32), (1, 4)).astype(bf)
    c["onesb"] = np.ones((128, 128), np.float32).astype(bf)
    c["onesf"] = np.ones((128, 128), np.float32)
    s = np.arange(128)
    c["triub"] = (s[:, None] <= s[None, :]).astype(np.float32).astype(bf)
    m0 = np.zeros((128, 128), np.float32)
    m0[:64, 64:] = NEG
    c["m0"] = m0
    slopes = 2.0 ** (-8.0 * np.arange(1, 9) / 8.0)
    altd = np.zeros((128, 8, 128), np.float32)
    for h in range(8):
        altd[:, h, :] = -slopes[h] * np.abs(s[None, :] - s[:, None])
    c["altd"] = altd.reshape(128, 1024).astype(bf)
    at3 = np.zeros((3, 16, 128), np.float32)
    at3[0] = 1.0
    at3[1] = (128.0 * np.arange(16))[:, None]
    at3[2] = s[None, :].astype(np.float32)
    at3p = np.zeros((128, 2048), np.float32)
    at3p[0:3] = at3.reshape(3, 2048)
    c["at3"] = at3p.astype(bf)
    bt3 = np.zeros((3, 8, 128), np.float32)
    for h in range(8):
        bt3[0, h] = -slopes[h] * s
        bt3[1, h] = -slopes[h]
        bt3[2, h] = slopes[h]
    bt3p = np.zeros((128, 1024), np.float32)
    bt3p[0:3] = bt3.reshape(3, 1024)
    c["bt3"] = bt3p.astype(bf)
    mk4 = np.zeros((128, 4), np.float32)
    for r in range(4):
        mk4[r * 32:(r + 1) * 32, r] = 1.0
    c["mk4"] = mk4
    sg = np.ones((128, 2), np.float32)
    sg[:64, 0] = -1.0
    sg[64:, 1] = -1.0
    c["sgn"] = sg
    bm = np.zeros((128, 8), np.float32)
    for p in range(128):
        bm[p, p // 16] = 1.0
    c["bmask"] = bm
    m2 = np.zeros((128, 16, 8), np.float32)
    for g in range(16):
        m2[:, g, g % 8] = 1.0
    c["mask2"] = m2
    c["pow2"] = np.tile((2.0 ** -(np.arange(NIT + 1) + 1.0))[None, :], (128, 1)).astype(np.float32)
    c["invt"] = np.tile((1.0 / (np.arange(16) + 1.0))[None, :], (128, 1)).astype(np.float32)
    return c


def _layout_weights(inp):
    f = lambda a: np.ascontiguousarray(a, dtype=np.float32)
    w = {}
    w["modw"] = f(inp["mod_w"].reshape(NL, 8, 128, 12, 512).transpose(0, 3, 2, 1, 4))
    w["modb"] = f(inp["mod_b"].reshape(NL, 48, 128).transpose(0, 2, 1))
    gv = np.stack([inp["mix_pre_g"], inp["mix_post_g"], inp["ffn_pre_g"], inp["ffn_post_g"]], axis=1)
    w["gvec"] = f(gv.reshape(NL, 4, 8, 128).transpose(0, 1, 3, 2))
    win = inp["w_in"].reshape(NL, 8, 128, 4008)
    w["w1"] = f(win[:, :, :, :936].transpose(0, 2, 1, 3))
    w["wg"] = f(win[:, :, :, 936:].reshape(NL, 8, 128, 24, 128).transpose(0, 3, 2, 1, 4))
    w["gcq"] = f(inp["g_cq"].reshape(NL, 2, 128).transpose(0, 2, 1))
    w["gckv"] = f(inp["g_ckv"])
    w["wuq"] = f(inp["w_uq"].reshape(NL, 2, 128, 1024).transpose(0, 2, 1, 3))
    w["wqi"] = f(inp["w_qi"].reshape(NL, 2, 128, 256).transpose(0, 2, 1, 3))
    w["wuv"] = f(inp["w_uv"].transpose(0, 2, 1, 3))
    dup = lambda a: np.concatenate([a, a], axis=1)
    w["are"] = f(dup(inp["a_re"].transpose(0, 2, 1)))
    w["aim"] = f(dup(inp["a_im"].transpose(0, 2, 1)))
    w["lstep"] = f(np.broadcast_to(inp["log_step"][:, None, :], (NL, 128, 16)))
    w["bre"] = f(inp["b_re"].transpose(0, 2, 1, 3))
    w["bim"] = f(inp["b_im"].transpose(0, 2, 1, 3))
    w["cre"] = f(inp["c_re"].transpose(0, 3, 1, 2))
    w["cim"] = f(inp["c_im"].transpose(0, 3, 1, 2))
    w["dskip"] = f(inp["d_skip"].reshape(NL, 2, 128).transpose(0, 2, 1))
    w["wglu"] = f(inp["w_glu"].reshape(NL, 2, 128, 256).transpose(0, 2, 1, 3))
    w["bglu"] = f(inp["b_glu"].reshape(NL, 2, 128).transpose(0, 2, 1))
    w["wpool"] = f(inp["w_pool"])
    w["pscale"] = f(inp["pool_scale"].reshape(NL, 2, 128).transpose(0, 2, 1))
    w["pa"] = f(inp["p_a"].reshape(NL, 4, 128, 1024).transpose(0, 2, 1, 3))
    w["pb"] = f(inp["p_b"].reshape(NL, 2, 128, 1024).transpose(0, 2, 1, 3))
    w["pc"] = f(inp["p_c"].reshape(NL, 2, 128, 1024).transpose(0, 2, 1, 3))
    w["wout"] = f(inp["w_out"].reshape(NL, 8, 128, 1024).transpose(0, 2, 1, 3))
    w["wup"] = f(inp["w_up"].reshape(NL, 8, 128, 32, 128).transpose(0, 3, 2, 1, 4))
    w["convw"] = f(inp["conv_w"].reshape(NL, 3, 32, 128).transpose(0, 3, 1, 2))
    w["convb"] = f(inp["conv_b"].reshape(NL, 32, 128).transpose(0, 2, 1))
    w["wdown"] = f(inp["w_down"].reshape(NL, 16, 128, 1024).transpose(0, 2, 1, 3))
    return w


def build_nc(shapes, nb=NB, nl=NL, do_mix=True, do_ffn=True, att=True, ssm=True, pool=True):
    nc = bass.Bass("TRN2", target_bir_lowering=False)
    DBG["nc"] = nc
    DBG["outs"] = {}
    dr = {}
    for name, (shape, dt) in shapes.items():
        dr[name] = nc.dram_tensor("d_" + name, list(shape), dt, kind="ExternalInput").ap()
    out = nc.dram_tensor("out", [nb, SEQ, D], F32, kind="ExternalOutput").ap()
    TABW = 2 * 1024 + 2 * 2048 + 64
    tab = nc.dram_tensor("tabscratch", [NL, 128, TABW], F32, kind="Internal").ap()
    tabb = nc.dram_tensor("tabscratchb", [NL, 128, 2048 + 2048], BF16, kind="Internal").ap()

    with ExitStack() as es:
        S = Sched(nc, es)

        def sb(st, name, shape, dt):
            _UNIQ[0] += 1
            actual = "%s__%d" % (name, _UNIQ[0])
            _BASE[actual] = name
            return st.enter_context(nc.sbuf_tensor(actual, shape, dt))

        def ps(st, name, shape, dt):
            S.excl.add(name)
            _UNIQ[0] += 1
            actual = "%s__%d" % (name, _UNIQ[0])
            _BASE[actual] = name
            return st.enter_context(nc.psum_tensor(actual, shape, dt))

        X = sb(es, "X", [128, NT, D], F32)
        hT = sb(es, "hT", [128, 8, SEQ], BF16)
        identf = sb(es, "identf", [128, 128], F32)
        identb = sb(es, "identb", [128, 128], BF16)
        onesb = sb(es, "onesb", [128, 128], BF16)
        onesf = sb(es, "onesf", [128, 128], F32)
        cols = sb(es, "cols", [128, NL, 6, 8, NB], F32)
        GG = sb(es, "GG", [128, D], F32)
        for nm, t in (("identf", identf), ("identb", identb), ("onesb", onesb), ("onesf", onesf)):
            S.dma(t[:], dr[nm], writes=[nm])

        with ExitStack() as ph:
            condT = sb(ph, "condT", [128, 8, NB], F32)
            modT = sb(ph, "modT", [128, 48, NB], F32)
            modb = sb(ph, "modb", [128, 48], F32)
            gvec = sb(ph, "gvec", [128, 4, 8], F32)
            mw = [sb(ph, "mw%d" % i, [128, 8, 512], F32) for i in range(2)]
            pm = ps(ph, "pm", [128, 512], F32)
            S.dma(condT[:], dr["cT"], writes=["condT"])
            S.op("act", lambda e: e.activation(out=condT[:], in_=condT[:], func=AF.Silu),
                 reads=["condT"], writes=["condT"])
            for l in range(nl):
                S.dma(modb[:], dr["modb"][l], writes=["modb"])
                S.dma(gvec[:], dr["gvec"][l].rearrange("w p c -> p w c"), writes=["gvec"])
                for j in range(12):
                    buf = mw[j % 2]
                    S.dma(buf[:], dr["modw"][l, j], writes=["mw%d" % (j % 2)])
                    for f4 in range(4):
                        fc = j * 4 + f4
                        for k in range(8):
                            S.op("pe", lambda e, buf=buf, f4=f4, k=k, fc=fc: e.matmul(
                                pm[:, fc * NB:(fc + 1) * NB], lhsT=buf[:, k, f4 * 128:(f4 + 1) * 128],
                                rhs=condT[:, k, :], start=(k == 0), stop=(k == 7)),
                                reads=["mw%d" % (j % 2), "condT"], writes=["pm"])
                S.op("dve", lambda e: e.tensor_tensor(
                    out=modT[:], in0=pm[:, 0:48 * NB].rearrange("p (f b) -> p f b", b=NB),
                    in1=modb[:].unsqueeze(2).to_broadcast([128, 48, NB]), op=ALU.add),
                    reads=["pm", "modb"], writes=["modT"])
                def gb(w):
                    return gvec[:, w, :].unsqueeze(2).to_broadcast([128, 8, NB])
                for (dst, sc0, gw) in ((0, 8, 0), (3, 32, 2)):
                    S.op("dve", lambda e, dst=dst, sc0=sc0, gw=gw: e.scalar_tensor_tensor(
                        out=cols[:, l, dst], in0=modT[:, sc0:sc0 + 8, :], scalar=1.0, in1=gb(gw),
                        op0=ALU.add, op1=ALU.mult), reads=["modT", "gvec"], writes=["cols"])
                for (dst, sh0) in ((1, 0), (4, 24)):
                    S.op("dve", lambda e, dst=dst, sh0=sh0: e.tensor_copy(
                        out=cols[:, l, dst], in_=modT[:, sh0:sh0 + 8, :]), reads=["modT"], writes=["cols"])
                for (dst, g0, gw) in ((2, 16, 1), (5, 40, 3)):
                    S.op("dve", lambda e, dst=dst, g0=g0, gw=gw: e.tensor_tensor(
                        out=cols[:, l, dst], in0=modT[:, g0:g0 + 8, :], in1=gb(gw), op=ALU.mult),
                        reads=["modT", "gvec"], writes=["cols"])
            dbg(S, "cols", cols[:].rearrange("p l s c b -> p (l s c b)"), [128, NL * 6 * 8 * NB], F32, ["cols"])
            S.end_phase()

        def rstd_from_ss(ss, n, width):
            S.op("dve", lambda e: e.tensor_scalar(out=ss[:, 0:width], in0=ss[:, 0:width], scalar1=1.0 / n,
                                                  scalar2=EPS, op0=ALU.mult, op1=ALU.add),
                 reads=[_nm(ss)], writes=[_nm(ss)])
            S.op("act", lambda e: e.activation(out=ss[:, 0:width], in_=ss[:, 0:width], func=AF.Sqrt),
                 reads=[_nm(ss)], writes=[_nm(ss)])
            S.op("dve", lambda e: e.reciprocal(out=ss[:, 0:width], in_=ss[:, 0:width]),
                 reads=[_nm(ss)], writes=[_nm(ss)])

        def norm_to_hT(ph, l, b, ca, cb):
            ss = sb(ph, "n_ss", [128, NT], F32)
            junk = sb(ph, "n_junk", [128, D], BF16)
            xn = [sb(ph, "n_xn%d" % i, [128, D], BF16) for i in range(4)]
            pt = [ps(ph, "n_pt%d" % i, [128, 1024], BF16) for i in range(4)]
            for t in range(NT):
                S.op("act", lambda e, t=t: e.activation(out=junk[:], in_=X[:, t, :], func=AF.Square,
                                                        accum_out=ss[:, t:t + 1]),
                     reads=[("X", t)], writes=["n_junk", "n_ss"])
            rstd_from_ss(ss, float(D), NT)
            for t in range(NT):
                xb = xn[t % 4]
                pp = pt[t % 4]
                S.op("dve", lambda e, t=t, xb=xb: e.tensor_scalar(out=xb[:], in0=X[:, t, :],
                                                               scalar1=ss[:, t:t + 1], scalar2=None, op0=ALU.mult),
                     reads=[("X", t), "n_ss"], writes=[_nm(xb)])
                for c in range(8):
                    S.op("pe", lambda e, c=c, xb=xb, pp=pp: e.transpose(
                        out=pp[:, c * 128:(c + 1) * 128], in_=xb[:, c * 128:(c + 1) * 128], identity=identb[:]),
                        reads=[_nm(xb), "identb"], writes=[_nm(pp)])
                for c in range(8):
                    eng = "act" if c % 2 else "dve"
                    if eng == "dve":
                        S.op("dve", lambda e, c=c, t=t, pp=pp: e.tensor_scalar(
                            out=hT[:, c, t * 128:(t + 1) * 128], in0=pp[:, c * 128:(c + 1) * 128],
                            scalar1=cols[:, l, ca, c, b:b + 1], scalar2=cols[:, l, cb, c, b:b + 1],
                            op0=ALU.mult, op1=ALU.add), reads=[_nm(pp), "cols"], writes=[("hTc", t, c)])
                    else:
                        S.op("act", lambda e, c=c, t=t, pp=pp: e.activation(
                            out=hT[:, c, t * 128:(t + 1) * 128], in_=pp[:, c * 128:(c + 1) * 128],
                            func=AF.Identity, scale=cols[:, l, ca, c, b:b + 1], bias=cols[:, l, cb, c, b:b + 1]),
                            reads=[_nm(pp), "cols"], writes=[("hTc", t, c)])

        def make_GG(ph, l, b, cg):
            dg = sb(ph, "gg_diag", [128, 128], F32)
            pg = [ps(ph, "gg_ps%d" % i, [128, 512], F32) for i in range(2)]
            for c in range(8):
                S.op("dve", lambda e, c=c: e.tensor_scalar(out=dg[:], in0=identf[:], scalar1=cols[:, l, cg, c, b:b + 1],
                                                        scalar2=None, op0=ALU.mult),
                     reads=["identf", "cols"], writes=["gg_diag"])
                S.op("pe", lambda e, c=c: e.matmul(pg[c // 4][:, (c % 4) * 128:(c % 4 + 1) * 128], lhsT=onesf[:],
                                                   rhs=dg[:], start=True, stop=True),
                     reads=["gg_diag", "onesf"], writes=[_nm(pg[c // 4])])
            for i in range(2):
                S.op("act", lambda e, i=i: e.copy(out=GG[:, i * 512:(i + 1) * 512], in_=pg[i][:]),
                     reads=[_nm(pg[i])], writes=["GG"])

        def resid_update(ph_tiles, t, py):
            ssy, junk, tmp = ph_tiles
            for i in range(2):
                S.op("act", lambda e, i=i: e.activation(out=junk[:, 0:512], in_=py[i][:], func=AF.Square,
                                                        accum_out=ssy[:, i:i + 1]),
                     reads=[_nm(py[i])], writes=[_nm(junk), _nm(ssy)])
            S.op("dve", lambda e: e.tensor_tensor(out=ssy[:, 2:3], in0=ssy[:, 0:1], in1=ssy[:, 1:2], op=ALU.add),
                 reads=[_nm(ssy)], writes=[_nm(ssy)])
            S.op("dve", lambda e: e.tensor_scalar(out=ssy[:, 2:3], in0=ssy[:, 2:3], scalar1=1.0 / D, scalar2=EPS,
                                                  op0=ALU.mult, op1=ALU.add), reads=[_nm(ssy)], writes=[_nm(ssy)])
            S.op("act", lambda e: e.activation(out=ssy[:, 2:3], in_=ssy[:, 2:3], func=AF.Sqrt),
                 reads=[_nm(ssy)], writes=[_nm(ssy)])
            S.op("dve", lambda e: e.reciprocal(out=ssy[:, 2:3], in_=ssy[:, 2:3]), reads=[_nm(ssy)], writes=[_nm(ssy)])
            for i in range(2):
                S.op("dve", lambda e, i=i: e.scalar_tensor_tensor(
                    out=tmp[:, i * 512:(i + 1) * 512], in0=py[i][:], scalar=ssy[:, 2:3],
                    in1=GG[:, i * 512:(i + 1) * 512], op0=ALU.mult, op1=ALU.mult),
                    reads=[_nm(py[i]), _nm(ssy), "GG"], writes=[_nm(tmp)])
            if t == 8:
                dbg(S, "r_ssy", ssy[:], [128, 4], F32, [_nm(ssy)])
                dbg(S, "r_tmp", tmp[:], [128, D], F32, [_nm(tmp)])
            S.op("dve", lambda e: e.tensor_tensor(out=X[:, t, :], in0=X[:, t, :], in1=tmp[:], op=ALU.add),
                 reads=[_nm(tmp), ("X", t)], writes=[("X", t)])

        if do_mix and ssm:
            for l in range(nl):
                ssm_tables(nc, S, dr, l, tab, tabb, sb, ps, identf)

        for b in range(nb):
            with ExitStack() as ph:
                for t in range(NT):
                    S.dma(X[:, t, :], dr["x"][b, t * 128:(t + 1) * 128, :], writes=[("X", t)])
                S.end_phase()
            for l in range(nl):
                if do_mix:
                    mixer(nc, S, dr, l, b, X, hT, GG, cols, tab, tabb, sb, ps, norm_to_hT, make_GG, resid_update,
                          rstd_from_ss, identb, identf, onesb, att, ssm, pool)
                if do_ffn:
                    ffn(nc, S, dr, l, b, X, hT, GG, sb, ps, norm_to_hT, make_GG, resid_update)
            with ExitStack() as ph:
                for t in range(NT):
                    S.dma(out[b, t * 128:(t + 1) * 128, :], X[:, t, :], reads=[("X", t)])
                S.end_phase()
        print("instructions:", S.ninst)
    return nc


DBG = {"on": False, "nc": None, "outs": {}}


def dbg(S, name, ap, shape, dt, reads, st=None):
    if not DBG["on"] or name in DBG["outs"]:
        return
    nc = DBG["nc"]
    d = nc.dram_tensor("dbg_" + name, list(shape), F32, kind="ExternalOutput").ap()
    DBG["outs"][name] = d
    if dt != F32:
        tmpf = st.enter_context(nc.sbuf_tensor("dbgt_" + name, list(shape), F32))
        S.op("dve", lambda e: e.tensor_copy(out=tmpf[:], in_=ap), reads=reads, writes=["dbgt_" + name])
        S.dma(d, tmpf[:], reads=["dbgt_" + name])
    else:
        S.dma(d, ap, reads=reads)


def cast_load(S, dst, src, wname):
    S.dma(dst, src, writes=[wname], q="pool")


def ffn(nc, S, dr, l, b, X, hT, GG, sb, ps, norm_to_hT, make_GG, resid_update):
    with ExitStack() as ph:
        norm_to_hT(ph, l, b, 3, 4)
        make_GG(ph, l, b, 5)
        dbg(S, "f_hT", hT[:].rearrange("p c t -> p (c t)"), [128, 8 * SEQ], BF16, [("hT", t) for t in range(NT)], ph)
        dbg(S, "f_GG", GG[:], [128, D], F32, ["GG"])
        S.end_phase()
    with ExitStack() as ph:
        wd = sb(ph, "f_wd", [128, 16, D], BF16)
        m = sb(ph, "f_m", [128, 16, 1024], BF16)
        cw = sb(ph, "f_cw", [128, 3, 32], F32)
        cbt = sb(ph, "f_cb", [128, 32], F32)
        halo = sb(ph, "f_halo", [128, 32, 2], F32)
        wu = [sb(ph, "f_wu%d" % i, [128, 8, 128], BF16) for i in range(3)]
        ub = [sb(ph, "f_ub%d" % i, [128, 1026], F32) for i in range(2)]
        acc = [sb(ph, "f_acc%d" % i, [128, 1024], F32) for i in range(2)]
        gl = sb(ph, "f_gl", [128, 1024], F32)
        ssy = sb(ph, "f_ssy", [128, 4], F32)
        junk = sb(ph, "f_junk", [128, 512], BF16)
        tmp = sb(ph, "f_tmp", [128, D], F32)
        pu = [ps(ph, "f_pu%d" % i, [128, 512], F32) for i in range(4)]
        py = [ps(ph, "f_py%d" % i, [128, 512], F32) for i in range(4)]
        S.dma(cw[:], dr["convw"][l], writes=["f_cw"])
        S.dma(cbt[:], dr["convb"][l], writes=["f_cb"])
        for k in range(16):
            cast_load(S, wd[:, k, :], dr["wdown"][l][:, k, :], ("f_wd", k))
        nload = 0
        for half in range(2):
            t0 = half * 1024
            for cc in range(16):
                for gv in range(2):
                    j = gv * 16 + cc
                    wb = wu[nload % 3]
                    nload += 1
                    cast_load(S, wb[:], dr["wup"][l, j], _nm(wb))
                    pp = pu[(gv * 2):(gv * 2 + 2)]
                    for s2 in range(2):
                        for k in range(8):
                            S.op("pe", lambda e, wb=wb, k=k, s2=s2, pp=pp: e.matmul(
                                pp[s2][:], lhsT=wb[:, k, :], rhs=hT[:, k, t0 + s2 * 512:t0 + (s2 + 1) * 512],
                                start=(k == 0), stop=(k == 7)),
                                reads=[_nm(wb)] + [("hT", (t0 + s2 * 512) // 128 + q) for q in range(4)],
                                writes=[_nm(pp[s2])])
                    u = ub[gv]
                    a = acc[gv]
                    dbg(S, "f_wb", wb[:].rearrange("p k n -> p (k n)"), [128, 1024], BF16, [_nm(wb)], ph)
                    if half == 0:
                        S.op("dve", lambda e, u=u: e.memset(u[:, 0:2], 0.0), writes=[_nm(u)])
                    else:
                        S.op("dve", lambda e, u=u, j=j: e.tensor_copy(out=u[:, 0:2], in_=halo[:, j, :]),
                             reads=["f_halo"], writes=[_nm(u)])
                    for s2 in range(2):
                        S.op("act", lambda e, u=u, s2=s2, pp=pp: e.copy(out=u[:, 2 + s2 * 512:2 + (s2 + 1) * 512],
                                                                        in_=pp[s2][:]),
                             reads=[_nm(pp[s2])], writes=[_nm(u)])
                    if half == 0:
                        S.op("dve", lambda e, u=u, j=j: e.tensor_copy(out=halo[:, j, :], in_=u[:, 1024:1026]),
                             reads=[_nm(u)], writes=["f_halo"])
                    dbg(S, "f_u", u[:], [128, 1026], F32, [_nm(u)], ph)
                    S.op("dve", lambda e, u=u, a=a, j=j: e.tensor_scalar(
                        out=a[:], in0=u[:, 2:1026], scalar1=cw[:, 2, j:j + 1], scalar2=cbt[:, j:j + 1],
                        op0=ALU.mult, op1=ALU.add), reads=[_nm(u), "f_cw", "f_cb"], writes=[_nm(a)])
                    S.op("dve", lambda e, u=u, a=a, j=j: e.scalar_tensor_tensor(
                        out=a[:], in0=u[:, 1:1025], scalar=cw[:, 1, j:j + 1], in1=a[:], op0=ALU.mult, op1=ALU.add),
                        reads=[_nm(u), "f_cw"], writes=[_nm(a)])
                    S.op("dve", lambda e, u=u, a=a, j=j: e.scalar_tensor_tensor(
                        out=a[:], in0=u[:, 0:1024], scalar=cw[:, 0, j:j + 1], in1=a[:], op0=ALU.mult, op1=ALU.add),
                        reads=[_nm(u), "f_cw"], writes=[_nm(a)])
                dbg(S, "f_acc0", acc[0][:], [128, 1024], F32, [_nm(acc[0])], ph)
                dbg(S, "f_acc1", acc[1][:], [128, 1024], F32, [_nm(acc[1])], ph)
                S.op("act", lambda e: e.activation(out=gl[:], in_=acc[0][:], func=AF.Gelu_apprx_tanh),
                     reads=[_nm(acc[0])], writes=["f_gl"])
                S.op("dve", lambda e, cc=cc: e.tensor_tensor(out=m[:, cc, :], in0=gl[:], in1=acc[1][:], op=ALU.mult),
                     reads=["f_gl", _nm(acc[1])], writes=[("f_m", cc)])
            for tt in range(8):
                t = half * 8 + tt
                pyy = py[(tt % 2) * 2:(tt % 2) * 2 + 2]
                for i in range(2):
                    for k in range(16):
                        S.op("pe", lambda e, i=i, k=k, tt=tt, pyy=pyy: e.matmul(
                            pyy[i][:], lhsT=m[:, k, tt * 128:(tt + 1) * 128], rhs=wd[:, k, i * 512:(i + 1) * 512],
                            start=(k == 0), stop=(k == 15)),
                            reads=[("f_m", k), ("f_wd", k)], writes=[_nm(pyy[i])])
                resid_update((ssy, junk, tmp), t, pyy)
        S.end_phase()


def ssm_tables(nc, S, dr, l, tab, tabb, sb, ps, identf):
    with ExitStack() as ph:
        are = sb(ph, "t_are", [128, 16], F32)
        aim = sb(ph, "t_aim", [128, 16], F32)
        stp = sb(ph, "t_stp", [128, 16], F32)
        th = sb(ph, "t_th", [128, 16], F32)
        r = sb(ph, "t_r", [128, 16], F32)
        sn = sb(ph, "t_sn", [128, 16], F32)
        cs = sb(ph, "t_cs", [128, 16], F32)
        t1 = sb(ph, "t_t1", [128, 16], F32)
        t2 = sb(ph, "t_t2", [128, 16], F32)
        lre = sb(ph, "t_lre", [128, 16], F32)
        lim = sb(ph, "t_lim", [128, 16], F32)
        ire = sb(ph, "t_ire", [128, 16], F32)
        iim = sb(ph, "t_iim", [128, 16], F32)
        pr = sb(ph, "t_pr", [128, 16], F32)
        pi = sb(ph, "t_pi", [128, 16], F32)
        cfr = sb(ph, "t_cfr", [128, 16], F32)
        cfi = sb(ph, "t_cfi", [128, 16], F32)
        Ere = sb(ph, "t_Ere", [128, 16, 256], F32)
        Eim = sb(ph, "t_Eim", [128, 16, 256], F32)
        ta = sb(ph, "t_ta", [128, 16, 128], F32)
        tb = sb(ph, "t_tb", [128, 16, 128], F32)
        sgn = sb(ph, "t_sgn", [128, 2], F32)
        outc = sb(ph, "t_outc", [128, 64], F32)
        halfpi = sb(ph, "t_hpi", [128, 1], F32)
        R = ["t"]

        def dv(fn, w=("t",)):
            S.op("dve", fn, reads=R, writes=list(w))

        S.dma(are[:], dr["are"][l], writes=R)
        S.dma(aim[:], dr["aim"][l], writes=R)
        S.dma(stp[:], dr["lstep"][l], writes=R)
        S.dma(sgn[:], dr["sgn"], writes=R)
        S.op("act", lambda e: e.activation(out=stp[:], in_=stp[:], func=AF.Exp), reads=R, writes=R)
        dv(lambda e: e.tensor_tensor(out=th[:], in0=aim[:], in1=stp[:], op=ALU.mult))
        dv(lambda e: e.tensor_tensor(out=t1[:], in0=are[:], in1=stp[:], op=ALU.mult))
        S.op("act", lambda e: e.activation(out=r[:], in_=t1[:], func=AF.Exp), reads=R, writes=R)
        dv(lambda e: e.memset(halfpi[:], float(np.pi / 2)))
        S.op("act", lambda e: e.activation(out=sn[:], in_=th[:], func=AF.Sin, scale=1.0 / 16), reads=R, writes=R)
        S.op("act", lambda e: e.activation(out=cs[:], in_=th[:], func=AF.Sin, scale=1.0 / 16, bias=halfpi[:]),
             reads=R, writes=R)
        for _ in range(4):
            dv(lambda e: e.tensor_tensor(out=t1[:], in0=sn[:], in1=cs[:], op=ALU.mult))
            dv(lambda e: e.tensor_tensor(out=t2[:], in0=sn[:], in1=sn[:], op=ALU.mult))
            dv(lambda e: e.tensor_scalar(out=sn[:], in0=t1[:], scalar1=2.0, scalar2=None, op0=ALU.mult))
            dv(lambda e: e.tensor_scalar(out=cs[:], in0=t2[:], scalar1=-2.0, scalar2=1.0, op0=ALU.mult, op1=ALU.add))
        dv(lambda e: e.tensor_tensor(out=lre[:], in0=r[:], in1=cs[:], op=ALU.mult))
        dv(lambda e: e.tensor_tensor(out=lim[:], in0=r[:], in1=sn[:], op=ALU.mult))
        dv(lambda e: e.reciprocal(out=t1[:], in_=r[:]))
        dv(lambda e: e.tensor_tensor(out=ire[:], in0=t1[:], in1=cs[:], op=ALU.mult))
        dv(lambda e: e.tensor_tensor(out=iim[:], in0=t1[:], in1=sn[:], op=ALU.mult))
        dv(lambda e: e.tensor_scalar(out=iim[:], in0=iim[:], scalar1=-1.0, scalar2=None, op0=ALU.mult))
        dv(lambda e: e.tensor_tensor(out=t1[:], in0=are[:], in1=are[:], op=ALU.mult))
        dv(lambda e: e.tensor_tensor(out=t2[:], in0=aim[:], in1=aim[:], op=ALU.mult))
        dv(lambda e: e.tensor_tensor(out=t1[:], in0=t1[:], in1=t2[:], op=ALU.add))
        dv(lambda e: e.reciprocal(out=t1[:], in_=t1[:]))
        dv(lambda e: e.tensor_scalar(out=t2[:], in0=lre[:], scalar1=-1.0, scalar2=None, op0=ALU.add))
        dv(lambda e: e.tensor_tensor(out=cfr[:], in0=t2[:], in1=are[:], op=ALU.mult))
        dv(lambda e: e.tensor_tensor(out=pr[:], in0=lim[:], in1=aim[:], op=ALU.mult))
        dv(lambda e: e.tensor_tensor(out=cfr[:], in0=cfr[:], in1=pr[:], op=ALU.add))
        dv(lambda e: e.tensor_tensor(out=cfr[:], in0=cfr[:], in1=t1[:], op=ALU.mult))
        dv(lambda e: e.tensor_tensor(out=cfi[:], in0=lim[:], in1=are[:], op=ALU.mult))
        dv(lambda e: e.tensor_tensor(out=pr[:], in0=t2[:], in1=aim[:], op=ALU.mult))
        dv(lambda e: e.tensor_tensor(out=cfi[:], in0=cfi[:], in1=pr[:], op=ALU.subtract))
        dv(lambda e: e.tensor_tensor(out=cfi[:], in0=cfi[:], in1=t1[:], op=ALU.mult))

        def power_table(bre_, bim_, nlev):
            dv(lambda e: e.memset(Ere[:, :, 0:1], 1.0))
            dv(lambda e: e.memset(Eim[:, :, 0:1], 0.0))
            dv(lambda e: e.tensor_copy(out=pr[:], in_=bre_[:]))
            dv(lambda e: e.tensor_copy(out=pi[:], in_=bim_[:]))
            for k in range(nlev):
                n = 1 << k
                prb = pr[:].unsqueeze(2).to_broadcast([128, 16, n])
                pib = pi[:].unsqueeze(2).to_broadcast([128, 16, n])
                dv(lambda e, n=n, prb=prb: e.tensor_tensor(out=ta[:, :, 0:n], in0=Ere[:, :, 0:n], in1=prb, op=ALU.mult))
                dv(lambda e, n=n, pib=pib: e.tensor_tensor(out=tb[:, :, 0:n], in0=Eim[:, :, 0:n], in1=pib, op=ALU.mult))
                dv(lambda e, n=n: e.tensor_tensor(out=Ere[:, :, n:2 * n], in0=ta[:, :, 0:n], in1=tb[:, :, 0:n],
                                                  op=ALU.subtract))
                dv(lambda e, n=n, pib=pib: e.tensor_tensor(out=ta[:, :, 0:n], in0=Ere[:, :, 0:n], in1=pib, op=ALU.mult))
                dv(lambda e, n=n, prb=prb: e.tensor_tensor(out=tb[:, :, 0:n], in0=Eim[:, :, 0:n], in1=prb, op=ALU.mult))
                dv(lambda e, n=n: e.tensor_tensor(out=Eim[:, :, n:2 * n], in0=ta[:, :, 0:n], in1=tb[:, :, 0:n],
                                                  op=ALU.add))
                if k < nlev - 1:
                    dv(lambda e: e.tensor_tensor(out=t1[:], in0=pr[:], in1=pr[:], op=ALU.mult))
                    dv(lambda e: e.tensor_tensor(out=t2[:], in0=pi[:], in1=pi[:], op=ALU.mult))
                    dv(lambda e: e.tensor_tensor(out=pi[:], in0=pr[:], in1=pi[:], op=ALU.mult))
                    dv(lambda e: e.tensor_scalar(out=pi[:], in0=pi[:], scalar1=2.0, scalar2=None, op0=ALU.mult))
                    dv(lambda e: e.tensor_tensor(out=pr[:], in0=t1[:], in1=t2[:], op=ALU.subtract))

        power_table(lre, lim, 8)
        o_t1 = 2048
        o_t2 = 2048 + 2048
        o_c = 2048 + 4096
        S.dma(tab[l][:, o_t1:o_t1 + 2048].rearrange("p (g t) -> p g t", t=128), Ere[:, :, 0:128], reads=R)
        dv(lambda e: e.tensor_scalar(out=ta[:], in0=Eim[:, :, 0:128], scalar1=sgn[:, 0:1], scalar2=None, op0=ALU.mult))
        S.dma(tab[l][:, o_t2:o_t2 + 2048].rearrange("p (g t) -> p g t", t=128), ta[:], reads=R)
        dv(lambda e: e.tensor_copy(out=outc[:, 0:16], in_=Ere[:, :, 128]))
        dv(lambda e: e.tensor_scalar(out=outc[:, 16:32], in0=Eim[:, :, 128], scalar1=sgn[:, 0:1], scalar2=None,
                                     op0=ALU.mult))
        dv(lambda e: e.tensor_scalar(out=outc[:, 32:48], in0=Eim[:, :, 128], scalar1=sgn[:, 1:2], scalar2=None,
                                     op0=ALU.mult))
        dv(lambda e: e.memset(outc[:, 48:64], 0.0))
        S.dma(tab[l][:, o_c:o_c + 64], outc[:], reads=R)
        S.end_phase()
        power_table(ire, iim, 7)
        ptr = [ps(ph, "t_ptr%d" % i, [128, 512], F32) for i in range(4)]
        arai = sb(ph, "t_arai", [128, 2, 16, 64], F32)
        for ri, E in ((0, Ere), (1, Eim)):
            for g in range(16):
                S.op("pe", lambda e, g=g, E=E, ri=ri: e.transpose(
                    out=ptr[ri * 2 + g // 8][:, (g % 8) * 64:(g % 8 + 1) * 64], in_=E[0:64, g, 0:128],
                    identity=identf[0:64, 0:64]), reads=R + ["identf"], writes=["t_ptr"])
            for hh in range(2):
                S.op("act", lambda e, hh=hh, ri=ri: e.copy(
                    out=arai[:, ri, hh * 8:(hh + 1) * 8, :].rearrange("p g n -> p (g n)"), in_=ptr[ri * 2 + hh][:]),
                    reads=["t_ptr"], writes=["t_arai"])
        S.dma(tab[l][:, 0:2048], arai[:].rearrange("p r g n -> p (r g n)"), reads=["t_arai"])
        bre = sb(ph, "t_bre", [64, 16, 16], F32)
        bim = sb(ph, "t_bim", [64, 16, 16], F32)
        Bre = sb(ph, "t_Bre", [64, 16, 16], F32)
        Bim = sb(ph, "t_Bim", [64, 16, 16], F32)
        tq = sb(ph, "t_tq", [64, 16, 16], F32)
        bmask = sb(ph, "t_bmask", [128, 8], F32)
        BT = sb(ph, "t_BT", [128, 2, 128], F32)
        Bblk = sb(ph, "t_Bblk", [128, 2, 8, 128], BF16)
        S.dma(bre[:], dr["bre"][l], writes=R)
        S.dma(bim[:], dr["bim"][l], writes=R)
        S.dma(bmask[:], dr["bmask"], writes=R)
        cfrb = cfr[0:64, :].unsqueeze(2).to_broadcast([64, 16, 16])
        cfib = cfi[0:64, :].unsqueeze(2).to_broadcast([64, 16, 16])
        dv(lambda e: e.tensor_tensor(out=Bre[:], in0=bre[:], in1=cfrb, op=ALU.mult))
        dv(lambda e: e.tensor_tensor(out=tq[:], in0=bim[:], in1=cfib, op=ALU.mult))
        dv(lambda e: e.tensor_tensor(out=Bre[:], in0=Bre[:], in1=tq[:], op=ALU.subtract))
        dv(lambda e: e.tensor_tensor(out=Bim[:], in0=bim[:], in1=cfrb, op=ALU.mult))
        dv(lambda e: e.tensor_tensor(out=tq[:], in0=bre[:], in1=cfib, op=ALU.mult))
        dv(lambda e: e.tensor_tensor(out=Bim[:], in0=Bim[:], in1=tq[:], op=ALU.add))
        for half in range(2):
            for ri, Bsrc in ((0, Bre), (1, Bim)):
                S.op("pe", lambda e, half=half, ri=ri, Bsrc=Bsrc: e.transpose(
                    out=ptr[0][:, (half * 2 + ri) * 64:(half * 2 + ri + 1) * 64],
                    in_=Bsrc[:, half * 8:(half + 1) * 8, :].rearrange("n g p -> n (g p)"),
                    identity=identf[0:64, 0:64]), reads=R + ["identf", "t_arai"], writes=["t_ptr0b"])
        S.op("act", lambda e: e.copy(out=BT[:].rearrange("p h c -> p (h c)"), in_=ptr[0][:, 0:256]),
             reads=["t_ptr0b"], writes=R)
        for half in range(2):
            S.op("dve", lambda e, half=half: e.tensor_tensor(
                out=Bblk[:, half], in0=BT[:, half, :].unsqueeze(1).to_broadcast([128, 8, 128]),
                in1=bmask[:].unsqueeze(2).to_broadcast([128, 8, 128]), op=ALU.mult), reads=R, writes=R)
        S.dma(tabb[l][:, 0:2048], Bblk[:].rearrange("p h g c -> p (h g c)"), reads=R)
        Cm = sb(ph, "t_Cm", [128, 16, 16], F32)
        mask2 = sb(ph, "t_mask2", [128, 16, 8], F32)
        Cpad = sb(ph, "t_Cpad", [128, 16, 8, 16], BF16)
        S.dma(Cm[0:64], dr["cre"][l], writes=R)
        S.dma(Cm[64:128], dr["cim"][l], writes=R)
        S.dma(mask2[:], dr["mask2"], writes=R)
        dv(lambda e: e.tensor_scalar(out=Cm[64:128], in0=Cm[64:128], scalar1=-1.0, scalar2=None, op0=ALU.mult))
        dv(lambda e: e.tensor_tensor(out=Cpad[:], in0=Cm[:].unsqueeze(2).to_broadcast([128, 16, 8, 16]),
                                     in1=mask2[:].unsqueeze(3).to_broadcast([128, 16, 8, 16]), op=ALU.mult))
        S.dma(tabb[l][:, 2048:4096], Cpad[:].rearrange("p g a b -> p (g a b)"), reads=R)
        S.end_phase()


def mixer(nc, S, dr, l, b, X, hT, GG, cols, tab, tabb, sb, ps, norm_to_hT, make_GG, resid_update, rstd_from_ss,
          identb, identf, onesb, att, ssm, pool):
    with ExitStack() as ph:
        norm_to_hT(ph, l, b, 0, 1)
        make_GG(ph, l, b, 2)
        S.end_phase()
    with ExitStack() as mx:
        yaT = sb(mx, "yaT", [128, 4, SEQ], BF16)
        if att:
            attention(nc, S, dr, l, b, hT, yaT, sb, ps, rstd_from_ss, identb, onesb)
        else:
            S.op("dve", lambda e: e.memset(yaT[:], 0.0), writes=["yaT"])
            S.end_phase()
        ysT = sb(mx, "ysT", [128, 2, SEQ], BF16)
        ypT = sb(mx, "ypT", [128, 2, SEQ], BF16)
        if ssm:
            ssm_branch(nc, S, dr, l, b, hT, ysT, tab, tabb, sb, ps)
        else:
            S.op("dve", lambda e: e.memset(ysT[:], 0.0), writes=["ysT"])
            S.end_phase()
        if pool:
            pool_branch(nc, S, dr, l, b, hT, ypT, sb, ps)
        else:
            S.op("dve", lambda e: e.memset(ypT[:], 0.0), writes=["ypT"])
            S.end_phase()
        merge(nc, S, dr, l, b, X, hT, GG, yaT, ysT, ypT, sb, ps, resid_update)


def merge(nc, S, dr, l, b, X, hT, GG, yaT, ysT, ypT, sb, ps, resid_update):
    with ExitStack() as ph:
        mT = sb(ph, "m_mT", [128, 8, SEQ], BF16)
        ssy = sb(ph, "m_ssy", [128, 4], F32)
        junk = sb(ph, "m_junk", [128, 512], BF16)
        tmp = sb(ph, "m_tmp", [128, D], F32)
        py = [ps(ph, "m_py%d" % i, [128, 512], F32) for i in range(4)]
        with ExitStack() as p1:
            wg = [sb(p1, "m_wg%d" % i, [128, 8, 128], BF16) for i in range(4)]
            pa = sb(p1, "m_pa", [128, 4, D], BF16)
            pb = sb(p1, "m_pb", [128, 2, D], BF16)
            pc = sb(p1, "m_pc", [128, 2, D], BF16)
            sg = [sb(p1, "m_sg%d" % i, [128, 512], F32) for i in range(2)]
            tm = sb(p1, "m_tm", [128, 512], F32)
            ac = sb(p1, "m_ac", [128, 512], F32)
            pg = [ps(p1, "m_pg%d" % i, [128, 512], F32) for i in range(2)]
            pp = [ps(p1, "m_pp%d" % i, [128, 512], F32) for i in range(2)]
            for k in range(4):
                cast_load(S, pa[:, k, :], dr["pa"][l][:, k, :], "m_pa")
            cast_load(S, pb[:], dr["pb"][l], "m_pb")
            cast_load(S, pc[:], dr["pc"][l], "m_pc")
            brs = ((yaT, pa, 4, "yaT", "m_pa"), (ysT, pb, 2, "ysT", "m_pb"), (ypT, pc, 2, "ypT", "m_pc"))
            nl_ = 0
            ng = 0
            for dc in range(8):
                wgs = []
                for br in range(3):
                    wb = wg[nl_ % 4]
                    nl_ += 1
                    cast_load(S, wb[:], dr["wg"][l, br * 8 + dc], _nm(wb))
                    wgs.append(wb)
                for s4 in range(4):
                    t0 = s4 * 512
                    hreads = [("hT", t0 // 128 + q) for q in range(4)]
                    for br in range(3):
                        yT, pw, nk, yn, pn = brs[br]
                        g_ = pg[ng % 2]
                        p_ = pp[ng % 2]
                        s_ = sg[ng % 2]
                        ng += 1
                        for k in range(8):
                            S.op("pe", lambda e, k=k, g_=g_, wb=wgs[br], t0=t0: e.matmul(
                                g_[:], lhsT=wb[:, k, :], rhs=hT[:, k, t0:t0 + 512], start=(k == 0), stop=(k == 7)),
                                reads=[_nm(wgs[br])] + hreads, writes=[_nm(g_)])
                        for k in range(nk):
                            S.op("pe", lambda e, k=k, p_=p_, pw=pw, yT=yT, t0=t0, nk=nk, dc=dc: e.matmul(
                                p_[:], lhsT=pw[:, k, dc * 128:(dc + 1) * 128], rhs=yT[:, k, t0:t0 + 512],
                                start=(k == 0), stop=(k == nk - 1)), reads=[pn, yn], writes=[_nm(p_)])
                        S.op("act", lambda e, g_=g_, s_=s_: e.activation(out=s_[:], in_=g_[:], func=AF.Sigmoid),
                             reads=[_nm(g_)], writes=[_nm(s_)])
                        if br == 0:
                            S.op("dve", lambda e, p_=p_, s_=s_: e.tensor_tensor(out=ac[:], in0=p_[:], in1=s_[:], op=ALU.mult),
                                 reads=[_nm(p_), _nm(s_)], writes=["m_ac"])
                        elif br == 1:
                            S.op("dve", lambda e, p_=p_, s_=s_: e.tensor_tensor(out=tm[:], in0=p_[:], in1=s_[:], op=ALU.mult),
                                 reads=[_nm(p_), _nm(s_)], writes=["m_tm"])
                            S.op("dve", lambda e: e.tensor_tensor(out=ac[:], in0=ac[:], in1=tm[:], op=ALU.add),
                                 reads=["m_tm"], writes=["m_ac"])
                        else:
                            S.op("dve", lambda e, p_=p_, s_=s_: e.tensor_tensor(out=tm[:], in0=p_[:], in1=s_[:], op=ALU.mult),
                                 reads=[_nm(p_), _nm(s_)], writes=["m_tm"])
                            S.op("dve", lambda e, dc=dc, t0=t0: e.tensor_tensor(
                                out=mT[:, dc, t0:t0 + 512], in0=ac[:], in1=tm[:], op=ALU.add),
                                reads=["m_tm", "m_ac"], writes=[("m_mT", dc)])
            S.end_phase()
        wo = sb(ph, "m_wo", [128, 8, D], BF16)
        for k in range(8):
            cast_load(S, wo[:, k, :], dr["wout"][l][:, k, :], ("m_wo", k))
        for t in range(NT):
            pyy = py[(t % 2) * 2:(t % 2) * 2 + 2]
            for i in range(2):
                for k in range(8):
                    S.op("pe", lambda e, i=i, k=k, t=t, pyy=pyy: e.matmul(
                        pyy[i][:], lhsT=mT[:, k, t * 128:(t + 1) * 128], rhs=wo[:, k, i * 512:(i + 1) * 512],
                        start=(k == 0), stop=(k == 7)), reads=[("m_mT", k), ("m_wo", k)], writes=[_nm(pyy[i])])
            resid_update((ssy, junk, tmp), t, pyy)
        S.end_phase()


def pool_branch(nc, S, dr, l, b, hT, ypT, sb, ps):
    with ExitStack() as ph:
        w1 = sb(ph, "p_w1", [128, 8, 256], BF16)
        wp = sb(ph, "p_wp", [128, 2, 128], F32)
        wpb = sb(ph, "p_wpb", [128, 2, 128], BF16)
        psc = sb(ph, "p_psc", [128, 2], F32)
        invt = sb(ph, "p_invt", [128, 16], F32)
        U = sb(ph, "p_U", [128, 16 + SEQ], F32)
        s_a = sb(ph, "p_sa", [128, 16 + SEQ], F32)
        s_b = sb(ph, "p_sb", [128, 16 + SEQ], F32)
        pl = sb(ph, "p_pl", [128, SEQ], BF16)
        fx = sb(ph, "p_fx", [128, 16], F32)
        pu = [ps(ph, "p_pu%d" % i, [128, 512], F32) for i in range(4)]
        cast_load(S, w1[:], dr["w1"][l][:, :, 680:936], "p_w1")
        S.dma(psc[:], dr["pscale"][l], writes=["p_psc"])
        S.dma(invt[:], dr["invt"], writes=["p_invt"])
        S.op("dve", lambda e: e.memset(wp[:], 0.0), writes=["p_wp"])
        for g in range(4):
            S.dma(wp[(g % 2) * 64:(g % 2 + 1) * 64, g // 2, (g % 2) * 64:(g % 2 + 1) * 64], dr["wpool"][l, g],
                  reads=[], writes=["p_wp"])
        S.op("dve", lambda e: e.tensor_copy(out=wpb[:], in_=wp[:]), reads=["p_wp"], writes=["p_wpb"])
        for buf in (U, s_a, s_b):
            S.op("dve", lambda e, buf=buf: e.memset(buf[:, 0:16], 0.0), writes=[_nm(buf)])
        for ct in range(2):
            for tc in range(4):
                for k in range(8):
                    S.op("pe", lambda e, k=k, tc=tc, ct=ct: e.matmul(
                        pu[tc][:], lhsT=w1[:, k, ct * 128:(ct + 1) * 128], rhs=hT[:, k, tc * 512:(tc + 1) * 512],
                        start=(k == 0), stop=(k == 7)), reads=["p_w1"] + [("hT", tc * 4 + q) for q in range(4)],
                        writes=[_nm(pu[tc])])
                S.op("act", lambda e, tc=tc: e.copy(out=U[:, 16 + tc * 512:16 + (tc + 1) * 512], in_=pu[tc][:]),
                     reads=[_nm(pu[tc])], writes=["p_U"])
            def shadd(dst, src, sh):
                S.op("dve", lambda e: e.tensor_tensor(out=dst[:, 16:16 + SEQ], in0=src[:, 16:16 + SEQ],
                                                      in1=src[:, 16 - sh:16 - sh + SEQ], op=ALU.add),
                     reads=[_nm(src)], writes=[_nm(dst)])
            shadd(s_a, U, 1)
            shadd(s_b, s_a, 2)
            if ct == 0:
                halves = ((0, s_a, 2), (64, s_b, 4))
            else:
                shadd(s_a, s_b, 4)
                shadd(s_b, s_a, 8)
                halves = ((0, s_a, 8), (64, s_b, 16))
            for (p0, sbuf, win) in halves:
                S.op("dve", lambda e, p0=p0, sbuf=sbuf, win=win: e.scalar_tensor_tensor(
                    out=pl[p0:p0 + 64, :], in0=sbuf[p0:p0 + 64, 16:16 + SEQ], scalar=1.0 / win,
                    in1=U[p0:p0 + 64, 16:16 + SEQ], op0=ALU.mult, op1=ALU.subtract),
                    reads=[_nm(sbuf), "p_U"], writes=["p_pl"])
                S.op("dve", lambda e, p0=p0, sbuf=sbuf, win=win: e.tensor_tensor(
                    out=fx[p0:p0 + 64, 0:win - 1], in0=sbuf[p0:p0 + 64, 16:16 + win - 1],
                    in1=invt[p0:p0 + 64, 0:win - 1], op=ALU.mult), reads=[_nm(sbuf), "p_invt"], writes=["p_fx"])
                S.op("dve", lambda e, p0=p0, win=win: e.tensor_tensor(
                    out=pl[p0:p0 + 64, 0:win - 1], in0=fx[p0:p0 + 64, 0:win - 1], in1=U[p0:p0 + 64, 16:16 + win - 1],
                    op=ALU.subtract), reads=["p_fx", "p_U"], writes=["p_pl"])
            for tc in range(4):
                S.op("pe", lambda e, tc=tc, ct=ct: e.matmul(pu[tc][:], lhsT=wpb[:, ct, :],
                                                         rhs=pl[:, tc * 512:(tc + 1) * 512], start=True, stop=True),
                     reads=["p_wpb", "p_pl"], writes=[_nm(pu[tc])])
                S.op("act", lambda e, tc=tc, ct=ct: e.activation(
                    out=ypT[:, ct, tc * 512:(tc + 1) * 512], in_=pu[tc][:], func=AF.Copy, scale=psc[:, ct:ct + 1]),
                    reads=[_nm(pu[tc]), "p_psc"], writes=["ypT"])
        S.end_phase()


def ssm_branch(nc, S, dr, l, b, hT, ysT, tab, tabb, sb, ps):
    with ExitStack() as ph:
        uT = sb(ph, "s_uT", [128, 2, SEQ], BF16)
        with ExitStack() as p1:
            w1 = sb(p1, "s_w1", [128, 8, 256], BF16)
            pj = [ps(p1, "s_pj%d" % i, [128, 512], F32) for i in range(2)]
            cast_load(S, w1[:], dr["w1"][l][:, :, 424:680], "s_w1")
            for ct in range(2):
                for tc in range(4):
                    pp = pj[tc % 2]
                    for k in range(8):
                        S.op("pe", lambda e, k=k, tc=tc, ct=ct, pp=pp: e.matmul(
                            pp[:], lhsT=w1[:, k, ct * 128:(ct + 1) * 128], rhs=hT[:, k, tc * 512:(tc + 1) * 512],
                            start=(k == 0), stop=(k == 7)), reads=["s_w1"] + [("hT", tc * 4 + q) for q in range(4)],
                            writes=[_nm(pp)])
                    S.op("act", lambda e, tc=tc, ct=ct, pp=pp: e.copy(out=uT[:, ct, tc * 512:(tc + 1) * 512], in_=pp[:]),
                         reads=[_nm(pp)], writes=["s_uT"])
            S.end_phase()
        zT = sb(ph, "s_zT", [128, 2, SEQ], BF16)
        arai = sb(ph, "s_arai", [128, 2, 16, 64], F32)
        T1 = sb(ph, "s_T1", [128, 16, 128], F32)
        T2 = sb(ph, "s_T2", [128, 16, 128], F32)
        pcs = sb(ph, "s_pcs", [128, 64], F32)
        Bblk = sb(ph, "s_Bblk", [128, 2, 1024], BF16)
        Cpad = sb(ph, "s_Cpad", [128, 16, 128], BF16)
        triub = sb(ph, "s_triub", [128, 128], BF16)
        dsk = sb(ph, "s_dsk", [128, 2], F32)
        bgl = sb(ph, "s_bgl", [128, 2], F32)
        wgl = sb(ph, "s_wgl", [128, 2, 256], BF16)
        t1s = [sb(ph, "s_t1%d" % i, [128, 4, 2, 64], BF16) for i in range(2)]
        t2s = [sb(ph, "s_t2%d" % i, [128, 4, 2, 64], BF16) for i in range(2)]
        v = sb(ph, "s_v", [128, 16, 2, 64], BF16)
        vsw = sb(ph, "s_vsw", [128, 16, 2, 64], BF16)
        xs = sb(ph, "s_xs", [128, 16, 128], BF16)
        xas = [sb(ph, "s_xa%d" % i, [128, 128], F32) for i in range(2)]
        xbs = [sb(ph, "s_xb%d" % i, [128, 128], F32) for i in range(2)]
        cA = [sb(ph, "s_cA%d" % i, [128, 16], F32) for i in range(2)]
        cB = [sb(ph, "s_cB%d" % i, [128, 16], F32) for i in range(2)]
        sA = sb(ph, "s_sA", [128, 16], F32)
        sB = sb(ph, "s_sB", [128, 16], F32)
        q1 = sb(ph, "s_q1", [128, 16], F32)
        yv = sb(ph, "s_yv", [128, 256], F32)
        sg = sb(ph, "s_sg", [128, 512], BF16)
        pbu = [ps(ph, "s_pbu%d" % i, [128, 512], F32) for i in range(2)]
        pA = [ps(ph, "s_pA%d" % i, [128, 512], F32) for i in range(2)]
        pB = [ps(ph, "s_pB%d" % i, [128, 512], F32) for i in range(2)]
        pyy = ps(ph, "s_py", [128, 512], F32)
        pg = ps(ph, "s_pg", [128, 512], F32)
        S.dma(arai[:].rearrange("p r g n -> p (r g n)"), tab[l][:, 0:2048], writes=["s_arai"])
        S.dma(T1[:].rearrange("p g t -> p (g t)"), tab[l][:, 2048:4096], writes=["s_T1"])
        S.dma(T2[:].rearrange("p g t -> p (g t)"), tab[l][:, 4096:6144], writes=["s_T2"])
        S.dma(pcs[:], tab[l][:, 6144:6208], writes=["s_pcs"])
        S.dma(Bblk[:].rearrange("p h c -> p (h c)"), tabb[l][:, 0:2048], writes=["s_Bblk"])
        S.dma(Cpad[:].rearrange("p g c -> p (g c)"), tabb[l][:, 2048:4096], writes=["s_Cpad"])
        S.dma(triub[:], dr["triub"], writes=["s_triub"])
        S.dma(dsk[:], dr["dskip"][l], writes=["s_dsk"])
        S.dma(bgl[:], dr["bglu"][l], writes=["s_bgl"])
        cast_load(S, wgl[:], dr["wglu"][l], "s_wgl")
        for i in range(2):
            S.op("dve", lambda e, i=i: e.memset(cA[i][:], 0.0), writes=[_nm(cA[i])])
            S.op("dve", lambda e, i=i: e.memset(cB[i][:], 0.0), writes=[_nm(cB[i])])
        Pr = pcs[:, 0:16]
        PiA = pcs[:, 16:32]
        PiB = pcs[:, 32:48]
        def stage1(t, half, q2):
            g0 = half * 8 + q2 * 4
            t1 = t1s[q2]
            t2 = t2s[q2]
            S.op("pe", lambda e: e.matmul(
                pbu[q2][:], lhsT=uT[:, half, t * 128:(t + 1) * 128], rhs=Bblk[:, half, q2 * 512:(q2 + 1) * 512],
                start=True, stop=True), reads=["s_uT", "s_Bblk"], writes=[_nm(pbu[q2])])
            bu = pbu[q2][:].rearrange("p (g r n) -> p g r n", g=4, r=2)
            Ar = arai[:, 0, g0:g0 + 4, :]
            Ai = arai[:, 1, g0:g0 + 4, :]
            S.op("dve", lambda e: e.tensor_tensor(
                out=t1[:], in0=bu, in1=Ar.unsqueeze(2).to_broadcast([128, 4, 2, 64]), op=ALU.mult),
                reads=[_nm(pbu[q2]), "s_arai"], writes=[_nm(t1)])
            S.op("dve", lambda e: e.tensor_tensor(
                out=t2[:, :, 0, :], in0=bu[:, :, 1, :], in1=Ai, op=ALU.mult),
                reads=[_nm(pbu[q2]), "s_arai"], writes=[_nm(t2)])
            S.op("dve", lambda e: e.tensor_tensor(
                out=t2[:, :, 1, :], in0=bu[:, :, 0, :], in1=Ai, op=ALU.mult),
                reads=[_nm(pbu[q2]), "s_arai"], writes=[_nm(t2)])
            S.op("pool", lambda e: e.tensor_tensor(
                out=v[:, g0:g0 + 4, 0, :], in0=t1[:, :, 0, :], in1=t2[:, :, 0, :], op=ALU.subtract),
                reads=[_nm(t1), _nm(t2)], writes=[("s_v", g0)])
            S.op("pool", lambda e: e.tensor_tensor(
                out=v[:, g0:g0 + 4, 1, :], in0=t1[:, :, 1, :], in1=t2[:, :, 1, :], op=ALU.add),
                reads=[_nm(t1), _nm(t2)], writes=[("s_v", g0)])
            S.op("pool", lambda e: e.tensor_tensor(
                out=vsw[:, g0:g0 + 4, 1, :], in0=t1[:, :, 0, :], in1=t2[:, :, 0, :], op=ALU.subtract),
                reads=[_nm(t1), _nm(t2)], writes=[("s_vsw", g0)])
            S.op("pool", lambda e: e.tensor_tensor(
                out=vsw[:, g0:g0 + 4, 0, :], in0=t1[:, :, 1, :], in1=t2[:, :, 1, :], op=ALU.add),
                reads=[_nm(t1), _nm(t2)], writes=[("s_vsw", g0)])
            pa_, pb_ = pA[q2], pB[q2]
            for gg in range(4):
                g = g0 + gg
                S.op("pe", lambda e: e.matmul(
                    pa_[:, gg * 128:(gg + 1) * 128], lhsT=v[:, g].rearrange("p r n -> p (r n)"), rhs=triub[:],
                    start=True, stop=True), reads=[("s_v", g0), "s_triub"], writes=[_nm(pa_)])
                S.op("pe", lambda e: e.matmul(
                    pb_[:, gg * 128:(gg + 1) * 128], lhsT=vsw[:, g].rearrange("p r n -> p (r n)"), rhs=triub[:],
                    start=True, stop=True), reads=[("s_vsw", g0), "s_triub"], writes=[_nm(pb_)])

        def stage2(t, half, q2):
            cAo, cBo = cA[t % 2], cB[t % 2]
            cAn, cBn = cA[(t + 1) % 2], cB[(t + 1) % 2]
            g0 = half * 8 + q2 * 4
            pa_, pb_ = pA[q2], pB[q2]
            A127 = pa_[:].rearrange("p (g t) -> p g t", t=128)[:, :, 127]
            B127 = pb_[:].rearrange("p (g t) -> p g t", t=128)[:, :, 127]
            gs = slice(g0, g0 + 4)
            S.op("dve", lambda e: e.tensor_tensor(out=sA[:, gs], in0=A127, in1=cAo[:, gs], op=ALU.add),
                 reads=[_nm(pa_), _nm(cAo)], writes=["s_sA"])
            S.op("dve", lambda e: e.tensor_tensor(out=sB[:, gs], in0=B127, in1=cBo[:, gs], op=ALU.add),
                 reads=[_nm(pb_), _nm(cBo)], writes=["s_sB"])
            for gg in range(4):
                g = g0 + gg
                xa = xas[gg % 2]
                xb = xbs[gg % 2]
                S.op("dve", lambda e: e.scalar_tensor_tensor(
                    out=xa[:], in0=pa_[:, gg * 128:(gg + 1) * 128], scalar=cAo[:, g:g + 1], in1=T1[:, g, :],
                    op0=ALU.add, op1=ALU.mult), reads=[_nm(pa_), _nm(cAo), "s_T1"], writes=[_nm(xa)])
                S.op("dve", lambda e: e.scalar_tensor_tensor(
                    out=xb[:], in0=pb_[:, gg * 128:(gg + 1) * 128], scalar=cBo[:, g:g + 1], in1=T2[:, g, :],
                    op0=ALU.add, op1=ALU.mult), reads=[_nm(pb_), _nm(cBo), "s_T2"], writes=[_nm(xb)])
                S.op("pool", lambda e: e.tensor_tensor(out=xs[:, g, :], in0=xa[:], in1=xb[:], op=ALU.add),
                     reads=[_nm(xa), _nm(xb)], writes=[("s_xs", g)])
            S.op("dve", lambda e: e.tensor_tensor(out=cAn[:, gs], in0=sA[:, gs], in1=Pr[:, gs], op=ALU.mult),
                 reads=["s_sA", "s_pcs"], writes=[_nm(cAn)])
            S.op("dve", lambda e: e.tensor_tensor(out=q1[:, gs], in0=sB[:, gs], in1=PiA[:, gs], op=ALU.mult),
                 reads=["s_sB", "s_pcs"], writes=["s_q1"])
            S.op("dve", lambda e: e.tensor_tensor(out=cAn[:, gs], in0=cAn[:, gs], in1=q1[:, gs], op=ALU.add),
                 reads=["s_q1"], writes=[_nm(cAn)])
            S.op("dve", lambda e: e.tensor_tensor(out=cBn[:, gs], in0=sB[:, gs], in1=Pr[:, gs], op=ALU.mult),
                 reads=["s_sB", "s_pcs"], writes=[_nm(cBn)])
            S.op("dve", lambda e: e.tensor_tensor(out=q1[:, gs], in0=sA[:, gs], in1=PiB[:, gs], op=ALU.mult),
                 reads=["s_sA", "s_pcs"], writes=["s_q1"])
            S.op("dve", lambda e: e.tensor_tensor(out=cBn[:, gs], in0=cBn[:, gs], in1=q1[:, gs], op=ALU.add),
                 reads=["s_q1"], writes=[_nm(cBn)])
            if q2 == 1:
                for g8 in range(8):
                    g = half * 8 + g8
                    S.op("pe", lambda e: e.matmul(
                        pyy[:, half * 128:(half + 1) * 128], lhsT=Cpad[:, g, :], rhs=xs[:, g, :],
                        start=(g8 == 0), stop=(g8 == 7)), reads=[("s_xs", g), "s_Cpad"], writes=["s_py"])
                S.op("dve", lambda e: e.scalar_tensor_tensor(
                    out=yv[:, half * 128:(half + 1) * 128], in0=uT[:, half, t * 128:(t + 1) * 128],
                    scalar=dsk[:, half:half + 1], in1=pyy[:, half * 128:(half + 1) * 128], op0=ALU.mult, op1=ALU.add),
                    reads=["s_py", "s_uT", "s_dsk"], writes=["s_yv"])
                S.op("act", lambda e: e.activation(
                    out=zT[:, half, t * 128:(t + 1) * 128], in_=yv[:, half * 128:(half + 1) * 128],
                    func=AF.Gelu_apprx_tanh), reads=["s_yv"], writes=[("s_zT", t // 4)])
            if q2 == 1 and half == 1 and t % 4 == 3:
                tc = t // 4
                for ct in range(2):
                    for k in range(2):
                        S.op("pe", lambda e: e.matmul(
                            pg[:], lhsT=wgl[:, k, ct * 128:(ct + 1) * 128], rhs=zT[:, k, tc * 512:(tc + 1) * 512],
                            start=(k == 0), stop=(k == 1)), reads=["s_wgl", ("s_zT", tc)], writes=["s_pg"])
                    S.op("act", lambda e: e.activation(out=sg[:], in_=pg[:], func=AF.Sigmoid,
                                                       bias=bgl[:, ct:ct + 1]),
                         reads=["s_pg", "s_bgl"], writes=["s_sg"])
                    S.op("dve", lambda e: e.tensor_tensor(
                        out=ysT[:, ct, tc * 512:(tc + 1) * 512], in0=zT[:, ct, tc * 512:(tc + 1) * 512], in1=sg[:],
                        op=ALU.mult), reads=["s_sg", ("s_zT", tc)], writes=["ysT"])

        units = [(t, half, q2) for t in range(NT) for half in range(2) for q2 in range(2)]
        stage1(*units[0])
        for ui, u in enumerate(units):
            if ui + 1 < len(units):
                stage1(*units[ui + 1])
            stage2(*u)
        S.end_phase()


def attention(nc, S, dr, l, b, hT, yaT, sb, ps, rstd_from_ss, identb, onesb):
    with ExitStack() as at:
        cqnT = sb(at, "a_cqnT", [128, 2, SEQ], BF16)
        ckvT = sb(at, "a_ckvT", [128, SEQ], BF16)
        Vg = sb(at, "a_V", [128, NT, 128], BF16)
        kidT = sb(at, "a_kidT", [128, SEQ], BF16)
        Wsc = sb(at, "a_Wsc", [128, NT, 8], F32)
        with ExitStack() as ph:
            w1 = sb(ph, "a_w1", [128, 8, 424], BF16)
            gcq = sb(ph, "a_gcq", [128, 2], F32)
            gkv = sb(ph, "a_gkv", [128, 128], F32)
            ss = sb(ph, "a_ss", [128, 2], F32)
            junk = sb(ph, "a_junk", [128, 256], BF16)
            cqn = sb(ph, "a_cqn", [128, 256], BF16)
            kid4 = sb(ph, "a_kid4", [128, 4, 32], BF16)
            pz = [ps(ph, "a_pz%d" % i, [128, 512], F32) for i in range(2)]
            pt = [ps(ph, "a_pt%d" % i, [128, 1024], BF16) for i in range(2)]
            cast_load(S, w1[:], dr["w1"][l][:, :, 0:424], "a_w1")
            S.dma(gcq[:], dr["gcq"][l], writes=["a_gcq"])
            S.dma(gkv[:], dr["gckv"][l:l + 1, :].to_broadcast([128, 128]), writes=["a_gkv"])
            for t in range(NT):
                p = pz[t % 2]
                q = pt[t % 2]
                for k in range(8):
                    S.op("pe", lambda e, k=k, t=t, p=p: e.matmul(p[:, 0:424], lhsT=hT[:, k, t * 128:(t + 1) * 128],
                                                                 rhs=w1[:, k, :], start=(k == 0), stop=(k == 7)),
                         reads=["a_w1", ("hT", t)], writes=[_nm(p)])
                S.op("act", lambda e, p=p: e.activation(out=junk[:], in_=p[:, 0:256], func=AF.Square,
                                                        accum_out=ss[:, 0:1]), reads=[_nm(p)], writes=["a_junk", "a_ss"])
                S.op("act", lambda e, p=p: e.activation(out=junk[:, 0:128], in_=p[:, 256:384], func=AF.Square,
                                                        accum_out=ss[:, 1:2]), reads=[_nm(p)], writes=["a_junk", "a_ss"])
                S.op("dve", lambda e: e.tensor_scalar(out=ss[:, 0:1], in0=ss[:, 0:1], scalar1=1.0 / 256, scalar2=EPS,
                                                      op0=ALU.mult, op1=ALU.add), reads=["a_ss"], writes=["a_ss"])
                S.op("dve", lambda e: e.tensor_scalar(out=ss[:, 1:2], in0=ss[:, 1:2], scalar1=1.0 / 128, scalar2=EPS,
                                                      op0=ALU.mult, op1=ALU.add), reads=["a_ss"], writes=["a_ss"])
                S.op("act", lambda e: e.activation(out=ss[:], in_=ss[:], func=AF.Sqrt), reads=["a_ss"], writes=["a_ss"])
                S.op("dve", lambda e: e.reciprocal(out=ss[:], in_=ss[:]), reads=["a_ss"], writes=["a_ss"])
                S.op("dve", lambda e, p=p: e.tensor_scalar(out=cqn[:], in0=p[:, 0:256], scalar1=ss[:, 0:1], scalar2=None,
                                                           op0=ALU.mult), reads=[_nm(p), "a_ss"], writes=["a_cqn"])
                S.op("dve", lambda e, p=p, t=t: e.scalar_tensor_tensor(
                    out=Vg[:, t, :], in0=p[:, 256:384], scalar=ss[:, 1:2], in1=gkv[:], op0=ALU.mult, op1=ALU.mult),
                    reads=[_nm(p), "a_ss", "a_gkv"], writes=[("a_V", t)])
                S.op("dve", lambda e, p=p: e.tensor_copy(
                    out=kid4[:], in_=p[:, 384:416].unsqueeze(1).to_broadcast([128, 4, 32])),
                    reads=[_nm(p)], writes=["a_kid4"])
                S.op("dve", lambda e, p=p, t=t: e.tensor_scalar(out=Wsc[:, t, :], in0=p[:, 416:424], scalar1=IDXW,
                                                                scalar2=None, op0=ALU.mult),
                     reads=[_nm(p)], writes=["a_Wsc"])
                for c in range(2):
                    S.op("pe", lambda e, c=c, q=q: e.transpose(out=q[:, c * 128:(c + 1) * 128],
                                                              in_=cqn[:, c * 128:(c + 1) * 128], identity=identb[:]),
                         reads=["a_cqn", "identb"], writes=[_nm(q)])
                S.op("pe", lambda e, q=q, t=t: e.transpose(out=q[:, 256:384], in_=Vg[:, t, :], identity=identb[:]),
                     reads=[("a_V", t), "identb"], writes=[_nm(q)])
                S.op("pe", lambda e, q=q: e.transpose(out=q[:, 384:512], in_=kid4[:].rearrange("p a b -> p (a b)"),
                                                      identity=identb[:]), reads=["a_kid4", "identb"], writes=[_nm(q)])
                for c in range(2):
                    S.op("act", lambda e, c=c, q=q, t=t: e.activation(
                        out=cqnT[:, c, t * 128:(t + 1) * 128], in_=q[:, c * 128:(c + 1) * 128], func=AF.Copy,
                        scale=gcq[:, c:c + 1]), reads=[_nm(q), "a_gcq"], writes=[("a_cqnT", t)])
                S.op("dve", lambda e, q=q, t=t: e.tensor_copy(out=ckvT[:, t * 128:(t + 1) * 128], in_=q[:, 256:384]),
                     reads=[_nm(q)], writes=[("a_ckvT", t)])
                S.op("dve", lambda e, q=q, t=t: e.tensor_copy(out=kidT[:, t * 128:(t + 1) * 128], in_=q[:, 384:512]),
                     reads=[_nm(q)], writes=[("a_kidT", t)])
            S.end_phase()
        with ExitStack() as ph:
            wuq = sb(ph, "a_wuq", [128, 2, 1024], BF16)
            wqi = sb(ph, "a_wqi", [128, 2, 256], BF16)
            wuv = sb(ph, "a_wuv", [128, 8, 64], BF16)
            i4b = sb(ph, "a_i4b", [128, 512], BF16)
            altd = sb(ph, "a_altd", [128, 1024], BF16)
            at3 = sb(ph, "a_at3", [128, 2048], BF16)
            bt3 = sb(ph, "a_bt3", [128, 1024], BF16)
            mk4 = sb(ph, "a_mk4", [128, 4], F32)
            wuvp = sb(ph, "a_wuvp", [128, 8, 128], BF16)
            m0 = sb(ph, "a_m0", [128, 128], F32)
            pow2 = sb(ph, "a_pow2", [128, NIT + 1], F32)
            qT = sb(ph, "a_qT", [128, 8, 128], BF16)
            qiT = sb(ph, "a_qiT", [128, 8, 128], BF16)
            accs = [sb(ph, "a_acc%d" % i, [128, SEQ], F32) for i in range(2)]
            jk = sb(ph, "a_jk", [128, SEQ], mybir.dt.uint8)
            MBs = [sb(ph, "a_MB%d" % i, [128, SEQ], BF16) for i in range(2)]
            Rb = [sb(ph, "a_R%d" % i, [128, 512], BF16) for i in range(2)]
            wdg = sb(ph, "a_wdg", [128, 8, 128], BF16)
            PT = [sb(ph, "a_PT%d" % i, [128, 512], BF16) for i in range(2)]
            rec = sb(ph, "a_rec", [128, 512], F32)
            oT = sb(ph, "a_oT", [128, 1024], BF16)
            sm = sb(ph, "a_sm", [128, 8], F32)
            Wk = sb(ph, "a_Wk", [128, NIT + 1], F32)
            pS = [ps(ph, "a_pS%d" % i, [128, 512], F32) for i in range(2)]
            pL = [ps(ph, "a_pL%d" % i, [128, 512], F32) for i in range(2)]
            pQ = ps(ph, "a_pQ", [128, 512], F32)
            pACC = ps(ph, "a_pACC", [128, 512], F32)
            pO = ps(ph, "a_pO", [128, 512], F32)
            pD = ps(ph, "a_pD", [128, 512], F32)
            pY = pQ
            cast_load(S, wuq[:], dr["wuq"][l], "a_wuq")
            cast_load(S, wqi[:], dr["wqi"][l], "a_wqi")
            cast_load(S, wuv[:], dr["wuv"][l], "a_wuv")
            for nm, tl in (("i4b", i4b), ("altd", altd), ("at3", at3), ("bt3", bt3), ("m0", m0), ("pow2", pow2),
                           ("mk4", mk4)):
                S.dma(tl[:], dr[nm], writes=["a_" + nm])
            S.op("dve", lambda e: e.memset(wuvp[:], 0.0), writes=["a_wuvp"])
            for h in range(8):
                S.op("dve", lambda e, h=h: e.tensor_copy(out=wuvp[:, h, (h % 2) * 64:(h % 2 + 1) * 64], in_=wuv[:, h, :]),
                     reads=["a_wuv"], writes=["a_wuvp"])
            hi, lo, w0, mid, cnt, tt, thr = (sm[:, i:i + 1] for i in range(7))

            def stageA(i, part):
                steps = []
                N = 128 * (i + 1)
                qs = slice(i * 128, (i + 1) * 128)
                MB = MBs[i % 2]
                mbn = "a_MB%d" % (i % 2)
                acc = accs[i % 2]
                ACCR = [("a_acc%d" % (i % 2), c_) for c_ in range(4)]

                def qi_proj(tl):
                    for k in range(2):
                        S.op("pe", lambda e: e.matmul(
                            pQ[:, tl * 128:(tl + 1) * 128], lhsT=wqi[:, k, tl * 128:(tl + 1) * 128],
                            rhs=cqnT[:, k, qs], start=(k == 0), stop=(k == 1)),
                            reads=["a_wqi", ("a_cqnT", i)], writes=["a_pQ"])
                    for h in range(tl * 4, tl * 4 + 4):
                        S.op("act", lambda e: e.activation(out=qiT[:, h, :], in_=pQ[:, tl * 128:(tl + 1) * 128],
                                                           func=AF.Copy, scale=mk4[:, h % 4:h % 4 + 1]),
                             reads=["a_pQ", "a_mk4"], writes=["a_qiT"])
                for tl in range(2):
                    steps.append(partial(qi_proj, tl))

                nch = (N + 511) // 512

                def diag_build():
                    for h in range(8):
                        S.op("act", lambda e: e.activation(out=wdg[:, h, :], in_=identb[:], func=AF.Copy,
                                                           scale=Wsc[:, i, h:h + 1]),
                             reads=["identb", "a_Wsc"], writes=["a_wdg"])
                steps.append(diag_build)
                seq = [(cc, h) for cc in range(nch) for h in range(8)]

                def emitL(x):
                    cc, h = seq[x]
                    n = min(512, N - cc * 512)
                    pl_ = pL[x % 2]
                    S.op("pe", lambda e: e.matmul(
                        pl_[:, 0:n], lhsT=qiT[:, h, :], rhs=kidT[:, cc * 512:cc * 512 + n],
                        start=True, stop=True), reads=["a_qiT"] + [("a_kidT", cc * 4 + q) for q in range((n + 127) // 128)],
                        writes=[_nm(pl_)])

                def idx_step(x):
                    cc, h = seq[x]
                    n = min(512, N - cc * 512)
                    if x == 0:
                        emitL(0)
                    if x + 1 < len(seq):
                        emitL(x + 1)
                    pl_ = pL[x % 2]
                    rb = Rb[x % 2]
                    S.op("act", lambda e: e.activation(out=rb[:, 0:n], in_=pl_[:, 0:n], func=AF.Relu),
                         reads=[_nm(pl_)], writes=[_nm(rb)])
                    S.op("pe", lambda e: e.matmul(pACC[:, 0:n], lhsT=wdg[:, h, :], rhs=rb[:, 0:n],
                                                  start=(h == 0), stop=(h == 7)),
                         reads=["a_wdg", _nm(rb)], writes=["a_pACC"])
                    if h == 7:
                        S.op("act", lambda e: e.copy(out=acc[:, cc * 512:cc * 512 + n], in_=pACC[:, 0:n]),
                             reads=["a_pACC"], writes=[("a_acc%d" % (i % 2), cc)])
                for x in range(len(seq)):
                    steps.append(partial(idx_step, x))
                if part == 1:
                    return steps
                steps = []

                def bounds():
                    if i >= 2:
                        S.op("dve", lambda e: e.tensor_reduce(out=hi, in_=acc[:, 0:N], axis=AX.X, op=ALU.max),
                             reads=ACCR, writes=["a_sm"])
                        S.op("dve", lambda e: e.tensor_reduce(out=lo, in_=acc[:, 0:N], axis=AX.X, op=ALU.min),
                             reads=ACCR, writes=["a_sm"])
                    S.op("dve", lambda e: e.tensor_tensor(out=acc[:, N - 128:N], in0=acc[:, N - 128:N], in1=m0[:],
                                                          op=ALU.add), reads=["a_m0"] + ACCR, writes=ACCR)
                    if i >= 2:
                        S.op("dve", lambda e: e.tensor_tensor(out=w0, in0=hi, in1=lo, op=ALU.subtract),
                             reads=["a_sm"], writes=["a_sm"])
                        S.op("dve", lambda e: e.tensor_scalar(out=Wk[:], in0=pow2[:], scalar1=w0, scalar2=None,
                                                              op0=ALU.mult), reads=["a_sm", "a_pow2"], writes=["a_Wk"])
                        S.op("dve", lambda e: e.tensor_tensor(out=mid, in0=lo, in1=Wk[:, 0:1], op=ALU.add),
                             reads=["a_sm", "a_Wk"], writes=["a_sm"])
                    else:
                        S.op("dve", lambda e: e.memset(thr, -1.0e29), writes=["a_sm"])
                steps.append(bounds)

                def bis(k):
                    S.op("dve", lambda e: e.tensor_scalar(
                        out=jk[:, 0:N], in0=acc[:, 0:N], scalar1=mid, scalar2=0.0, op0=ALU.is_ge, op1=ALU.add,
                        accum_out=cnt), reads=ACCR + ["a_sm"], writes=["a_jk", "a_sm"])
                    S.op("dve", lambda e: e.tensor_scalar(out=tt, in0=cnt, scalar1=255.5, scalar2=0.5, op0=ALU.is_ge,
                                                          op1=ALU.subtract), reads=["a_sm"], writes=["a_sm"])
                    S.op("dve", lambda e: e.scalar_tensor_tensor(out=mid, in0=tt, scalar=Wk[:, k:k + 1], in1=mid,
                                                                 op0=ALU.mult, op1=ALU.add),
                         reads=["a_sm", "a_Wk"], writes=["a_sm"])
                if i >= 2:
                    for k in range(NIT):
                        steps.append(partial(bis, k))

                def fin():
                    if i >= 2:
                        S.op("dve", lambda e: e.tensor_tensor(out=thr, in0=mid, in1=Wk[:, NIT:NIT + 1], op=ALU.subtract),
                             reads=["a_sm", "a_Wk"], writes=["a_sm"])
                    S.op("dve", lambda e: e.tensor_scalar(out=MB[:, 0:N], in0=acc[:, 0:N], scalar1=thr, scalar2=-30000.0,
                                                          op0=ALU.is_lt, op1=ALU.mult),
                         reads=ACCR + ["a_sm"], writes=[mbn])
                steps.append(fin)
                return steps

            def stageB(i):
                steps = []
                qs = slice(i * 128, (i + 1) * 128)
                MB = MBs[i % 2]
                mbn = "a_MB%d" % (i % 2)

                def q_proj(g2):
                    for h in range(g2 * 4, g2 * 4 + 4):
                        for k in range(2):
                            S.op("pe", lambda e: e.matmul(
                                pS[g2][:, (h % 4) * 128:(h % 4 + 1) * 128], lhsT=wuq[:, k, h * 128:(h + 1) * 128],
                                rhs=cqnT[:, k, qs], start=(k == 0), stop=(k == 1)),
                                reads=["a_wuq", ("a_cqnT", i)], writes=[_nm(pS[g2])])
                    S.op("act", lambda e: e.activation(
                        out=qT[:, g2 * 4:(g2 + 1) * 4, :].rearrange("p h q -> p (h q)"), in_=pS[g2][:], func=AF.Copy,
                        scale=ATT_SC), reads=[_nm(pS[g2])], writes=["a_qT"])
                steps.append(partial(q_proj, 0))
                steps.append(partial(q_proj, 1))
                its = [(g, j) for g in range(2) for j in range(i + 1)]

                def emitS(n):
                    grp, j = its[n]
                    s_ = pS[n % 2]
                    S.op("pe", lambda e: e.matmul(
                        s_[:], lhsT=ckvT[:, j * 128:(j + 1) * 128],
                        rhs=qT[:, grp * 4:(grp + 1) * 4, :].rearrange("p h q -> p (h q)"), start=True, stop=False),
                        reads=[("a_ckvT", j), "a_qT"], writes=[_nm(s_)])
                    S.op("pe", lambda e: e.matmul(
                        s_[:], lhsT=MB[:, j * 128:(j + 1) * 128], rhs=i4b[:], start=False, stop=False),
                        reads=[mbn, "a_i4b"], writes=[_nm(s_)])
                    if j < i:
                        dd = i - j
                        S.op("pe", lambda e: e.matmul(
                            s_[:], lhsT=at3[:, dd * 128:(dd + 1) * 128], rhs=bt3[:, grp * 512:(grp + 1) * 512],
                            start=False, stop=True), reads=["a_at3", "a_bt3"], writes=[_nm(s_)])
                    else:
                        S.op("pe", lambda e: e.matmul(
                            s_[:], lhsT=identb[:], rhs=altd[:, grp * 512:(grp + 1) * 512], start=False, stop=True),
                            reads=["identb", "a_altd"], writes=[_nm(s_)])

                def it_step(n):
                    grp, j = its[n]
                    if n == 0:
                        emitS(0)
                    if n + 1 < len(its):
                        emitS(n + 1)
                    s_ = pS[n % 2]
                    ptb = PT[n % 2]
                    S.op("act", lambda e: e.activation(out=ptb[:], in_=s_[:], func=AF.Exp),
                         reads=[_nm(s_)], writes=[_nm(ptb)])
                    S.op("pe", lambda e: e.matmul(pO[:], lhsT=Vg[:, j, :], rhs=ptb[:], start=(j == 0), stop=(j == i)),
                         reads=[("a_V", j), _nm(ptb)], writes=["a_pO"])
                    S.op("pe", lambda e: e.matmul(pD[:], lhsT=onesb[:], rhs=ptb[:], start=(j == 0), stop=(j == i)),
                         reads=["onesb", _nm(ptb)], writes=["a_pD"])
                    if j == i:
                        S.op("dve", lambda e: e.reciprocal(out=rec[:], in_=pD[:]), reads=["a_pD"], writes=["a_rec"])
                        S.op("dve", lambda e: e.tensor_tensor(out=oT[:, grp * 512:(grp + 1) * 512], in0=pO[:],
                                                              in1=rec[:], op=ALU.mult),
                             reads=["a_pO", "a_rec"], writes=["a_oT"])
                for n in range(len(its)):
                    steps.append(partial(it_step, n))

                def y_proj():
                    for h in range(8):
                        S.op("pe", lambda e: e.matmul(
                            pY[:, (h // 2) * 128:(h // 2 + 1) * 128], lhsT=wuvp[:, h, :],
                            rhs=oT[:, h * 128:(h + 1) * 128], start=(h % 2 == 0), stop=(h % 2 == 1)),
                            reads=["a_wuvp", "a_oT"], writes=["a_pQ"])
                    S.op("act", lambda e: e.copy(out=yaT[:, :, qs], in_=pY[:].rearrange("p (c q) -> p c q", q=128)),
                         reads=["a_pQ"], writes=["yaT"])
                steps.append(y_proj)
                return steps

            def run_merged(*lists):
                pos = [0] * len(lists)
                total = sum(len(x) for x in lists)
                for _ in range(total):
                    best, bf = None, None
                    for q, lst in enumerate(lists):
                        if pos[q] < len(lst):
                            frac = pos[q] / len(lst)
                            if bf is None or frac < bf:
                                best, bf = q, frac
                    lists[best][pos[best]]()
                    pos[best] += 1

            run_merged(stageA(0, 1))
            run_merged(stageA(1, 1), stageA(0, 2))
            for i in range(NT):
                run_merged(stageA(i + 2, 1) if i + 2 < NT else [], stageA(i + 1, 2) if i + 1 < NT else [], stageB(i))
            S.end_phase()


_BF = {"identb", "i4b", "onesb", "triub", "altd", "at3", "bt3"}


def kernel(**inputs):
    inp = {k: np.asarray(v) for k, v in inputs.items()}
    n_cores = 8
    shared = _layout_weights(inp)
    shared.update(_consts())
    x = np.ascontiguousarray(inp["x"], dtype=np.float32)
    c = np.asarray(inp["c"], dtype=np.float32)
    in_maps = []
    for ci in range(n_cores):
        m = dict(shared)
        m["x"] = x[ci * NB:(ci + 1) * NB]
        cc = c[ci * NB:(ci + 1) * NB]
        m["cT"] = np.ascontiguousarray(cc.reshape(NB, 8, 128).transpose(2, 1, 0))
        in_maps.append(m)
    shapes = {k: (v.shape, BF16 if k in _BF else F32) for k, v in in_maps[0].items()}
    nc = build_nc(shapes)
    in_maps = [{"d_" + k: v for k, v in m.items()} for m in in_maps]
    res = run_bass_kernel_spmd(nc, in_maps, core_ids=list(range(n_cores)))
    return np.concatenate([np.asarray(r["out"], dtype=np.float32) for r in res.results], axis=0)
```
